# Optimizing a Trainium2 kernel written in Bass

```python
import jax, jax.numpy as jnp
from jax import lax
import numpy as np

D_MODEL = 1024
BATCH = 4
SEQ = 4096
DEPTH = 4

GRID_W = 64
CTX_LEN = 256
N_MIXERS = 3
ROPE_THETA = 10000.0
DEEPNORM_ALPHA = (2 * DEPTH) ** 0.25
DEEPNORM_BETA = (8 * DEPTH) ** -0.25
LN_EPS = 1e-5
RMS_EPS = 1e-6
ATTN_HEAD_DIM = 128
ATTN_Q_HEADS = D_MODEL // ATTN_HEAD_DIM
ATTN_KV_HEADS = 2
ATTN_GROUP = ATTN_Q_HEADS // ATTN_KV_HEADS
Q_BLOCK = 128
CMLP_CHUNK = 128
CMLP_INNER = 2 * D_MODEL
CMLP_GROUPS = 8
CMLP_GROUP_DIM = CMLP_INNER // CMLP_GROUPS
RET_HEADS = 4
RET_KEY_DIM = D_MODEL // RET_HEADS
RET_VALUE_DIM = 2 * RET_KEY_DIM
RET_CHUNK = 128
FFN_DIM = (7 * D_MODEL) // 2
MOE_EXPERTS = 8
MOE_TOP_K = 2

kernel_name = 'hybrid_diffusion_interleaved_gqa_gmlp_retention_moe'


def layer_norm(x, g, b):
    xf = x.astype(jnp.float32)
    mu = jnp.mean(xf, axis=-1, keepdims=True)
    var = jnp.mean(jnp.square(xf - mu), axis=-1, keepdims=True)
    y = (xf - mu) * lax.rsqrt(var + LN_EPS) * g.astype(jnp.float32) + b.astype(jnp.float32)
    return y.astype(x.dtype)


def rms_norm(x, g=None):
    xf = x.astype(jnp.float32)
    y = xf * lax.rsqrt(jnp.mean(jnp.square(xf), axis=-1, keepdims=True) + RMS_EPS)
    if g is not None:
        y = y * g.astype(jnp.float32)
    return y.astype(x.dtype)


def axial_rope(x, row, col):
    d = x.shape[-1]
    nf = d // 4
    inv_freq = ROPE_THETA ** (-jnp.arange(nf, dtype=jnp.float32) / nf)
    xf = x.astype(jnp.float32)

    def rotate(seg, pos):
        ang = pos.astype(jnp.float32)[:, None] * inv_freq
        cos = jnp.cos(ang)[None, :, None, :]
        sin = jnp.sin(ang)[None, :, None, :]
        a, b = seg[..., :nf], seg[..., nf:]
        return jnp.concatenate([a * cos - b * sin, b * cos + a * sin], axis=-1)

    out = jnp.concatenate([rotate(xf[..., : d // 2], row), rotate(xf[..., d // 2:], col)], axis=-1)
    return out.astype(x.dtype)


def adaln(cond, w, b):
    return jnp.split(jax.nn.silu(cond) @ w + b, 6, axis=-1)


def modulate(x, shift, scale):
    return x * (1.0 + scale) + shift


def attend(q, k, v):
    s = jnp.einsum('bqhgd,bkhd->bhgqk', q, k).astype(jnp.float32)
    p = jax.nn.softmax(s, axis=-1).astype(v.dtype)
    return jnp.einsum('bhgqk,bkhd->bqhgd', p, v)


def attention_mixer(hc, hx, row, col, wqkv, q_norm, k_norm, wo, need_ctx):
    B, S, _ = hx.shape
    C = hc.shape[1]
    nq = ATTN_Q_HEADS * ATTN_HEAD_DIM
    nkv = ATTN_KV_HEADS * ATTN_HEAD_DIM
    scale = ATTN_HEAD_DIM ** -0.5

    def project(h):
        b_, L, _ = h.shape
        t = h @ wqkv
        q = rms_norm(t[..., :nq].reshape(b_, L, ATTN_Q_HEADS, ATTN_HEAD_DIM), q_norm)
        k = rms_norm(t[..., nq:nq + nkv].reshape(b_, L, ATTN_KV_HEADS, ATTN_HEAD_DIM), k_norm)
        v = t[..., nq + nkv:].reshape(b_, L, ATTN_KV_HEADS, ATTN_HEAD_DIM)
        return q, k, v

    qc, kc, vc = project(hc)
    qx, kx, vx = project(hx)
    qx = axial_rope(qx, row, col)
    kx = axial_rope(kx, row, col)
    k_all = jnp.concatenate([kc, kx], axis=1)
    v_all = jnp.concatenate([vc, vx], axis=1)
    n_blk = S // Q_BLOCK
    q_blocks = (qx * scale).reshape(B, n_blk, Q_BLOCK, ATTN_KV_HEADS, ATTN_GROUP, ATTN_HEAD_DIM).swapaxes(0, 1)
    o_blocks = lax.map(lambda qb: attend(qb, k_all, v_all), q_blocks)
    yx = o_blocks.swapaxes(0, 1).reshape(B, S, nq) @ wo
    yc = None
    if need_ctx:
        oc = attend((qc * scale).reshape(B, C, ATTN_KV_HEADS, ATTN_GROUP, ATTN_HEAD_DIM), kc, vc)
        yc = oc.reshape(B, C, nq) @ wo
    return yc, yx


def chunk_mlp_mixer(hc, hx, w_in, b_in, v_norm_g, v_norm_b, w_s, b_s, w_out, b_out, need_ctx):
    def run(h):
        B, L, _ = h.shape
        z = jax.nn.gelu(h @ w_in + b_in)
        u, v = z[..., :CMLP_INNER], z[..., CMLP_INNER:]
        v = layer_norm(v, v_norm_g, v_norm_b)
        v = v.reshape(B, L // CMLP_CHUNK, CMLP_CHUNK, CMLP_GROUPS, CMLP_GROUP_DIM)
        v = jnp.einsum('gpq,bnqgc->bnpgc', w_s, v) + b_s.T[:, :, None]
        return (u * v.reshape(B, L, CMLP_INNER)) @ w_out + b_out

    yx = run(hx)
    yc = run(hc) if need_ctx else None
    return yc, yx


def retention_scan(q, k, v, log_gamma, state0):
    B, L, H, _ = q.shape
    n = L // RET_CHUNK

    def to_chunks(t):
        return t.reshape(B, n, RET_CHUNK, H, t.shape[-1]).transpose(1, 0, 3, 2, 4)

    idx = jnp.arange(RET_CHUNK, dtype=jnp.float32)
    diff = idx[:, None] - idx[None, :]
    inner_decay = jnp.where(diff >= 0, jnp.exp(jnp.maximum(diff, 0.0) * log_gamma[:, None, None]), 0.0)
    cross_decay = jnp.exp((idx + 1.0) * log_gamma[:, None])
    state_weight = jnp.exp((RET_CHUNK - 1.0 - idx) * log_gamma[:, None])
    chunk_decay = jnp.exp(RET_CHUNK * log_gamma)

    def step(state, qkv):
        qc, kc, vc = qkv
        scores = jnp.einsum('bhid,bhjd->bhij', qc, kc) * inner_decay
        out = (jnp.einsum('bhij,bhje->bhie', scores, vc)
               + jnp.einsum('bhid,bhde->bhie', qc, state) * cross_decay[..., None])
        state = (state * chunk_decay[:, None, None]
                 + jnp.einsum('bhjd,bhje->bhde', kc * state_weight[..., None], vc))
        return state, out

    state, out = lax.scan(step, state0, (to_chunks(q), to_chunks(k), to_chunks(v)))
    out = out.transpose(1, 0, 3, 2, 4).reshape(B, L, H, v.shape[-1])
    return out, state


def retention_mixer(hc, hx, row, col, wqkvg, decay_raw, wo, need_ctx):
    nk = RET_HEADS * RET_KEY_DIM
    nv = RET_HEADS * RET_VALUE_DIM
    log_gamma = -jnp.exp(decay_raw.astype(jnp.float32))

    def project(h, rotate):
        B, L, _ = h.shape
        t = h @ wqkvg
        q = t[..., :nk].reshape(B, L, RET_HEADS, RET_KEY_DIM)
        k = t[..., nk:2 * nk].reshape(B, L, RET_HEADS, RET_KEY_DIM) * (RET_KEY_DIM ** -0.5)
        v = t[..., 2 * nk:2 * nk + nv].reshape(B, L, RET_HEADS, RET_VALUE_DIM)
        g = t[..., 2 * nk + nv:]
        if rotate:
            q = axial_rope(q, row, col)
            k = axial_rope(k, row, col)
        return q.astype(jnp.float32), k.astype(jnp.float32), v.astype(jnp.float32), g

    def flip(t):
        return jnp.flip(t, axis=1)

    def finish(o_fwd, o_bwd_rev, g):
        o = rms_norm(o_fwd + flip(o_bwd_rev))
        B, L = o.shape[:2]
        return (jax.nn.silu(g) * o.reshape(B, L, nv).astype(g.dtype)) @ wo

    qc, kc, vc, gc = project(hc, False)
    qx, kx, vx, gx = project(hx, True)
    zero = jnp.zeros((hx.shape[0], RET_HEADS, RET_KEY_DIM, RET_VALUE_DIM), jnp.float32)
    oc_f, state_f = retention_scan(qc, kc, vc, log_gamma[0], zero)
    oc_b, state_b = retention_scan(flip(qc), flip(kc), flip(vc), log_gamma[1], zero)
    ox_f, _ = retention_scan(qx, kx, vx, log_gamma[0], state_f)
    ox_b, _ = retention_scan(flip(qx), flip(kx), flip(vx), log_gamma[1], state_b)
    yx = finish(ox_f, ox_b, gx)
    yc = finish(oc_f, oc_b, gc) if need_ctx else None
    return yc, yx


def swiglu(h, w_gu, w_down):
    g, u = jnp.split(h @ w_gu, 2, axis=-1)
    return (jax.nn.silu(g) * u) @ w_down


def moe_swiglu(h, router, w_gu, w_down):
    shp = h.shape
    t = h.reshape(-1, shp[-1])
    logits = (t @ router).astype(jnp.float32)
    top_logit, top_idx = lax.top_k(logits, MOE_TOP_K)
    top_w = jax.nn.softmax(top_logit, axis=-1)
    gates = jnp.sum(jax.nn.one_hot(top_idx, MOE_EXPERTS, dtype=jnp.float32) * top_w[..., None], axis=1)
    out = jnp.zeros_like(t)
    for e in range(MOE_EXPERTS):
        out = out + gates[:, e:e + 1].astype(t.dtype) * swiglu(t, w_gu[e], w_down[e])
    return out.reshape(shp)


def setup_inputs(seed: int = 0) -> dict:
    key = jax.random.key(seed)
    keys = iter(jax.random.split(key, 96))

    def nrm(shape, scale):
        return scale * jax.random.normal(next(keys), shape, jnp.float32)

    D = D_MODEL
    p = {}
    p['x'] = nrm((BATCH, SEQ, D), 1.0)
    p['c'] = nrm((BATCH, D), 1.0)
    p['ctx'] = nrm((BATCH, CTX_LEN, D), 1.0)
    p['c_ctx'] = nrm((D,), 1.0)
    for i in range(DEPTH):
        pre = 'l%d_' % i
        p[pre + 'ada_w'] = nrm((D, 6 * D), D ** -0.5)
        p[pre + 'ada_b'] = nrm((6 * D,), 0.02)
        p[pre + 'ln1_g'] = 1.0 + nrm((D,), 0.02)
        p[pre + 'ln1_b'] = nrm((D,), 0.02)
        p[pre + 'ln2_g'] = 1.0 + nrm((D,), 0.02)
        p[pre + 'ln2_b'] = nrm((D,), 0.02)
        kind = i % N_MIXERS
        if kind == 0:
            nq = ATTN_Q_HEADS * ATTN_HEAD_DIM
            p[pre + 'attn_wqkv'] = nrm((D, nq + 2 * ATTN_KV_HEADS * ATTN_HEAD_DIM), D ** -0.5)
            p[pre + 'attn_q_norm'] = 1.0 + nrm((ATTN_HEAD_DIM,), 0.02)
            p[pre + 'attn_k_norm'] = 1.0 + nrm((ATTN_HEAD_DIM,), 0.02)
            p[pre + 'attn_wo'] = nrm((nq, D), nq ** -0.5 * DEEPNORM_BETA)
        elif kind == 1:
            p[pre + 'cmlp_w_in'] = nrm((D, 2 * CMLP_INNER), D ** -0.5)
            p[pre + 'cmlp_b_in'] = nrm((2 * CMLP_INNER,), 0.02)
            p[pre + 'cmlp_v_norm_g'] = 1.0 + nrm((CMLP_INNER,), 0.02)
            p[pre + 'cmlp_v_norm_b'] = nrm((CMLP_INNER,), 0.02)
            p[pre + 'cmlp_w_s'] = nrm((CMLP_GROUPS, CMLP_CHUNK, CMLP_CHUNK), CMLP_CHUNK ** -0.5)
            p[pre + 'cmlp_b_s'] = 1.0 + nrm((CMLP_GROUPS, CMLP_CHUNK), 0.02)
            p[pre + 'cmlp_w_out'] = nrm((CMLP_INNER, D), CMLP_INNER ** -0.5 * DEEPNORM_BETA)
            p[pre + 'cmlp_b_out'] = nrm((D,), 0.02)
        else:
            nk = RET_HEADS * RET_KEY_DIM
            nv = RET_HEADS * RET_VALUE_DIM
            p[pre + 'ret_wqkvg'] = nrm((D, 2 * nk + 2 * nv), D ** -0.5)
            gamma = 1.0 - jnp.exp2(-5.0 - jnp.arange(RET_HEADS, dtype=jnp.float32))
            base = jnp.log(-jnp.log(gamma))
            p[pre + 'ret_decay'] = base[None, :] + nrm((2, RET_HEADS), 0.1)
            p[pre + 'ret_wo'] = nrm((nv, D), nv ** -0.5 * DEEPNORM_BETA)
        if i % 2 == 0:
            p[pre + 'ffn_w_gu'] = nrm((D, 2 * FFN_DIM), D ** -0.5)
            p[pre + 'ffn_w_down'] = nrm((FFN_DIM, D), FFN_DIM ** -0.5 * DEEPNORM_BETA)
        else:
            p[pre + 'moe_router'] = nrm((D, MOE_EXPERTS), D ** -0.5)
            p[pre + 'moe_w_gu'] = nrm((MOE_EXPERTS, D, 2 * FFN_DIM), D ** -0.5)
            p[pre + 'moe_w_down'] = nrm((MOE_EXPERTS, FFN_DIM, D), FFN_DIM ** -0.5 * DEEPNORM_BETA)
    return p


def reference(x, c, ctx, c_ctx,
              l0_ada_w, l0_ada_b, l0_ln1_g, l0_ln1_b, l0_ln2_g, l0_ln2_b,
              l0_attn_wqkv, l0_attn_q_norm, l0_attn_k_norm, l0_attn_wo,
              l0_ffn_w_gu, l0_ffn_w_down,
              l1_ada_w, l1_ada_b, l1_ln1_g, l1_ln1_b, l1_ln2_g, l1_ln2_b,
              l1_cmlp_w_in, l1_cmlp_b_in, l1_cmlp_v_norm_g, l1_cmlp_v_norm_b,
              l1_cmlp_w_s, l1_cmlp_b_s, l1_cmlp_w_out, l1_cmlp_b_out,
              l1_moe_router, l1_moe_w_gu, l1_moe_w_down,
              l2_ada_w, l2_ada_b, l2_ln1_g, l2_ln1_b, l2_ln2_g, l2_ln2_b,
              l2_ret_wqkvg, l2_ret_decay, l2_ret_wo,
              l2_ffn_w_gu, l2_ffn_w_down,
              l3_ada_w, l3_ada_b, l3_ln1_g, l3_ln1_b, l3_ln2_g, l3_ln2_b,
              l3_attn_wqkv, l3_attn_q_norm, l3_attn_k_norm, l3_attn_wo,
              l3_moe_router, l3_moe_w_gu, l3_moe_w_down):
    layers = [
        (l0_ada_w, l0_ada_b, (l0_ln1_g, l0_ln1_b), (l0_ln2_g, l0_ln2_b),
         (l0_attn_wqkv, l0_attn_q_norm, l0_attn_k_norm, l0_attn_wo),
         (l0_ffn_w_gu, l0_ffn_w_down)),
        (l1_ada_w, l1_ada_b, (l1_ln1_g, l1_ln1_b), (l1_ln2_g, l1_ln2_b),
         (l1_cmlp_w_in, l1_cmlp_b_in, l1_cmlp_v_norm_g, l1_cmlp_v_norm_b,
          l1_cmlp_w_s, l1_cmlp_b_s, l1_cmlp_w_out, l1_cmlp_b_out),
         (l1_moe_router, l1_moe_w_gu, l1_moe_w_down)),
        (l2_ada_w, l2_ada_b, (l2_ln1_g, l2_ln1_b), (l2_ln2_g, l2_ln2_b),
         (l2_ret_wqkvg, l2_ret_decay, l2_ret_wo),
         (l2_ffn_w_gu, l2_ffn_w_down)),
        (l3_ada_w, l3_ada_b, (l3_ln1_g, l3_ln1_b), (l3_ln2_g, l3_ln2_b),
         (l3_attn_wqkv, l3_attn_q_norm, l3_attn_k_norm, l3_attn_wo),
         (l3_moe_router, l3_moe_w_gu, l3_moe_w_down)),
    ]
    n_tok = x.shape[1]
    ROWS = n_tok // GRID_W
    row = jnp.repeat(jnp.arange(ROWS, dtype=jnp.int32), GRID_W)
    col = jnp.arange(ROWS * GRID_W, dtype=jnp.int32) % GRID_W
    C = ctx.shape[1]
    xc = ctx
    for i in range(DEPTH):
        ada_w, ada_b, ln1, ln2, mix_p, ffn_p = layers[i]
        need_ctx = i < DEPTH - 1
        sh1x, sc1x, g1x, sh2x, sc2x, g2x = [t[:, None, :] for t in adaln(c, ada_w, ada_b)]
        sh1c, sc1c, g1c, sh2c, sc2c, g2c = adaln(c_ctx, ada_w, ada_b)

        hx = modulate(x, sh1x, sc1x)
        hc = modulate(xc, sh1c, sc1c)
        kind = i % N_MIXERS
        if kind == 0:
            yc, yx = attention_mixer(hc, hx, row, col, *mix_p, need_ctx=need_ctx)
        elif kind == 1:
            yc, yx = chunk_mlp_mixer(hc, hx, *mix_p, need_ctx=need_ctx)
        else:
            yc, yx = retention_mixer(hc, hx, row, col, *mix_p, need_ctx=need_ctx)
        x = layer_norm(DEEPNORM_ALPHA * x + g1x * yx, *ln1)
        if need_ctx:
            xc = layer_norm(DEEPNORM_ALPHA * xc + g1c * yc, *ln1)

        channel = swiglu if i % 2 == 0 else moe_swiglu
        hx = modulate(x, sh2x, sc2x)
        if need_ctx:
            hc = modulate(xc, sh2c, sc2c)
            f = channel(jnp.concatenate([hc, hx], axis=1), *ffn_p)
            fc, fx = f[:, :C], f[:, C:]
            xc = layer_norm(DEEPNORM_ALPHA * xc + g2c * fc, *ln2)
        else:
            fx = channel(hx, *ffn_p)
        x = layer_norm(DEEPNORM_ALPHA * x + g2x * fx, *ln2)
    return x
```

```python
import os
import numpy as np
import concourse.bass as bass
import concourse.mybir as mybir
from concourse.bass_utils import run_bass_kernel_spmd

F32 = mybir.dt.float32
BF16 = mybir.dt.bfloat16
I32 = mybir.dt.int32
AF = mybir.ActivationFunctionType
ALU = mybir.AluOpType
AX = mybir.AxisListType

ENGS = ("pe", "act", "dve", "pool", "sp")
N_DMA_SEMS = 12


class _Ins:
    __slots__ = ("eng", "fn", "deps", "dma", "idx", "signal", "sig_count", "dma_slot", "dma_round", "waits")

    def __init__(self, eng, fn, dma):
        self.eng = eng
        self.fn = fn
        self.dma = dma
        self.deps = set()
        self.signal = False
        self.sig_count = 0
        self.waits = None


class Prog:
    def __init__(self, nc, sync_same_engine=True):
        self.nc = nc
        self.lists = {e: [] for e in ENGS}
        self.state = {}
        self.sync_same_engine = sync_same_engine
        self.dma_count = {"sp": 0, "pool": 0, "act": 0}

    def op(self, eng, fn, reads=(), writes=(), dma=False):
        ins = _Ins(eng, fn, dma)
        ins.idx = len(self.lists[eng])
        for k in reads:
            st = self.state.get(k)
            if st is not None and st[0] is not None:
                ins.deps.add(st[0])
        for k in writes:
            st = self.state.get(k)
            if st is not None:
                if st[0] is not None:
                    ins.deps.add(st[0])
                for r in st[1]:
                    ins.deps.add(r)
        ins.deps.discard(ins)
        for k in reads:
            st = self.state.setdefault(k, [None, []])
            st[1].append(ins)
        for k in writes:
            self.state[k] = [ins, []]
        if dma:
            n = self.dma_count[eng]
            self.dma_count[eng] = n + 1
            ins.dma_slot = n % N_DMA_SEMS
            ins.dma_round = n // N_DMA_SEMS + 1
        self.lists[eng].append(ins)
        return ins

    def finalize(self, final_waits=()):
        nc = self.nc
        for e in ENGS:
            for ins in self.lists[e]:
                for d in ins.deps:
                    if d.dma:
                        continue
                    if d.eng == ins.eng and not ins.dma:
                        if d.eng == "pe" or not self.sync_same_engine:
                            continue
                    d.signal = True
        for ins in final_waits:
            if not ins.dma:
                ins.signal = True
        for e in ENGS:
            c = 0
            for ins in self.lists[e]:
                if ins.signal and not ins.dma:
                    c += 1
                    ins.sig_count = c
        self.sig_totals = {e: sum(1 for i in self.lists[e] if i.signal and not i.dma) for e in ENGS}
        import contextlib
        with contextlib.ExitStack() as es:
            sems = {e: es.enter_context(nc.semaphore("s_" + e)) for e in ENGS}
            dsems = {q: [es.enter_context(nc.semaphore("d_%s_%d" % (q, i))) for i in range(N_DMA_SEMS)]
                     for q in ("sp", "pool", "act")}
            block = es.enter_context(nc.Block())
            engobj = {"pe": "tensor", "act": "scalar", "dve": "vector", "pool": "gpsimd", "sp": "sync"}

            def make_body(e):
                lst = self.lists[e]

                def body(engine):
                    waited = {}

                    def wait(sem, val, key):
                        if waited.get(key, 0) >= val:
                            return
                        waited[key] = val
                        engine.wait_ge(sem, val)

                    for ins in lst:
                        for d in ins.deps:
                            if d.dma:
                                wait(dsems[d.eng][d.dma_slot], 16 * d.dma_round, ("d", d.eng, d.dma_slot))
                            else:
                                if d.eng == e and not ins.dma and (e == "pe" or not self.sync_same_engine):
                                    continue
                                wait(sems[d.eng], d.sig_count, ("c", d.eng))
                        if ins.dma:
                            if ins.dma_round > 1:
                                wait(dsems[e][ins.dma_slot], 16 * (ins.dma_round - 1), ("d", e, ins.dma_slot))
                        r = ins.fn(engine)
                        if ins.dma:
                            r.then_inc(dsems[e][ins.dma_slot], 16)
                        elif ins.signal:
                            r.then_inc(sems[e], 1)
                    if e == "sp":
                        for ins in final_waits:
                            if ins.dma:
                                wait(dsems[ins.eng][ins.dma_slot], 16 * ins.dma_round, ("d", ins.eng, ins.dma_slot))
                            else:
                                wait(sems[ins.eng], ins.sig_count, ("c", ins.eng))
                return body

            for e in ENGS:
                if not self.lists[e] and e != "sp":
                    continue
                getattr(block, engobj[e])(make_body(e))
        return self


D = 1024
DEPTH = 4
SEQ = 4096
BATCH = 4
CTX = 256
TOK = 2048
NT = 18
NTOK = NT * 128
FFN = 3584
NEXP = 8
NEXP_RUN = int(os.environ.get("MK_NEXP_RUN", "8"))
ALPHA = (2 * DEPTH) ** 0.25
LN_EPS = 1e-5
RMS_EPS = 1e-6
GROUPS = [(0, 2), (2, 4), (6, 4), (10, 4), (14, 4)]
KC = D // 128


class Arena:
    def __init__(self, ap, ncols):
        self.ap = ap
        self.ncols = ncols
        self.top = 0
        self.marks = []

    def alloc(self, free_shape, dtype=F32, parts=128):
        n = 1
        for s in free_shape:
            n *= s
        words = n if dtype in (F32, I32) else (n + 1) // 2
        words = (words + 7) // 8 * 8
        assert self.top + words <= self.ncols, ("arena overflow", self.top, words, self.ncols)
        v = self.ap[0:parts, self.top:self.top + words]
        self.top += words
        if dtype not in (F32,):
            v = v.bitcast(dtype)
        v = v[:, 0:n]
        if len(free_shape) > 1:
            names = "abcdefg"[:len(free_shape)]
            pat = "p (%s) -> p %s" % (" ".join(names), " ".join(names))
            v = v.rearrange(pat, **{names[q]: free_shape[q] for q in range(1, len(free_shape))})
        return v

    def push(self):
        self.marks.append(self.top)

    def pop(self):
        self.top = self.marks.pop()


class Ctx:
    pass


def _barrier(P):
    last = []
    for e in ENGS:
        lst = P.lists[e]
        if not lst:
            continue
        for ins in reversed(lst):
            if not ins.dma:
                last.append(ins)
                break
        seen = set()
        for ins in reversed(lst):
            if ins.dma and ins.dma_slot not in seen:
                seen.add(ins.dma_slot)
                last.append(ins)
            if len(seen) == N_DMA_SEMS:
                break
    P.barrier_set = last
    P.state = {}
    P.after_barrier = {e: True for e in ENGS}


_orig_op = Prog.op


def _op_with_barrier(self, eng, fn, reads=(), writes=(), dma=False):
    ins = _orig_op(self, eng, fn, reads, writes, dma)
    if getattr(self, "after_barrier", None) and self.after_barrier.get(eng):
        for b in self.barrier_set:
            if b is not ins:
                ins.deps.add(b)
        self.after_barrier[eng] = False
    return ins


Prog.op = _op_with_barrier
Prog.barrier = _barrier


ARENA_COLS = 53200


def _mm(P, out, lhsT, rhs, start, stop, reads, writes):
    return P.op("pe", lambda e: e.matmul(out, lhsT=lhsT, rhs=rhs, start=start, stop=stop), reads, writes)


def layer_param_names(i):
    pre = "l%d_" % i
    names = ["ada_w", "ada_b", "ln1_g", "ln1_b", "ln2_g", "ln2_b"]
    kind = i % 3
    if kind == 0:
        names += ["attn_wqkv", "attn_q_norm", "attn_k_norm", "attn_wo"]
    elif kind == 1:
        names += ["cmlp_w_in", "cmlp_b_in", "cmlp_v_norm_g", "cmlp_v_norm_b", "cmlp_w_s", "cmlp_b_s",
                  "cmlp_w_out", "cmlp_b_out"]
    else:
        names += ["ret_wqkvg", "ret_decay", "ret_wo"]
    if i % 2 == 0:
        names += ["ffn_w_gu", "ffn_w_down"]
    else:
        names += ["moe_router", "moe_w_gu", "moe_w_down"]
    return [pre + n for n in names]


PARAM_SHAPES = {}


def _param_shape(name):
    n = name[3:]
    shp = {
        "ada_w": [D, 6 * D], "ada_b": [6 * D], "ln1_g": [D], "ln1_b": [D], "ln2_g": [D], "ln2_b": [D],
        "attn_wqkv": [D, 1536], "attn_q_norm": [128], "attn_k_norm": [128], "attn_wo": [D, D],
        "cmlp_w_in": [D, 4096], "cmlp_b_in": [4096], "cmlp_v_norm_g": [2048], "cmlp_v_norm_b": [2048],
        "cmlp_w_s": [8, 128, 128], "cmlp_b_s": [8, 128], "cmlp_w_out": [2048, D], "cmlp_b_out": [D],
        "ret_wqkvg": [D, 6144], "ret_decay": [2, 4], "ret_wo": [2048, D],
        "ffn_w_gu": [D, 2 * FFN], "ffn_w_down": [FFN, D],
        "moe_router": [D, NEXP], "moe_w_gu": [NEXP_RUN, D, 2 * FFN], "moe_w_down": [NEXP_RUN, FFN, D],
    }[n]
    return shp


def build_program(stages, layers_needed):
    nc = bass.Bass("TRN2", target_bir_lowering=False)
    C = Ctx()
    C.nc = nc
    dram = {}

    def din(name, shape, dtype=F32):
        dram[name] = nc.dram_tensor(name, list(shape), dtype, kind="ExternalInput").ap()
        return dram[name]

    din("x_in", [TOK, D])
    din("ctx_in", [CTX, D])
    din("cc", [128, KC, 2])
    din("ident", [128, 128])
    if any(k == "mix" and i % 3 == 0 for k, i in stages):
        din("rope_a", [128, 2, TOK])
        din("rope_o", [128, 2, TOK])
    if any(k == "mix" and i % 3 != 1 for k, i in stages):
        din("x_oth", [TOK, D])
    if any(k == "mix" and i % 3 == 2 for k, i in stages):
        din("rope_r", [128, 2, 2, TOK])
        din("rope_ro", [128, 2, 2, TOK])
        din("ret_dec", [2, 4])
        C.xacc = nc.dram_tensor("xacc", [NTOK, D], F32, kind="Internal").ap()
    for i in layers_needed:
        for n in layer_param_names(i):
            din(n, _param_shape(n))
    x_out = nc.dram_tensor("x_out", [TOK, D], F32, kind="ExternalOutput").ap()
    xc_out = nc.dram_tensor("xc_out", [CTX, D], F32, kind="ExternalOutput").ap()
    C.ada_scr = nc.dram_tensor("ada_scr", [DEPTH, 2, 6 * D], F32, kind="Internal").ap()
    C.dram = dram

    import contextlib
    with contextlib.ExitStack() as es:
        arena_t = es.enter_context(nc.sbuf_tensor("arena", [128, ARENA_COLS], F32))
        A = Arena(arena_t[:], ARENA_COLS)
        C.A = A
        C.psum = [es.enter_context(nc.psum_tensor("ps%d" % i, [128, 512], F32)) for i in range(8)]
        P = Prog(nc)
        C.P = P
        C.Xraw = A.alloc((NT * D,))
        C.X = C.Xraw.rearrange("p (t d) -> p t d", d=D)
        C.hT = A.alloc((KC, NTOK), BF16)
        C.ident = A.alloc((128,))
        C.adaT = A.alloc((DEPTH, 48, 2))
        C.sc1p = A.alloc((DEPTH, 2, KC, 2))
        C.ones_bf2 = A.alloc((128,), BF16)

        P.op("sp", lambda e: e.dma_start(out=C.ident, in_=dram["ident"]), writes=["ident"], dma=True)
        P.op("pool", lambda e: e.memset(C.ones_bf2, 1.0), writes=["ones_bf"])
        for t in range(NT):
            src = dram["ctx_in"][t * 128:(t + 1) * 128, :] if t < 2 else dram["x_in"][(t - 2) * 128:(t - 1) * 128, :]
            P.op("sp", (lambda e, t=t, src=src: e.dma_start(out=C.X[:, t, :], in_=src)), writes=[("X", t)], dma=True)

        emit_adaln(C, layers_needed)
        for kind, i in stages:
            if kind == "ffn":
                emit_ffn(C, i)
            else:
                emit_mixer(C, i)
        P.barrier()
        outs = []
        for t in range(NT):
            dst = xc_out[t * 128:(t + 1) * 128, :] if t < 2 else x_out[(t - 2) * 128:(t - 1) * 128, :]
            outs.append(P.op("sp", (lambda e, t=t, dst=dst: e.dma_start(out=dst, in_=C.X[:, t, :])),
                             reads=[("X", t)], dma=True))
        P.finalize(final_waits=outs)
    nc.mk_inputs = set(dram.keys())
    return nc


def emit_adaln(C, layers):
    P, A, nc = C.P, C.A, C.nc
    P.barrier()
    A.push()
    cc = A.alloc((KC, 2))
    scc = A.alloc((KC, 2))
    wch = [A.alloc((KC, 512)) for _ in range(2)]
    bch = [A.alloc((512,), parts=2) for _ in range(2)]
    rowc = [A.alloc((512,), parts=2) for _ in range(2)]
    psT = C.psum[2]
    P.op("sp", lambda e: e.dma_start(out=cc, in_=C.dram["cc"]), writes=["cc"], dma=True)
    P.op("act", lambda e: e.activation(out=scc, in_=cc, func=AF.Silu), reads=["cc"], writes=["scc"])
    n = 0
    for i in layers:
        w = C.dram["l%d_ada_w" % i].rearrange("(k p) n -> p k n", p=128)
        b = C.dram["l%d_ada_b" % i]
        for cch in range(12):
            s = n % 2
            n += 1
            cs = slice(cch * 512, (cch + 1) * 512)
            P.op("sp", (lambda e, s=s, cs=cs, w=w: e.dma_start(out=wch[s], in_=w[:, :, cs])), writes=[("wch", s)], dma=True)
            P.op("sp", (lambda e, s=s, cs=cs, b=b: e.dma_start(out=bch[s], in_=b[cs].partition_broadcast(2))),
                 writes=[("bch", s)], dma=True)
            ps = C.psum[s]
            for k in range(KC):
                _mm(P, ps[0:2, :], scc[:, k, :], wch[s][:, k, :], k == 0, k == KC - 1,
                    reads=["scc", ("wch", s)], writes=[("bank", s)])
            P.op("dve", (lambda e, s=s, ps=ps: e.tensor_tensor(out=rowc[s], in0=ps[0:2, :], in1=bch[s], op=ALU.add)),
                 reads=[("bank", s), ("bch", s)], writes=[("rowc", s)])
            P.op("sp", (lambda e, s=s, cs=cs, i=i: e.dma_start(out=C.ada_scr[i, :, cs], in_=rowc[s])),
                 reads=[("rowc", s)], writes=[("ada_scr", i)], dma=True)
            for q in range(4):
                ch = cch * 4 + q
                _mm(P, psT[:, ch * 2:(ch + 1) * 2], rowc[s][:, q * 128:(q + 1) * 128], C.ident[0:2, 0:2], True, True,
                    reads=[("rowc", s), "ident"], writes=[("bank", 2)])
        P.op("dve", (lambda e, i=i: e.tensor_copy(out=C.adaT[:, i, :, :], in_=psT[:, 0:96].rearrange("p (c j) -> p c j", j=2))),
             reads=[("bank", 2)], writes=[("adaT", i)])
        for sub, c0 in ((0, 8), (1, 32)):
            P.op("dve", (lambda e, i=i, sub=sub, c0=c0: e.tensor_scalar(
                out=C.sc1p[:, i, sub, :, :], in0=C.adaT[:, i, c0:c0 + 8, :], scalar1=1.0, scalar2=None, op0=ALU.add)),
                reads=[("adaT", i)], writes=[("sc1p", i, sub)])
    A.pop()
    P.barrier()


def emit_build_hT(C, i, sub, need_ctx=True, router=None):
    P, A = C.P, C.A
    shift_c0 = 0 if sub == 0 else 24
    tiles = range(NT) if need_ctx else range(2, NT)
    for t in tiles:
        j = 1 if t < 2 else 0
        s = t % 2
        ps = (C.psum[4 + 2 * s], C.psum[5 + 2 * s])
        for k in range(KC):
            pst = ps[k // 4][:, (k % 4) * 128:(k % 4 + 1) * 128]
            P.op("pe", (lambda e, t=t, k=k, pst=pst: e.transpose(pst, C.X[:, t, k * 128:(k + 1) * 128], C.ident)),
                 reads=[("X", t), "ident"], writes=[("bank", 4 + 2 * s + k // 4)])
        for k in range(KC):
            pst = ps[k // 4][:, (k % 4) * 128:(k % 4 + 1) * 128]
            P.op("dve", (lambda e, t=t, k=k, pst=pst, j=j: e.tensor_scalar(
                out=C.hT[:, k, t * 128:(t + 1) * 128], in0=pst,
                scalar1=C.sc1p[:, i, sub, k, j:j + 1], scalar2=C.adaT[:, i, shift_c0 + k, j:j + 1],
                op0=ALU.mult, op1=ALU.add)),
                reads=[("bank", 4 + 2 * s + k // 4)], writes=[("hT", t, k)])
            if router is not None:
                h32 = router["h32"][s]
                P.op("dve", (lambda e, t=t, k=k, pst=pst, j=j, h32=h32: e.tensor_scalar(
                    out=h32[:, k, :], in0=pst,
                    scalar1=C.sc1p[:, i, sub, k, j:j + 1], scalar2=C.adaT[:, i, shift_c0 + k, j:j + 1],
                    op0=ALU.mult, op1=ALU.add)),
                    reads=[("bank", 4 + 2 * s + k // 4)], writes=[("h32", s, k)])
        if router is not None and not os.environ.get("MK_DBG_NORMM"):
            psl = C.psum[s]
            for k in range(KC):
                _mm(P, psl[:, 0:NEXP], router["h32"][s][:, k, :], router["w"][:, k, :], k == 0, k == KC - 1,
                    reads=[("h32", s, k), "router_w"], writes=[("bank", s)])
            P.op("act", (lambda e, t=t, psl=psl: e.copy(out=router["logits"][:, t, :], in_=psl[:, 0:NEXP])),
                 reads=[("bank", s)], writes=[("logits", t)])


def emit_scale_x(C, need_ctx=True):
    P = C.P
    for t in (range(NT) if need_ctx else range(2, NT)):
        P.op("pool", (lambda e, t=t: e.tensor_scalar(out=C.X[:, t, :], in0=C.X[:, t, :], scalar1=float(ALPHA),
                                                     scalar2=None, op0=ALU.mult)),
             reads=[("X", t)], writes=[("X", t)])


def emit_load_bc(C, i, sub, tiles):
    P = C.P
    gate_c0 = 2 * D if sub == 0 else 5 * D
    srcs = {
        "gx": C.ada_scr[i, 0, gate_c0:gate_c0 + D], "gc": C.ada_scr[i, 1, gate_c0:gate_c0 + D],
        "lg": C.dram["l%d_ln%d_g" % (i, sub + 1)], "lb": C.dram["l%d_ln%d_b" % (i, sub + 1)],
    }
    for n, src in srcs.items():
        P.op("sp", (lambda e, n=n, src=src: e.dma_start(out=tiles[n], in_=src.partition_broadcast(128))),
             reads=[("ada_scr", i)], writes=[("bc", n)], dma=True)


def emit_ln(C, tiles, need_ctx=True):
    P, A = C.P, C.A
    A.push()
    stats = A.alloc((NT, 2, 6))
    mv = A.alloc((NT, 2))
    rstd = A.alloc((NT,))
    nmr = A.alloc((NT,))
    t0 = 0 if need_ctx else 2
    for t in range(t0, NT):
        for hh in range(2):
            P.op("dve", (lambda e, t=t, hh=hh: e.bn_stats(out=stats[:, t, hh, :], in_=C.X[:, t, hh * 512:(hh + 1) * 512])),
                 reads=[("X", t)], writes=[("stats", t, hh)])
        P.op("dve", (lambda e, t=t: e.bn_aggr(out=mv[:, t, :], in_=stats[:, t, :, :])),
             reads=[("stats", t, 0), ("stats", t, 1)], writes=[("mv", t)])
    mvk = [("mv", t) for t in range(t0, NT)]
    P.op("dve", lambda e: e.tensor_scalar(out=rstd[:, t0:NT], in0=mv[:, t0:NT, 1], scalar1=float(LN_EPS), scalar2=None, op0=ALU.add),
         reads=mvk, writes=["rstd"])
    P.op("act", lambda e: e.activation(out=rstd[:, t0:NT], in_=rstd[:, t0:NT], func=AF.Sqrt), reads=["rstd"], writes=["rstd"])
    P.op("dve", lambda e: e.reciprocal(out=rstd[:, t0:NT], in_=rstd[:, t0:NT]), reads=["rstd"], writes=["rstd"])
    P.op("dve", lambda e: e.scalar_tensor_tensor(out=nmr[:, t0:NT], in0=mv[:, t0:NT, 0], scalar=-1.0, in1=rstd[:, t0:NT],
                                                 op0=ALU.mult, op1=ALU.mult), reads=mvk + ["rstd"], writes=["nmr"])
    for t in range(t0, NT):
        P.op("act", (lambda e, t=t: e.activation(out=C.X[:, t, :], in_=C.X[:, t, :], func=AF.Identity,
                                                 bias=nmr[:, t:t + 1], scale=rstd[:, t:t + 1])),
             reads=[("X", t), "rstd", "nmr"], writes=[("X", t)])
        P.op("pool", (lambda e, t=t: e.tensor_tensor(out=C.X[:, t, :], in0=C.X[:, t, :], in1=tiles["lg"], op=ALU.mult)),
             reads=[("X", t), ("bc", "lg")], writes=[("X", t)])
        P.op("pool", (lambda e, t=t: e.tensor_tensor(out=C.X[:, t, :], in0=C.X[:, t, :], in1=tiles["lb"], op=ALU.add)),
             reads=[("X", t), ("bc", "lb")], writes=[("X", t)])
    A.pop()


def hT_keys(t0, n):
    return [("hT", t, k) for t in range(t0, t0 + n) for k in range(KC)]


def emit_ffn(C, i):
    P, A, nc = C.P, C.A, C.nc
    moe = (i % 2 == 1)
    need_ctx = i < DEPTH - 1
    P.barrier()
    A.push()
    bc = {n: A.alloc((D,)) for n in ("gx", "gc", "lg", "lb")}
    emit_load_bc(C, i, 1, bc)
    router = None
    if moe and not os.environ.get("MK_DBG_NOROUTER"):
        router = {
            "h32": [A.alloc((KC, 128)) for _ in range(2)],
            "w": A.alloc((KC, NEXP)),
            "logits": A.alloc((NT, NEXP)),
        }
        rw = C.dram["l%d_moe_router" % i].rearrange("(k p) e -> p k e", p=128)
        P.op("sp", lambda e: e.dma_start(out=router["w"], in_=rw), writes=["router_w"], dma=True)
    emit_build_hT(C, i, 1, need_ctx=need_ctx, router=router)
    emit_scale_x(C, need_ctx=need_ctx)
    gates = None
    if moe and not os.environ.get("MK_DBG_NOGATES"):
        gates = emit_gates(C, router, need_ctx)
    if moe:
        wgu = C.dram["l%d_moe_w_gu" % i]
        wdn = C.dram["l%d_moe_w_down" % i]
        blocks = [(e, j) for e in range(NEXP_RUN) for j in range(FFN // 512)]
    else:
        wgu = C.dram["l%d_ffn_w_gu" % i]
        wdn = C.dram["l%d_ffn_w_down" % i]
        blocks = [(None, j) for j in range(FFN // 512)]
    Wg = [A.alloc((KC, 512), BF16) for _ in range(2)]
    Wu = [A.alloc((KC, 512), BF16) for _ in range(2)]
    Wd = [A.alloc((4, D), BF16) for _ in range(2)]
    act = [A.alloc((4, 512), BF16) for _ in range(2)]
    sg = [A.alloc((512,)) for _ in range(2)]
    tmp = [A.alloc((D,)) for _ in range(2)]
    groups = GROUPS if need_ctx else GROUPS[1:]

    def load_block(bi):
        e_, j = blocks[bi]
        s = bi % 2
        gu = (wgu[e_] if moe else wgu).rearrange("(k p) n -> p k n", p=128)
        dn = (wdn[e_] if moe else wdn)[j * 512:(j + 1) * 512, :].rearrange("(f p) n -> p f n", p=128)
        P.op("pool", (lambda e: e.dma_start(out=Wg[s], in_=gu[:, :, j * 512:(j + 1) * 512])), writes=[("Wg", s)], dma=True)
        P.op("pool", (lambda e: e.dma_start(out=Wu[s], in_=gu[:, :, FFN + j * 512:FFN + (j + 1) * 512])),
             writes=[("Wu", s)], dma=True)
        P.op("pool", (lambda e: e.dma_start(out=Wd[s], in_=dn)), writes=[("Wd", s)], dma=True)

    load_block(0)
    cnt = 0
    for bi, (e_, j) in enumerate(blocks):
        s = bi % 2
        if bi + 1 < len(blocks):
            load_block(bi + 1)
        for (t0, ntile) in groups:
            ntok = ntile * 128
            a = cnt % 2
            cnt += 1
            for fb in range(4):
                pg = C.psum[(fb % 2) * 2]
                pu = C.psum[(fb % 2) * 2 + 1]
                for k in range(KC):
                    _mm(P, pg[:, 0:ntok], Wg[s][:, k, fb * 128:(fb + 1) * 128], C.hT[:, k, t0 * 128:t0 * 128 + ntok],
                        k == 0, k == KC - 1, reads=([("Wg", s)] + hT_keys(t0, ntile)) if k in (0, KC - 1) else [], writes=[("bank", (fb % 2) * 2)])
                for k in range(KC):
                    _mm(P, pu[:, 0:ntok], Wu[s][:, k, fb * 128:(fb + 1) * 128], C.hT[:, k, t0 * 128:t0 * 128 + ntok],
                        k == 0, k == KC - 1, reads=([("Wu", s)] + hT_keys(t0, ntile)) if k in (0, KC - 1) else [], writes=[("bank", (fb % 2) * 2 + 1)])
                sgt = sg[fb % 2]
                P.op("act", (lambda e, pg=pg, sgt=sgt, ntok=ntok: e.activation(out=sgt[:, 0:ntok], in_=pg[:, 0:ntok], func=AF.Silu)),
                     reads=[("bank", (fb % 2) * 2)], writes=[("sg", fb % 2)])
                P.op("dve", (lambda e, pu=pu, sgt=sgt, ntok=ntok, a=a, fb=fb: e.tensor_tensor(
                    out=act[a][:, fb, 0:ntok], in0=pu[:, 0:ntok], in1=sgt[:, 0:ntok], op=ALU.mult)),
                    reads=[("bank", (fb % 2) * 2 + 1), ("sg", fb % 2)], writes=[("act", a, fb)])
            for tt in range(ntile):
                t = t0 + tt
                o = (cnt + tt) % 2
                po = (C.psum[4 + 2 * o], C.psum[5 + 2 * o])
                for nh in range(2):
                    for fb in range(4):
                        _mm(P, po[nh][:, :], act[a][:, fb, tt * 128:(tt + 1) * 128], Wd[s][:, fb, nh * 512:(nh + 1) * 512],
                            fb == 0, fb == 3, reads=[("act", a, fb), ("Wd", s)], writes=[("bank", 4 + 2 * o + nh)])
                gbc = bc["gc"] if t < 2 else bc["gx"]
                tm = tmp[o]
                for nh in range(2):
                    if moe and gates is not None:
                        P.op("dve", (lambda e, po=po, nh=nh, tm=tm, gbc=gbc, t=t, e_=e_: e.scalar_tensor_tensor(
                            out=tm[:, nh * 512:(nh + 1) * 512], in0=po[nh][:, :], scalar=gates[:, t, e_:e_ + 1],
                            in1=gbc[:, nh * 512:(nh + 1) * 512], op0=ALU.mult, op1=ALU.mult)),
                            reads=[("bank", 4 + 2 * o + nh), ("bc", "gx"), ("bc", "gc"), "gates"], writes=[("tmp", o, nh)])
                    else:
                        P.op("dve", (lambda e, po=po, nh=nh, tm=tm, gbc=gbc: e.tensor_tensor(
                            out=tm[:, nh * 512:(nh + 1) * 512], in0=po[nh][:, :], in1=gbc[:, nh * 512:(nh + 1) * 512], op=ALU.mult)),
                            reads=[("bank", 4 + 2 * o + nh), ("bc", "gx"), ("bc", "gc")], writes=[("tmp", o, nh)])
                P.op("pool", (lambda e, t=t, tm=tm: e.tensor_tensor(out=C.X[:, t, :], in0=C.X[:, t, :], in1=tm, op=ALU.add)),
                     reads=[("X", t), ("tmp", o, 0), ("tmp", o, 1)], writes=[("X", t)])
    emit_ln(C, bc, need_ctx=need_ctx)
    A.pop()
    P.barrier()


def emit_gates(C, router, need_ctx):
    P, A = C.P, C.A
    L = router["logits"]
    t0 = 0 if need_ctx else 2
    n = NT - t0
    Lv = L[:, t0:NT, :]
    gates = A.alloc((NT, NEXP))
    m1 = A.alloc((NT,))
    m2 = A.alloc((NT,))
    mk1 = A.alloc((NT, NEXP))
    mk2 = A.alloc((NT, NEXP))
    l2 = A.alloc((NT, NEXP))
    w1 = A.alloc((NT,))
    w2 = A.alloc((NT,))
    lk = [("logits", t) for t in range(t0, NT)]

    def bcast(v):
        return v[:, t0:NT][:, :, None].broadcast_to([128, n, NEXP])

    P.op("dve", lambda e: e.tensor_reduce(out=m1[:, t0:NT], in_=Lv, axis=AX.X, op=ALU.max), reads=lk, writes=["m1"])
    P.op("dve", lambda e: e.tensor_tensor(out=mk1[:, t0:NT, :], in0=Lv, in1=bcast(m1), op=ALU.is_equal), reads=lk + ["m1"], writes=["mk1"])
    P.op("dve", lambda e: e.scalar_tensor_tensor(out=l2[:, t0:NT, :], in0=mk1[:, t0:NT, :], scalar=-1e30, in1=Lv,
                                                 op0=ALU.mult, op1=ALU.add), reads=lk + ["mk1"], writes=["l2"])
    P.op("dve", lambda e: e.tensor_reduce(out=m2[:, t0:NT], in_=l2[:, t0:NT, :], axis=AX.X, op=ALU.max), reads=["l2"], writes=["m2"])
    P.op("dve", lambda e: e.tensor_tensor(out=mk2[:, t0:NT, :], in0=l2[:, t0:NT, :], in1=bcast(m2), op=ALU.is_equal),
         reads=["l2", "m2"], writes=["mk2"])
    P.op("dve", lambda e: e.tensor_tensor(out=w2[:, t0:NT], in0=m2[:, t0:NT], in1=m1[:, t0:NT], op=ALU.subtract),
         reads=["m1", "m2"], writes=["w2"])
    P.op("act", lambda e: e.activation(out=w2[:, t0:NT], in_=w2[:, t0:NT], func=AF.Exp), reads=["w2"], writes=["w2"])
    P.op("dve", lambda e: e.tensor_scalar(out=w1[:, t0:NT], in0=w2[:, t0:NT], scalar1=1.0, scalar2=None, op0=ALU.add),
         reads=["w2"], writes=["w1"])
    P.op("dve", lambda e: e.reciprocal(out=w1[:, t0:NT], in_=w1[:, t0:NT]), reads=["w1"], writes=["w1"])
    P.op("dve", lambda e: e.tensor_tensor(out=w2[:, t0:NT], in0=w2[:, t0:NT], in1=w1[:, t0:NT], op=ALU.mult),
         reads=["w1", "w2"], writes=["w2"])
    P.op("dve", lambda e: e.tensor_tensor(out=mk1[:, t0:NT, :], in0=mk1[:, t0:NT, :], in1=bcast(w1), op=ALU.mult),
         reads=["mk1", "w1"], writes=["mk1"])
    P.op("dve", lambda e: e.tensor_tensor(out=mk2[:, t0:NT, :], in0=mk2[:, t0:NT, :], in1=bcast(w2), op=ALU.mult),
         reads=["mk2", "w2"], writes=["mk2"])
    P.op("dve", lambda e: e.tensor_tensor(out=gates[:, t0:NT, :], in0=mk1[:, t0:NT, :], in1=mk2[:, t0:NT, :], op=ALU.add),
         reads=["mk1", "mk2"], writes=["gates"])
    return gates


def emit_mixer(C, i):
    kind = i % 3
    if kind == 0:
        emit_attention(C, i)
    elif kind == 1:
        emit_cmlp(C, i)
    else:
        emit_retention(C, i)


PAIR_GROUPS = [[0, 1], [2, 3], [4, 5], [6, 7]]


def emit_rot_copy(P, dst, src, half, rkey, wkey):
    n = src.shape[-1]
    for b0 in range(0, n, 2 * half):
        P.op("pool", (lambda e, b0=b0: e.tensor_copy(out=dst[:, :, b0:b0 + half], in_=src[:, :, b0 + half:b0 + 2 * half])),
             reads=[rkey], writes=[wkey])
        P.op("pool", (lambda e, b0=b0: e.tensor_copy(out=dst[:, :, b0 + half:b0 + 2 * half], in_=src[:, :, b0:b0 + half])),
             reads=[rkey], writes=[wkey])


def emit_load_gain(C, dst, dst_rot, src, half, scale, key):
    P = C.P
    col = src.rearrange("(p o) -> p o", o=1)
    P.op("sp", lambda e: e.dma_start(out=dst, in_=col), writes=[key], dma=True)
    for b0 in range(0, 128, 2 * half):
        P.op("sp", (lambda e, b0=b0: e.dma_start(out=dst_rot[b0:b0 + half, :], in_=col[b0 + half:b0 + 2 * half, :])),
             writes=[key + "_r"], dma=True)
        P.op("sp", (lambda e, b0=b0: e.dma_start(out=dst_rot[b0 + half:b0 + 2 * half, :], in_=col[b0:b0 + half, :])),
             writes=[key + "_r"], dma=True)
    if scale != 1.0:
        P.op("dve", lambda e: e.tensor_scalar(out=dst, in0=dst, scalar1=float(scale), scalar2=None, op0=ALU.mult),
             reads=[key], writes=[key])
        P.op("dve", lambda e: e.tensor_scalar(out=dst_rot, in0=dst_rot, scalar1=float(scale), scalar2=None, op0=ALU.mult),
             reads=[key + "_r"], writes=[key + "_r"])


def emit_qk_norm_rope(C, ps_q, ps_qr, ps_ss, ntok, out_bf, gain, gain_r, cos, sin, tmp, eps_t, inv_d, tag, rope, out_key):
    P = C.P
    sq, rstd, ta, tb = tmp
    kq, kqr, kss = ("bank", ps_q[1]), ("bank", ps_qr[1]), ("bank", ps_ss[1])
    pq, pqr, pss = ps_q[0], ps_qr[0], ps_ss[0]
    P.op("act", lambda e: e.activation(out=sq[:, 0:ntok], in_=pq[:, 0:ntok], func=AF.Square), reads=[kq], writes=[tag + "sq"])
    _mm(P, pss[:, 0:ntok], C.ones_bf2, sq[:, 0:ntok], True, True, reads=[tag + "sq", "ones_bf"], writes=[kss])
    P.op("dve", lambda e: e.tensor_scalar(out=rstd[:, 0:ntok], in0=pss[:, 0:ntok], scalar1=float(inv_d), scalar2=float(RMS_EPS),
                                          op0=ALU.mult, op1=ALU.add), reads=[kss], writes=[tag + "rstd"])
    P.op("act", lambda e: e.activation(out=rstd[:, 0:ntok], in_=rstd[:, 0:ntok], func=AF.Ln),
         reads=[tag + "rstd"], writes=[tag + "rstd"])
    P.op("act", lambda e: e.activation(out=rstd[:, 0:ntok], in_=rstd[:, 0:ntok], func=AF.Exp, scale=-0.5),
         reads=[tag + "rstd"], writes=[tag + "rstd"])
    if not rope:
        P.op("dve", lambda e: e.scalar_tensor_tensor(out=out_bf, in0=pq[:, 0:ntok], scalar=gain, in1=rstd[:, 0:ntok],
                                                     op0=ALU.mult, op1=ALU.mult),
             reads=[kq, tag + "rstd", "gains"], writes=[out_key])
        return
    P.op("dve", lambda e: e.scalar_tensor_tensor(out=ta[:, 0:ntok], in0=pq[:, 0:ntok], scalar=gain, in1=rstd[:, 0:ntok],
                                                 op0=ALU.mult, op1=ALU.mult), reads=[kq, tag + "rstd", "gains"], writes=[tag + "ta"])
    P.op("dve", lambda e: e.scalar_tensor_tensor(out=tb[:, 0:ntok], in0=pqr[:, 0:ntok], scalar=gain_r, in1=rstd[:, 0:ntok],
                                                 op0=ALU.mult, op1=ALU.mult), reads=[kqr, tag + "rstd", "gains"], writes=[tag + "tb"])
    P.op("pool", lambda e: e.tensor_tensor(out=ta[:, 0:ntok], in0=ta[:, 0:ntok], in1=cos, op=ALU.mult),
         reads=[tag + "ta", "rope"], writes=[tag + "ta"])
    P.op("pool", lambda e: e.tensor_tensor(out=tb[:, 0:ntok], in0=tb[:, 0:ntok], in1=sin, op=ALU.mult),
         reads=[tag + "tb", "rope"], writes=[tag + "tb"])
    P.op("dve", lambda e: e.tensor_tensor(out=out_bf, in0=ta[:, 0:ntok], in1=tb[:, 0:ntok], op=ALU.add),
         reads=[tag + "ta", tag + "tb"], writes=[out_key])


def emit_attention(C, i):
    P, A, nc = C.P, C.A, C.nc
    need_ctx = i < DEPTH - 1
    wqkv = C.dram["l%d_attn_wqkv" % i].rearrange("(k p) n -> p k n", p=128)
    wo = C.dram["l%d_attn_wo" % i]
    P.barrier()
    A.push()
    gx = A.alloc((D,))
    gc = A.alloc((D,))
    bc = {"gx": gx, "gc": gc}
    gate_c0 = 2 * D
    P.op("sp", lambda e: e.dma_start(out=gx, in_=C.ada_scr[i, 0, gate_c0:gate_c0 + D].partition_broadcast(128)), writes=[("bc", "gx")], dma=True)
    P.op("sp", lambda e: e.dma_start(out=gc, in_=C.ada_scr[i, 1, gate_c0:gate_c0 + D].partition_broadcast(128)), writes=[("bc", "gc")], dma=True)
    emit_build_hT(C, i, 0, need_ctx=True)
    emit_scale_x(C, need_ctx=need_ctx)

    rope = A.alloc((2, TOK))
    P.op("sp", lambda e: e.dma_start(out=rope, in_=C.dram["rope_a"]), writes=["rope"], dma=True)
    Kall = A.alloc((2, CTX + SEQ), BF16)
    Vall = A.alloc((34, 256), BF16)
    gk, gkr, gq, gqr, eps_t = (A.alloc((1,)) for _ in range(5))
    emit_load_gain(C, gk, gkr, C.dram["l%d_attn_k_norm" % i], 32, 1.0, "gk")
    emit_load_gain(C, gq, gqr, C.dram["l%d_attn_q_norm" % i], 32, 128 ** -0.5, "gq")
    P.op("pool", lambda e: e.memset(eps_t, float(RMS_EPS)), writes=["eps"])
    tmp = (A.alloc((512,), BF16), A.alloc((512,)), A.alloc((512,)), A.alloc((512,)))
    bank = lambda n: (C.psum[n], n)

    A.push()
    Wk = A.alloc((KC, 256), BF16)
    Wkr = A.alloc((KC, 256), BF16)
    Wv = A.alloc((KC, 256), BF16)
    Xo = A.alloc((D,))
    hTo = A.alloc((KC, 512), BF16)
    rope_o = A.alloc((2, 512))
    P.op("pool", lambda e: e.dma_start(out=Wk, in_=wqkv[:, :, 1024:1280]), writes=["Wk"], dma=True)
    P.op("pool", lambda e: e.dma_start(out=Wv, in_=wqkv[:, :, 1280:1536]), writes=["Wv"], dma=True)
    emit_rot_copy(P, Wkr, Wk, 32, "Wk", "Wkr")

    def kproj(src, c0, ntok, hkeys, out, cos, sin, isctx, hk, okey):
        for k in range(KC):
            _mm(P, C.psum[0][:, 0:ntok], Wk[:, k, hk * 128:(hk + 1) * 128], src[:, k, c0:c0 + ntok],
                k == 0, k == KC - 1, reads=["Wk"] + hkeys, writes=[("bank", 0)])
        if not isctx:
            for k in range(KC):
                _mm(P, C.psum[1][:, 0:ntok], Wkr[:, k, hk * 128:(hk + 1) * 128], src[:, k, c0:c0 + ntok],
                    k == 0, k == KC - 1, reads=["Wkr"] + hkeys, writes=[("bank", 1)])
        emit_qk_norm_rope(C, bank(0), bank(1), bank(2), ntok, out, gk, gkr, cos, sin, tmp, eps_t, 1.0 / 128, "k",
                          not isctx, okey)

    def vproj(src, c0, hkeys, dst, okey, pb):
        pv = C.psum[pb]
        for k in range(KC):
            _mm(P, pv[:, 0:256], src[:, k, c0:c0 + 128], Wv[:, k, :], k == 0, k == KC - 1,
                reads=["Wv"] + hkeys, writes=[("bank", pb)])
        P.op("act", (lambda e: e.copy(out=dst, in_=pv[:, 0:256])), reads=[("bank", pb)], writes=[okey])

    for (t0, ntile) in GROUPS:
        ntok = ntile * 128
        isctx = t0 < 2
        for hk in range(2):
            if isctx:
                kproj(C.hT, 0, ntok, hT_keys(t0, ntile), Kall[:, hk, 0:CTX], None, None, True, hk, ("Kall", hk, "c"))
            else:
                c0 = (t0 - 2) * 128
                kproj(C.hT, t0 * 128, ntok, hT_keys(t0, ntile), Kall[:, hk, CTX + c0:CTX + c0 + ntok],
                      rope[:, 0, c0:c0 + ntok], rope[:, 1, c0:c0 + ntok], False, hk, ("Kall", hk, t0))
        for tt in range(ntile):
            t = t0 + tt
            vproj(C.hT, t * 128, hT_keys(t, 1), Vall[:, t, :], ("Vall", t), 4 + t % 2)
    xoth = C.dram["x_oth"]
    for g in range(4):
        P.op("sp", (lambda e, g=g: e.dma_start(out=rope_o, in_=C.dram["rope_o"][:, :, g * 512:(g + 1) * 512])),
             writes=["rope_o"], dma=True)
        for tt in range(4):
            tg = g * 4 + tt
            P.op("sp", (lambda e, tg=tg: e.dma_start(out=Xo, in_=xoth[tg * 128:(tg + 1) * 128, :])), writes=["Xo"], dma=True)
            for k in range(KC):
                pst = C.psum[6 + k // 4][:, (k % 4) * 128:(k % 4 + 1) * 128]
                P.op("pe", (lambda e, k=k, pst=pst: e.transpose(pst, Xo[:, k * 128:(k + 1) * 128], C.ident)),
                     reads=["Xo", "ident"], writes=[("bank", 6 + k // 4)])
            for k in range(KC):
                pst = C.psum[6 + k // 4][:, (k % 4) * 128:(k % 4 + 1) * 128]
                P.op("dve", (lambda e, k=k, pst=pst, tt=tt: e.tensor_scalar(
                    out=hTo[:, k, tt * 128:(tt + 1) * 128], in0=pst,
                    scalar1=C.sc1p[:, i, 0, k, 0:1], scalar2=C.adaT[:, i, k, 0:1], op0=ALU.mult, op1=ALU.add)),
                    reads=[("bank", 6 + k // 4)], writes=[("hTo", tt, k)])
        hk_o = [("hTo", tt, k) for tt in range(4) for k in range(KC)]
        c0 = CTX + TOK + g * 512
        for hk in range(2):
            kproj(hTo, 0, 512, hk_o, Kall[:, hk, c0:c0 + 512], rope_o[:, 0, :], rope_o[:, 1, :], False, hk, ("Kall", hk, "o", g))
        for tt in range(4):
            vproj(hTo, tt * 128, [("hTo", tt, k) for k in range(KC)], Vall[:, 18 + g * 4 + tt, :], ("Vall", 18 + g * 4 + tt), 4 + tt % 2)
    A.pop()
    P.barrier()

    Wq = [A.alloc((KC, 128), BF16) for _ in range(2)]
    Wqr = [A.alloc((KC, 128), BF16) for _ in range(2)]
    Wo = [A.alloc((D,), BF16) for _ in range(2)]
    qT = [A.alloc((512,), BF16) for _ in range(2)]
    PT = [A.alloc((512,), BF16) for _ in range(3)]
    oT = [A.alloc((512,), BF16) for _ in range(2)]
    rden = A.alloc((512,))
    ytmp = [A.alloc((D,)) for _ in range(2)]
    groups = GROUPS if need_ctx else GROUPS[1:]

    def load_head(h):
        s = h % 2
        P.op("pool", lambda e: e.dma_start(out=Wq[s], in_=wqkv[:, :, h * 128:(h + 1) * 128]), writes=[("Wq", s)], dma=True)
        P.op("pool", lambda e: e.dma_start(out=Wo[s], in_=wo[h * 128:(h + 1) * 128, :]), writes=[("Wo", s)], dma=True)
        emit_rot_copy(P, Wqr[s], Wq[s], 32, ("Wq", s), ("Wqr", s))

    att_phase = int(os.environ.get("MK_ATT_PHASE", "5"))
    load_head(0)
    cnt = 0
    ycnt = 0
    for h in range(8 if att_phase >= 2 else 0):
        s = h % 2
        hk = h // 4
        if h + 1 < 8:
            load_head(h + 1)
        for (t0, ntile) in groups:
            ntok = ntile * 128
            isctx = t0 < 2
            a = cnt % 2
            cnt += 1
            for k in range(KC):
                _mm(P, C.psum[0][:, 0:ntok], Wq[s][:, k, :], C.hT[:, k, t0 * 128:t0 * 128 + ntok],
                    k == 0, k == KC - 1, reads=[("Wq", s)] + hT_keys(t0, ntile), writes=[("bank", 0)])
            if not isctx:
                for k in range(KC):
                    _mm(P, C.psum[1][:, 0:ntok], Wqr[s][:, k, :], C.hT[:, k, t0 * 128:t0 * 128 + ntok],
                        k == 0, k == KC - 1, reads=[("Wqr", s)] + hT_keys(t0, ntile), writes=[("bank", 1)])
                c0 = (t0 - 2) * 128
                emit_qk_norm_rope(C, bank(0), bank(1), bank(2), ntok, qT[a][:, 0:ntok], gq, gqr, rope[:, 0, c0:c0 + ntok],
                                  rope[:, 1, c0:c0 + ntok], tmp, eps_t, 1.0 / 128, "q", True, ("qT", a))
            else:
                emit_qk_norm_rope(C, bank(0), bank(1), bank(2), ntok, qT[a][:, 0:ntok], gq, gqr, None, None, tmp, eps_t,
                                  1.0 / 128, "q", False, ("qT", a))
            nkt = 2 if isctx else 34
            if att_phase < 3:
                continue
            for kt in range(nkt):
                sb = 4 + kt % 2
                pp = kt % 3
                kkey = "Kall"
                vkey = "Vall"
                _mm(P, C.psum[sb][:, 0:ntok], Kall[:, hk, kt * 128:(kt + 1) * 128], qT[a][:, 0:ntok], True, True,
                    reads=[kkey, ("qT", a)], writes=[("bank", sb)])
                P.op("act", (lambda e, sb=sb, pp=pp, ntok=ntok: e.activation(out=PT[pp][:, 0:ntok], in_=C.psum[sb][:, 0:ntok], func=AF.Exp)),
                     reads=[("bank", sb)], writes=[("PT", pp)])
                if att_phase < 4:
                    continue
                _mm(P, C.psum[3][:, 0:ntok], Vall[:, kt, hk * 128:(hk + 1) * 128], PT[pp][:, 0:ntok], kt == 0, kt == nkt - 1,
                    reads=[vkey, ("PT", pp)], writes=[("bank", 3)])
                _mm(P, C.psum[2][:, 0:ntok], C.ones_bf2, PT[pp][:, 0:ntok], kt == 0, kt == nkt - 1,
                    reads=["ones_bf", ("PT", pp)], writes=[("bank", 2)])
            if att_phase < 4:
                continue
            P.op("dve", (lambda e, ntok=ntok: e.reciprocal(out=rden[:, 0:ntok], in_=C.psum[2][:, 0:ntok])),
                 reads=[("bank", 2)], writes=["rden"])
            P.op("dve", (lambda e, ntok=ntok, a=a: e.tensor_tensor(out=oT[a][:, 0:ntok], in0=C.psum[3][:, 0:ntok], in1=rden[:, 0:ntok], op=ALU.mult)),
                 reads=[("bank", 3), "rden"], writes=[("oT", a)])
            for tt in range(ntile if att_phase >= 5 else 0):
                t = t0 + tt
                o = ycnt % 2
                ycnt += 1
                gbc = gc if t < 2 else gx
                for nh in range(2):
                    _mm(P, C.psum[6 + nh][:, :], oT[a][:, tt * 128:(tt + 1) * 128], Wo[s][:, nh * 512:(nh + 1) * 512], True, True,
                        reads=[("oT", a), ("Wo", s)], writes=[("bank", 6 + nh)])
                    P.op("dve", (lambda e, nh=nh, o=o, gbc=gbc: e.tensor_tensor(
                        out=ytmp[o][:, nh * 512:(nh + 1) * 512], in0=C.psum[6 + nh][:, :], in1=gbc[:, nh * 512:(nh + 1) * 512], op=ALU.mult)),
                        reads=[("bank", 6 + nh), ("bc", "gx"), ("bc", "gc")], writes=[("ytmp", o, nh)])
                P.op("pool", (lambda e, t=t, o=o: e.tensor_tensor(out=C.X[:, t, :], in0=C.X[:, t, :], in1=ytmp[o], op=ALU.add)),
                     reads=[("X", t), ("ytmp", o, 0), ("ytmp", o, 1)], writes=[("X", t)])
    A.pop()
    P.barrier()
    A.push()
    bc2 = {"lg": A.alloc((D,)), "lb": A.alloc((D,))}
    for n, src in (("lg", C.dram["l%d_ln1_g" % i]), ("lb", C.dram["l%d_ln1_b" % i])):
        P.op("sp", (lambda e, n=n, src=src: e.dma_start(out=bc2[n], in_=src.partition_broadcast(128))), writes=[("bc", n)], dma=True)
    emit_ln(C, bc2, need_ctx=need_ctx)
    A.pop()
    P.barrier()


def emit_ln_bc(C, i, sub, need_ctx):
    P, A = C.P, C.A
    P.barrier()
    A.push()
    bc2 = {"lg": A.alloc((D,)), "lb": A.alloc((D,))}
    for n, src in (("lg", C.dram["l%d_ln%d_g" % (i, sub + 1)]), ("lb", C.dram["l%d_ln%d_b" % (i, sub + 1)])):
        P.op("sp", (lambda e, n=n, src=src: e.dma_start(out=bc2[n], in_=src.partition_broadcast(128))), writes=[("bc", n)], dma=True)
    emit_ln(C, bc2, need_ctx=need_ctx)
    A.pop()
    P.barrier()


def emit_cmlp(C, i):
    P, A, nc = C.P, C.A, C.nc
    need_ctx = i < DEPTH - 1
    pre = "l%d_cmlp_" % i
    w_in = C.dram[pre + "w_in"].rearrange("(k p) n -> p k n", p=128)
    w_out = C.dram[pre + "w_out"].rearrange("(c p) n -> p c n", p=128)
    P.barrier()
    A.push()
    gx = A.alloc((D,))
    gc = A.alloc((D,))
    gate_c0 = 2 * D
    P.op("sp", lambda e: e.dma_start(out=gx, in_=C.ada_scr[i, 0, gate_c0:gate_c0 + D].partition_broadcast(128)), writes=[("bc", "gx")], dma=True)
    P.op("sp", lambda e: e.dma_start(out=gc, in_=C.ada_scr[i, 1, gate_c0:gate_c0 + D].partition_broadcast(128)), writes=[("bc", "gc")], dma=True)
    emit_build_hT(C, i, 0, need_ctx=need_ctx)
    emit_scale_x(C, need_ctx=need_ctx)
    gv, bv, bu = A.alloc((16,)), A.alloc((16,)), A.alloc((16,))
    P.op("sp", lambda e: e.dma_start(out=gv, in_=C.dram[pre + "v_norm_g"].rearrange("(c p) -> p c", p=128), allow_slow_non_contiguous=True), writes=["gv"], dma=True)
    P.op("sp", lambda e: e.dma_start(out=bv, in_=C.dram[pre + "v_norm_b"].rearrange("(c p) -> p c", p=128), allow_slow_non_contiguous=True), writes=["bv"], dma=True)
    P.op("sp", lambda e: e.dma_start(out=bu, in_=C.dram[pre + "b_in"][0:2048].rearrange("(c p) -> p c", p=128), allow_slow_non_contiguous=True), writes=["bu"], dma=True)
    brow_v = A.alloc((2048,), BF16, parts=1)
    brow_o = A.alloc((D,), BF16, parts=1)
    P.op("pool", lambda e: e.dma_start(out=brow_v, in_=C.dram[pre + "b_in"][2048:4096].rearrange("(o n) -> o n", o=1)), writes=["brow_v"], dma=True)
    P.op("pool", lambda e: e.dma_start(out=brow_o, in_=C.dram[pre + "b_out"].rearrange("(o n) -> o n", o=1)), writes=["brow_o"], dma=True)
    WsT = A.alloc((8, 128), BF16)
    Bt = A.alloc((16, 128))
    ones_row = C.ones_bf2[0:1, :]
    A.push()
    Wsl = [A.alloc((128,)) for _ in range(2)]
    bsbc = A.alloc((8, 128))
    for g in range(8):
        s = g % 2
        P.op("sp", (lambda e, g=g, s=s: e.dma_start(out=Wsl[s], in_=C.dram[pre + "w_s"][g])), writes=[("Wsl", s)], dma=True)
        P.op("sp", (lambda e, g=g: e.dma_start(out=bsbc[:, g, :], in_=C.dram[pre + "b_s"][g].partition_broadcast(128))), writes=[("bsbc", g)], dma=True)
        P.op("pe", (lambda e, s=s: e.transpose(C.psum[s][:, 0:128], Wsl[s], C.ident)), reads=[("Wsl", s), "ident"], writes=[("bank", s)])
        P.op("act", (lambda e, g=g, s=s: e.copy(out=WsT[:, g, :], in_=C.psum[s][:, 0:128])), reads=[("bank", s)], writes=[("WsT", g)])
        _mm(P, C.psum[2 + s][:, 0:128], C.ones_bf2, WsT[:, g, :], True, True, reads=["ones_bf", ("WsT", g)], writes=[("bank", 2 + s)])
        for cb in (2 * g, 2 * g + 1):
            P.op("dve", (lambda e, g=g, s=s, cb=cb: e.scalar_tensor_tensor(
                out=Bt[:, cb, :], in0=C.psum[2 + s][:, 0:128], scalar=bv[:, cb:cb + 1], in1=bsbc[:, g, :], op0=ALU.mult, op1=ALU.add)),
                reads=[("bank", 2 + s), "bv", ("bsbc", g)], writes=[("Bt", cb)])
    A.pop()
    P.barrier()
    Wv = A.alloc((KC, 512), BF16)
    Wu = A.alloc((KC, 256), BF16)
    Wo = A.alloc((16, 512), BF16)
    z = A.alloc((4, 2048), BF16)
    uvT = A.alloc((16, 512), BF16)
    uT = A.alloc((512,))
    tmp = A.alloc((512,))
    ytmp = [A.alloc((512,)) for _ in range(2)]
    stats = A.alloc((4, 4, 6))
    mv = A.alloc((4, 2))
    rstd = A.alloc((4,))
    nmr = A.alloc((4,))
    groups = GROUPS if need_ctx else GROUPS[1:]
    ycnt = 0
    for (t0, ntile) in groups:
        ntok = ntile * 128
        for vb in range(4):
            P.op("pool", (lambda e, vb=vb: e.dma_start(out=Wv, in_=w_in[:, :, 2048 + vb * 512:2048 + (vb + 1) * 512])), writes=["Wv"], dma=True)
            for tt in range(ntile):
                t = t0 + tt
                pb = tt % 2
                _mm(P, C.psum[pb][:, :], ones_row, brow_v[:, vb * 512:(vb + 1) * 512], True, False, reads=["ones_bf", "brow_v"], writes=[("bank", pb)])
                for k in range(KC):
                    _mm(P, C.psum[pb][:, :], C.hT[:, k, t * 128:(t + 1) * 128], Wv[:, k, :], False, k == KC - 1,
                        reads=["Wv"] + hT_keys(t, 1), writes=[("bank", pb)])
                P.op("act", (lambda e, tt=tt, vb=vb, pb=pb: e.activation(out=z[:, tt, vb * 512:(vb + 1) * 512], in_=C.psum[pb][:, :], func=AF.Gelu_apprx_tanh)),
                     reads=[("bank", pb)], writes=[("z", tt, vb)])
        for tt in range(ntile):
            for vb in range(4):
                P.op("dve", (lambda e, tt=tt, vb=vb: e.bn_stats(out=stats[:, tt, vb, :], in_=z[:, tt, vb * 512:(vb + 1) * 512])),
                     reads=[("z", tt, vb)], writes=[("zst", tt, vb)])
            P.op("dve", (lambda e, tt=tt: e.bn_aggr(out=mv[:, tt, :], in_=stats[:, tt, :, :])), reads=[("zst", tt, vb) for vb in range(4)], writes=[("zmv", tt)])
        zk = [("zmv", tt) for tt in range(ntile)]
        P.op("dve", (lambda e, ntile=ntile: e.tensor_scalar(out=rstd[:, 0:ntile], in0=mv[:, 0:ntile, 1], scalar1=float(LN_EPS), scalar2=None, op0=ALU.add)),
             reads=zk, writes=["zrstd"])
        P.op("act", (lambda e, ntile=ntile: e.activation(out=rstd[:, 0:ntile], in_=rstd[:, 0:ntile], func=AF.Sqrt)), reads=["zrstd"], writes=["zrstd"])
        P.op("dve", (lambda e, ntile=ntile: e.reciprocal(out=rstd[:, 0:ntile], in_=rstd[:, 0:ntile])), reads=["zrstd"], writes=["zrstd"])
        P.op("dve", (lambda e, ntile=ntile: e.scalar_tensor_tensor(out=nmr[:, 0:ntile], in0=mv[:, 0:ntile, 0], scalar=-1.0, in1=rstd[:, 0:ntile],
                                                                  op0=ALU.mult, op1=ALU.mult)), reads=zk + ["zrstd"], writes=["znmr"])
        for tt in range(ntile):
            P.op("act", (lambda e, tt=tt: e.activation(out=z[:, tt, :], in_=z[:, tt, :], func=AF.Identity, bias=nmr[:, tt:tt + 1], scale=rstd[:, tt:tt + 1])),
                 reads=[("z", tt, vb) for vb in range(4)] + ["zrstd", "znmr"], writes=[("zn", tt)])
        for cb in range(16):
            g = cb // 2
            if cb % 2 == 0:
                P.op("pool", (lambda e, cb=cb: e.dma_start(out=Wu, in_=w_in[:, :, cb * 128:(cb + 2) * 128])), writes=["Wu"], dma=True)
            pu = C.psum[2 + cb % 2]
            ps = C.psum[4 + cb % 2]
            for k in range(KC):
                _mm(P, pu[:, 0:ntok], Wu[:, k, (cb % 2) * 128:(cb % 2 + 1) * 128], C.hT[:, k, t0 * 128:t0 * 128 + ntok],
                    k == 0, k == KC - 1, reads=["Wu"] + hT_keys(t0, ntile), writes=[("bank", 2 + cb % 2)])
            P.op("dve", (lambda e, pu=pu, cb=cb, ntok=ntok: e.tensor_scalar(out=uT[:, 0:ntok], in0=pu[:, 0:ntok], scalar1=bu[:, cb:cb + 1], scalar2=None, op0=ALU.add)),
                 reads=[("bank", 2 + cb % 2), "bu"], writes=["uT"])
            P.op("act", (lambda e, ntok=ntok: e.activation(out=uT[:, 0:ntok], in_=uT[:, 0:ntok], func=AF.Gelu_apprx_tanh)),
                 reads=["uT"], writes=["uT"])
            for tt in range(ntile):
                _mm(P, ps[:, tt * 128:(tt + 1) * 128], z[:, tt, cb * 128:(cb + 1) * 128], WsT[:, g, :], True, True,
                    reads=[("zn", tt), ("WsT", g)], writes=[("bank", 4 + cb % 2)])
            P.op("dve", (lambda e, ps=ps, cb=cb, ntile=ntile, ntok=ntok: e.scalar_tensor_tensor(
                out=tmp[:, 0:ntok].rearrange("p (a b) -> p a b", b=128), in0=ps[:, 0:ntok].rearrange("p (a b) -> p a b", b=128),
                scalar=gv[:, cb:cb + 1], in1=Bt[:, cb:cb + 1, :].broadcast_to([128, ntile, 128]), op0=ALU.mult, op1=ALU.add)),
                reads=[("bank", 4 + cb % 2), "gv", ("Bt", cb)], writes=["cm_tmp"])
            P.op("dve", (lambda e, cb=cb, ntok=ntok: e.tensor_tensor(out=uvT[:, cb, 0:ntok], in0=tmp[:, 0:ntok], in1=uT[:, 0:ntok], op=ALU.mult)),
                 reads=["cm_tmp", "uT"], writes=[("uvT", cb)])
        uk = [("uvT", cb) for cb in range(16)]
        for nh in range(2):
            P.op("pool", (lambda e, nh=nh: e.dma_start(out=Wo, in_=w_out[:, :, nh * 512:(nh + 1) * 512])), writes=["Wo"], dma=True)
            for tt in range(ntile):
                t = t0 + tt
                o = ycnt % 2
                ycnt += 1
                py = C.psum[6 + o]
                gbc = gc if t < 2 else gx
                _mm(P, py[:, :], ones_row, brow_o[:, nh * 512:(nh + 1) * 512], True, False, reads=["ones_bf", "brow_o"], writes=[("bank", 6 + o)])
                for cb in range(16):
                    _mm(P, py[:, :], uvT[:, cb, tt * 128:(tt + 1) * 128], Wo[:, cb, :], False, cb == 15,
                        reads=["Wo"] + (uk if cb in (0, 15) else []), writes=[("bank", 6 + o)])
                P.op("dve", (lambda e, py=py, o=o, gbc=gbc, nh=nh: e.tensor_tensor(out=ytmp[o], in0=py[:, :], in1=gbc[:, nh * 512:(nh + 1) * 512], op=ALU.mult)),
                     reads=[("bank", 6 + o), ("bc", "gx"), ("bc", "gc")], writes=[("ytmp", o)])
                P.op("pool", (lambda e, t=t, o=o, nh=nh: e.tensor_tensor(out=C.X[:, t, nh * 512:(nh + 1) * 512], in0=C.X[:, t, nh * 512:(nh + 1) * 512], in1=ytmp[o], op=ALU.add)),
                     reads=[("X", t), ("ytmp", o)], writes=[("X", t)])
    A.pop()
    emit_ln_bc(C, i, 0, need_ctx)


def emit_retention(C, i):
    P, A, nc = C.P, C.A, C.nc
    need_ctx = i < DEPTH - 1
    w = C.dram["l%d_ret_wqkvg" % i].rearrange("(k p) n -> p k n", p=128)
    wo = C.dram["l%d_ret_wo" % i]
    xacc = C.xacc
    P.barrier()
    A.push()
    gx = A.alloc((D,))
    gc = A.alloc((D,))
    gate_c0 = 2 * D
    P.op("sp", lambda e: e.dma_start(out=gx, in_=C.ada_scr[i, 0, gate_c0:gate_c0 + D].partition_broadcast(128)), writes=[("bc", "gx")], dma=True)
    P.op("sp", lambda e: e.dma_start(out=gc, in_=C.ada_scr[i, 1, gate_c0:gate_c0 + D].partition_broadcast(128)), writes=[("bc", "gc")], dma=True)
    emit_build_hT(C, i, 0, need_ctx=True)
    emit_scale_x(C, need_ctx=True)
    for t in range(NT):
        P.op("sp", (lambda e, t=t: e.dma_start(out=xacc[t * 128:(t + 1) * 128, :], in_=C.X[:, t, :])), reads=[("X", t)], writes=[("xacc", t)], dma=True)
    P.barrier()
    A2 = Arena(C.Xraw, NT * D)
    Kh = A2.alloc((2, CTX + SEQ), BF16)
    Vh = A2.alloc((34, 512), BF16)
    oT32 = A2.alloc((4, 512))
    ogT = A2.alloc((4, 512), BF16)
    Xt = A2.alloc((D,))
    hTo = A.alloc((KC, 512), BF16)
    ropeg = A.alloc((2, 2, 512))
    wreg_top = A.top
    Wreg = A.alloc((6144,))
    qT = [A.alloc((2, 512), BF16) for _ in range(2)]
    PT = [A.alloc((512,), BF16) for _ in range(3)]
    tmp = [A.alloc((512,)) for _ in range(4)]
    sq = A.alloc((4, 512), BF16)
    rstd = A.alloc((512,))
    ytmp = [A.alloc((D,)) for _ in range(2)]
    dec = A.alloc((8,))
    lg = A.alloc((8,))
    nlg = A.alloc((8,))
    lg128 = A.alloc((8,))
    nlg128 = A.alloc((8,))
    dji_i = A.alloc((128,), I32)
    dji = A.alloc((128,))
    mrow_i = A.alloc((40,), I32)
    mrow = A.alloc((40,))
    Ef, Eb, Df, Db, Dd = (A.alloc((128,)) for _ in range(5))
    Tf = A.alloc((4, 128))
    Tb = A.alloc((4, 128))
    pwf, pwb, npwb = A.alloc((40,)), A.alloc((40,)), A.alloc((40,))
    P.op("sp", lambda e: e.dma_start(out=dec, in_=C.dram["ret_dec"].rearrange("a b -> (a b)").partition_broadcast(128)), writes=["dec"], dma=True)
    P.op("act", lambda e: e.activation(out=lg, in_=dec, func=AF.Exp), reads=["dec"], writes=["nlg"])
    P.op("dve", lambda e: e.tensor_scalar(out=nlg, in0=lg, scalar1=1.0, scalar2=None, op0=ALU.mult), reads=["nlg"], writes=["nlg2"])
    P.op("dve", lambda e: e.tensor_scalar(out=lg, in0=nlg, scalar1=-1.0, scalar2=None, op0=ALU.mult), reads=["nlg2"], writes=["lg"])
    P.op("dve", lambda e: e.tensor_scalar(out=lg128, in0=lg, scalar1=128.0, scalar2=None, op0=ALU.mult), reads=["lg"], writes=["lg128"])
    P.op("dve", lambda e: e.tensor_scalar(out=nlg128, in0=nlg, scalar1=128.0, scalar2=None, op0=ALU.mult), reads=["nlg2"], writes=["nlg128"])
    P.op("pool", lambda e: e.iota(dji_i, pattern=[[1, 128]], base=0, channel_multiplier=-1), writes=["dji_i"])
    P.op("pool", lambda e: e.iota(mrow_i, pattern=[[1, 40]], base=0, channel_multiplier=0), writes=["mrow_i"])
    P.op("dve", lambda e: e.tensor_copy(out=dji, in_=dji_i), reads=["dji_i"], writes=["dji"])
    P.op("dve", lambda e: e.tensor_copy(out=mrow, in_=mrow_i), reads=["mrow_i"], writes=["mrow"])
    P.barrier()
    bank = lambda n: ("bank", n)
    hk_all = [("hT", t, k) for t in range(NT) for k in range(KC)]

    def do_head(hd):
        f, bb = hd, 4 + hd
        P.op("act", lambda e: e.activation(out=Ef, in_=dji, func=AF.Exp, scale=lg[:, f:f + 1]), reads=["dji", "lg"], writes=["Ef"])
        P.op("act", lambda e: e.activation(out=Eb, in_=dji, func=AF.Exp, scale=nlg[:, bb:bb + 1]), reads=["dji", "nlg2"], writes=["Eb"])
        P.op("act", lambda e: e.activation(out=pwf, in_=mrow, func=AF.Exp, scale=lg128[:, f:f + 1]), reads=["mrow", "lg128"], writes=["pwf"])
        P.op("act", lambda e: e.activation(out=pwb, in_=mrow, func=AF.Exp, scale=lg128[:, bb:bb + 1]), reads=["mrow", "lg128"], writes=["pwb"])
        P.op("act", lambda e: e.activation(out=npwb, in_=mrow, func=AF.Exp, scale=nlg128[:, bb:bb + 1]), reads=["mrow", "nlg128"], writes=["npwb"])
        P.op("pool", lambda e: e.affine_select(out=Df, in_=Ef, pattern=[[1, 128]], compare_op=ALU.is_ge, fill=0.0, base=0, channel_multiplier=-1),
             reads=["Ef"], writes=["Df"])
        P.op("pool", lambda e: e.affine_select(out=Db, in_=Eb, pattern=[[-1, 128]], compare_op=ALU.is_ge, fill=0.0, base=0, channel_multiplier=1),
             reads=["Eb"], writes=["Db"])
        P.op("dve", lambda e: e.tensor_tensor(out=Dd, in0=Df, in1=Db, op=ALU.add), reads=["Df", "Db"], writes=["Dd"])
        for m in range(4):
            P.op("dve", (lambda e, m=m: e.tensor_scalar(out=Tf[:, m, :], in0=Ef, scalar1=pwf[:, m:m + 1], scalar2=None, op0=ALU.mult)),
                 reads=["Ef", "pwf"], writes=[("Tf", m)])
            P.op("dve", (lambda e, m=m: e.tensor_scalar(out=Tb[:, m, :], in0=Eb, scalar1=npwb[:, m:m + 1], scalar2=None, op0=ALU.mult)),
                 reads=["Eb", "npwb"], writes=[("Tb", m)])
        ret_phase = int(os.environ.get("MK_RET_PHASE", "5"))
        if ret_phase < 2:
            return
        A.top = wreg_top
        Wk = A.alloc((KC, 256), BF16)
        Wkr = A.alloc((KC, 256), BF16)
        Wv = A.alloc((KC, 512), BF16)
        P.op("pool", lambda e: e.dma_start(out=Wk, in_=w[:, :, 1024 + hd * 256:1024 + (hd + 1) * 256]), writes=["Wk"], dma=True)
        P.op("pool", lambda e: e.dma_start(out=Wv, in_=w[:, :, 2048 + hd * 512:2048 + (hd + 1) * 512]), writes=["Wv"], dma=True)
        P.op("pool", lambda e: e.tensor_scalar(out=Wk, in0=Wk, scalar1=0.0625, scalar2=None, op0=ALU.mult), reads=["Wk"], writes=["Wk"])
        emit_rot_copy(P, Wkr, Wk, 64, "Wk", "Wkr")

        def kv_group(src, c0, ntile, hkeys, kcol0, ktile0, isctx, ropet):
            ntok = ntile * 128
            for dc in range(2):
                for k in range(KC):
                    _mm(P, C.psum[0][:, 0:ntok], Wk[:, k, dc * 128:(dc + 1) * 128], src[:, k, c0:c0 + ntok], k == 0, k == KC - 1,
                        reads=["Wk"] + hkeys, writes=[bank(0)])
                if isctx:
                    P.op("act", (lambda e, dc=dc: e.copy(out=Kh[:, dc, kcol0:kcol0 + ntok], in_=C.psum[0][:, 0:ntok])), reads=[bank(0)], writes=["Kh"])
                    continue
                for k in range(KC):
                    _mm(P, C.psum[1][:, 0:ntok], Wkr[:, k, dc * 128:(dc + 1) * 128], src[:, k, c0:c0 + ntok], k == 0, k == KC - 1,
                        reads=["Wkr"] + hkeys, writes=[bank(1)])
                P.op("dve", (lambda e, dc=dc: e.tensor_tensor(out=tmp[0][:, 0:ntok], in0=C.psum[0][:, 0:ntok], in1=ropet[:, dc, 0, 0:ntok], op=ALU.mult)),
                     reads=[bank(0), "ropeg"], writes=["t0"])
                P.op("dve", (lambda e, dc=dc: e.tensor_tensor(out=tmp[1][:, 0:ntok], in0=C.psum[1][:, 0:ntok], in1=ropet[:, dc, 1, 0:ntok], op=ALU.mult)),
                     reads=[bank(1), "ropeg"], writes=["t1"])
                P.op("pool", (lambda e, dc=dc: e.tensor_tensor(out=Kh[:, dc, kcol0:kcol0 + ntok], in0=tmp[0][:, 0:ntok], in1=tmp[1][:, 0:ntok], op=ALU.add)),
                     reads=["t0", "t1"], writes=["Kh"])
            for tt in range(ntile):
                pb = 2 + tt % 2
                for k in range(KC):
                    _mm(P, C.psum[pb][:, :], src[:, k, c0 + tt * 128:c0 + (tt + 1) * 128], Wv[:, k, :], k == 0, k == KC - 1,
                        reads=["Wv"] + hkeys, writes=[bank(pb)])
                P.op("act", (lambda e, tt=tt, pb=pb: e.copy(out=Vh[:, ktile0 + tt, :], in_=C.psum[pb][:, :])), reads=[bank(pb)], writes=["Vh"])

        for (t0, ntile) in GROUPS:
            if t0 < 2:
                kv_group(C.hT, 0, ntile, hk_all, 0, 0, True, None)
            else:
                c0 = (t0 - 2) * 128
                P.op("sp", (lambda e, c0=c0: e.dma_start(out=ropeg, in_=C.dram["rope_r"][:, :, :, c0:c0 + 512])), writes=["ropeg"], dma=True)
                kv_group(C.hT, t0 * 128, ntile, hk_all, CTX + c0, t0, False, ropeg)
        for g in range(4):
            P.op("sp", (lambda e, g=g: e.dma_start(out=ropeg, in_=C.dram["rope_ro"][:, :, :, g * 512:(g + 1) * 512])), writes=["ropeg"], dma=True)
            for tt in range(4):
                tg = g * 4 + tt
                P.op("sp", (lambda e, tg=tg: e.dma_start(out=Xt, in_=C.dram["x_oth"][tg * 128:(tg + 1) * 128, :])), writes=["Xt"], dma=True)
                for k in range(KC):
                    pst = C.psum[6 + k // 4][:, (k % 4) * 128:(k % 4 + 1) * 128]
                    P.op("pe", (lambda e, k=k, pst=pst: e.transpose(pst, Xt[:, k * 128:(k + 1) * 128], C.ident)),
                         reads=["Xt", "ident"], writes=[bank(6 + k // 4)])
                for k in range(KC):
                    pst = C.psum[6 + k // 4][:, (k % 4) * 128:(k % 4 + 1) * 128]
                    P.op("dve", (lambda e, k=k, pst=pst, tt=tt: e.tensor_scalar(
                        out=hTo[:, k, tt * 128:(tt + 1) * 128], in0=pst,
                        scalar1=C.sc1p[:, i, 0, k, 0:1], scalar2=C.adaT[:, i, k, 0:1], op0=ALU.mult, op1=ALU.add)),
                        reads=[bank(6 + k // 4)], writes=[("hTo", tt, k)])
            kv_group(hTo, 0, 4, [("hTo", tt, k) for tt in range(4) for k in range(KC)], CTX + TOK + g * 512, 18 + g * 4, False, ropeg)
        P.barrier()
        if ret_phase < 3:
            return
        A.top = wreg_top
        Wq = A.alloc((KC, 256), BF16)
        Wqr = A.alloc((KC, 256), BF16)
        Wg = A.alloc((KC, 512), BF16)
        Wo = A.alloc((4, D), BF16)
        P.op("pool", lambda e: e.dma_start(out=Wq, in_=w[:, :, hd * 256:(hd + 1) * 256]), writes=["Wq"], dma=True)
        P.op("pool", lambda e: e.dma_start(out=Wg, in_=w[:, :, 4096 + hd * 512:4096 + (hd + 1) * 512]), writes=["Wg"], dma=True)
        P.op("pool", lambda e: e.dma_start(out=Wo, in_=wo[hd * 512:(hd + 1) * 512, :].rearrange("(c p) n -> p c n", p=128)), writes=["Wo"], dma=True)
        emit_rot_copy(P, Wqr, Wq, 64, "Wq", "Wqr")
        ycnt_box = [0]

        def do_group(t0, ntile, a):
            ntok = ntile * 128
            isctx = t0 < 2
            lq0 = t0 - 2
            ycnt = ycnt_box[0]
            hkeys = hT_keys(t0, ntile)
            if not isctx:
                P.op("sp", (lambda e, lq0=lq0: e.dma_start(out=ropeg, in_=C.dram["rope_r"][:, :, :, lq0 * 128:lq0 * 128 + 512])), writes=["ropeg"], dma=True)
            for dc in range(2):
                for k in range(KC):
                    _mm(P, C.psum[0][:, 0:ntok], Wq[:, k, dc * 128:(dc + 1) * 128], C.hT[:, k, t0 * 128:t0 * 128 + ntok], k == 0, k == KC - 1,
                        reads=["Wq"] + hkeys, writes=[bank(0)])
                if isctx:
                    P.op("act", (lambda e, dc=dc, a=a: e.copy(out=qT[a][:, dc, 0:ntok], in_=C.psum[0][:, 0:ntok])), reads=[bank(0)], writes=[("qT", a, dc)])
                    continue
                for k in range(KC):
                    _mm(P, C.psum[1][:, 0:ntok], Wqr[:, k, dc * 128:(dc + 1) * 128], C.hT[:, k, t0 * 128:t0 * 128 + ntok], k == 0, k == KC - 1,
                        reads=["Wqr"] + hkeys, writes=[bank(1)])
                P.op("dve", (lambda e, dc=dc: e.tensor_tensor(out=tmp[0][:, 0:ntok], in0=C.psum[0][:, 0:ntok], in1=ropeg[:, dc, 0, 0:ntok], op=ALU.mult)),
                     reads=[bank(0), "ropeg"], writes=["t0"])
                P.op("dve", (lambda e, dc=dc: e.tensor_tensor(out=tmp[1][:, 0:ntok], in0=C.psum[1][:, 0:ntok], in1=ropeg[:, dc, 1, 0:ntok], op=ALU.mult)),
                     reads=[bank(1), "ropeg"], writes=["t1"])
                P.op("pool", (lambda e, dc=dc, a=a: e.tensor_tensor(out=qT[a][:, dc, 0:ntok], in0=tmp[0][:, 0:ntok], in1=tmp[1][:, 0:ntok], op=ALU.add)),
                     reads=["t0", "t1"], writes=[("qT", a, dc)])
            ktiles = [0, 1] if isctx else list(range(34))
            for ki, kt in enumerate(ktiles):
                sb = 2 + ki % 2
                pp = ki % 3
                ps = C.psum[sb]
                for dc in range(2):
                    _mm(P, ps[:, 0:ntok], Kh[:, dc, kt * 128:(kt + 1) * 128], qT[a][:, dc, 0:ntok], dc == 0, dc == 1,
                        reads=["Kh", ("qT", a, dc)], writes=[bank(sb)])
                pt = PT[pp]
                rk = [bank(sb), "tables"]
                wk_ = [("PT", pp)]

                def one(outv, inv, scal, tab, rk=rk, wk_=wk_, wkeys=None):
                    P.op("dve", (lambda e: e.scalar_tensor_tensor(out=outv, in0=inv, scalar=scal, in1=tab, op0=ALU.mult, op1=ALU.mult)),
                         reads=rk, writes=(wk_ if wkeys is None else wkeys))

                def sub(m, mode, idx, ps=ps, pt=pt):
                    sl = slice(m * 128, (m + 1) * 128)
                    if mode == "f":
                        one(pt[:, sl], ps[:, sl], pwf[:, idx:idx + 1], Ef)
                    elif mode == "b":
                        one(pt[:, sl], ps[:, sl], pwb[:, idx:idx + 1], Eb)
                    else:
                        P.op("dve", (lambda e: e.tensor_tensor(out=pt[:, sl], in0=ps[:, sl], in1=Dd, op=ALU.mult)), reads=rk, writes=wk_)

                Tfv = Tf.rearrange("p a b -> p (a b)")
                Tbv = Tb.rearrange("p a b -> p (a b)")
                if isctx:
                    for m in range(2):
                        if kt < m:
                            sub(m, "f", 1)
                        elif kt == m:
                            sub(m, "d", 0)
                        else:
                            sub(m, "b", 1)
                elif kt < 2:
                    one(tmp[2][:, 0:ntok], ps[:, 0:ntok], pwf[:, lq0 + 2 - kt:lq0 + 3 - kt], Tfv, wkeys=["t2"])
                    P.op("dve", (lambda e, ps=ps, kt=kt: e.scalar_tensor_tensor(out=tmp[3][:, 0:ntok], in0=ps[:, 0:ntok],
                                                                               scalar=pwb[:, 32 + kt - lq0:33 + kt - lq0], in1=Tbv, op0=ALU.mult, op1=ALU.mult)),
                         reads=rk, writes=["t3"])
                    P.op("pool", (lambda e, pt=pt: e.tensor_tensor(out=pt[:, 0:ntok], in0=tmp[2][:, 0:ntok], in1=tmp[3][:, 0:ntok], op=ALU.add)),
                         reads=["t2", "t3"], writes=wk_)
                elif kt < 18:
                    lk = kt - 2
                    if lk < lq0:
                        one(pt[:, 0:ntok], ps[:, 0:ntok], pwf[:, lq0 - lk:lq0 - lk + 1], Tfv)
                    elif lk > lq0 + 3:
                        one(pt[:, 0:ntok], ps[:, 0:ntok], pwb[:, lk - lq0:lk - lq0 + 1], Tbv)
                    else:
                        for m in range(4):
                            dlt = lk - lq0 - m
                            if dlt > 0:
                                sub(m, "b", dlt)
                            elif dlt == 0:
                                sub(m, "d", 0)
                            else:
                                sub(m, "f", -dlt)
                else:
                    lk = 16 + (kt - 18)
                    one(pt[:, 0:ntok], ps[:, 0:ntok], pwb[:, lk - lq0:lk - lq0 + 1], Tbv)
                for eb in range(4):
                    _mm(P, C.psum[4 + eb][:, 0:ntok], Vh[:, kt, eb * 128:(eb + 1) * 128], pt[:, 0:ntok], ki == 0, ki == len(ktiles) - 1,
                        reads=["Vh", ("PT", pp)], writes=[bank(4 + eb)])
            if ret_phase < 4:
                return
            for eb in range(4):
                P.op("act", (lambda e, eb=eb: e.copy(out=oT32[:, eb, 0:ntok], in_=C.psum[4 + eb][:, 0:ntok])), reads=[bank(4 + eb)], writes=[("oT32", eb)])
                P.op("act", (lambda e, eb=eb: e.activation(out=sq[:, eb, 0:ntok], in_=C.psum[4 + eb][:, 0:ntok], func=AF.Square)),
                     reads=[bank(4 + eb)], writes=[("sq", eb)])
            for eb in range(4):
                _mm(P, C.psum[0][:, 0:ntok], C.ones_bf2, sq[:, eb, 0:ntok], eb == 0, eb == 3, reads=[("sq", eb), "ones_bf"], writes=[bank(0)])
            P.op("dve", lambda e: e.tensor_scalar(out=rstd[:, 0:ntok], in0=C.psum[0][:, 0:ntok], scalar1=1.0 / 512, scalar2=float(RMS_EPS),
                                                  op0=ALU.mult, op1=ALU.add), reads=[bank(0)], writes=["rstd"])
            P.op("act", lambda e: e.activation(out=rstd[:, 0:ntok], in_=rstd[:, 0:ntok], func=AF.Ln), reads=["rstd"], writes=["rstd"])
            P.op("act", lambda e: e.activation(out=rstd[:, 0:ntok], in_=rstd[:, 0:ntok], func=AF.Exp, scale=-0.5), reads=["rstd"], writes=["rstd"])
            for eb in range(4):
                pg = C.psum[1 + eb % 2] if False else C.psum[2 + eb % 2]
                pgk = bank(2 + eb % 2)
                for k in range(KC):
                    _mm(P, pg[:, 0:ntok], Wg[:, k, eb * 128:(eb + 1) * 128], C.hT[:, k, t0 * 128:t0 * 128 + ntok], k == 0, k == KC - 1,
                        reads=["Wg"] + hkeys, writes=[pgk])
                P.op("act", (lambda e, pg=pg: e.copy(out=tmp[3][:, 0:ntok], in_=pg[:, 0:ntok])), reads=[pgk], writes=["t3"])
                P.op("act", (lambda e: e.activation(out=tmp[0][:, 0:ntok], in_=tmp[3][:, 0:ntok], func=AF.Exp, scale=-1.0)), reads=["t3"], writes=["t0"])
                P.op("dve", lambda e: e.tensor_scalar(out=tmp[0][:, 0:ntok], in0=tmp[0][:, 0:ntok], scalar1=1.0, scalar2=None, op0=ALU.add),
                     reads=["t0"], writes=["t0"])
                P.op("dve", lambda e: e.reciprocal(out=tmp[0][:, 0:ntok], in_=tmp[0][:, 0:ntok]), reads=["t0"], writes=["t0"])
                P.op("dve", (lambda e: e.tensor_tensor(out=tmp[1][:, 0:ntok], in0=tmp[3][:, 0:ntok], in1=tmp[0][:, 0:ntok], op=ALU.mult)),
                     reads=["t3", "t0"], writes=["t1"])
                P.op("pool", (lambda e, eb=eb: e.tensor_tensor(out=tmp[2][:, 0:ntok], in0=oT32[:, eb, 0:ntok], in1=rstd[:, 0:ntok], op=ALU.mult)),
                     reads=[("oT32", eb), "rstd"], writes=["t2"])
                P.op("dve", (lambda e, eb=eb: e.tensor_tensor(out=ogT[:, eb, 0:ntok], in0=tmp[2][:, 0:ntok], in1=tmp[1][:, 0:ntok], op=ALU.mult)),
                     reads=["t1", "t2"], writes=[("ogT", eb)])
            if ret_phase < 5:
                return
            ok_ = [("ogT", eb) for eb in range(4)]
            for tt in range(ntile):
                t = t0 + tt
                o = ycnt % 2
                ycnt += 1
                gbc = gc if t < 2 else gx
                for nh in range(2):
                    py = C.psum[nh]
                    for eb in range(4):
                        _mm(P, py[:, :], ogT[:, eb, tt * 128:(tt + 1) * 128], Wo[:, eb, nh * 512:(nh + 1) * 512], eb == 0, eb == 3,
                            reads=ok_ + ["Wo"], writes=[bank(nh)])
                    P.op("dve", (lambda e, py=py, nh=nh, o=o, gbc=gbc: e.tensor_tensor(out=ytmp[o][:, nh * 512:(nh + 1) * 512], in0=py[:, :],
                                                                                        in1=gbc[:, nh * 512:(nh + 1) * 512], op=ALU.mult)),
                         reads=[bank(nh), ("bc", "gx"), ("bc", "gc")], writes=[("ytmp", o, nh)])
                P.op("pool", (lambda e, t=t, o=o: e.dma_start(out=xacc[t * 128:(t + 1) * 128, :], in_=ytmp[o], accum_op=ALU.add)),
                     reads=[("ytmp", o, 0), ("ytmp", o, 1)], writes=[("xacc", t)], dma=True)
            ycnt_box[0] = ycnt

        for gi, (t0_, ntile_) in enumerate(GROUPS):
            do_group(t0_, ntile_, gi % 2)
        P.barrier()

    for hd_ in range(4):
        do_head(hd_)
    for t in range(NT):
        P.op("sp", (lambda e, t=t: e.dma_start(out=C.X[:, t, :], in_=xacc[t * 128:(t + 1) * 128, :])), writes=[("X", t)], dma=True)
    A.pop()
    emit_ln_bc(C, i, 0, need_ctx)


_PROG_CACHE = {}


def _rope_tables(hd, nf):
    theta = np.float32(10000.0)
    inv = (theta ** (-(np.arange(nf, dtype=np.float32) / np.float32(nf)))).astype(np.float32)
    n = np.arange(SEQ)
    row = (n // 64).astype(np.float32)
    col = (n % 64).astype(np.float32)
    d = np.arange(hd)
    pos = np.where((d < hd // 2)[:, None], row[None, :], col[None, :]).astype(np.float32)
    ang = (pos * inv[d % nf][:, None]).astype(np.float32)
    sign = np.where((d % (hd // 2)) < nf, -1.0, 1.0).astype(np.float32)[:, None]
    return np.stack([np.cos(ang), np.sin(ang) * sign], axis=1).astype(np.float32)


def _core_inputs(inputs, layers_needed, x0=None, xc0=None, flip_odd=False):
    x = np.asarray(inputs["x"] if x0 is None else x0, dtype=np.float32)
    ctx = np.asarray(inputs["ctx"] if xc0 is None else xc0, dtype=np.float32)
    c = np.asarray(inputs["c"], dtype=np.float32)
    c_ctx = np.asarray(inputs["c_ctx"], dtype=np.float32)
    ident = np.eye(128, dtype=np.float32)
    shared = {"ident": ident}
    rope_a = _rope_tables(128, 32)
    rope_r = _rope_tables(256, 64).reshape(2, 128, 2, SEQ).transpose(1, 0, 2, 3)
    for i in layers_needed:
        for n in layer_param_names(i):
            shared[n] = np.ascontiguousarray(np.asarray(inputs[n], dtype=np.float32))
            if NEXP_RUN < NEXP and ("moe_w_gu" in n or "moe_w_down" in n):
                shared[n] = np.ascontiguousarray(shared[n][:NEXP_RUN])
    maps = []
    for r in range(8):
        b, h = r // 2, r % 2
        cc = np.stack([c[b].reshape(KC, 128).T, c_ctx.reshape(KC, 128).T], axis=-1)
        m = dict(shared)
        own = slice(h * TOK, (h + 1) * TOK)
        oth = slice((1 - h) * TOK, (2 - h) * TOK)
        rev = flip_odd and h == 1
        st = -1 if rev else 1
        m["x_in"] = np.ascontiguousarray(x[b, own][::st])
        m["ctx_in"] = np.ascontiguousarray(ctx[b][::st])
        m["x_oth"] = np.ascontiguousarray(x[b, oth][::st])
        m["cc"] = np.ascontiguousarray(cc.astype(np.float32))
        m["rope_a"] = np.ascontiguousarray(rope_a[:, :, own][:, :, ::st])
        m["rope_o"] = np.ascontiguousarray(rope_a[:, :, oth][:, :, ::st])
        m["rope_r"] = np.ascontiguousarray(rope_r[:, :, :, own][:, :, :, ::st])
        m["rope_ro"] = np.ascontiguousarray(rope_r[:, :, :, oth][:, :, :, ::st])
        for i in layers_needed:
            if i % 3 == 2:
                dec = np.asarray(inputs["l%d_ret_decay" % i], dtype=np.float32)
                m["ret_dec"] = np.ascontiguousarray(dec[::st])
            if i % 3 == 1 and rev:
                m["l%d_cmlp_w_s" % i] = np.ascontiguousarray(shared["l%d_cmlp_w_s" % i][:, ::-1, ::-1])
                m["l%d_cmlp_b_s" % i] = np.ascontiguousarray(shared["l%d_cmlp_b_s" % i][:, ::-1])
        maps.append(m)
    return maps


def run_stages(inputs, stages, x0=None, xc0=None):
    layers_needed = sorted(set(i for _, i in stages))
    key = tuple(stages)
    if key not in _PROG_CACHE:
        _PROG_CACHE[key] = build_program(stages, layers_needed)
    nc = _PROG_CACHE[key]
    flip_odd = any(k == "mix" and i % 3 == 2 for k, i in stages)
    maps = _core_inputs(inputs, layers_needed, x0, xc0, flip_odd)
    maps = [{k: v for k, v in m.items() if k in nc.mk_inputs} for m in maps]
    res = run_bass_kernel_spmd(nc, maps, core_ids=list(range(8)))
    xo = np.zeros((BATCH, SEQ, D), np.float32)
    xco = np.zeros((BATCH, CTX, D), np.float32)
    for r in range(8):
        b, h = r // 2, r % 2
        st = -1 if (flip_odd and h == 1) else 1
        xo[b, h * TOK:(h + 1) * TOK] = res.results[r]["x_out"][::st]
        if h == 0:
            xco[b] = res.results[r]["xc_out"]
    return xo, xco


def kernel(**inputs):
    x, xc = inputs["x"], inputs["ctx"]
    for i in range(DEPTH):
        x, xc = run_stages(inputs, [("mix", i), ("ffn", i)], x0=x, xc0=xc)
    return x
```

```python
import os
import numpy as np
import concourse.bass as bass
import concourse.mybir as mybir
from concourse.bass_utils import run_bass_kernel_spmd

F32 = mybir.dt.float32
BF16 = mybir.dt.bfloat16
I32 = mybir.dt.int32
AF = mybir.ActivationFunctionType
ALU = mybir.AluOpType
AX = mybir.AxisListType

ENGS = ("pe", "act", "dve", "pool", "sp")
N_DMA_SEMS = 12


class _Ins:
    __slots__ = ("eng", "fn", "deps", "dma", "idx", "signal", "sig_count", "dma_slot", "dma_round", "waits")

    def __init__(self, eng, fn, dma):
        self.eng = eng
        self.fn = fn
        self.dma = dma
        self.deps = set()
        self.signal = False
        self.sig_count = 0
        self.waits = None


class Prog:
    def __init__(self, nc, sync_same_engine=True):
        self.nc = nc
        self.lists = {e: [] for e in ENGS}
        self.state = {}
        self.sync_same_engine = sync_same_engine
        self.dma_count = {"sp": 0, "pool": 0, "act": 0}

    def op(self, eng, fn, reads=(), writes=(), dma=False):
        ins = _Ins(eng, fn, dma)
        ins.idx = len(self.lists[eng])
        for k in reads:
            st = self.state.get(k)
            if st is not None and st[0] is not None:
                ins.deps.add(st[0])
        for k in writes:
            st = self.state.get(k)
            if st is not None:
                if st[0] is not None:
                    ins.deps.add(st[0])
                for r in st[1]:
                    ins.deps.add(r)
        ins.deps.discard(ins)
        for k in reads:
            st = self.state.setdefault(k, [None, []])
            st[1].append(ins)
        for k in writes:
            self.state[k] = [ins, []]
        if dma:
            n = self.dma_count[eng]
            self.dma_count[eng] = n + 1
            ins.dma_slot = n % N_DMA_SEMS
            ins.dma_round = n // N_DMA_SEMS + 1
        self.lists[eng].append(ins)
        return ins

    def finalize(self, final_waits=()):
        nc = self.nc
        for e in ENGS:
            for ins in self.lists[e]:
                for d in ins.deps:
                    if d.dma:
                        continue
                    if d.eng == ins.eng and not ins.dma:
                        if d.eng == "pe" or not self.sync_same_engine:
                            continue
                    d.signal = True
        for ins in final_waits:
            if not ins.dma:
                ins.signal = True
        for e in ENGS:
            c = 0
            for ins in self.lists[e]:
                if ins.signal and not ins.dma:
                    c += 1
                    ins.sig_count = c
        self.sig_totals = {e: sum(1 for i in self.lists[e] if i.signal and not i.dma) for e in ENGS}
        import contextlib
        with contextlib.ExitStack() as es:
            sems = {e: es.enter_context(nc.semaphore("s_" + e)) for e in ENGS}
            dsems = {q: [es.enter_context(nc.semaphore("d_%s_%d" % (q, i))) for i in range(N_DMA_SEMS)]
                     for q in ("sp", "pool", "act")}
            block = es.enter_context(nc.Block())
            engobj = {"pe": "tensor", "act": "scalar", "dve": "vector", "pool": "gpsimd", "sp": "sync"}

            def make_body(e):
                lst = self.lists[e]

                def body(engine):
                    waited = {}

                    def wait(sem, val, key):
                        if waited.get(key, 0) >= val:
                            return
                        waited[key] = val
                        engine.wait_ge(sem, val)

                    for ins in lst:
                        for d in ins.deps:
                            if d.dma:
                                wait(dsems[d.eng][d.dma_slot], 16 * d.dma_round, ("d", d.eng, d.dma_slot))
                            else:
                                if d.eng == e and not ins.dma and (e == "pe" or not self.sync_same_engine):
                                    continue
                                wait(sems[d.eng], d.sig_count, ("c", d.eng))
                        if ins.dma:
                            if ins.dma_round > 1:
                                wait(dsems[e][ins.dma_slot], 16 * (ins.dma_round - 1), ("d", e, ins.dma_slot))
                        r = ins.fn(engine)
                        if ins.dma:
                            r.then_inc(dsems[e][ins.dma_slot], 16)
                        elif ins.signal:
                            r.then_inc(sems[e], 1)
                    if e == "sp":
                        for ins in final_waits:
                            if ins.dma:
                                wait(dsems[ins.eng][ins.dma_slot], 16 * ins.dma_round, ("d", ins.eng, ins.dma_slot))
                            else:
                                wait(sems[ins.eng], ins.sig_count, ("c", ins.eng))
                return body

            for e in ENGS:
                if not self.lists[e] and e != "sp":
                    continue
                getattr(block, engobj[e])(make_body(e))
        return self


D = 1024
DEPTH = 4
SEQ = 4096
BATCH = 4
CTX = 256
TOK = 2048
NT = 18
NTOK = NT * 128
FFN = 3584
NEXP = 8
NEXP_RUN = int(os.environ.get("MK_NEXP_RUN", "8"))
ALPHA = (2 * DEPTH) ** 0.25
LN_EPS = 1e-5
RMS_EPS = 1e-6
GROUPS = [(0, 2), (2, 4), (6, 4), (10, 4), (14, 4)]
KC = D // 128


class Arena:
    def __init__(self, ap, ncols):
        self.ap = ap
        self.ncols = ncols
        self.top = 0
        self.marks = []

    def alloc(self, free_shape, dtype=F32, parts=128):
        n = 1
        for s in free_shape:
            n *= s
        words = n if dtype in (F32, I32) else (n + 1) // 2
        words = (words + 7) // 8 * 8
        assert self.top + words <= self.ncols, ("arena overflow", self.top, words, self.ncols)
        v = self.ap[0:parts, self.top:self.top + words]
        self.top += words
        if dtype not in (F32,):
            v = v.bitcast(dtype)
        v = v[:, 0:n]
        if len(free_shape) > 1:
            names = "abcdefg"[:len(free_shape)]
            pat = "p (%s) -> p %s" % (" ".join(names), " ".join(names))
            v = v.rearrange(pat, **{names[q]: free_shape[q] for q in range(1, len(free_shape))})
        return v

    def push(self):
        self.marks.append(self.top)

    def pop(self):
        self.top = self.marks.pop()


class Ctx:
    pass


def _barrier(P):
    last = []
    for e in ENGS:
        lst = P.lists[e]
        if not lst:
            continue
        for ins in reversed(lst):
            if not ins.dma:
                last.append(ins)
                break
        seen = set()
        for ins in reversed(lst):
            if ins.dma and ins.dma_slot not in seen:
                seen.add(ins.dma_slot)
                last.append(ins)
            if len(seen) == N_DMA_SEMS:
                break
    P.barrier_set = last
    P.state = {}
    P.after_barrier = {e: True for e in ENGS}


_orig_op = Prog.op


def _op_with_barrier(self, eng, fn, reads=(), writes=(), dma=False):
    ins = _orig_op(self, eng, fn, reads, writes, dma)
    if getattr(self, "after_barrier", None) and self.after_barrier.get(eng):
        for b in self.barrier_set:
            if b is not ins:
                ins.deps.add(b)
        self.after_barrier[eng] = False
    return ins


Prog.op = _op_with_barrier
Prog.barrier = _barrier


ARENA_COLS = 53200


def _mm(P, out, lhsT, rhs, start, stop, reads, writes):
    return P.op("pe", lambda e: e.matmul(out, lhsT=lhsT, rhs=rhs, start=start, stop=stop), reads, writes)


def layer_param_names(i):
    pre = "l%d_" % i
    names = ["ada_w", "ada_b", "ln1_g", "ln1_b", "ln2_g", "ln2_b"]
    kind = i % 3
    if kind == 0:
        names += ["attn_wqkv", "attn_q_norm", "attn_k_norm", "attn_wo"]
    elif kind == 1:
        names += ["cmlp_w_in", "cmlp_b_in", "cmlp_v_norm_g", "cmlp_v_norm_b", "cmlp_w_s", "cmlp_b_s",
                  "cmlp_w_out", "cmlp_b_out"]
    else:
        names += ["ret_wqkvg", "ret_decay", "ret_wo"]
    if i % 2 == 0:
        names += ["ffn_w_gu", "ffn_w_down"]
    else:
        names += ["moe_router", "moe_w_gu", "moe_w_down"]
    return [pre + n for n in names]


PARAM_SHAPES = {}


def _param_shape(name):
    n = name[3:]
    shp = {
        "ada_w": [D, 6 * D], "ada_b": [6 * D], "ln1_g": [D], "ln1_b": [D], "ln2_g": [D], "ln2_b": [D],
        "attn_wqkv": [D, 1536], "attn_q_norm": [128], "attn_k_norm": [128], "attn_wo": [D, D],
        "cmlp_w_in": [D, 4096], "cmlp_b_in": [4096], "cmlp_v_norm_g": [2048], "cmlp_v_norm_b": [2048],
        "cmlp_w_s": [8, 128, 128], "cmlp_b_s": [8, 128], "cmlp_w_out": [2048, D], "cmlp_b_out": [D],
        "ret_wqkvg": [D, 6144], "ret_decay": [2, 4], "ret_wo": [2048, D],
        "ffn_w_gu": [D, 2 * FFN], "ffn_w_down": [FFN, D],
        "moe_router": [D, NEXP], "moe_w_gu": [NEXP_RUN, D, 2 * FFN], "moe_w_down": [NEXP_RUN, FFN, D],
    }[n]
    return shp


def build_program(stages, layers_needed):
    nc = bass.Bass("TRN2", target_bir_lowering=False)
    C = Ctx()
    C.nc = nc
    dram = {}

    def din(name, shape, dtype=F32):
        dram[name] = nc.dram_tensor(name, list(shape), dtype, kind="ExternalInput").ap()
        return dram[name]

    din("x_in", [TOK, D])
    din("ctx_in", [CTX, D])
    din("cc", [128, KC, 2])
    din("ident", [128, 128])
    if any(k == "mix" and i % 3 == 0 for k, i in stages):
        din("rope_a", [128, 2, TOK])
        din("rope_o", [128, 2, TOK])
    if any(k == "mix" and i % 3 != 1 for k, i in stages):
        din("x_oth", [TOK, D])
    if any(k == "mix" and i % 3 == 2 for k, i in stages):
        din("rope_r", [128, 2, 2, TOK])
        din("rope_ro", [128, 2, 2, TOK])
        din("ret_dec", [2, 4])
        C.xacc = nc.dram_tensor("xacc", [NTOK, D], F32, kind="Internal").ap()
    for i in layers_needed:
        for n in layer_param_names(i):
            din(n, _param_shape(n))
    x_out = nc.dram_tensor("x_out", [TOK, D], F32, kind="ExternalOutput").ap()
    xc_out = nc.dram_tensor("xc_out", [CTX, D], F32, kind="ExternalOutput").ap()
    C.ada_scr = nc.dram_tensor("ada_scr", [DEPTH, 2, 6 * D], F32, kind="Internal").ap()
    C.dram = dram

    import contextlib
    with contextlib.ExitStack() as es:
        arena_t = es.enter_context(nc.sbuf_tensor("arena", [128, ARENA_COLS], F32))
        A = Arena(arena_t[:], ARENA_COLS)
        C.A = A
        C.psum = [es.enter_context(nc.psum_tensor("ps%d" % i, [128, 512], F32)) for i in range(8)]
        P = Prog(nc)
        C.P = P
        C.Xraw = A.alloc((NT * D,))
        C.X = C.Xraw.rearrange("p (t d) -> p t d", d=D)
        C.hT = A.alloc((KC, NTOK), BF16)
        C.ident = A.alloc((128,))
        C.adaT = A.alloc((DEPTH, 48, 2))
        C.sc1p = A.alloc((DEPTH, 2, KC, 2))
        C.ones_bf2 = A.alloc((128,), BF16)

        P.op("sp", lambda e: e.dma_start(out=C.ident, in_=dram["ident"]), writes=["ident"], dma=True)
        P.op("pool", lambda e: e.memset(C.ones_bf2, 1.0), writes=["ones_bf"])
        for t in range(NT):
            src = dram["ctx_in"][t * 128:(t + 1) * 128, :] if t < 2 else dram["x_in"][(t - 2) * 128:(t - 1) * 128, :]
            P.op("sp", (lambda e, t=t, src=src: e.dma_start(out=C.X[:, t, :], in_=src)), writes=[("X", t)], dma=True)

        emit_adaln(C, layers_needed)
        for kind, i in stages:
            if kind == "ffn":
                emit_ffn(C, i)
            else:
                emit_mixer(C, i)
        P.barrier()
        outs = []
        for t in range(NT):
            dst = xc_out[t * 128:(t + 1) * 128, :] if t < 2 else x_out[(t - 2) * 128:(t - 1) * 128, :]
            outs.append(P.op("sp", (lambda e, t=t, dst=dst: e.dma_start(out=dst, in_=C.X[:, t, :])),
                             reads=[("X", t)], dma=True))
        P.finalize(final_waits=outs)
    nc.mk_inputs = set(dram.keys())
    return nc


def emit_adaln(C, layers):
    P, A, nc = C.P, C.A, C.nc
    P.barrier()
    A.push()
    cc = A.alloc((KC, 2))
    scc = A.alloc((KC, 2))
    wch = [A.alloc((KC, 512)) for _ in range(2)]
    bch = [A.alloc((512,), parts=2) for _ in range(2)]
    rowc = [A.alloc((512,), parts=2) for _ in range(2)]
    psT = C.psum[2]
    P.op("sp", lambda e: e.dma_start(out=cc, in_=C.dram["cc"]), writes=["cc"], dma=True)
    P.op("act", lambda e: e.activation(out=scc, in_=cc, func=AF.Silu), reads=["cc"], writes=["scc"])
    n = 0
    for i in layers:
        w = C.dram["l%d_ada_w" % i].rearrange("(k p) n -> p k n", p=128)
        b = C.dram["l%d_ada_b" % i]
        for cch in range(12):
            s = n % 2
            n += 1
            cs = slice(cch * 512, (cch + 1) * 512)
            P.op("sp", (lambda e, s=s, cs=cs, w=w: e.dma_start(out=wch[s], in_=w[:, :, cs])), writes=[("wch", s)], dma=True)
            P.op("sp", (lambda e, s=s, cs=cs, b=b: e.dma_start(out=bch[s], in_=b[cs].partition_broadcast(2))),
                 writes=[("bch", s)], dma=True)
            ps = C.psum[s]
            for k in range(KC):
                _mm(P, ps[0:2, :], scc[:, k, :], wch[s][:, k, :], k == 0, k == KC - 1,
                    reads=["scc", ("wch", s)], writes=[("bank", s)])
            P.op("dve", (lambda e, s=s, ps=ps: e.tensor_tensor(out=rowc[s], in0=ps[0:2, :], in1=bch[s], op=ALU.add)),
                 reads=[("bank", s), ("bch", s)], writes=[("rowc", s)])
            P.op("sp", (lambda e, s=s, cs=cs, i=i: e.dma_start(out=C.ada_scr[i, :, cs], in_=rowc[s])),
                 reads=[("rowc", s)], writes=[("ada_scr", i)], dma=True)
            for q in range(4):
                ch = cch * 4 + q
                _mm(P, psT[:, ch * 2:(ch + 1) * 2], rowc[s][:, q * 128:(q + 1) * 128], C.ident[0:2, 0:2], True, True,
                    reads=[("rowc", s), "ident"], writes=[("bank", 2)])
        P.op("dve", (lambda e, i=i: e.tensor_copy(out=C.adaT[:, i, :, :], in_=psT[:, 0:96].rearrange("p (c j) -> p c j", j=2))),
             reads=[("bank", 2)], writes=[("adaT", i)])
        for sub, c0 in ((0, 8), (1, 32)):
            P.op("dve", (lambda e, i=i, sub=sub, c0=c0: e.tensor_scalar(
                out=C.sc1p[:, i, sub, :, :], in0=C.adaT[:, i, c0:c0 + 8, :], scalar1=1.0, scalar2=None, op0=ALU.add)),
                reads=[("adaT", i)], writes=[("sc1p", i, sub)])
    A.pop()
    P.barrier()


def emit_build_hT(C, i, sub, need_ctx=True, router=None):
    P, A = C.P, C.A
    shift_c0 = 0 if sub == 0 else 24
    tiles = range(NT) if need_ctx else range(2, NT)
    for t in tiles:
        j = 1 if t < 2 else 0
        s = t % 2
        ps = (C.psum[4 + 2 * s], C.psum[5 + 2 * s])
        for k in range(KC):
            pst = ps[k // 4][:, (k % 4) * 128:(k % 4 + 1) * 128]
            P.op("pe", (lambda e, t=t, k=k, pst=pst: e.transpose(pst, C.X[:, t, k * 128:(k + 1) * 128], C.ident)),
                 reads=[("X", t), "ident"], writes=[("bank", 4 + 2 * s + k // 4)])
        for k in range(KC):
            pst = ps[k // 4][:, (k % 4) * 128:(k % 4 + 1) * 128]
            P.op("dve", (lambda e, t=t, k=k, pst=pst, j=j: e.tensor_scalar(
                out=C.hT[:, k, t * 128:(t + 1) * 128], in0=pst,
                scalar1=C.sc1p[:, i, sub, k, j:j + 1], scalar2=C.adaT[:, i, shift_c0 + k, j:j + 1],
                op0=ALU.mult, op1=ALU.add)),
                reads=[("bank", 4 + 2 * s + k // 4)], writes=[("hT", t, k)])
            if router is not None:
                h32 = router["h32"][s]
                P.op("dve", (lambda e, t=t, k=k, pst=pst, j=j, h32=h32: e.tensor_scalar(
                    out=h32[:, k, :], in0=pst,
                    scalar1=C.sc1p[:, i, sub, k, j:j + 1], scalar2=C.adaT[:, i, shift_c0 + k, j:j + 1],
                    op0=ALU.mult, op1=ALU.add)),
                    reads=[("bank", 4 + 2 * s + k // 4)], writes=[("h32", s, k)])
        if router is not None and not os.environ.get("MK_DBG_NORMM"):
            psl = C.psum[s]
            for k in range(KC):
                _mm(P, psl[:, 0:NEXP], router["h32"][s][:, k, :], router["w"][:, k, :], k == 0, k == KC - 1,
                    reads=[("h32", s, k), "router_w"], writes=[("bank", s)])
            P.op("act", (lambda e, t=t, psl=psl: e.copy(out=router["logits"][:, t, :], in_=psl[:, 0:NEXP])),
                 reads=[("bank", s)], writes=[("logits", t)])


def emit_scale_x(C, need_ctx=True):
    P = C.P
    for t in (range(NT) if need_ctx else range(2, NT)):
        P.op("pool", (lambda e, t=t: e.tensor_scalar(out=C.X[:, t, :], in0=C.X[:, t, :], scalar1=float(ALPHA),
                                                     scalar2=None, op0=ALU.mult)),
             reads=[("X", t)], writes=[("X", t)])


def emit_load_bc(C, i, sub, tiles):
    P = C.P
    gate_c0 = 2 * D if sub == 0 else 5 * D
    srcs = {
        "gx": C.ada_scr[i, 0, gate_c0:gate_c0 + D], "gc": C.ada_scr[i, 1, gate_c0:gate_c0 + D],
        "lg": C.dram["l%d_ln%d_g" % (i, sub + 1)], "lb": C.dram["l%d_ln%d_b" % (i, sub + 1)],
    }
    for n, src in srcs.items():
        P.op("sp", (lambda e, n=n, src=src: e.dma_start(out=tiles[n], in_=src.partition_broadcast(128))),
             reads=[("ada_scr", i)], writes=[("bc", n)], dma=True)


def emit_ln(C, tiles, need_ctx=True):
    P, A = C.P, C.A
    A.push()
    stats = A.alloc((NT, 2, 6))
    mv = A.alloc((NT, 2))
    rstd = A.alloc((NT,))
    nmr = A.alloc((NT,))
    t0 = 0 if need_ctx else 2
    for t in range(t0, NT):
        for hh in range(2):
            P.op("dve", (lambda e, t=t, hh=hh: e.bn_stats(out=stats[:, t, hh, :], in_=C.X[:, t, hh * 512:(hh + 1) * 512])),
                 reads=[("X", t)], writes=[("stats", t, hh)])
        P.op("dve", (lambda e, t=t: e.bn_aggr(out=mv[:, t, :], in_=stats[:, t, :, :])),
             reads=[("stats", t, 0), ("stats", t, 1)], writes=[("mv", t)])
    mvk = [("mv", t) for t in range(t0, NT)]
    P.op("dve", lambda e: e.tensor_scalar(out=rstd[:, t0:NT], in0=mv[:, t0:NT, 1], scalar1=float(LN_EPS), scalar2=None, op0=ALU.add),
         reads=mvk, writes=["rstd"])
    P.op("act", lambda e: e.activation(out=rstd[:, t0:NT], in_=rstd[:, t0:NT], func=AF.Sqrt), reads=["rstd"], writes=["rstd"])
    P.op("dve", lambda e: e.reciprocal(out=rstd[:, t0:NT], in_=rstd[:, t0:NT]), reads=["rstd"], writes=["rstd"])
    P.op("dve", lambda e: e.scalar_tensor_tensor(out=nmr[:, t0:NT], in0=mv[:, t0:NT, 0], scalar=-1.0, in1=rstd[:, t0:NT],
                                                 op0=ALU.mult, op1=ALU.mult), reads=mvk + ["rstd"], writes=["nmr"])
    for t in range(t0, NT):
        P.op("act", (lambda e, t=t: e.activation(out=C.X[:, t, :], in_=C.X[:, t, :], func=AF.Identity,
                                                 bias=nmr[:, t:t + 1], scale=rstd[:, t:t + 1])),
             reads=[("X", t), "rstd", "nmr"], writes=[("X", t)])
        P.op("pool", (lambda e, t=t: e.tensor_tensor(out=C.X[:, t, :], in0=C.X[:, t, :], in1=tiles["lg"], op=ALU.mult)),
             reads=[("X", t), ("bc", "lg")], writes=[("X", t)])
        P.op("pool", (lambda e, t=t: e.tensor_tensor(out=C.X[:, t, :], in0=C.X[:, t, :], in1=tiles["lb"], op=ALU.add)),
             reads=[("X", t), ("bc", "lb")], writes=[("X", t)])
    A.pop()


def hT_keys(t0, n):
    return [("hT", t, k) for t in range(t0, t0 + n) for k in range(KC)]


def emit_ffn(C, i):
    P, A, nc = C.P, C.A, C.nc
    moe = (i % 2 == 1)
    need_ctx = i < DEPTH - 1
    P.barrier()
    A.push()
    bc = {n: A.alloc((D,)) for n in ("gx", "gc", "lg", "lb")}
    emit_load_bc(C, i, 1, bc)
    router = None
    if moe and not os.environ.get("MK_DBG_NOROUTER"):
        router = {
            "h32": [A.alloc((KC, 128)) for _ in range(2)],
            "w": A.alloc((KC, NEXP)),
            "logits": A.alloc((NT, NEXP)),
        }
        rw = C.dram["l%d_moe_router" % i].rearrange("(k p) e -> p k e", p=128)
        P.op("sp", lambda e: e.dma_start(out=router["w"], in_=rw), writes=["router_w"], dma=True)
    emit_build_hT(C, i, 1, need_ctx=need_ctx, router=router)
    emit_scale_x(C, need_ctx=need_ctx)
    gates = None
    if moe and not os.environ.get("MK_DBG_NOGATES"):
        gates = emit_gates(C, router, need_ctx)
    if moe:
        wgu = C.dram["l%d_moe_w_gu" % i]
        wdn = C.dram["l%d_moe_w_down" % i]
        blocks = [(e, j) for e in range(NEXP_RUN) for j in range(FFN // 512)]
    else:
        wgu = C.dram["l%d_ffn_w_gu" % i]
        wdn = C.dram["l%d_ffn_w_down" % i]
        blocks = [(None, j) for j in range(FFN // 512)]
    if os.environ.get("MK_DBG_NBLK"):
        blocks = blocks[:int(os.environ["MK_DBG_NBLK"])]
    Wg = [A.alloc((KC, 512), BF16) for _ in range(2)]
    Wu = [A.alloc((KC, 512), BF16) for _ in range(2)]
    Wd = [A.alloc((4, D), BF16) for _ in range(2)]
    act = [A.alloc((4, 512), BF16) for _ in range(2)]
    sg = [A.alloc((512,)) for _ in range(2)]
    tmp = [A.alloc((D,)) for _ in range(2)]
    groups = GROUPS if need_ctx else GROUPS[1:]

    def load_block(bi):
        e_, j = blocks[bi]
        s = bi % 2
        gu = (wgu[e_] if moe else wgu).rearrange("(k p) n -> p k n", p=128)
        dn = (wdn[e_] if moe else wdn)[j * 512:(j + 1) * 512, :].rearrange("(f p) n -> p f n", p=128)
        P.op("pool", (lambda e: e.dma_start(out=Wg[s], in_=gu[:, :, j * 512:(j + 1) * 512])), writes=[("Wg", s)], dma=True)
        if os.environ.get("MK_DBG_SKIPW") and bi > 1:
            return
        P.op("pool", (lambda e: e.dma_start(out=Wu[s], in_=gu[:, :, FFN + j * 512:FFN + (j + 1) * 512])),
             writes=[("Wu", s)], dma=True)
        P.op("pool", (lambda e: e.dma_start(out=Wd[s], in_=dn)), writes=[("Wd", s)], dma=True)

    acc_eng = os.environ.get("MK_ACC_ENG", "pool")
    items = [(bi, gi) for bi in range(len(blocks)) for gi in range(len(groups))]

    def emit_gu(n):
        bi, gi = items[n]
        s = bi % 2
        if gi == 0 and bi + 1 < len(blocks):
            load_block(bi + 1)
        (t0, ntile) = groups[gi]
        ntok = ntile * 128
        a = n % 2
        for fb in range(4):
            pg = C.psum[(fb % 2) * 2]
            pu = C.psum[(fb % 2) * 2 + 1]
            for k in range(KC):
                _mm(P, pg[:, 0:ntok], Wg[s][:, k, fb * 128:(fb + 1) * 128], C.hT[:, k, t0 * 128:t0 * 128 + ntok],
                    k == 0, k == KC - 1, reads=([("Wg", s)] + hT_keys(t0, ntile)) if k in (0, KC - 1) else [], writes=[("bank", (fb % 2) * 2)])
            for k in range(KC):
                _mm(P, pu[:, 0:ntok], Wu[s][:, k, fb * 128:(fb + 1) * 128], C.hT[:, k, t0 * 128:t0 * 128 + ntok],
                    k == 0, k == KC - 1, reads=([("Wu", s)] + hT_keys(t0, ntile)) if k in (0, KC - 1) else [], writes=[("bank", (fb % 2) * 2 + 1)])
            sgt = sg[fb % 2]
            P.op("act", (lambda e, pg=pg, sgt=sgt: e.activation(out=sgt[:, 0:ntok], in_=pg[:, 0:ntok], func=AF.Silu)),
                 reads=[("bank", (fb % 2) * 2)], writes=[("sg", fb % 2)])
            P.op("dve", (lambda e, pu=pu, sgt=sgt, fb=fb: e.tensor_tensor(
                out=act[a][:, fb, 0:ntok], in0=pu[:, 0:ntok], in1=sgt[:, 0:ntok], op=ALU.mult)),
                reads=[("bank", (fb % 2) * 2 + 1), ("sg", fb % 2)], writes=[("act", a, fb)])

    ocnt = [0]

    def emit_down(n):
        bi, gi = items[n]
        s = bi % 2
        e_, j = blocks[bi]
        (t0, ntile) = groups[gi]
        a = n % 2
        for tt in range(ntile):
            t = t0 + tt
            o = ocnt[0] % 2
            ocnt[0] += 1
            po = (C.psum[4 + 2 * o], C.psum[5 + 2 * o])
            for nh in range(2):
                for fb in range(4):
                    _mm(P, po[nh][:, :], act[a][:, fb, tt * 128:(tt + 1) * 128], Wd[s][:, fb, nh * 512:(nh + 1) * 512],
                        fb == 0, fb == 3, reads=[("act", a, fb), ("Wd", s)], writes=[("bank", 4 + 2 * o + nh)])
            gbc = bc["gc"] if t < 2 else bc["gx"]
            tm = tmp[o]
            for nh in range(2):
                if moe and gates is not None:
                    P.op("dve", (lambda e, po=po, nh=nh, tm=tm, gbc=gbc, t=t, e_=e_: e.scalar_tensor_tensor(
                        out=tm[:, nh * 512:(nh + 1) * 512], in0=po[nh][:, :], scalar=gates[:, t, e_:e_ + 1],
                        in1=gbc[:, nh * 512:(nh + 1) * 512], op0=ALU.mult, op1=ALU.mult)),
                        reads=[("bank", 4 + 2 * o + nh), ("bc", "gx"), ("bc", "gc"), "gates"], writes=[("tmp", o, nh)])
                else:
                    P.op("dve", (lambda e, po=po, nh=nh, tm=tm, gbc=gbc: e.tensor_tensor(
                        out=tm[:, nh * 512:(nh + 1) * 512], in0=po[nh][:, :], in1=gbc[:, nh * 512:(nh + 1) * 512], op=ALU.mult)),
                        reads=[("bank", 4 + 2 * o + nh), ("bc", "gx"), ("bc", "gc")], writes=[("tmp", o, nh)])
            eng = acc_eng if acc_eng != "alt" else ("dve" if ocnt[0] % 2 else "pool")
            P.op(eng, (lambda e, t=t, tm=tm: e.tensor_tensor(out=C.X[:, t, :], in0=C.X[:, t, :], in1=tm, op=ALU.add)),
                 reads=[("X", t), ("tmp", o, 0), ("tmp", o, 1)], writes=[("X", t)])

    if blocks:
        load_block(0)
    pipelined = bool(os.environ.get("MK_PIPE"))
    if pipelined and items:
        emit_gu(0)
        for n in range(len(items)):
            if n + 1 < len(items):
                emit_gu(n + 1)
            emit_down(n)
    else:
        for n in range(len(items)):
            emit_gu(n)
            emit_down(n)
    emit_ln(C, bc, need_ctx=need_ctx)
    A.pop()
    P.barrier()


def emit_gates(C, router, need_ctx):
    P, A = C.P, C.A
    L = router["logits"]
    t0 = 0 if need_ctx else 2
    n = NT - t0
    Lv = L[:, t0:NT, :]
    gates = A.alloc((NT, NEXP))
    m1 = A.alloc((NT,))
    m2 = A.alloc((NT,))
    mk1 = A.alloc((NT, NEXP))
    mk2 = A.alloc((NT, NEXP))
    l2 = A.alloc((NT, NEXP))
    w1 = A.alloc((NT,))
    w2 = A.alloc((NT,))
    lk = [("logits", t) for t in range(t0, NT)]

    def bcast(v):
        return v[:, t0:NT][:, :, None].broadcast_to([128, n, NEXP])

    P.op("dve", lambda e: e.tensor_reduce(out=m1[:, t0:NT], in_=Lv, axis=AX.X, op=ALU.max), reads=lk, writes=["m1"])
    P.op("dve", lambda e: e.tensor_tensor(out=mk1[:, t0:NT, :], in0=Lv, in1=bcast(m1), op=ALU.is_equal), reads=lk + ["m1"], writes=["mk1"])
    P.op("dve", lambda e: e.scalar_tensor_tensor(out=l2[:, t0:NT, :], in0=mk1[:, t0:NT, :], scalar=-1e30, in1=Lv,
                                                 op0=ALU.mult, op1=ALU.add), reads=lk + ["mk1"], writes=["l2"])
    P.op("dve", lambda e: e.tensor_reduce(out=m2[:, t0:NT], in_=l2[:, t0:NT, :], axis=AX.X, op=ALU.max), reads=["l2"], writes=["m2"])
    P.op("dve", lambda e: e.tensor_tensor(out=mk2[:, t0:NT, :], in0=l2[:, t0:NT, :], in1=bcast(m2), op=ALU.is_equal),
         reads=["l2", "m2"], writes=["mk2"])
    P.op("dve", lambda e: e.tensor_tensor(out=w2[:, t0:NT], in0=m2[:, t0:NT], in1=m1[:, t0:NT], op=ALU.subtract),
         reads=["m1", "m2"], writes=["w2"])
    P.op("act", lambda e: e.activation(out=w2[:, t0:NT], in_=w2[:, t0:NT], func=AF.Exp), reads=["w2"], writes=["w2"])
    P.op("dve", lambda e: e.tensor_scalar(out=w1[:, t0:NT], in0=w2[:, t0:NT], scalar1=1.0, scalar2=None, op0=ALU.add),
         reads=["w2"], writes=["w1"])
    P.op("dve", lambda e: e.reciprocal(out=w1[:, t0:NT], in_=w1[:, t0:NT]), reads=["w1"], writes=["w1"])
    P.op("dve", lambda e: e.tensor_tensor(out=w2[:, t0:NT], in0=w2[:, t0:NT], in1=w1[:, t0:NT], op=ALU.mult),
         reads=["w1", "w2"], writes=["w2"])
    P.op("dve", lambda e: e.tensor_tensor(out=mk1[:, t0:NT, :], in0=mk1[:, t0:NT, :], in1=bcast(w1), op=ALU.mult),
         reads=["mk1", "w1"], writes=["mk1"])
    P.op("dve", lambda e: e.tensor_tensor(out=mk2[:, t0:NT, :], in0=mk2[:, t0:NT, :], in1=bcast(w2), op=ALU.mult),
         reads=["mk2", "w2"], writes=["mk2"])
    P.op("dve", lambda e: e.tensor_tensor(out=gates[:, t0:NT, :], in0=mk1[:, t0:NT, :], in1=mk2[:, t0:NT, :], op=ALU.add),
         reads=["mk1", "mk2"], writes=["gates"])
    return gates


def emit_mixer(C, i):
    kind = i % 3
    if kind == 0:
        emit_attention(C, i)
    elif kind == 1:
        emit_cmlp(C, i)
    else:
        emit_retention(C, i)


PAIR_GROUPS = [[0, 1], [2, 3], [4, 5], [6, 7]]


def emit_rot_copy(P, dst, src, half, rkey, wkey):
    n = src.shape[-1]
    for b0 in range(0, n, 2 * half):
        P.op("pool", (lambda e, b0=b0: e.tensor_copy(out=dst[:, :, b0:b0 + half], in_=src[:, :, b0 + half:b0 + 2 * half])),
             reads=[rkey], writes=[wkey])
        P.op("pool", (lambda e, b0=b0: e.tensor_copy(out=dst[:, :, b0 + half:b0 + 2 * half], in_=src[:, :, b0:b0 + half])),
             reads=[rkey], writes=[wkey])


def emit_load_gain(C, dst, dst_rot, src, half, scale, key):
    P = C.P
    col = src.rearrange("(p o) -> p o", o=1)
    P.op("sp", lambda e: e.dma_start(out=dst, in_=col), writes=[key], dma=True)
    for b0 in range(0, 128, 2 * half):
        P.op("sp", (lambda e, b0=b0: e.dma_start(out=dst_rot[b0:b0 + half, :], in_=col[b0 + half:b0 + 2 * half, :])),
             writes=[key + "_r"], dma=True)
        P.op("sp", (lambda e, b0=b0: e.dma_start(out=dst_rot[b0 + half:b0 + 2 * half, :], in_=col[b0:b0 + half, :])),
             writes=[key + "_r"], dma=True)
    if scale != 1.0:
        P.op("dve", lambda e: e.tensor_scalar(out=dst, in0=dst, scalar1=float(scale), scalar2=None, op0=ALU.mult),
             reads=[key], writes=[key])
        P.op("dve", lambda e: e.tensor_scalar(out=dst_rot, in0=dst_rot, scalar1=float(scale), scalar2=None, op0=ALU.mult),
             reads=[key + "_r"], writes=[key + "_r"])


def emit_qk_norm_rope(C, ps_q, ps_qr, ps_ss, ntok, out_bf, gain, gain_r, cos, sin, tmp, eps_t, inv_d, tag, rope, out_key):
    P = C.P
    sq, rstd, ta, tb = tmp
    kq, kqr, kss = ("bank", ps_q[1]), ("bank", ps_qr[1]), ("bank", ps_ss[1])
    pq, pqr, pss = ps_q[0], ps_qr[0], ps_ss[0]
    P.op("act", lambda e: e.activation(out=sq[:, 0:ntok], in_=pq[:, 0:ntok], func=AF.Square), reads=[kq], writes=[tag + "sq"])
    _mm(P, pss[:, 0:ntok], C.ones_bf2, sq[:, 0:ntok], True, True, reads=[tag + "sq", "ones_bf"], writes=[kss])
    P.op("dve", lambda e: e.tensor_scalar(out=rstd[:, 0:ntok], in0=pss[:, 0:ntok], scalar1=float(inv_d), scalar2=float(RMS_EPS),
                                          op0=ALU.mult, op1=ALU.add), reads=[kss], writes=[tag + "rstd"])
    P.op("act", lambda e: e.activation(out=rstd[:, 0:ntok], in_=rstd[:, 0:ntok], func=AF.Ln),
         reads=[tag + "rstd"], writes=[tag + "rstd"])
    P.op("act", lambda e: e.activation(out=rstd[:, 0:ntok], in_=rstd[:, 0:ntok], func=AF.Exp, scale=-0.5),
         reads=[tag + "rstd"], writes=[tag + "rstd"])
    if not rope:
        P.op("dve", lambda e: e.scalar_tensor_tensor(out=out_bf, in0=pq[:, 0:ntok], scalar=gain, in1=rstd[:, 0:ntok],
                                                     op0=ALU.mult, op1=ALU.mult),
             reads=[kq, tag + "rstd", "gains"], writes=[out_key])
        return
    P.op("dve", lambda e: e.scalar_tensor_tensor(out=ta[:, 0:ntok], in0=pq[:, 0:ntok], scalar=gain, in1=rstd[:, 0:ntok],
                                                 op0=ALU.mult, op1=ALU.mult), reads=[kq, tag + "rstd", "gains"], writes=[tag + "ta"])
    P.op("dve", lambda e: e.scalar_tensor_tensor(out=tb[:, 0:ntok], in0=pqr[:, 0:ntok], scalar=gain_r, in1=rstd[:, 0:ntok],
                                                 op0=ALU.mult, op1=ALU.mult), reads=[kqr, tag + "rstd", "gains"], writes=[tag + "tb"])
    P.op("pool", lambda e: e.tensor_tensor(out=ta[:, 0:ntok], in0=ta[:, 0:ntok], in1=cos, op=ALU.mult),
         reads=[tag + "ta", "rope"], writes=[tag + "ta"])
    P.op("pool", lambda e: e.tensor_tensor(out=tb[:, 0:ntok], in0=tb[:, 0:ntok], in1=sin, op=ALU.mult),
         reads=[tag + "tb", "rope"], writes=[tag + "tb"])
    P.op("dve", lambda e: e.tensor_tensor(out=out_bf, in0=ta[:, 0:ntok], in1=tb[:, 0:ntok], op=ALU.add),
         reads=[tag + "ta", tag + "tb"], writes=[out_key])


def emit_attention(C, i):
    P, A, nc = C.P, C.A, C.nc
    need_ctx = i < DEPTH - 1
    wqkv = C.dram["l%d_attn_wqkv" % i].rearrange("(k p) n -> p k n", p=128)
    wo = C.dram["l%d_attn_wo" % i]
    P.barrier()
    A.push()
    gx = A.alloc((D,))
    gc = A.alloc((D,))
    bc = {"gx": gx, "gc": gc}
    gate_c0 = 2 * D
    P.op("sp", lambda e: e.dma_start(out=gx, in_=C.ada_scr[i, 0, gate_c0:gate_c0 + D].partition_broadcast(128)), writes=[("bc", "gx")], dma=True)
    P.op("sp", lambda e: e.dma_start(out=gc, in_=C.ada_scr[i, 1, gate_c0:gate_c0 + D].partition_broadcast(128)), writes=[("bc", "gc")], dma=True)
    emit_build_hT(C, i, 0, need_ctx=True)
    emit_scale_x(C, need_ctx=need_ctx)

    rope = A.alloc((2, TOK))
    P.op("sp", lambda e: e.dma_start(out=rope, in_=C.dram["rope_a"]), writes=["rope"], dma=True)
    Kall = A.alloc((2, CTX + SEQ), BF16)
    Vall = A.alloc((34, 256), BF16)
    gk, gkr, gq, gqr, eps_t = (A.alloc((1,)) for _ in range(5))
    emit_load_gain(C, gk, gkr, C.dram["l%d_attn_k_norm" % i], 32, 1.0, "gk")
    emit_load_gain(C, gq, gqr, C.dram["l%d_attn_q_norm" % i], 32, 128 ** -0.5, "gq")
    P.op("pool", lambda e: e.memset(eps_t, float(RMS_EPS)), writes=["eps"])
    tmp = (A.alloc((512,), BF16), A.alloc((512,)), A.alloc((512,)), A.alloc((512,)))
    bank = lambda n: (C.psum[n], n)

    A.push()
    Wk = A.alloc((KC, 256), BF16)
    Wkr = A.alloc((KC, 256), BF16)
    Wv = A.alloc((KC, 256), BF16)
    Xo = A.alloc((D,))
    hTo = A.alloc((KC, 512), BF16)
    rope_o = A.alloc((2, 512))
    P.op("pool", lambda e: e.dma_start(out=Wk, in_=wqkv[:, :, 1024:1280]), writes=["Wk"], dma=True)
    P.op("pool", lambda e: e.dma_start(out=Wv, in_=wqkv[:, :, 1280:1536]), writes=["Wv"], dma=True)
    emit_rot_copy(P, Wkr, Wk, 32, "Wk", "Wkr")

    def kproj(src, c0, ntok, hkeys, out, cos, sin, isctx, hk, okey):
        for k in range(KC):
            _mm(P, C.psum[0][:, 0:ntok], Wk[:, k, hk * 128:(hk + 1) * 128], src[:, k, c0:c0 + ntok],
                k == 0, k == KC - 1, reads=["Wk"] + hkeys, writes=[("bank", 0)])
        if not isctx:
            for k in range(KC):
                _mm(P, C.psum[1][:, 0:ntok], Wkr[:, k, hk * 128:(hk + 1) * 128], src[:, k, c0:c0 + ntok],
                    k == 0, k == KC - 1, reads=["Wkr"] + hkeys, writes=[("bank", 1)])
        emit_qk_norm_rope(C, bank(0), bank(1), bank(2), ntok, out, gk, gkr, cos, sin, tmp, eps_t, 1.0 / 128, "k",
                          not isctx, okey)

    def vproj(src, c0, hkeys, dst, okey, pb):
        pv = C.psum[pb]
        for k in range(KC):
            _mm(P, pv[:, 0:256], src[:, k, c0:c0 + 128], Wv[:, k, :], k == 0, k == KC - 1,
                reads=["Wv"] + hkeys, writes=[("bank", pb)])
        P.op("act", (lambda e: e.copy(out=dst, in_=pv[:, 0:256])), reads=[("bank", pb)], writes=[okey])

    for (t0, ntile) in GROUPS:
        ntok = ntile * 128
        isctx = t0 < 2
        for hk in range(2):
            if isctx:
                kproj(C.hT, 0, ntok, hT_keys(t0, ntile), Kall[:, hk, 0:CTX], None, None, True, hk, ("Kall", hk, "c"))
            else:
                c0 = (t0 - 2) * 128
                kproj(C.hT, t0 * 128, ntok, hT_keys(t0, ntile), Kall[:, hk, CTX + c0:CTX + c0 + ntok],
                      rope[:, 0, c0:c0 + ntok], rope[:, 1, c0:c0 + ntok], False, hk, ("Kall", hk, t0))
        for tt in range(ntile):
            t = t0 + tt
            vproj(C.hT, t * 128, hT_keys(t, 1), Vall[:, t, :], ("Vall", t), 4 + t % 2)
    xoth = C.dram["x_oth"]
    for g in range(4):
        P.op("sp", (lambda e, g=g: e.dma_start(out=rope_o, in_=C.dram["rope_o"][:, :, g * 512:(g + 1) * 512])),
             writes=["rope_o"], dma=True)
        for tt in range(4):
            tg = g * 4 + tt
            P.op("sp", (lambda e, tg=tg: e.dma_start(out=Xo, in_=xoth[tg * 128:(tg + 1) * 128, :])), writes=["Xo"], dma=True)
            for k in range(KC):
                pst = C.psum[6 + k // 4][:, (k % 4) * 128:(k % 4 + 1) * 128]
                P.op("pe", (lambda e, k=k, pst=pst: e.transpose(pst, Xo[:, k * 128:(k + 1) * 128], C.ident)),
                     reads=["Xo", "ident"], writes=[("bank", 6 + k // 4)])
            for k in range(KC):
                pst = C.psum[6 + k // 4][:, (k % 4) * 128:(k % 4 + 1) * 128]
                P.op("dve", (lambda e, k=k, pst=pst, tt=tt: e.tensor_scalar(
                    out=hTo[:, k, tt * 128:(tt + 1) * 128], in0=pst,
                    scalar1=C.sc1p[:, i, 0, k, 0:1], scalar2=C.adaT[:, i, k, 0:1], op0=ALU.mult, op1=ALU.add)),
                    reads=[("bank", 6 + k // 4)], writes=[("hTo", tt, k)])
        hk_o = [("hTo", tt, k) for tt in range(4) for k in range(KC)]
        c0 = CTX + TOK + g * 512
        for hk in range(2):
            kproj(hTo, 0, 512, hk_o, Kall[:, hk, c0:c0 + 512], rope_o[:, 0, :], rope_o[:, 1, :], False, hk, ("Kall", hk, "o", g))
        for tt in range(4):
            vproj(hTo, tt * 128, [("hTo", tt, k) for k in range(KC)], Vall[:, 18 + g * 4 + tt, :], ("Vall", 18 + g * 4 + tt), 4 + tt % 2)
    A.pop()
    P.barrier()

    Wq = [A.alloc((KC, 128), BF16) for _ in range(2)]
    Wqr = [A.alloc((KC, 128), BF16) for _ in range(2)]
    Wo = [A.alloc((D,), BF16) for _ in range(2)]
    qT = [A.alloc((512,), BF16) for _ in range(2)]
    PT = [A.alloc((512,), BF16) for _ in range(3)]
    oT = [A.alloc((512,), BF16) for _ in range(2)]
    rden = A.alloc((512,))
    ytmp = [A.alloc((D,)) for _ in range(2)]
    groups = GROUPS if need_ctx else GROUPS[1:]

    def load_head(h):
        s = h % 2
        P.op("pool", lambda e: e.dma_start(out=Wq[s], in_=wqkv[:, :, h * 128:(h + 1) * 128]), writes=[("Wq", s)], dma=True)
        P.op("pool", lambda e: e.dma_start(out=Wo[s], in_=wo[h * 128:(h + 1) * 128, :]), writes=[("Wo", s)], dma=True)
        emit_rot_copy(P, Wqr[s], Wq[s], 32, ("Wq", s), ("Wqr", s))

    items = [(h, gi) for h in range(8) for gi in range(len(groups))]
    SB = [4, 5, 6]
    LOOK = 2
    ycnt = [0]

    def qproj(n):
        h, gi = items[n]
        s = h % 2
        (t0, ntile) = groups[gi]
        ntok = ntile * 128
        isctx = t0 < 2
        a = n % 2
        for k in range(KC):
            _mm(P, C.psum[0][:, 0:ntok], Wq[s][:, k, :], C.hT[:, k, t0 * 128:t0 * 128 + ntok],
                k == 0, k == KC - 1, reads=[("Wq", s)] + hT_keys(t0, ntile), writes=[("bank", 0)])
        if not isctx:
            for k in range(KC):
                _mm(P, C.psum[1][:, 0:ntok], Wqr[s][:, k, :], C.hT[:, k, t0 * 128:t0 * 128 + ntok],
                    k == 0, k == KC - 1, reads=[("Wqr", s)] + hT_keys(t0, ntile), writes=[("bank", 1)])
            c0 = (t0 - 2) * 128
            emit_qk_norm_rope(C, bank(0), bank(1), bank(2), ntok, qT[a][:, 0:ntok], gq, gqr, rope[:, 0, c0:c0 + ntok],
                              rope[:, 1, c0:c0 + ntok], tmp, eps_t, 1.0 / 128, "q", True, ("qT", a))
        else:
            emit_qk_norm_rope(C, bank(0), bank(1), bank(2), ntok, qT[a][:, 0:ntok], gq, gqr, None, None, tmp, eps_t,
                              1.0 / 128, "q", False, ("qT", a))

    def keyloop(n):
        h, gi = items[n]
        hk = h // 4
        (t0, ntile) = groups[gi]
        ntok = ntile * 128
        isctx = t0 < 2
        a = n % 2
        nkt = 2 if isctx else 34

        def qk(kt):
            sb = SB[kt % 3]
            pp = kt % 3
            _mm(P, C.psum[sb][:, 0:ntok], Kall[:, hk, kt * 128:(kt + 1) * 128], qT[a][:, 0:ntok], True, True,
                reads=["Kall", ("qT", a)], writes=[("bank", sb)])
            P.op("act", (lambda e: e.activation(out=PT[pp][:, 0:ntok], in_=C.psum[sb][:, 0:ntok], func=AF.Exp)),
                 reads=[("bank", sb)], writes=[("PT", pp)])

        def pv(kt):
            pp = kt % 3
            _mm(P, C.psum[3][:, 0:ntok], Vall[:, kt, hk * 128:(hk + 1) * 128], PT[pp][:, 0:ntok], kt == 0, kt == nkt - 1,
                reads=["Vall", ("PT", pp)], writes=[("bank", 3)])
            _mm(P, C.psum[2][:, 0:ntok], C.ones_bf2, PT[pp][:, 0:ntok], kt == 0, kt == nkt - 1,
                reads=["ones_bf", ("PT", pp)], writes=[("bank", 2)])

        for kt in range(min(LOOK, nkt)):
            qk(kt)
        for kt in range(nkt):
            if kt + LOOK < nkt:
                qk(kt + LOOK)
            pv(kt)
        P.op("dve", (lambda e: e.reciprocal(out=rden[:, 0:ntok], in_=C.psum[2][:, 0:ntok])),
             reads=[("bank", 2)], writes=["rden"])
        P.op("dve", (lambda e: e.tensor_tensor(out=oT[a][:, 0:ntok], in0=C.psum[3][:, 0:ntok], in1=rden[:, 0:ntok], op=ALU.mult)),
             reads=[("bank", 3), "rden"], writes=[("oT", a)])

    def yproj(n):
        h, gi = items[n]
        s = h % 2
        (t0, ntile) = groups[gi]
        a = n % 2
        for tt in range(ntile):
            t = t0 + tt
            o = ycnt[0] % 2
            ycnt[0] += 1
            gbc = gc if t < 2 else gx
            yb = (7, 0) if o == 0 else (1, 2)
            for nh in range(2):
                _mm(P, C.psum[yb[nh]][:, :], oT[a][:, tt * 128:(tt + 1) * 128], Wo[s][:, nh * 512:(nh + 1) * 512], True, True,
                    reads=[("oT", a), ("Wo", s)], writes=[("bank", yb[nh])])
                P.op("dve", (lambda e, nh=nh, o=o, gbc=gbc, yb=yb: e.tensor_tensor(
                    out=ytmp[o][:, nh * 512:(nh + 1) * 512], in0=C.psum[yb[nh]][:, :], in1=gbc[:, nh * 512:(nh + 1) * 512], op=ALU.mult)),
                    reads=[("bank", yb[nh]), ("bc", "gx"), ("bc", "gc")], writes=[("ytmp", o, nh)])
            P.op("pool", (lambda e, t=t, o=o: e.tensor_tensor(out=C.X[:, t, :], in0=C.X[:, t, :], in1=ytmp[o], op=ALU.add)),
                 reads=[("X", t), ("ytmp", o, 0), ("ytmp", o, 1)], writes=[("X", t)])

    load_head(0)
    load_head(1)
    qproj(0)
    for n in range(len(items)):
        keyloop(n)
        if n + 1 < len(items):
            qproj(n + 1)
        yproj(n)
        h_, gi_ = items[n]
        if gi_ == len(groups) - 1 and h_ + 2 < 8:
            load_head(h_ + 2)
    A.pop()
    P.barrier()
    A.push()
    bc2 = {"lg": A.alloc((D,)), "lb": A.alloc((D,))}
    for n, src in (("lg", C.dram["l%d_ln1_g" % i]), ("lb", C.dram["l%d_ln1_b" % i])):
        P.op("sp", (lambda e, n=n, src=src: e.dma_start(out=bc2[n], in_=src.partition_broadcast(128))), writes=[("bc", n)], dma=True)
    emit_ln(C, bc2, need_ctx=need_ctx)
    A.pop()
    P.barrier()


def emit_ln_bc(C, i, sub, need_ctx):
    P, A = C.P, C.A
    P.barrier()
    A.push()
    bc2 = {"lg": A.alloc((D,)), "lb": A.alloc((D,))}
    for n, src in (("lg", C.dram["l%d_ln%d_g" % (i, sub + 1)]), ("lb", C.dram["l%d_ln%d_b" % (i, sub + 1)])):
        P.op("sp", (lambda e, n=n, src=src: e.dma_start(out=bc2[n], in_=src.partition_broadcast(128))), writes=[("bc", n)], dma=True)
    emit_ln(C, bc2, need_ctx=need_ctx)
    A.pop()
    P.barrier()


def emit_cmlp(C, i):
    P, A, nc = C.P, C.A, C.nc
    need_ctx = i < DEPTH - 1
    pre = "l%d_cmlp_" % i
    w_in = C.dram[pre + "w_in"].rearrange("(k p) n -> p k n", p=128)
    w_out = C.dram[pre + "w_out"].rearrange("(c p) n -> p c n", p=128)
    P.barrier()
    A.push()
    gx = A.alloc((D,))
    gc = A.alloc((D,))
    gate_c0 = 2 * D
    P.op("sp", lambda e: e.dma_start(out=gx, in_=C.ada_scr[i, 0, gate_c0:gate_c0 + D].partition_broadcast(128)), writes=[("bc", "gx")], dma=True)
    P.op("sp", lambda e: e.dma_start(out=gc, in_=C.ada_scr[i, 1, gate_c0:gate_c0 + D].partition_broadcast(128)), writes=[("bc", "gc")], dma=True)
    emit_build_hT(C, i, 0, need_ctx=need_ctx)
    emit_scale_x(C, need_ctx=need_ctx)
    gv, bv, bu = A.alloc((16,)), A.alloc((16,)), A.alloc((16,))
    P.op("sp", lambda e: e.dma_start(out=gv, in_=C.dram[pre + "v_norm_g"].rearrange("(c p) -> p c", p=128), allow_slow_non_contiguous=True), writes=["gv"], dma=True)
    P.op("sp", lambda e: e.dma_start(out=bv, in_=C.dram[pre + "v_norm_b"].rearrange("(c p) -> p c", p=128), allow_slow_non_contiguous=True), writes=["bv"], dma=True)
    P.op("sp", lambda e: e.dma_start(out=bu, in_=C.dram[pre + "b_in"][0:2048].rearrange("(c p) -> p c", p=128), allow_slow_non_contiguous=True), writes=["bu"], dma=True)
    brow_v = A.alloc((2048,), BF16, parts=1)
    brow_o = A.alloc((D,), BF16, parts=1)
    P.op("pool", lambda e: e.dma_start(out=brow_v, in_=C.dram[pre + "b_in"][2048:4096].rearrange("(o n) -> o n", o=1)), writes=["brow_v"], dma=True)
    P.op("pool", lambda e: e.dma_start(out=brow_o, in_=C.dram[pre + "b_out"].rearrange("(o n) -> o n", o=1)), writes=["brow_o"], dma=True)
    WsT = A.alloc((8, 128), BF16)
    Bt = A.alloc((16, 128))
    ones_row = C.ones_bf2[0:1, :]
    A.push()
    Wsl = [A.alloc((128,)) for _ in range(2)]
    bsbc = A.alloc((8, 128))
    for g in range(8):
        s = g % 2
        P.op("sp", (lambda e, g=g, s=s: e.dma_start(out=Wsl[s], in_=C.dram[pre + "w_s"][g])), writes=[("Wsl", s)], dma=True)
        P.op("sp", (lambda e, g=g: e.dma_start(out=bsbc[:, g, :], in_=C.dram[pre + "b_s"][g].partition_broadcast(128))), writes=[("bsbc", g)], dma=True)
        P.op("pe", (lambda e, s=s: e.transpose(C.psum[s][:, 0:128], Wsl[s], C.ident)), reads=[("Wsl", s), "ident"], writes=[("bank", s)])
        P.op("act", (lambda e, g=g, s=s: e.copy(out=WsT[:, g, :], in_=C.psum[s][:, 0:128])), reads=[("bank", s)], writes=[("WsT", g)])
        _mm(P, C.psum[2 + s][:, 0:128], C.ones_bf2, WsT[:, g, :], True, True, reads=["ones_bf", ("WsT", g)], writes=[("bank", 2 + s)])
        for cb in (2 * g, 2 * g + 1):
            P.op("dve", (lambda e, g=g, s=s, cb=cb: e.scalar_tensor_tensor(
                out=Bt[:, cb, :], in0=C.psum[2 + s][:, 0:128], scalar=bv[:, cb:cb + 1], in1=bsbc[:, g, :], op0=ALU.mult, op1=ALU.add)),
                reads=[("bank", 2 + s), "bv", ("bsbc", g)], writes=[("Bt", cb)])
    A.pop()
    P.barrier()
    Wv = A.alloc((KC, 512), BF16)
    Wu = A.alloc((KC, 256), BF16)
    Wo = A.alloc((16, 512), BF16)
    z = A.alloc((4, 2048), BF16)
    uvT = A.alloc((16, 512), BF16)
    uT = A.alloc((512,))
    tmp = A.alloc((512,))
    ytmp = [A.alloc((512,)) for _ in range(2)]
    stats = A.alloc((4, 4, 6))
    mv = A.alloc((4, 2))
    rstd = A.alloc((4,))
    nmr = A.alloc((4,))
    groups = GROUPS if need_ctx else GROUPS[1:]
    ycnt = 0
    for (t0, ntile) in groups:
        ntok = ntile * 128
        for vb in range(4):
            P.op("pool", (lambda e, vb=vb: e.dma_start(out=Wv, in_=w_in[:, :, 2048 + vb * 512:2048 + (vb + 1) * 512])), writes=["Wv"], dma=True)
            for tt in range(ntile):
                t = t0 + tt
                pb = tt % 2
                _mm(P, C.psum[pb][:, :], ones_row, brow_v[:, vb * 512:(vb + 1) * 512], True, False, reads=["ones_bf", "brow_v"], writes=[("bank", pb)])
                for k in range(KC):
                    _mm(P, C.psum[pb][:, :], C.hT[:, k, t * 128:(t + 1) * 128], Wv[:, k, :], False, k == KC - 1,
                        reads=["Wv"] + hT_keys(t, 1), writes=[("bank", pb)])
                P.op("act", (lambda e, tt=tt, vb=vb, pb=pb: e.activation(out=z[:, tt, vb * 512:(vb + 1) * 512], in_=C.psum[pb][:, :], func=AF.Gelu_apprx_tanh)),
                     reads=[("bank", pb)], writes=[("z", tt, vb)])
        for tt in range(ntile):
            for vb in range(4):
                P.op("dve", (lambda e, tt=tt, vb=vb: e.bn_stats(out=stats[:, tt, vb, :], in_=z[:, tt, vb * 512:(vb + 1) * 512])),
                     reads=[("z", tt, vb)], writes=[("zst", tt, vb)])
            P.op("dve", (lambda e, tt=tt: e.bn_aggr(out=mv[:, tt, :], in_=stats[:, tt, :, :])), reads=[("zst", tt, vb) for vb in range(4)], writes=[("zmv", tt)])
        zk = [("zmv", tt) for tt in range(ntile)]
        P.op("dve", (lambda e, ntile=ntile: e.tensor_scalar(out=rstd[:, 0:ntile], in0=mv[:, 0:ntile, 1], scalar1=float(LN_EPS), scalar2=None, op0=ALU.add)),
             reads=zk, writes=["zrstd"])
        P.op("act", (lambda e, ntile=ntile: e.activation(out=rstd[:, 0:ntile], in_=rstd[:, 0:ntile], func=AF.Sqrt)), reads=["zrstd"], writes=["zrstd"])
        P.op("dve", (lambda e, ntile=ntile: e.reciprocal(out=rstd[:, 0:ntile], in_=rstd[:, 0:ntile])), reads=["zrstd"], writes=["zrstd"])
        P.op("dve", (lambda e, ntile=ntile: e.scalar_tensor_tensor(out=nmr[:, 0:ntile], in0=mv[:, 0:ntile, 0], scalar=-1.0, in1=rstd[:, 0:ntile],
                                                                  op0=ALU.mult, op1=ALU.mult)), reads=zk + ["zrstd"], writes=["znmr"])
        for tt in range(ntile):
            P.op("act", (lambda e, tt=tt: e.activation(out=z[:, tt, :], in_=z[:, tt, :], func=AF.Identity, bias=nmr[:, tt:tt + 1], scale=rstd[:, tt:tt + 1])),
                 reads=[("z", tt, vb) for vb in range(4)] + ["zrstd", "znmr"], writes=[("zn", tt)])
        for cb in range(16):
            g = cb // 2
            if cb % 2 == 0:
                P.op("pool", (lambda e, cb=cb: e.dma_start(out=Wu, in_=w_in[:, :, cb * 128:(cb + 2) * 128])), writes=["Wu"], dma=True)
            pu = C.psum[2 + cb % 2]
            ps = C.psum[4 + cb % 2]
            for k in range(KC):
                _mm(P, pu[:, 0:ntok], Wu[:, k, (cb % 2) * 128:(cb % 2 + 1) * 128], C.hT[:, k, t0 * 128:t0 * 128 + ntok],
                    k == 0, k == KC - 1, reads=["Wu"] + hT_keys(t0, ntile), writes=[("bank", 2 + cb % 2)])
            P.op("dve", (lambda e, pu=pu, cb=cb, ntok=ntok: e.tensor_scalar(out=uT[:, 0:ntok], in0=pu[:, 0:ntok], scalar1=bu[:, cb:cb + 1], scalar2=None, op0=ALU.add)),
                 reads=[("bank", 2 + cb % 2), "bu"], writes=["uT"])
            P.op("act", (lambda e, ntok=ntok: e.activation(out=uT[:, 0:ntok], in_=uT[:, 0:ntok], func=AF.Gelu_apprx_tanh)),
                 reads=["uT"], writes=["uT"])
            for tt in range(ntile):
                _mm(P, ps[:, tt * 128:(tt + 1) * 128], z[:, tt, cb * 128:(cb + 1) * 128], WsT[:, g, :], True, True,
                    reads=[("zn", tt), ("WsT", g)], writes=[("bank", 4 + cb % 2)])
            P.op("dve", (lambda e, ps=ps, cb=cb, ntile=ntile, ntok=ntok: e.scalar_tensor_tensor(
                out=tmp[:, 0:ntok].rearrange("p (a b) -> p a b", b=128), in0=ps[:, 0:ntok].rearrange("p (a b) -> p a b", b=128),
                scalar=gv[:, cb:cb + 1], in1=Bt[:, cb:cb + 1, :].broadcast_to([128, ntile, 128]), op0=ALU.mult, op1=ALU.add)),
                reads=[("bank", 4 + cb % 2), "gv", ("Bt", cb)], writes=["cm_tmp"])
            P.op("dve", (lambda e, cb=cb, ntok=ntok: e.tensor_tensor(out=uvT[:, cb, 0:ntok], in0=tmp[:, 0:ntok], in1=uT[:, 0:ntok], op=ALU.mult)),
                 reads=["cm_tmp", "uT"], writes=[("uvT", cb)])
        uk = [("uvT", cb) for cb in range(16)]
        for nh in range(2):
            P.op("pool", (lambda e, nh=nh: e.dma_start(out=Wo, in_=w_out[:, :, nh * 512:(nh + 1) * 512])), writes=["Wo"], dma=True)
            for tt in range(ntile):
                t = t0 + tt
                o = ycnt % 2
                ycnt += 1
                py = C.psum[6 + o]
                gbc = gc if t < 2 else gx
                _mm(P, py[:, :], ones_row, brow_o[:, nh * 512:(nh + 1) * 512], True, False, reads=["ones_bf", "brow_o"], writes=[("bank", 6 + o)])
                for cb in range(16):
                    _mm(P, py[:, :], uvT[:, cb, tt * 128:(tt + 1) * 128], Wo[:, cb, :], False, cb == 15,
                        reads=["Wo"] + (uk if cb in (0, 15) else []), writes=[("bank", 6 + o)])
                P.op("dve", (lambda e, py=py, o=o, gbc=gbc, nh=nh: e.tensor_tensor(out=ytmp[o], in0=py[:, :], in1=gbc[:, nh * 512:(nh + 1) * 512], op=ALU.mult)),
                     reads=[("bank", 6 + o), ("bc", "gx"), ("bc", "gc")], writes=[("ytmp", o)])
                P.op("pool", (lambda e, t=t, o=o, nh=nh: e.tensor_tensor(out=C.X[:, t, nh * 512:(nh + 1) * 512], in0=C.X[:, t, nh * 512:(nh + 1) * 512], in1=ytmp[o], op=ALU.add)),
                     reads=[("X", t), ("ytmp", o)], writes=[("X", t)])
    A.pop()
    emit_ln_bc(C, i, 0, need_ctx)


def emit_retention(C, i):
    P, A, nc = C.P, C.A, C.nc
    need_ctx = i < DEPTH - 1
    w = C.dram["l%d_ret_wqkvg" % i].rearrange("(k p) n -> p k n", p=128)
    wo = C.dram["l%d_ret_wo" % i]
    xacc = C.xacc
    P.barrier()
    A.push()
    gx = A.alloc((D,))
    gc = A.alloc((D,))
    gate_c0 = 2 * D
    P.op("sp", lambda e: e.dma_start(out=gx, in_=C.ada_scr[i, 0, gate_c0:gate_c0 + D].partition_broadcast(128)), writes=[("bc", "gx")], dma=True)
    P.op("sp", lambda e: e.dma_start(out=gc, in_=C.ada_scr[i, 1, gate_c0:gate_c0 + D].partition_broadcast(128)), writes=[("bc", "gc")], dma=True)
    emit_build_hT(C, i, 0, need_ctx=True)
    emit_scale_x(C, need_ctx=True)
    for t in range(NT):
        P.op("sp", (lambda e, t=t: e.dma_start(out=xacc[t * 128:(t + 1) * 128, :], in_=C.X[:, t, :])), reads=[("X", t)], writes=[("xacc", t)], dma=True)
    P.barrier()
    A2 = Arena(C.Xraw, NT * D)
    Kh = A2.alloc((2, CTX + SEQ), BF16)
    Vh = A2.alloc((34, 512), BF16)
    oT32 = A2.alloc((4, 512))
    ogT = A2.alloc((4, 512), BF16)
    Xt = A2.alloc((D,))
    hTo = A.alloc((KC, 512), BF16)
    ropeg = A.alloc((2, 2, 512))
    wreg_top = A.top
    Wreg = A.alloc((6144,))
    qT = [A.alloc((2, 512), BF16) for _ in range(2)]
    PT = [A.alloc((512,), BF16) for _ in range(3)]
    tmp = [A.alloc((512,)) for _ in range(4)]
    sq = A.alloc((4, 512), BF16)
    rstd = A.alloc((512,))
    ytmp = [A.alloc((D,)) for _ in range(2)]
    dec = A.alloc((8,))
    lg = A.alloc((8,))
    nlg = A.alloc((8,))
    lg128 = A.alloc((8,))
    nlg128 = A.alloc((8,))
    dji_i = A.alloc((128,), I32)
    dji = A.alloc((128,))
    mrow_i = A.alloc((40,), I32)
    mrow = A.alloc((40,))
    Ef, Eb, Df, Db, Dd = (A.alloc((128,)) for _ in range(5))
    Tf = A.alloc((4, 128))
    Tb = A.alloc((4, 128))
    pwf, pwb, npwb = A.alloc((40,)), A.alloc((40,)), A.alloc((40,))
    P.op("sp", lambda e: e.dma_start(out=dec, in_=C.dram["ret_dec"].rearrange("a b -> (a b)").partition_broadcast(128)), writes=["dec"], dma=True)
    P.op("act", lambda e: e.activation(out=lg, in_=dec, func=AF.Exp), reads=["dec"], writes=["nlg"])
    P.op("dve", lambda e: e.tensor_scalar(out=nlg, in0=lg, scalar1=1.0, scalar2=None, op0=ALU.mult), reads=["nlg"], writes=["nlg2"])
    P.op("dve", lambda e: e.tensor_scalar(out=lg, in0=nlg, scalar1=-1.0, scalar2=None, op0=ALU.mult), reads=["nlg2"], writes=["lg"])
    P.op("dve", lambda e: e.tensor_scalar(out=lg128, in0=lg, scalar1=128.0, scalar2=None, op0=ALU.mult), reads=["lg"], writes=["lg128"])
    P.op("dve", lambda e: e.tensor_scalar(out=nlg128, in0=nlg, scalar1=128.0, scalar2=None, op0=ALU.mult), reads=["nlg2"], writes=["nlg128"])
    P.op("pool", lambda e: e.iota(dji_i, pattern=[[1, 128]], base=0, channel_multiplier=-1), writes=["dji_i"])
    P.op("pool", lambda e: e.iota(mrow_i, pattern=[[1, 40]], base=0, channel_multiplier=0), writes=["mrow_i"])
    P.op("dve", lambda e: e.tensor_copy(out=dji, in_=dji_i), reads=["dji_i"], writes=["dji"])
    P.op("dve", lambda e: e.tensor_copy(out=mrow, in_=mrow_i), reads=["mrow_i"], writes=["mrow"])
    P.barrier()
    bank = lambda n: ("bank", n)
    hk_all = [("hT", t, k) for t in range(NT) for k in range(KC)]

    def do_head(hd):
        f, bb = hd, 4 + hd
        P.op("act", lambda e: e.activation(out=Ef, in_=dji, func=AF.Exp, scale=lg[:, f:f + 1]), reads=["dji", "lg"], writes=["Ef"])
        P.op("act", lambda e: e.activation(out=Eb, in_=dji, func=AF.Exp, scale=nlg[:, bb:bb + 1]), reads=["dji", "nlg2"], writes=["Eb"])
        P.op("act", lambda e: e.activation(out=pwf, in_=mrow, func=AF.Exp, scale=lg128[:, f:f + 1]), reads=["mrow", "lg128"], writes=["pwf"])
        P.op("act", lambda e: e.activation(out=pwb, in_=mrow, func=AF.Exp, scale=lg128[:, bb:bb + 1]), reads=["mrow", "lg128"], writes=["pwb"])
        P.op("act", lambda e: e.activation(out=npwb, in_=mrow, func=AF.Exp, scale=nlg128[:, bb:bb + 1]), reads=["mrow", "nlg128"], writes=["npwb"])
        P.op("pool", lambda e: e.affine_select(out=Df, in_=Ef, pattern=[[1, 128]], compare_op=ALU.is_ge, fill=0.0, base=0, channel_multiplier=-1),
             reads=["Ef"], writes=["Df"])
        P.op("pool", lambda e: e.affine_select(out=Db, in_=Eb, pattern=[[-1, 128]], compare_op=ALU.is_ge, fill=0.0, base=0, channel_multiplier=1),
             reads=["Eb"], writes=["Db"])
        P.op("dve", lambda e: e.tensor_tensor(out=Dd, in0=Df, in1=Db, op=ALU.add), reads=["Df", "Db"], writes=["Dd"])
        for m in range(4):
            P.op("dve", (lambda e, m=m: e.tensor_scalar(out=Tf[:, m, :], in0=Ef, scalar1=pwf[:, m:m + 1], scalar2=None, op0=ALU.mult)),
                 reads=["Ef", "pwf"], writes=[("Tf", m)])
            P.op("dve", (lambda e, m=m: e.tensor_scalar(out=Tb[:, m, :], in0=Eb, scalar1=npwb[:, m:m + 1], scalar2=None, op0=ALU.mult)),
                 reads=["Eb", "npwb"], writes=[("Tb", m)])
        ret_phase = int(os.environ.get("MK_RET_PHASE", "5"))
        if ret_phase < 2:
            return
        A.top = wreg_top
        Wk = A.alloc((KC, 256), BF16)
        Wkr = A.alloc((KC, 256), BF16)
        Wv = A.alloc((KC, 512), BF16)
        P.op("pool", lambda e: e.dma_start(out=Wk, in_=w[:, :, 1024 + hd * 256:1024 + (hd + 1) * 256]), writes=["Wk"], dma=True)
        P.op("pool", lambda e: e.dma_start(out=Wv, in_=w[:, :, 2048 + hd * 512:2048 + (hd + 1) * 512]), writes=["Wv"], dma=True)
        P.op("pool", lambda e: e.tensor_scalar(out=Wk, in0=Wk, scalar1=0.0625, scalar2=None, op0=ALU.mult), reads=["Wk"], writes=["Wk"])
        emit_rot_copy(P, Wkr, Wk, 64, "Wk", "Wkr")

        def kv_group(src, c0, ntile, hkeys, kcol0, ktile0, isctx, ropet):
            ntok = ntile * 128
            for dc in range(2):
                for k in range(KC):
                    _mm(P, C.psum[0][:, 0:ntok], Wk[:, k, dc * 128:(dc + 1) * 128], src[:, k, c0:c0 + ntok], k == 0, k == KC - 1,
                        reads=["Wk"] + hkeys, writes=[bank(0)])
                if isctx:
                    P.op("act", (lambda e, dc=dc: e.copy(out=Kh[:, dc, kcol0:kcol0 + ntok], in_=C.psum[0][:, 0:ntok])), reads=[bank(0)], writes=["Kh"])
                    continue
                for k in range(KC):
                    _mm(P, C.psum[1][:, 0:ntok], Wkr[:, k, dc * 128:(dc + 1) * 128], src[:, k, c0:c0 + ntok], k == 0, k == KC - 1,
                        reads=["Wkr"] + hkeys, writes=[bank(1)])
                P.op("dve", (lambda e, dc=dc: e.tensor_tensor(out=tmp[0][:, 0:ntok], in0=C.psum[0][:, 0:ntok], in1=ropet[:, dc, 0, 0:ntok], op=ALU.mult)),
                     reads=[bank(0), "ropeg"], writes=["t0"])
                P.op("dve", (lambda e, dc=dc: e.tensor_tensor(out=tmp[1][:, 0:ntok], in0=C.psum[1][:, 0:ntok], in1=ropet[:, dc, 1, 0:ntok], op=ALU.mult)),
                     reads=[bank(1), "ropeg"], writes=["t1"])
                P.op("pool", (lambda e, dc=dc: e.tensor_tensor(out=Kh[:, dc, kcol0:kcol0 + ntok], in0=tmp[0][:, 0:ntok], in1=tmp[1][:, 0:ntok], op=ALU.add)),
                     reads=["t0", "t1"], writes=["Kh"])
            for tt in range(ntile):
                pb = 2 + tt % 2
                for k in range(KC):
                    _mm(P, C.psum[pb][:, :], src[:, k, c0 + tt * 128:c0 + (tt + 1) * 128], Wv[:, k, :], k == 0, k == KC - 1,
                        reads=["Wv"] + hkeys, writes=[bank(pb)])
                P.op("act", (lambda e, tt=tt, pb=pb: e.copy(out=Vh[:, ktile0 + tt, :], in_=C.psum[pb][:, :])), reads=[bank(pb)], writes=["Vh"])

        for (t0, ntile) in GROUPS:
            if t0 < 2:
                kv_group(C.hT, 0, ntile, hk_all, 0, 0, True, None)
            else:
                c0 = (t0 - 2) * 128
                P.op("sp", (lambda e, c0=c0: e.dma_start(out=ropeg, in_=C.dram["rope_r"][:, :, :, c0:c0 + 512])), writes=["ropeg"], dma=True)
                kv_group(C.hT, t0 * 128, ntile, hk_all, CTX + c0, t0, False, ropeg)
        for g in range(4):
            P.op("sp", (lambda e, g=g: e.dma_start(out=ropeg, in_=C.dram["rope_ro"][:, :, :, g * 512:(g + 1) * 512])), writes=["ropeg"], dma=True)
            for tt in range(4):
                tg = g * 4 + tt
                P.op("sp", (lambda e, tg=tg: e.dma_start(out=Xt, in_=C.dram["x_oth"][tg * 128:(tg + 1) * 128, :])), writes=["Xt"], dma=True)
                for k in range(KC):
                    pst = C.psum[6 + k // 4][:, (k % 4) * 128:(k % 4 + 1) * 128]
                    P.op("pe", (lambda e, k=k, pst=pst: e.transpose(pst, Xt[:, k * 128:(k + 1) * 128], C.ident)),
                         reads=["Xt", "ident"], writes=[bank(6 + k // 4)])
                for k in range(KC):
                    pst = C.psum[6 + k // 4][:, (k % 4) * 128:(k % 4 + 1) * 128]
                    P.op("dve", (lambda e, k=k, pst=pst, tt=tt: e.tensor_scalar(
                        out=hTo[:, k, tt * 128:(tt + 1) * 128], in0=pst,
                        scalar1=C.sc1p[:, i, 0, k, 0:1], scalar2=C.adaT[:, i, k, 0:1], op0=ALU.mult, op1=ALU.add)),
                        reads=[bank(6 + k // 4)], writes=[("hTo", tt, k)])
            kv_group(hTo, 0, 4, [("hTo", tt, k) for tt in range(4) for k in range(KC)], CTX + TOK + g * 512, 18 + g * 4, False, ropeg)
        P.barrier()
        if ret_phase < 3:
            return
        A.top = wreg_top
        Wq = A.alloc((KC, 256), BF16)
        Wqr = A.alloc((KC, 256), BF16)
        Wg = A.alloc((KC, 512), BF16)
        Wo = A.alloc((4, D), BF16)
        P.op("pool", lambda e: e.dma_start(out=Wq, in_=w[:, :, hd * 256:(hd + 1) * 256]), writes=["Wq"], dma=True)
        P.op("pool", lambda e: e.dma_start(out=Wg, in_=w[:, :, 4096 + hd * 512:4096 + (hd + 1) * 512]), writes=["Wg"], dma=True)
        P.op("pool", lambda e: e.dma_start(out=Wo, in_=wo[hd * 512:(hd + 1) * 512, :].rearrange("(c p) n -> p c n", p=128)), writes=["Wo"], dma=True)
        emit_rot_copy(P, Wqr, Wq, 64, "Wq", "Wqr")
        ycnt_box = [0]

        def do_group(t0, ntile, a):
            ntok = ntile * 128
            isctx = t0 < 2
            lq0 = t0 - 2
            ycnt = ycnt_box[0]
            hkeys = hT_keys(t0, ntile)
            if not isctx:
                P.op("sp", (lambda e, lq0=lq0: e.dma_start(out=ropeg, in_=C.dram["rope_r"][:, :, :, lq0 * 128:lq0 * 128 + 512])), writes=["ropeg"], dma=True)
            for dc in range(2):
                for k in range(KC):
                    _mm(P, C.psum[0][:, 0:ntok], Wq[:, k, dc * 128:(dc + 1) * 128], C.hT[:, k, t0 * 128:t0 * 128 + ntok], k == 0, k == KC - 1,
                        reads=["Wq"] + hkeys, writes=[bank(0)])
                if isctx:
                    P.op("act", (lambda e, dc=dc, a=a: e.copy(out=qT[a][:, dc, 0:ntok], in_=C.psum[0][:, 0:ntok])), reads=[bank(0)], writes=[("qT", a, dc)])
                    continue
                for k in range(KC):
                    _mm(P, C.psum[1][:, 0:ntok], Wqr[:, k, dc * 128:(dc + 1) * 128], C.hT[:, k, t0 * 128:t0 * 128 + ntok], k == 0, k == KC - 1,
                        reads=["Wqr"] + hkeys, writes=[bank(1)])
                P.op("dve", (lambda e, dc=dc: e.tensor_tensor(out=tmp[0][:, 0:ntok], in0=C.psum[0][:, 0:ntok], in1=ropeg[:, dc, 0, 0:ntok], op=ALU.mult)),
                     reads=[bank(0), "ropeg"], writes=["t0"])
                P.op("dve", (lambda e, dc=dc: e.tensor_tensor(out=tmp[1][:, 0:ntok], in0=C.psum[1][:, 0:ntok], in1=ropeg[:, dc, 1, 0:ntok], op=ALU.mult)),
                     reads=[bank(1), "ropeg"], writes=["t1"])
                P.op("pool", (lambda e, dc=dc, a=a: e.tensor_tensor(out=qT[a][:, dc, 0:ntok], in0=tmp[0][:, 0:ntok], in1=tmp[1][:, 0:ntok], op=ALU.add)),
                     reads=["t0", "t1"], writes=[("qT", a, dc)])
            ktiles = [0, 1] if isctx else list(range(34))
            SBR = [2, 3, 1]

            def qk_r(ki):
                kt = ktiles[ki]
                sb = SBR[ki % 3]
                pp = ki % 3
                ps = C.psum[sb]
                for dc in range(2):
                    _mm(P, ps[:, 0:ntok], Kh[:, dc, kt * 128:(kt + 1) * 128], qT[a][:, dc, 0:ntok], dc == 0, dc == 1,
                        reads=["Kh", ("qT", a, dc)], writes=[bank(sb)])
                pt = PT[pp]
                rk = [bank(sb), "tables"]
                wk_ = [("PT", pp)]

                def one(outv, inv, scal, tab, rk=rk, wk_=wk_, wkeys=None):
                    P.op("dve", (lambda e: e.scalar_tensor_tensor(out=outv, in0=inv, scalar=scal, in1=tab, op0=ALU.mult, op1=ALU.mult)),
                         reads=rk, writes=(wk_ if wkeys is None else wkeys))

                def sub(m, mode, idx, ps=ps, pt=pt):
                    sl = slice(m * 128, (m + 1) * 128)
                    if mode == "f":
                        one(pt[:, sl], ps[:, sl], pwf[:, idx:idx + 1], Ef)
                    elif mode == "b":
                        one(pt[:, sl], ps[:, sl], pwb[:, idx:idx + 1], Eb)
                    else:
                        P.op("dve", (lambda e: e.tensor_tensor(out=pt[:, sl], in0=ps[:, sl], in1=Dd, op=ALU.mult)), reads=rk, writes=wk_)

                Tfv = Tf.rearrange("p a b -> p (a b)")
                Tbv = Tb.rearrange("p a b -> p (a b)")
                if isctx:
                    for m in range(2):
                        if kt < m:
                            sub(m, "f", 1)
                        elif kt == m:
                            sub(m, "d", 0)
                        else:
                            sub(m, "b", 1)
                elif kt < 2:
                    one(tmp[2][:, 0:ntok], ps[:, 0:ntok], pwf[:, lq0 + 2 - kt:lq0 + 3 - kt], Tfv, wkeys=["t2"])
                    P.op("dve", (lambda e, ps=ps, kt=kt: e.scalar_tensor_tensor(out=tmp[3][:, 0:ntok], in0=ps[:, 0:ntok],
                                                                               scalar=pwb[:, 32 + kt - lq0:33 + kt - lq0], in1=Tbv, op0=ALU.mult, op1=ALU.mult)),
                         reads=rk, writes=["t3"])
                    P.op("pool", (lambda e, pt=pt: e.tensor_tensor(out=pt[:, 0:ntok], in0=tmp[2][:, 0:ntok], in1=tmp[3][:, 0:ntok], op=ALU.add)),
                         reads=["t2", "t3"], writes=wk_)
                elif kt < 18:
                    lk = kt - 2
                    if lk < lq0:
                        one(pt[:, 0:ntok], ps[:, 0:ntok], pwf[:, lq0 - lk:lq0 - lk + 1], Tfv)
                    elif lk > lq0 + 3:
                        one(pt[:, 0:ntok], ps[:, 0:ntok], pwb[:, lk - lq0:lk - lq0 + 1], Tbv)
                    else:
                        for m in range(4):
                            dlt = lk - lq0 - m
                            if dlt > 0:
                                sub(m, "b", dlt)
                            elif dlt == 0:
                                sub(m, "d", 0)
                            else:
                                sub(m, "f", -dlt)
                else:
                    lk = 16 + (kt - 18)
                    one(pt[:, 0:ntok], ps[:, 0:ntok], pwb[:, lk - lq0:lk - lq0 + 1], Tbv)

            def pv_r(ki):
                kt = ktiles[ki]
                pp = ki % 3
                pt = PT[pp]
                for eb in range(4):
                    _mm(P, C.psum[4 + eb][:, 0:ntok], Vh[:, kt, eb * 128:(eb + 1) * 128], pt[:, 0:ntok], ki == 0, ki == len(ktiles) - 1,
                        reads=["Vh", ("PT", pp)], writes=[bank(4 + eb)])

            for ki in range(min(2, len(ktiles))):
                qk_r(ki)
            for ki in range(len(ktiles)):
                if ki + 2 < len(ktiles):
                    qk_r(ki + 2)
                pv_r(ki)
            if ret_phase < 4:
                return
            for eb in range(4):
                P.op("act", (lambda e, eb=eb: e.copy(out=oT32[:, eb, 0:ntok], in_=C.psum[4 + eb][:, 0:ntok])), reads=[bank(4 + eb)], writes=[("oT32", eb)])
                P.op("act", (lambda e, eb=eb: e.activation(out=sq[:, eb, 0:ntok], in_=C.psum[4 + eb][:, 0:ntok], func=AF.Square)),
                     reads=[bank(4 + eb)], writes=[("sq", eb)])
            for eb in range(4):
                _mm(P, C.psum[0][:, 0:ntok], C.ones_bf2, sq[:, eb, 0:ntok], eb == 0, eb == 3, reads=[("sq", eb), "ones_bf"], writes=[bank(0)])
            P.op("dve", lambda e: e.tensor_scalar(out=rstd[:, 0:ntok], in0=C.psum[0][:, 0:ntok], scalar1=1.0 / 512, scalar2=float(RMS_EPS),
                                                  op0=ALU.mult, op1=ALU.add), reads=[bank(0)], writes=["rstd"])
            P.op("act", lambda e: e.activation(out=rstd[:, 0:ntok], in_=rstd[:, 0:ntok], func=AF.Ln), reads=["rstd"], writes=["rstd"])
            P.op("act", lambda e: e.activation(out=rstd[:, 0:ntok], in_=rstd[:, 0:ntok], func=AF.Exp, scale=-0.5), reads=["rstd"], writes=["rstd"])
            for eb in range(4):
                pg = C.psum[1 + eb % 2] if False else C.psum[2 + eb % 2]
                pgk = bank(2 + eb % 2)
                for k in range(KC):
                    _mm(P, pg[:, 0:ntok], Wg[:, k, eb * 128:(eb + 1) * 128], C.hT[:, k, t0 * 128:t0 * 128 + ntok], k == 0, k == KC - 1,
                        reads=["Wg"] + hkeys, writes=[pgk])
                P.op("act", (lambda e, pg=pg: e.copy(out=tmp[3][:, 0:ntok], in_=pg[:, 0:ntok])), reads=[pgk], writes=["t3"])
                P.op("act", (lambda e: e.activation(out=tmp[0][:, 0:ntok], in_=tmp[3][:, 0:ntok], func=AF.Exp, scale=-1.0)), reads=["t3"], writes=["t0"])
                P.op("dve", lambda e: e.tensor_scalar(out=tmp[0][:, 0:ntok], in0=tmp[0][:, 0:ntok], scalar1=1.0, scalar2=None, op0=ALU.add),
                     reads=["t0"], writes=["t0"])
                P.op("dve", lambda e: e.reciprocal(out=tmp[0][:, 0:ntok], in_=tmp[0][:, 0:ntok]), reads=["t0"], writes=["t0"])
                P.op("dve", (lambda e: e.tensor_tensor(out=tmp[1][:, 0:ntok], in0=tmp[3][:, 0:ntok], in1=tmp[0][:, 0:ntok], op=ALU.mult)),
                     reads=["t3", "t0"], writes=["t1"])
                P.op("pool", (lambda e, eb=eb: e.tensor_tensor(out=tmp[2][:, 0:ntok], in0=oT32[:, eb, 0:ntok], in1=rstd[:, 0:ntok], op=ALU.mult)),
                     reads=[("oT32", eb), "rstd"], writes=["t2"])
                P.op("dve", (lambda e, eb=eb: e.tensor_tensor(out=ogT[:, eb, 0:ntok], in0=tmp[2][:, 0:ntok], in1=tmp[1][:, 0:ntok], op=ALU.mult)),
                     reads=["t1", "t2"], writes=[("ogT", eb)])
            if ret_phase < 5:
                return
            ok_ = [("ogT", eb) for eb in range(4)]
            for tt in range(ntile):
                t = t0 + tt
                o = ycnt % 2
                ycnt += 1
                gbc = gc if t < 2 else gx
                for nh in range(2):
                    py = C.psum[nh]
                    for eb in range(4):
                        _mm(P, py[:, :], ogT[:, eb, tt * 128:(tt + 1) * 128], Wo[:, eb, nh * 512:(nh + 1) * 512], eb == 0, eb == 3,
                            reads=ok_ + ["Wo"], writes=[bank(nh)])
                    P.op("dve", (lambda e, py=py, nh=nh, o=o, gbc=gbc: e.tensor_tensor(out=ytmp[o][:, nh * 512:(nh + 1) * 512], in0=py[:, :],
                                                                                        in1=gbc[:, nh * 512:(nh + 1) * 512], op=ALU.mult)),
                         reads=[bank(nh), ("bc", "gx"), ("bc", "gc")], writes=[("ytmp", o, nh)])
                P.op("pool", (lambda e, t=t, o=o: e.dma_start(out=xacc[t * 128:(t + 1) * 128, :], in_=ytmp[o], accum_op=ALU.add)),
                     reads=[("ytmp", o, 0), ("ytmp", o, 1)], writes=[("xacc", t)], dma=True)
            ycnt_box[0] = ycnt

        for gi, (t0_, ntile_) in enumerate(GROUPS):
            do_group(t0_, ntile_, gi % 2)
        P.barrier()

    for hd_ in range(4):
        do_head(hd_)
    for t in range(NT):
        P.op("sp", (lambda e, t=t: e.dma_start(out=C.X[:, t, :], in_=xacc[t * 128:(t + 1) * 128, :])), writes=[("X", t)], dma=True)
    A.pop()
    emit_ln_bc(C, i, 0, need_ctx)


_PROG_CACHE = {}


def _rope_tables(hd, nf):
    theta = np.float32(10000.0)
    inv = (theta ** (-(np.arange(nf, dtype=np.float32) / np.float32(nf)))).astype(np.float32)
    n = np.arange(SEQ)
    row = (n // 64).astype(np.float32)
    col = (n % 64).astype(np.float32)
    d = np.arange(hd)
    pos = np.where((d < hd // 2)[:, None], row[None, :], col[None, :]).astype(np.float32)
    ang = (pos * inv[d % nf][:, None]).astype(np.float32)
    sign = np.where((d % (hd // 2)) < nf, -1.0, 1.0).astype(np.float32)[:, None]
    return np.stack([np.cos(ang), np.sin(ang) * sign], axis=1).astype(np.float32)


def _core_inputs(inputs, layers_needed, x0=None, xc0=None, flip_odd=False):
    x = np.asarray(inputs["x"] if x0 is None else x0, dtype=np.float32)
    ctx = np.asarray(inputs["ctx"] if xc0 is None else xc0, dtype=np.float32)
    c = np.asarray(inputs["c"], dtype=np.float32)
    c_ctx = np.asarray(inputs["c_ctx"], dtype=np.float32)
    ident = np.eye(128, dtype=np.float32)
    shared = {"ident": ident}
    rope_a = _rope_tables(128, 32)
    rope_r = _rope_tables(256, 64).reshape(2, 128, 2, SEQ).transpose(1, 0, 2, 3)
    for i in layers_needed:
        for n in layer_param_names(i):
            shared[n] = np.ascontiguousarray(np.asarray(inputs[n], dtype=np.float32))
            if NEXP_RUN < NEXP and ("moe_w_gu" in n or "moe_w_down" in n):
                shared[n] = np.ascontiguousarray(shared[n][:NEXP_RUN])
    maps = []
    for r in range(8):
        b, h = r // 2, r % 2
        cc = np.stack([c[b].reshape(KC, 128).T, c_ctx.reshape(KC, 128).T], axis=-1)
        m = dict(shared)
        own = slice(h * TOK, (h + 1) * TOK)
        oth = slice((1 - h) * TOK, (2 - h) * TOK)
        rev = flip_odd and h == 1
        st = -1 if rev else 1
        m["x_in"] = np.ascontiguousarray(x[b, own][::st])
        m["ctx_in"] = np.ascontiguousarray(ctx[b][::st])
        m["x_oth"] = np.ascontiguousarray(x[b, oth][::st])
        m["cc"] = np.ascontiguousarray(cc.astype(np.float32))
        m["rope_a"] = np.ascontiguousarray(rope_a[:, :, own][:, :, ::st])
        m["rope_o"] = np.ascontiguousarray(rope_a[:, :, oth][:, :, ::st])
        m["rope_r"] = np.ascontiguousarray(rope_r[:, :, :, own][:, :, :, ::st])
        m["rope_ro"] = np.ascontiguousarray(rope_r[:, :, :, oth][:, :, :, ::st])
        for i in layers_needed:
            if i % 3 == 2:
                dec = np.asarray(inputs["l%d_ret_decay" % i], dtype=np.float32)
                m["ret_dec"] = np.ascontiguousarray(dec[::st])
            if i % 3 == 1 and rev:
                m["l%d_cmlp_w_s" % i] = np.ascontiguousarray(shared["l%d_cmlp_w_s" % i][:, ::-1, ::-1])
                m["l%d_cmlp_b_s" % i] = np.ascontiguousarray(shared["l%d_cmlp_b_s" % i][:, ::-1])
        maps.append(m)
    return maps


def run_stages(inputs, stages, x0=None, xc0=None):
    layers_needed = sorted(set(i for _, i in stages))
    key = tuple(stages)
    if key not in _PROG_CACHE:
        _PROG_CACHE[key] = build_program(stages, layers_needed)
    nc = _PROG_CACHE[key]
    flip_odd = any(k == "mix" and i % 3 == 2 for k, i in stages)
    maps = _core_inputs(inputs, layers_needed, x0, xc0, flip_odd)
    maps = [{k: v for k, v in m.items() if k in nc.mk_inputs} for m in maps]
    if os.environ.get("MK_TRACE"):
        res = run_bass_kernel_spmd(nc, maps, core_ids=list(range(8)), trace=True)
        print("MK_TRACE exec_time_ns", stages, res.exec_time_ns)
        try:
            import json, collections
            pj = res.profile_json
            if isinstance(pj, (list, tuple)):
                pj = pj[0]
            if isinstance(pj, str):
                pj = json.loads(pj)
            print("MK_TRACE profile keys", list(pj.keys())[:40] if isinstance(pj, dict) else type(pj))
            if isinstance(pj, dict):
                for k, v in pj.items():
                    if isinstance(v, (int, float, str)):
                        print("  ", k, v)
                    elif isinstance(v, dict):
                        print("  ", k, {kk: vv for kk, vv in list(v.items())[:30] if isinstance(vv, (int, float, str))})
        except Exception as ex:
            print("MK_TRACE profile dump failed", ex)
    else:
        res = run_bass_kernel_spmd(nc, maps, core_ids=list(range(8)))
    xo = np.zeros((BATCH, SEQ, D), np.float32)
    xco = np.zeros((BATCH, CTX, D), np.float32)
    for r in range(8):
        b, h = r // 2, r % 2
        st = -1 if (flip_odd and h == 1) else 1
        xo[b, h * TOK:(h + 1) * TOK] = res.results[r]["x_out"][::st]
        if h == 0:
            xco[b] = res.results[r]["xc_out"]
    return xo, xco


INPUT_NAMES = (
    "x",
    "c",
    "ctx",
    "c_ctx",
    "l0_ada_w",
    "l0_ada_b",
    "l0_ln1_g",
    "l0_ln1_b",
    "l0_ln2_g",
    "l0_ln2_b",
    "l0_attn_wqkv",
    "l0_attn_q_norm",
    "l0_attn_k_norm",
    "l0_attn_wo",
    "l0_ffn_w_gu",
    "l0_ffn_w_down",
    "l1_ada_w",
    "l1_ada_b",
    "l1_ln1_g",
    "l1_ln1_b",
    "l1_ln2_g",
    "l1_ln2_b",
    "l1_cmlp_w_in",
    "l1_cmlp_b_in",
    "l1_cmlp_v_norm_g",
    "l1_cmlp_v_norm_b",
    "l1_cmlp_w_s",
    "l1_cmlp_b_s",
    "l1_cmlp_w_out",
    "l1_cmlp_b_out",
    "l1_moe_router",
    "l1_moe_w_gu",
    "l1_moe_w_down",
    "l2_ada_w",
    "l2_ada_b",
    "l2_ln1_g",
    "l2_ln1_b",
    "l2_ln2_g",
    "l2_ln2_b",
    "l2_ret_wqkvg",
    "l2_ret_decay",
    "l2_ret_wo",
    "l2_ffn_w_gu",
    "l2_ffn_w_down",
    "l3_ada_w",
    "l3_ada_b",
    "l3_ln1_g",
    "l3_ln1_b",
    "l3_ln2_g",
    "l3_ln2_b",
    "l3_attn_wqkv",
    "l3_attn_q_norm",
    "l3_attn_k_norm",
    "l3_attn_wo",
    "l3_moe_router",
    "l3_moe_w_gu",
    "l3_moe_w_down",
)


def kernel(**inputs):
    missing = [n for n in INPUT_NAMES if n not in inputs]
    assert not missing, missing
    x, xc = inputs["x"], inputs["ctx"]
    for i in range(DEPTH):
        x, xc = run_stages(inputs, [("mix", i), ("ffn", i)], x0=x, xc0=xc)
    return x
```

```python
import os
import numpy as np
import concourse.bass as bass
import concourse.mybir as mybir
from concourse.bass_utils import run_bass_kernel_spmd

F32 = mybir.dt.float32
BF16 = mybir.dt.bfloat16
I32 = mybir.dt.int32
AF = mybir.ActivationFunctionType
ALU = mybir.AluOpType
AX = mybir.AxisListType

ENGS = ("pe", "act", "dve", "pool", "sp")
N_DMA_SEMS = 12


class _Ins:
    __slots__ = ("eng", "fn", "deps", "dma", "idx", "signal", "sig_count", "dma_slot", "dma_round", "waits")

    def __init__(self, eng, fn, dma):
        self.eng = eng
        self.fn = fn
        self.dma = dma
        self.deps = set()
        self.signal = False
        self.sig_count = 0
        self.waits = None


class Prog:
    def __init__(self, nc, sync_same_engine=True):
        self.nc = nc
        self.lists = {e: [] for e in ENGS}
        self.state = {}
        self.sync_same_engine = sync_same_engine
        self.dma_count = {"sp": 0, "pool": 0, "act": 0}

    def op(self, eng, fn, reads=(), writes=(), dma=False):
        ins = _Ins(eng, fn, dma)
        ins.idx = len(self.lists[eng])
        for k in reads:
            st = self.state.get(k)
            if st is not None and st[0] is not None:
                ins.deps.add(st[0])
        for k in writes:
            st = self.state.get(k)
            if st is not None:
                if st[0] is not None:
                    ins.deps.add(st[0])
                for r in st[1]:
                    ins.deps.add(r)
        ins.deps.discard(ins)
        for k in reads:
            st = self.state.setdefault(k, [None, []])
            st[1].append(ins)
        for k in writes:
            self.state[k] = [ins, []]
        if dma:
            n = self.dma_count[eng]
            self.dma_count[eng] = n + 1
            ins.dma_slot = n % N_DMA_SEMS
            ins.dma_round = n // N_DMA_SEMS + 1
        self.lists[eng].append(ins)
        return ins

    def finalize(self, final_waits=()):
        nc = self.nc
        for e in ENGS:
            for ins in self.lists[e]:
                for d in ins.deps:
                    if d.dma:
                        continue
                    if d.eng == ins.eng and not ins.dma:
                        if d.eng == "pe" or not self.sync_same_engine:
                            continue
                    d.signal = True
        for ins in final_waits:
            if not ins.dma:
                ins.signal = True
        for e in ENGS:
            c = 0
            for ins in self.lists[e]:
                if ins.signal and not ins.dma:
                    c += 1
                    ins.sig_count = c
        self.sig_totals = {e: sum(1 for i in self.lists[e] if i.signal and not i.dma) for e in ENGS}
        import contextlib
        with contextlib.ExitStack() as es:
            sems = {e: es.enter_context(nc.semaphore("s_" + e)) for e in ENGS}
            dsems = {q: [es.enter_context(nc.semaphore("d_%s_%d" % (q, i))) for i in range(N_DMA_SEMS)]
                     for q in ("sp", "pool", "act")}
            block = es.enter_context(nc.Block())
            engobj = {"pe": "tensor", "act": "scalar", "dve": "vector", "pool": "gpsimd", "sp": "sync"}

            def make_body(e):
                lst = self.lists[e]

                def body(engine):
                    waited = {}

                    def wait(sem, val, key):
                        if waited.get(key, 0) >= val:
                            return
                        waited[key] = val
                        engine.wait_ge(sem, val)

                    for ins in lst:
                        for d in ins.deps:
                            if d.dma:
                                wait(dsems[d.eng][d.dma_slot], 16 * d.dma_round, ("d", d.eng, d.dma_slot))
                            else:
                                if d.eng == e and not ins.dma and (e == "pe" or not self.sync_same_engine):
                                    continue
                                wait(sems[d.eng], d.sig_count, ("c", d.eng))
                        if ins.dma:
                            if ins.dma_round > 1:
                                wait(dsems[e][ins.dma_slot], 16 * (ins.dma_round - 1), ("d", e, ins.dma_slot))
                        r = ins.fn(engine)
                        if ins.dma:
                            r.then_inc(dsems[e][ins.dma_slot], 16)
                        elif ins.signal:
                            r.then_inc(sems[e], 1)
                    if e == "sp":
                        for ins in final_waits:
                            if ins.dma:
                                wait(dsems[ins.eng][ins.dma_slot], 16 * ins.dma_round, ("d", ins.eng, ins.dma_slot))
                            else:
                                wait(sems[ins.eng], ins.sig_count, ("c", ins.eng))
                return body

            for e in ENGS:
                if not self.lists[e] and e != "sp":
                    continue
                getattr(block, engobj[e])(make_body(e))
        return self


D = 1024
DEPTH = 4
SEQ = 4096
BATCH = 4
CTX = 256
TOK = 2048
NT = 18
NTOK = NT * 128
FFN = 3584
NEXP = 8
NEXP_RUN = int(os.environ.get("MK_NEXP_RUN", "8"))
ALPHA = (2 * DEPTH) ** 0.25
LN_EPS = 1e-5
RMS_EPS = 1e-6
GROUPS = [(0, 2), (2, 4), (6, 4), (10, 4), (14, 4)]
KC = D // 128


class Arena:
    def __init__(self, ap, ncols):
        self.ap = ap
        self.ncols = ncols
        self.top = 0
        self.marks = []

    def alloc(self, free_shape, dtype=F32, parts=128):
        n = 1
        for s in free_shape:
            n *= s
        words = n if dtype in (F32, I32) else (n + 1) // 2
        words = (words + 7) // 8 * 8
        assert self.top + words <= self.ncols, ("arena overflow", self.top, words, self.ncols)
        v = self.ap[0:parts, self.top:self.top + words]
        self.top += words
        if dtype not in (F32,):
            v = v.bitcast(dtype)
        v = v[:, 0:n]
        if len(free_shape) > 1:
            names = "abcdefg"[:len(free_shape)]
            pat = "p (%s) -> p %s" % (" ".join(names), " ".join(names))
            v = v.rearrange(pat, **{names[q]: free_shape[q] for q in range(1, len(free_shape))})
        return v

    def push(self):
        self.marks.append(self.top)

    def pop(self):
        self.top = self.marks.pop()


class Ctx:
    pass


def _barrier(P):
    last = []
    for e in ENGS:
        lst = P.lists[e]
        if not lst:
            continue
        for ins in reversed(lst):
            if not ins.dma:
                last.append(ins)
                break
        seen = set()
        for ins in reversed(lst):
            if ins.dma and ins.dma_slot not in seen:
                seen.add(ins.dma_slot)
                last.append(ins)
            if len(seen) == N_DMA_SEMS:
                break
    P.barrier_set = last
    P.state = {}
    P.after_barrier = {e: True for e in ENGS}


_orig_op = Prog.op


def _op_with_barrier(self, eng, fn, reads=(), writes=(), dma=False):
    ins = _orig_op(self, eng, fn, reads, writes, dma)
    if getattr(self, "after_barrier", None) and self.after_barrier.get(eng):
        for b in self.barrier_set:
            if b is not ins:
                ins.deps.add(b)
        self.after_barrier[eng] = False
    return ins


Prog.op = _op_with_barrier
Prog.barrier = _barrier


ARENA_COLS = 53200


def _mm(P, out, lhsT, rhs, start, stop, reads, writes):
    return P.op("pe", lambda e: e.matmul(out, lhsT=lhsT, rhs=rhs, start=start, stop=stop), reads, writes)


def layer_param_names(i):
    pre = "l%d_" % i
    names = ["ada_w", "ada_b", "ln1_g", "ln1_b", "ln2_g", "ln2_b"]
    kind = i % 3
    if kind == 0:
        names += ["attn_wqkv", "attn_q_norm", "attn_k_norm", "attn_wo"]
    elif kind == 1:
        names += ["cmlp_w_in", "cmlp_b_in", "cmlp_v_norm_g", "cmlp_v_norm_b", "cmlp_w_s", "cmlp_b_s",
                  "cmlp_w_out", "cmlp_b_out"]
    else:
        names += ["ret_wqkvg", "ret_decay", "ret_wo"]
    if i % 2 == 0:
        names += ["ffn_w_gu", "ffn_w_down"]
    else:
        names += ["moe_router", "moe_w_gu", "moe_w_down"]
    return [pre + n for n in names]


PARAM_SHAPES = {}


def _param_shape(name):
    n = name[3:]
    shp = {
        "ada_w": [D, 6 * D], "ada_b": [6 * D], "ln1_g": [D], "ln1_b": [D], "ln2_g": [D], "ln2_b": [D],
        "attn_wqkv": [D, 1536], "attn_q_norm": [128], "attn_k_norm": [128], "attn_wo": [D, D],
        "cmlp_w_in": [D, 4096], "cmlp_b_in": [4096], "cmlp_v_norm_g": [2048], "cmlp_v_norm_b": [2048],
        "cmlp_w_s": [8, 128, 128], "cmlp_b_s": [8, 128], "cmlp_w_out": [2048, D], "cmlp_b_out": [D],
        "ret_wqkvg": [D, 6144], "ret_decay": [2, 4], "ret_wo": [2048, D],
        "ffn_w_gu": [D, 2 * FFN], "ffn_w_down": [FFN, D],
        "moe_router": [D, NEXP], "moe_w_gu": [NEXP_RUN, D, 2 * FFN], "moe_w_down": [NEXP_RUN, FFN, D],
    }[n]
    return shp


def build_program(stages, layers_needed):
    nc = bass.Bass("TRN2", target_bir_lowering=False)
    C = Ctx()
    C.nc = nc
    dram = {}

    def din(name, shape, dtype=F32):
        dram[name] = nc.dram_tensor(name, list(shape), dtype, kind="ExternalInput").ap()
        return dram[name]

    din("x_in", [TOK, D])
    din("ctx_in", [CTX, D])
    din("cc", [128, KC, 2])
    din("ident", [128, 128])
    if any(k == "mix" and i % 3 == 0 for k, i in stages):
        din("rope_a", [128, 2, TOK])
        din("rope_o", [128, 2, TOK])
    if any(k == "mix" and i % 3 != 1 for k, i in stages):
        din("x_oth", [TOK, D])
    if any(k == "mix" and i % 3 == 2 for k, i in stages):
        din("rope_r", [128, 2, 2, TOK])
        din("rope_ro", [128, 2, 2, TOK])
        din("ret_dec", [2, 4])
        C.xacc = nc.dram_tensor("xacc", [NTOK, D], F32, kind="Internal").ap()
    for i in layers_needed:
        for n in layer_param_names(i):
            din(n, _param_shape(n))
    x_out = nc.dram_tensor("x_out", [TOK, D], F32, kind="ExternalOutput").ap()
    xc_out = nc.dram_tensor("xc_out", [CTX, D], F32, kind="ExternalOutput").ap()
    C.ada_scr = nc.dram_tensor("ada_scr", [DEPTH, 2, 6 * D], F32, kind="Internal").ap()
    C.dram = dram

    import contextlib
    with contextlib.ExitStack() as es:
        arena_t = es.enter_context(nc.sbuf_tensor("arena", [128, ARENA_COLS], F32))
        A = Arena(arena_t[:], ARENA_COLS)
        C.A = A
        C.psum = [es.enter_context(nc.psum_tensor("ps%d" % i, [128, 512], F32)) for i in range(8)]
        P = Prog(nc)
        C.P = P
        C.Xraw = A.alloc((NT * D,))
        C.X = C.Xraw.rearrange("p (t d) -> p t d", d=D)
        C.hT = A.alloc((KC, NTOK), BF16)
        C.ident = A.alloc((128,))
        C.adaT = A.alloc((DEPTH, 48, 2))
        C.sc1p = A.alloc((DEPTH, 2, KC, 2))
        C.ones_bf2 = A.alloc((128,), BF16)

        P.op("sp", lambda e: e.dma_start(out=C.ident, in_=dram["ident"]), writes=["ident"], dma=True)
        P.op("pool", lambda e: e.memset(C.ones_bf2, 1.0), writes=["ones_bf"])
        for t in range(NT):
            src = dram["ctx_in"][t * 128:(t + 1) * 128, :] if t < 2 else dram["x_in"][(t - 2) * 128:(t - 1) * 128, :]
            P.op("sp", (lambda e, t=t, src=src: e.dma_start(out=C.X[:, t, :], in_=src)), writes=[("X", t)], dma=True)

        emit_adaln(C, layers_needed)
        for kind, i in stages:
            if kind == "ffn":
                emit_ffn(C, i)
            else:
                emit_mixer(C, i)
        P.barrier()
        outs = []
        for t in range(NT):
            dst = xc_out[t * 128:(t + 1) * 128, :] if t < 2 else x_out[(t - 2) * 128:(t - 1) * 128, :]
            outs.append(P.op("sp", (lambda e, t=t, dst=dst: e.dma_start(out=dst, in_=C.X[:, t, :])),
                             reads=[("X", t)], dma=True))
        P.finalize(final_waits=outs)
    nc.mk_inputs = set(dram.keys())
    return nc


def emit_adaln(C, layers):
    P, A, nc = C.P, C.A, C.nc
    P.barrier()
    A.push()
    cc = A.alloc((KC, 2))
    scc = A.alloc((KC, 2))
    wch = [A.alloc((KC, 512)) for _ in range(2)]
    bch = [A.alloc((512,), parts=2) for _ in range(2)]
    rowc = [A.alloc((512,), parts=2) for _ in range(2)]
    psT = C.psum[2]
    P.op("sp", lambda e: e.dma_start(out=cc, in_=C.dram["cc"]), writes=["cc"], dma=True)
    P.op("act", lambda e: e.activation(out=scc, in_=cc, func=AF.Silu), reads=["cc"], writes=["scc"])
    n = 0
    for i in layers:
        w = C.dram["l%d_ada_w" % i].rearrange("(k p) n -> p k n", p=128)
        b = C.dram["l%d_ada_b" % i]
        for cch in range(12):
            s = n % 2
            n += 1
            cs = slice(cch * 512, (cch + 1) * 512)
            P.op("sp", (lambda e, s=s, cs=cs, w=w: e.dma_start(out=wch[s], in_=w[:, :, cs])), writes=[("wch", s)], dma=True)
            P.op("sp", (lambda e, s=s, cs=cs, b=b: e.dma_start(out=bch[s], in_=b[cs].partition_broadcast(2))),
                 writes=[("bch", s)], dma=True)
            ps = C.psum[s]
            for k in range(KC):
                _mm(P, ps[0:2, :], scc[:, k, :], wch[s][:, k, :], k == 0, k == KC - 1,
                    reads=["scc", ("wch", s)], writes=[("bank", s)])
            P.op("dve", (lambda e, s=s, ps=ps: e.tensor_tensor(out=rowc[s], in0=ps[0:2, :], in1=bch[s], op=ALU.add)),
                 reads=[("bank", s), ("bch", s)], writes=[("rowc", s)])
            P.op("sp", (lambda e, s=s, cs=cs, i=i: e.dma_start(out=C.ada_scr[i, :, cs], in_=rowc[s])),
                 reads=[("rowc", s)], writes=[("ada_scr", i)], dma=True)
            for q in range(4):
                ch = cch * 4 + q
                _mm(P, psT[:, ch * 2:(ch + 1) * 2], rowc[s][:, q * 128:(q + 1) * 128], C.ident[0:2, 0:2], True, True,
                    reads=[("rowc", s), "ident"], writes=[("bank", 2)])
        P.op("dve", (lambda e, i=i: e.tensor_copy(out=C.adaT[:, i, :, :], in_=psT[:, 0:96].rearrange("p (c j) -> p c j", j=2))),
             reads=[("bank", 2)], writes=[("adaT", i)])
        for sub, c0 in ((0, 8), (1, 32)):
            P.op("dve", (lambda e, i=i, sub=sub, c0=c0: e.tensor_scalar(
                out=C.sc1p[:, i, sub, :, :], in0=C.adaT[:, i, c0:c0 + 8, :], scalar1=1.0, scalar2=None, op0=ALU.add)),
                reads=[("adaT", i)], writes=[("sc1p", i, sub)])
    A.pop()
    P.barrier()


def emit_build_hT(C, i, sub, need_ctx=True, router=None):
    P, A = C.P, C.A
    shift_c0 = 0 if sub == 0 else 24
    tiles = range(NT) if need_ctx else range(2, NT)
    for t in tiles:
        j = 1 if t < 2 else 0
        s = t % 2
        ps = (C.psum[4 + 2 * s], C.psum[5 + 2 * s])
        for k in range(KC):
            pst = ps[k // 4][:, (k % 4) * 128:(k % 4 + 1) * 128]
            P.op("pe", (lambda e, t=t, k=k, pst=pst: e.transpose(pst, C.X[:, t, k * 128:(k + 1) * 128], C.ident)),
                 reads=[("X", t), "ident"], writes=[("bank", 4 + 2 * s + k // 4)])
        for k in range(KC):
            pst = ps[k // 4][:, (k % 4) * 128:(k % 4 + 1) * 128]
            P.op("dve", (lambda e, t=t, k=k, pst=pst, j=j: e.tensor_scalar(
                out=C.hT[:, k, t * 128:(t + 1) * 128], in0=pst,
                scalar1=C.sc1p[:, i, sub, k, j:j + 1], scalar2=C.adaT[:, i, shift_c0 + k, j:j + 1],
                op0=ALU.mult, op1=ALU.add)),
                reads=[("bank", 4 + 2 * s + k // 4)], writes=[("hT", t, k)])
            if router is not None:
                h32 = router["h32"][s]
                P.op("dve", (lambda e, t=t, k=k, pst=pst, j=j, h32=h32: e.tensor_scalar(
                    out=h32[:, k, :], in0=pst,
                    scalar1=C.sc1p[:, i, sub, k, j:j + 1], scalar2=C.adaT[:, i, shift_c0 + k, j:j + 1],
                    op0=ALU.mult, op1=ALU.add)),
                    reads=[("bank", 4 + 2 * s + k // 4)], writes=[("h32", s, k)])
        if router is not None and not os.environ.get("MK_DBG_NORMM"):
            psl = C.psum[s]
            for k in range(KC):
                _mm(P, psl[:, 0:NEXP], router["h32"][s][:, k, :], router["w"][:, k, :], k == 0, k == KC - 1,
                    reads=[("h32", s, k), "router_w"], writes=[("bank", s)])
            P.op("act", (lambda e, t=t, psl=psl: e.copy(out=router["logits"][:, t, :], in_=psl[:, 0:NEXP])),
                 reads=[("bank", s)], writes=[("logits", t)])


def emit_scale_x(C, need_ctx=True):
    P = C.P
    for t in (range(NT) if need_ctx else range(2, NT)):
        eng = "act" if t % 2 == 0 else "dve"
        if eng == "act":
            P.op("act", (lambda e, t=t: e.activation(out=C.X[:, t, :], in_=C.X[:, t, :], func=AF.Copy, scale=float(ALPHA))),
                 reads=[("X", t)], writes=[("X", t)])
        else:
            P.op("dve", (lambda e, t=t: e.tensor_scalar(out=C.X[:, t, :], in0=C.X[:, t, :], scalar1=float(ALPHA),
                                                        scalar2=None, op0=ALU.mult)),
                 reads=[("X", t)], writes=[("X", t)])


def emit_load_bc(C, i, sub, tiles):
    P = C.P
    gate_c0 = 2 * D if sub == 0 else 5 * D
    srcs = {
        "gx": C.ada_scr[i, 0, gate_c0:gate_c0 + D], "gc": C.ada_scr[i, 1, gate_c0:gate_c0 + D],
        "lg": C.dram["l%d_ln%d_g" % (i, sub + 1)], "lb": C.dram["l%d_ln%d_b" % (i, sub + 1)],
    }
    for n, src in srcs.items():
        P.op("sp", (lambda e, n=n, src=src: e.dma_start(out=tiles[n], in_=src.partition_broadcast(128))),
             reads=[("ada_scr", i)], writes=[("bc", n)], dma=True)


def emit_ln(C, tiles, need_ctx=True):
    P, A = C.P, C.A
    A.push()
    stats = A.alloc((NT, 2, 6))
    mv = A.alloc((NT, 2))
    rstd = A.alloc((NT,))
    nmr = A.alloc((NT,))
    t0 = 0 if need_ctx else 2
    for t in range(t0, NT):
        for hh in range(2):
            P.op("dve", (lambda e, t=t, hh=hh: e.bn_stats(out=stats[:, t, hh, :], in_=C.X[:, t, hh * 512:(hh + 1) * 512])),
                 reads=[("X", t)], writes=[("stats", t, hh)])
        P.op("dve", (lambda e, t=t: e.bn_aggr(out=mv[:, t, :], in_=stats[:, t, :, :])),
             reads=[("stats", t, 0), ("stats", t, 1)], writes=[("mv", t)])
    mvk = [("mv", t) for t in range(t0, NT)]
    P.op("dve", lambda e: e.tensor_scalar(out=rstd[:, t0:NT], in0=mv[:, t0:NT, 1], scalar1=float(LN_EPS), scalar2=None, op0=ALU.add),
         reads=mvk, writes=["rstd"])
    P.op("act", lambda e: e.activation(out=rstd[:, t0:NT], in_=rstd[:, t0:NT], func=AF.Sqrt), reads=["rstd"], writes=["rstd"])
    P.op("dve", lambda e: e.reciprocal(out=rstd[:, t0:NT], in_=rstd[:, t0:NT]), reads=["rstd"], writes=["rstd"])
    P.op("dve", lambda e: e.scalar_tensor_tensor(out=nmr[:, t0:NT], in0=mv[:, t0:NT, 0], scalar=-1.0, in1=rstd[:, t0:NT],
                                                 op0=ALU.mult, op1=ALU.mult), reads=mvk + ["rstd"], writes=["nmr"])
    for t in range(t0, NT):
        P.op("act", (lambda e, t=t: e.activation(out=C.X[:, t, :], in_=C.X[:, t, :], func=AF.Identity,
                                                 bias=nmr[:, t:t + 1], scale=rstd[:, t:t + 1])),
             reads=[("X", t), "rstd", "nmr"], writes=[("X", t)])
        P.op("dve", (lambda e, t=t: e.tensor_tensor(out=C.X[:, t, :], in0=C.X[:, t, :], in1=tiles["lg"], op=ALU.mult)),
             reads=[("X", t), ("bc", "lg")], writes=[("X", t)])
        P.op("pool" if t % 2 == 0 else "dve", (lambda e, t=t: e.tensor_tensor(out=C.X[:, t, :], in0=C.X[:, t, :], in1=tiles["lb"], op=ALU.add)),
             reads=[("X", t), ("bc", "lb")], writes=[("X", t)])
    A.pop()


def hT_keys(t0, n):
    return [("hT", t, k) for t in range(t0, t0 + n) for k in range(KC)]


def emit_ffn(C, i):
    P, A, nc = C.P, C.A, C.nc
    moe = (i % 2 == 1)
    need_ctx = i < DEPTH - 1
    P.barrier()
    A.push()
    bc = {n: A.alloc((D,)) for n in ("gx", "gc", "lg", "lb")}
    emit_load_bc(C, i, 1, bc)
    router = None
    if moe and not os.environ.get("MK_DBG_NOROUTER"):
        router = {
            "h32": [A.alloc((KC, 128)) for _ in range(2)],
            "w": A.alloc((KC, NEXP)),
            "logits": A.alloc((NT, NEXP)),
        }
        rw = C.dram["l%d_moe_router" % i].rearrange("(k p) e -> p k e", p=128)
        P.op("sp", lambda e: e.dma_start(out=router["w"], in_=rw), writes=["router_w"], dma=True)
    emit_build_hT(C, i, 1, need_ctx=need_ctx, router=router)
    emit_scale_x(C, need_ctx=need_ctx)
    gates = None
    if moe and not os.environ.get("MK_DBG_NOGATES"):
        gates = emit_gates(C, router, need_ctx)
    if moe:
        wgu = C.dram["l%d_moe_w_gu" % i]
        wdn = C.dram["l%d_moe_w_down" % i]
        blocks = [(e, j) for e in range(NEXP_RUN) for j in range(FFN // 512)]
    else:
        wgu = C.dram["l%d_ffn_w_gu" % i]
        wdn = C.dram["l%d_ffn_w_down" % i]
        blocks = [(None, j) for j in range(FFN // 512)]
    if os.environ.get("MK_DBG_NBLK"):
        blocks = blocks[:int(os.environ["MK_DBG_NBLK"])]
    Wg = [A.alloc((KC, 512), BF16) for _ in range(2)]
    Wu = [A.alloc((KC, 512), BF16) for _ in range(2)]
    Wd = [A.alloc((4, D), BF16) for _ in range(2)]
    act = [A.alloc((4, 512), BF16) for _ in range(2)]
    sg = [A.alloc((512,)) for _ in range(2)]
    tmp = [A.alloc((D,)) for _ in range(2)]
    groups = GROUPS if need_ctx else GROUPS[1:]

    def load_block(bi):
        e_, j = blocks[bi]
        s = bi % 2
        gu = (wgu[e_] if moe else wgu).rearrange("(k p) n -> p k n", p=128)
        dn = (wdn[e_] if moe else wdn)[j * 512:(j + 1) * 512, :].rearrange("(f p) n -> p f n", p=128)
        P.op("pool", (lambda e: e.dma_start(out=Wg[s], in_=gu[:, :, j * 512:(j + 1) * 512])), writes=[("Wg", s)], dma=True)
        if os.environ.get("MK_DBG_SKIPW") and bi > 1:
            return
        P.op("pool", (lambda e: e.dma_start(out=Wu[s], in_=gu[:, :, FFN + j * 512:FFN + (j + 1) * 512])),
             writes=[("Wu", s)], dma=True)
        P.op("pool", (lambda e: e.dma_start(out=Wd[s], in_=dn)), writes=[("Wd", s)], dma=True)

    acc_eng = os.environ.get("MK_ACC_ENG", "pool")
    items = [(bi, gi) for bi in range(len(blocks)) for gi in range(len(groups))]

    def emit_gu(n):
        bi, gi = items[n]
        s = bi % 2
        if gi == 0 and bi + 1 < len(blocks):
            load_block(bi + 1)
        (t0, ntile) = groups[gi]
        ntok = ntile * 128
        a = n % 2
        for fb in range(4):
            pg = C.psum[(fb % 2) * 2]
            pu = C.psum[(fb % 2) * 2 + 1]
            for k in range(KC):
                _mm(P, pg[:, 0:ntok], Wg[s][:, k, fb * 128:(fb + 1) * 128], C.hT[:, k, t0 * 128:t0 * 128 + ntok],
                    k == 0, k == KC - 1, reads=([("Wg", s)] + hT_keys(t0, ntile)) if k in (0, KC - 1) else [], writes=[("bank", (fb % 2) * 2)])
            for k in range(KC):
                _mm(P, pu[:, 0:ntok], Wu[s][:, k, fb * 128:(fb + 1) * 128], C.hT[:, k, t0 * 128:t0 * 128 + ntok],
                    k == 0, k == KC - 1, reads=([("Wu", s)] + hT_keys(t0, ntile)) if k in (0, KC - 1) else [], writes=[("bank", (fb % 2) * 2 + 1)])
            sgt = sg[fb % 2]
            P.op("act", (lambda e, pg=pg, sgt=sgt: e.activation(out=sgt[:, 0:ntok], in_=pg[:, 0:ntok], func=AF.Silu)),
                 reads=[("bank", (fb % 2) * 2)], writes=[("sg", fb % 2)])
            P.op("dve", (lambda e, pu=pu, sgt=sgt, fb=fb: e.tensor_tensor(
                out=act[a][:, fb, 0:ntok], in0=pu[:, 0:ntok], in1=sgt[:, 0:ntok], op=ALU.mult)),
                reads=[("bank", (fb % 2) * 2 + 1), ("sg", fb % 2)], writes=[("act", a, fb)])

    ocnt = [0]

    def emit_down(n):
        bi, gi = items[n]
        s = bi % 2
        e_, j = blocks[bi]
        (t0, ntile) = groups[gi]
        a = n % 2
        for tt in range(ntile):
            t = t0 + tt
            o = ocnt[0] % 2
            ocnt[0] += 1
            po = (C.psum[4 + 2 * o], C.psum[5 + 2 * o])
            for nh in range(2):
                for fb in range(4):
                    _mm(P, po[nh][:, :], act[a][:, fb, tt * 128:(tt + 1) * 128], Wd[s][:, fb, nh * 512:(nh + 1) * 512],
                        fb == 0, fb == 3, reads=[("act", a, fb), ("Wd", s)], writes=[("bank", 4 + 2 * o + nh)])
            gbc = bc["gc"] if t < 2 else bc["gx"]
            tm = tmp[o]
            for nh in range(2):
                if moe and gates is not None:
                    P.op("dve", (lambda e, po=po, nh=nh, tm=tm, gbc=gbc, t=t, e_=e_: e.scalar_tensor_tensor(
                        out=tm[:, nh * 512:(nh + 1) * 512], in0=po[nh][:, :], scalar=gates[:, t, e_:e_ + 1],
                        in1=gbc[:, nh * 512:(nh + 1) * 512], op0=ALU.mult, op1=ALU.mult)),
                        reads=[("bank", 4 + 2 * o + nh), ("bc", "gx"), ("bc", "gc"), "gates"], writes=[("tmp", o, nh)])
                else:
                    P.op("dve", (lambda e, po=po, nh=nh, tm=tm, gbc=gbc: e.tensor_tensor(
                        out=tm[:, nh * 512:(nh + 1) * 512], in0=po[nh][:, :], in1=gbc[:, nh * 512:(nh + 1) * 512], op=ALU.mult)),
                        reads=[("bank", 4 + 2 * o + nh), ("bc", "gx"), ("bc", "gc")], writes=[("tmp", o, nh)])
            eng = acc_eng if acc_eng != "alt" else ("dve" if ocnt[0] % 2 else "pool")
            P.op(eng, (lambda e, t=t, tm=tm: e.tensor_tensor(out=C.X[:, t, :], in0=C.X[:, t, :], in1=tm, op=ALU.add)),
                 reads=[("X", t), ("tmp", o, 0), ("tmp", o, 1)], writes=[("X", t)])

    if blocks:
        load_block(0)
    pipelined = bool(os.environ.get("MK_PIPE"))
    if pipelined and items:
        emit_gu(0)
        for n in range(len(items)):
            if n + 1 < len(items):
                emit_gu(n + 1)
            emit_down(n)
    else:
        for n in range(len(items)):
            emit_gu(n)
            emit_down(n)
    emit_ln(C, bc, need_ctx=need_ctx)
    A.pop()
    P.barrier()


def emit_gates(C, router, need_ctx):
    P, A = C.P, C.A
    L = router["logits"]
    t0 = 0 if need_ctx else 2
    n = NT - t0
    Lv = L[:, t0:NT, :]
    gates = A.alloc((NT, NEXP))
    m1 = A.alloc((NT,))
    m2 = A.alloc((NT,))
    mk1 = A.alloc((NT, NEXP))
    mk2 = A.alloc((NT, NEXP))
    l2 = A.alloc((NT, NEXP))
    w1 = A.alloc((NT,))
    w2 = A.alloc((NT,))
    lk = [("logits", t) for t in range(t0, NT)]

    def bcast(v):
        return v[:, t0:NT][:, :, None].broadcast_to([128, n, NEXP])

    P.op("dve", lambda e: e.tensor_reduce(out=m1[:, t0:NT], in_=Lv, axis=AX.X, op=ALU.max), reads=lk, writes=["m1"])
    P.op("dve", lambda e: e.tensor_tensor(out=mk1[:, t0:NT, :], in0=Lv, in1=bcast(m1), op=ALU.is_equal), reads=lk + ["m1"], writes=["mk1"])
    P.op("dve", lambda e: e.scalar_tensor_tensor(out=l2[:, t0:NT, :], in0=mk1[:, t0:NT, :], scalar=-1e30, in1=Lv,
                                                 op0=ALU.mult, op1=ALU.add), reads=lk + ["mk1"], writes=["l2"])
    P.op("dve", lambda e: e.tensor_reduce(out=m2[:, t0:NT], in_=l2[:, t0:NT, :], axis=AX.X, op=ALU.max), reads=["l2"], writes=["m2"])
    P.op("dve", lambda e: e.tensor_tensor(out=mk2[:, t0:NT, :], in0=l2[:, t0:NT, :], in1=bcast(m2), op=ALU.is_equal),
         reads=["l2", "m2"], writes=["mk2"])
    P.op("dve", lambda e: e.tensor_tensor(out=w2[:, t0:NT], in0=m2[:, t0:NT], in1=m1[:, t0:NT], op=ALU.subtract),
         reads=["m1", "m2"], writes=["w2"])
    P.op("act", lambda e: e.activation(out=w2[:, t0:NT], in_=w2[:, t0:NT], func=AF.Exp), reads=["w2"], writes=["w2"])
    P.op("dve", lambda e: e.tensor_scalar(out=w1[:, t0:NT], in0=w2[:, t0:NT], scalar1=1.0, scalar2=None, op0=ALU.add),
         reads=["w2"], writes=["w1"])
    P.op("dve", lambda e: e.reciprocal(out=w1[:, t0:NT], in_=w1[:, t0:NT]), reads=["w1"], writes=["w1"])
    P.op("dve", lambda e: e.tensor_tensor(out=w2[:, t0:NT], in0=w2[:, t0:NT], in1=w1[:, t0:NT], op=ALU.mult),
         reads=["w1", "w2"], writes=["w2"])
    P.op("dve", lambda e: e.tensor_tensor(out=mk1[:, t0:NT, :], in0=mk1[:, t0:NT, :], in1=bcast(w1), op=ALU.mult),
         reads=["mk1", "w1"], writes=["mk1"])
    P.op("dve", lambda e: e.tensor_tensor(out=mk2[:, t0:NT, :], in0=mk2[:, t0:NT, :], in1=bcast(w2), op=ALU.mult),
         reads=["mk2", "w2"], writes=["mk2"])
    P.op("dve", lambda e: e.tensor_tensor(out=gates[:, t0:NT, :], in0=mk1[:, t0:NT, :], in1=mk2[:, t0:NT, :], op=ALU.add),
         reads=["mk1", "mk2"], writes=["gates"])
    return gates


def emit_mixer(C, i):
    kind = i % 3
    if kind == 0:
        emit_attention(C, i)
    elif kind == 1:
        emit_cmlp(C, i)
    else:
        emit_retention(C, i)


PAIR_GROUPS = [[0, 1], [2, 3], [4, 5], [6, 7]]


def emit_rot_copy(P, dst, src, half, rkey, wkey):
    n = src.shape[-1]
    for b0 in range(0, n, 2 * half):
        P.op("pool", (lambda e, b0=b0: e.tensor_copy(out=dst[:, :, b0:b0 + half], in_=src[:, :, b0 + half:b0 + 2 * half])),
             reads=[rkey], writes=[wkey])
        P.op("pool", (lambda e, b0=b0: e.tensor_copy(out=dst[:, :, b0 + half:b0 + 2 * half], in_=src[:, :, b0:b0 + half])),
             reads=[rkey], writes=[wkey])


def emit_load_gain(C, dst, dst_rot, src, half, scale, key):
    P = C.P
    col = src.rearrange("(p o) -> p o", o=1)
    P.op("sp", lambda e: e.dma_start(out=dst, in_=col), writes=[key], dma=True)
    for b0 in range(0, 128, 2 * half):
        P.op("sp", (lambda e, b0=b0: e.dma_start(out=dst_rot[b0:b0 + half, :], in_=col[b0 + half:b0 + 2 * half, :])),
             writes=[key + "_r"], dma=True)
        P.op("sp", (lambda e, b0=b0: e.dma_start(out=dst_rot[b0 + half:b0 + 2 * half, :], in_=col[b0:b0 + half, :])),
             writes=[key + "_r"], dma=True)
    if scale != 1.0:
        P.op("dve", lambda e: e.tensor_scalar(out=dst, in0=dst, scalar1=float(scale), scalar2=None, op0=ALU.mult),
             reads=[key], writes=[key])
        P.op("dve", lambda e: e.tensor_scalar(out=dst_rot, in0=dst_rot, scalar1=float(scale), scalar2=None, op0=ALU.mult),
             reads=[key + "_r"], writes=[key + "_r"])


def emit_qk_norm_rope(C, ps_q, ps_qr, ps_ss, ntok, out_bf, gain, gain_r, cos, sin, tmp, eps_t, inv_d, tag, rope, out_key):
    P = C.P
    sq, rstd, ta, tb = tmp
    kq, kqr, kss = ("bank", ps_q[1]), ("bank", ps_qr[1]), ("bank", ps_ss[1])
    pq, pqr, pss = ps_q[0], ps_qr[0], ps_ss[0]
    P.op("act", lambda e: e.activation(out=sq[:, 0:ntok], in_=pq[:, 0:ntok], func=AF.Square), reads=[kq], writes=[tag + "sq"])
    _mm(P, pss[:, 0:ntok], C.ones_bf2, sq[:, 0:ntok], True, True, reads=[tag + "sq", "ones_bf"], writes=[kss])
    P.op("dve", lambda e: e.tensor_scalar(out=rstd[:, 0:ntok], in0=pss[:, 0:ntok], scalar1=float(inv_d), scalar2=float(RMS_EPS),
                                          op0=ALU.mult, op1=ALU.add), reads=[kss], writes=[tag + "rstd"])
    P.op("act", lambda e: e.activation(out=rstd[:, 0:ntok], in_=rstd[:, 0:ntok], func=AF.Ln),
         reads=[tag + "rstd"], writes=[tag + "rstd"])
    P.op("act", lambda e: e.activation(out=rstd[:, 0:ntok], in_=rstd[:, 0:ntok], func=AF.Exp, scale=-0.5),
         reads=[tag + "rstd"], writes=[tag + "rstd"])
    if not rope:
        P.op("dve", lambda e: e.scalar_tensor_tensor(out=out_bf, in0=pq[:, 0:ntok], scalar=gain, in1=rstd[:, 0:ntok],
                                                     op0=ALU.mult, op1=ALU.mult),
             reads=[kq, tag + "rstd", "gains"], writes=[out_key])
        return
    P.op("dve", lambda e: e.scalar_tensor_tensor(out=ta[:, 0:ntok], in0=pq[:, 0:ntok], scalar=gain, in1=rstd[:, 0:ntok],
                                                 op0=ALU.mult, op1=ALU.mult), reads=[kq, tag + "rstd", "gains"], writes=[tag + "ta"])
    P.op("dve", lambda e: e.scalar_tensor_tensor(out=tb[:, 0:ntok], in0=pqr[:, 0:ntok], scalar=gain_r, in1=rstd[:, 0:ntok],
                                                 op0=ALU.mult, op1=ALU.mult), reads=[kqr, tag + "rstd", "gains"], writes=[tag + "tb"])
    P.op("pool", lambda e: e.tensor_tensor(out=ta[:, 0:ntok], in0=ta[:, 0:ntok], in1=cos, op=ALU.mult),
         reads=[tag + "ta", "rope"], writes=[tag + "ta"])
    P.op("pool", lambda e: e.tensor_tensor(out=tb[:, 0:ntok], in0=tb[:, 0:ntok], in1=sin, op=ALU.mult),
         reads=[tag + "tb", "rope"], writes=[tag + "tb"])
    P.op("dve", lambda e: e.tensor_tensor(out=out_bf, in0=ta[:, 0:ntok], in1=tb[:, 0:ntok], op=ALU.add),
         reads=[tag + "ta", tag + "tb"], writes=[out_key])


def emit_attention(C, i):
    P, A, nc = C.P, C.A, C.nc
    need_ctx = i < DEPTH - 1
    wqkv = C.dram["l%d_attn_wqkv" % i].rearrange("(k p) n -> p k n", p=128)
    wo = C.dram["l%d_attn_wo" % i]
    P.barrier()
    A.push()
    gx = A.alloc((D,))
    gc = A.alloc((D,))
    bc = {"gx": gx, "gc": gc}
    gate_c0 = 2 * D
    P.op("sp", lambda e: e.dma_start(out=gx, in_=C.ada_scr[i, 0, gate_c0:gate_c0 + D].partition_broadcast(128)), writes=[("bc", "gx")], dma=True)
    P.op("sp", lambda e: e.dma_start(out=gc, in_=C.ada_scr[i, 1, gate_c0:gate_c0 + D].partition_broadcast(128)), writes=[("bc", "gc")], dma=True)
    emit_build_hT(C, i, 0, need_ctx=True)
    emit_scale_x(C, need_ctx=need_ctx)

    rope = A.alloc((2, TOK))
    P.op("sp", lambda e: e.dma_start(out=rope, in_=C.dram["rope_a"]), writes=["rope"], dma=True)
    Kall = A.alloc((2, CTX + SEQ), BF16)
    Vall = A.alloc((34, 256), BF16)
    gk, gkr, gq, gqr, eps_t = (A.alloc((1,)) for _ in range(5))
    emit_load_gain(C, gk, gkr, C.dram["l%d_attn_k_norm" % i], 32, 1.0, "gk")
    emit_load_gain(C, gq, gqr, C.dram["l%d_attn_q_norm" % i], 32, 128 ** -0.5, "gq")
    P.op("pool", lambda e: e.memset(eps_t, float(RMS_EPS)), writes=["eps"])
    tmp = (A.alloc((512,), BF16), A.alloc((512,)), A.alloc((512,)), A.alloc((512,)))
    bank = lambda n: (C.psum[n], n)

    A.push()
    Wk = A.alloc((KC, 256), BF16)
    Wkr = A.alloc((KC, 256), BF16)
    Wv = A.alloc((KC, 256), BF16)
    Xo = A.alloc((D,))
    hTo = A.alloc((KC, 512), BF16)
    rope_o = A.alloc((2, 512))
    P.op("pool", lambda e: e.dma_start(out=Wk, in_=wqkv[:, :, 1024:1280]), writes=["Wk"], dma=True)
    P.op("pool", lambda e: e.dma_start(out=Wv, in_=wqkv[:, :, 1280:1536]), writes=["Wv"], dma=True)
    emit_rot_copy(P, Wkr, Wk, 32, "Wk", "Wkr")

    def kproj(src, c0, ntok, hkeys, out, cos, sin, isctx, hk, okey):
        for k in range(KC):
            _mm(P, C.psum[0][:, 0:ntok], Wk[:, k, hk * 128:(hk + 1) * 128], src[:, k, c0:c0 + ntok],
                k == 0, k == KC - 1, reads=["Wk"] + hkeys, writes=[("bank", 0)])
        if not isctx:
            for k in range(KC):
                _mm(P, C.psum[1][:, 0:ntok], Wkr[:, k, hk * 128:(hk + 1) * 128], src[:, k, c0:c0 + ntok],
                    k == 0, k == KC - 1, reads=["Wkr"] + hkeys, writes=[("bank", 1)])
        emit_qk_norm_rope(C, bank(0), bank(1), bank(2), ntok, out, gk, gkr, cos, sin, tmp, eps_t, 1.0 / 128, "k",
                          not isctx, okey)

    def vproj(src, c0, hkeys, dst, okey, pb):
        pv = C.psum[pb]
        for k in range(KC):
            _mm(P, pv[:, 0:256], src[:, k, c0:c0 + 128], Wv[:, k, :], k == 0, k == KC - 1,
                reads=["Wv"] + hkeys, writes=[("bank", pb)])
        P.op("act", (lambda e: e.copy(out=dst, in_=pv[:, 0:256])), reads=[("bank", pb)], writes=[okey])

    for (t0, ntile) in GROUPS:
        ntok = ntile * 128
        isctx = t0 < 2
        for hk in range(2):
            if isctx:
                kproj(C.hT, 0, ntok, hT_keys(t0, ntile), Kall[:, hk, 0:CTX], None, None, True, hk, ("Kall", hk, "c"))
            else:
                c0 = (t0 - 2) * 128
                kproj(C.hT, t0 * 128, ntok, hT_keys(t0, ntile), Kall[:, hk, CTX + c0:CTX + c0 + ntok],
                      rope[:, 0, c0:c0 + ntok], rope[:, 1, c0:c0 + ntok], False, hk, ("Kall", hk, t0))
        for tt in range(ntile):
            t = t0 + tt
            vproj(C.hT, t * 128, hT_keys(t, 1), Vall[:, t, :], ("Vall", t), 4 + t % 2)
    xoth = C.dram["x_oth"]
    for g in range(4):
        P.op("sp", (lambda e, g=g: e.dma_start(out=rope_o, in_=C.dram["rope_o"][:, :, g * 512:(g + 1) * 512])),
             writes=["rope_o"], dma=True)
        for tt in range(4):
            tg = g * 4 + tt
            P.op("sp", (lambda e, tg=tg: e.dma_start(out=Xo, in_=xoth[tg * 128:(tg + 1) * 128, :])), writes=["Xo"], dma=True)
            for k in range(KC):
                pst = C.psum[6 + k // 4][:, (k % 4) * 128:(k % 4 + 1) * 128]
                P.op("pe", (lambda e, k=k, pst=pst: e.transpose(pst, Xo[:, k * 128:(k + 1) * 128], C.ident)),
                     reads=["Xo", "ident"], writes=[("bank", 6 + k // 4)])
            for k in range(KC):
                pst = C.psum[6 + k // 4][:, (k % 4) * 128:(k % 4 + 1) * 128]
                P.op("dve", (lambda e, k=k, pst=pst, tt=tt: e.tensor_scalar(
                    out=hTo[:, k, tt * 128:(tt + 1) * 128], in0=pst,
                    scalar1=C.sc1p[:, i, 0, k, 0:1], scalar2=C.adaT[:, i, k, 0:1], op0=ALU.mult, op1=ALU.add)),
                    reads=[("bank", 6 + k // 4)], writes=[("hTo", tt, k)])
        hk_o = [("hTo", tt, k) for tt in range(4) for k in range(KC)]
        c0 = CTX + TOK + g * 512
        for hk in range(2):
            kproj(hTo, 0, 512, hk_o, Kall[:, hk, c0:c0 + 512], rope_o[:, 0, :], rope_o[:, 1, :], False, hk, ("Kall", hk, "o", g))
        for tt in range(4):
            vproj(hTo, tt * 128, [("hTo", tt, k) for k in range(KC)], Vall[:, 18 + g * 4 + tt, :], ("Vall", 18 + g * 4 + tt), 4 + tt % 2)
    A.pop()
    P.barrier()

    Wq = [A.alloc((KC, 128), BF16) for _ in range(2)]
    Wqr = [A.alloc((KC, 128), BF16) for _ in range(2)]
    Wo = [A.alloc((D,), BF16) for _ in range(2)]
    qT = [A.alloc((512,), BF16) for _ in range(2)]
    PT = [A.alloc((512,), BF16) for _ in range(3)]
    oT = [A.alloc((512,), BF16) for _ in range(2)]
    rden = A.alloc((512,))
    ytmp = [A.alloc((D,)) for _ in range(2)]
    groups = GROUPS if need_ctx else GROUPS[1:]

    def load_head(h):
        s = h % 2
        P.op("pool", lambda e: e.dma_start(out=Wq[s], in_=wqkv[:, :, h * 128:(h + 1) * 128]), writes=[("Wq", s)], dma=True)
        P.op("pool", lambda e: e.dma_start(out=Wo[s], in_=wo[h * 128:(h + 1) * 128, :]), writes=[("Wo", s)], dma=True)
        emit_rot_copy(P, Wqr[s], Wq[s], 32, ("Wq", s), ("Wqr", s))

    items = [(h, gi) for h in range(8) for gi in range(len(groups))]
    SB = [4, 5, 6]
    LOOK = 2
    ycnt = [0]

    def qproj(n):
        h, gi = items[n]
        s = h % 2
        (t0, ntile) = groups[gi]
        ntok = ntile * 128
        isctx = t0 < 2
        a = n % 2
        for k in range(KC):
            _mm(P, C.psum[0][:, 0:ntok], Wq[s][:, k, :], C.hT[:, k, t0 * 128:t0 * 128 + ntok],
                k == 0, k == KC - 1, reads=[("Wq", s)] + hT_keys(t0, ntile), writes=[("bank", 0)])
        if not isctx:
            for k in range(KC):
                _mm(P, C.psum[1][:, 0:ntok], Wqr[s][:, k, :], C.hT[:, k, t0 * 128:t0 * 128 + ntok],
                    k == 0, k == KC - 1, reads=[("Wqr", s)] + hT_keys(t0, ntile), writes=[("bank", 1)])
            c0 = (t0 - 2) * 128
            emit_qk_norm_rope(C, bank(0), bank(1), bank(2), ntok, qT[a][:, 0:ntok], gq, gqr, rope[:, 0, c0:c0 + ntok],
                              rope[:, 1, c0:c0 + ntok], tmp, eps_t, 1.0 / 128, "q", True, ("qT", a))
        else:
            emit_qk_norm_rope(C, bank(0), bank(1), bank(2), ntok, qT[a][:, 0:ntok], gq, gqr, None, None, tmp, eps_t,
                              1.0 / 128, "q", False, ("qT", a))

    def keyloop(n):
        h, gi = items[n]
        hk = h // 4
        (t0, ntile) = groups[gi]
        ntok = ntile * 128
        isctx = t0 < 2
        a = n % 2
        nkt = 2 if isctx else 34

        def qk(kt):
            sb = SB[kt % 3]
            pp = kt % 3
            _mm(P, C.psum[sb][:, 0:ntok], Kall[:, hk, kt * 128:(kt + 1) * 128], qT[a][:, 0:ntok], True, True,
                reads=["Kall", ("qT", a)], writes=[("bank", sb)])
            P.op("act", (lambda e: e.activation(out=PT[pp][:, 0:ntok], in_=C.psum[sb][:, 0:ntok], func=AF.Exp)),
                 reads=[("bank", sb)], writes=[("PT", pp)])

        def pv(kt):
            pp = kt % 3
            _mm(P, C.psum[3][:, 0:ntok], Vall[:, kt, hk * 128:(hk + 1) * 128], PT[pp][:, 0:ntok], kt == 0, kt == nkt - 1,
                reads=["Vall", ("PT", pp)], writes=[("bank", 3)])
            _mm(P, C.psum[2][:, 0:ntok], C.ones_bf2, PT[pp][:, 0:ntok], kt == 0, kt == nkt - 1,
                reads=["ones_bf", ("PT", pp)], writes=[("bank", 2)])

        for kt in range(min(LOOK, nkt)):
            qk(kt)
        for kt in range(nkt):
            if kt + LOOK < nkt:
                qk(kt + LOOK)
            pv(kt)
        P.op("dve", (lambda e: e.reciprocal(out=rden[:, 0:ntok], in_=C.psum[2][:, 0:ntok])),
             reads=[("bank", 2)], writes=["rden"])
        P.op("dve", (lambda e: e.tensor_tensor(out=oT[a][:, 0:ntok], in0=C.psum[3][:, 0:ntok], in1=rden[:, 0:ntok], op=ALU.mult)),
             reads=[("bank", 3), "rden"], writes=[("oT", a)])

    def yproj(n):
        h, gi = items[n]
        s = h % 2
        (t0, ntile) = groups[gi]
        a = n % 2
        for tt in range(ntile):
            t = t0 + tt
            o = ycnt[0] % 2
            ycnt[0] += 1
            gbc = gc if t < 2 else gx
            yb = (7, 0) if o == 0 else (1, 2)
            for nh in range(2):
                _mm(P, C.psum[yb[nh]][:, :], oT[a][:, tt * 128:(tt + 1) * 128], Wo[s][:, nh * 512:(nh + 1) * 512], True, True,
                    reads=[("oT", a), ("Wo", s)], writes=[("bank", yb[nh])])
                P.op("dve", (lambda e, nh=nh, o=o, gbc=gbc, yb=yb: e.tensor_tensor(
                    out=ytmp[o][:, nh * 512:(nh + 1) * 512], in0=C.psum[yb[nh]][:, :], in1=gbc[:, nh * 512:(nh + 1) * 512], op=ALU.mult)),
                    reads=[("bank", yb[nh]), ("bc", "gx"), ("bc", "gc")], writes=[("ytmp", o, nh)])
            P.op("pool" if o == 0 else "dve", (lambda e, t=t, o=o: e.tensor_tensor(out=C.X[:, t, :], in0=C.X[:, t, :], in1=ytmp[o], op=ALU.add)),
                 reads=[("X", t), ("ytmp", o, 0), ("ytmp", o, 1)], writes=[("X", t)])

    load_head(0)
    load_head(1)
    qproj(0)
    for n in range(len(items)):
        keyloop(n)
        if n + 1 < len(items):
            qproj(n + 1)
        yproj(n)
        h_, gi_ = items[n]
        if gi_ == len(groups) - 1 and h_ + 2 < 8:
            load_head(h_ + 2)
    A.pop()
    P.barrier()
    A.push()
    bc2 = {"lg": A.alloc((D,)), "lb": A.alloc((D,))}
    for n, src in (("lg", C.dram["l%d_ln1_g" % i]), ("lb", C.dram["l%d_ln1_b" % i])):
        P.op("sp", (lambda e, n=n, src=src: e.dma_start(out=bc2[n], in_=src.partition_broadcast(128))), writes=[("bc", n)], dma=True)
    emit_ln(C, bc2, need_ctx=need_ctx)
    A.pop()
    P.barrier()


def emit_ln_bc(C, i, sub, need_ctx):
    P, A = C.P, C.A
    P.barrier()
    A.push()
    bc2 = {"lg": A.alloc((D,)), "lb": A.alloc((D,))}
    for n, src in (("lg", C.dram["l%d_ln%d_g" % (i, sub + 1)]), ("lb", C.dram["l%d_ln%d_b" % (i, sub + 1)])):
        P.op("sp", (lambda e, n=n, src=src: e.dma_start(out=bc2[n], in_=src.partition_broadcast(128))), writes=[("bc", n)], dma=True)
    emit_ln(C, bc2, need_ctx=need_ctx)
    A.pop()
    P.barrier()


def emit_cmlp(C, i):
    P, A, nc = C.P, C.A, C.nc
    need_ctx = i < DEPTH - 1
    pre = "l%d_cmlp_" % i
    w_in = C.dram[pre + "w_in"].rearrange("(k p) n -> p k n", p=128)
    w_out = C.dram[pre + "w_out"].rearrange("(c p) n -> p c n", p=128)
    P.barrier()
    A.push()
    gx = A.alloc((D,))
    gc = A.alloc((D,))
    gate_c0 = 2 * D
    P.op("sp", lambda e: e.dma_start(out=gx, in_=C.ada_scr[i, 0, gate_c0:gate_c0 + D].partition_broadcast(128)), writes=[("bc", "gx")], dma=True)
    P.op("sp", lambda e: e.dma_start(out=gc, in_=C.ada_scr[i, 1, gate_c0:gate_c0 + D].partition_broadcast(128)), writes=[("bc", "gc")], dma=True)
    emit_build_hT(C, i, 0, need_ctx=need_ctx)
    emit_scale_x(C, need_ctx=need_ctx)
    gv, bv, bu = A.alloc((16,)), A.alloc((16,)), A.alloc((16,))
    P.op("sp", lambda e: e.dma_start(out=gv, in_=C.dram[pre + "v_norm_g"].rearrange("(c p) -> p c", p=128), allow_slow_non_contiguous=True), writes=["gv"], dma=True)
    P.op("sp", lambda e: e.dma_start(out=bv, in_=C.dram[pre + "v_norm_b"].rearrange("(c p) -> p c", p=128), allow_slow_non_contiguous=True), writes=["bv"], dma=True)
    P.op("sp", lambda e: e.dma_start(out=bu, in_=C.dram[pre + "b_in"][0:2048].rearrange("(c p) -> p c", p=128), allow_slow_non_contiguous=True), writes=["bu"], dma=True)
    brow_v = A.alloc((2048,), BF16, parts=1)
    brow_o = A.alloc((D,), BF16, parts=1)
    P.op("pool", lambda e: e.dma_start(out=brow_v, in_=C.dram[pre + "b_in"][2048:4096].rearrange("(o n) -> o n", o=1)), writes=["brow_v"], dma=True)
    P.op("pool", lambda e: e.dma_start(out=brow_o, in_=C.dram[pre + "b_out"].rearrange("(o n) -> o n", o=1)), writes=["brow_o"], dma=True)
    WsT = A.alloc((8, 128), BF16)
    Bt = A.alloc((16, 128))
    ones_row = C.ones_bf2[0:1, :]
    A.push()
    Wsl = [A.alloc((128,)) for _ in range(2)]
    bsbc = A.alloc((8, 128))
    for g in range(8):
        s = g % 2
        P.op("sp", (lambda e, g=g, s=s: e.dma_start(out=Wsl[s], in_=C.dram[pre + "w_s"][g])), writes=[("Wsl", s)], dma=True)
        P.op("sp", (lambda e, g=g: e.dma_start(out=bsbc[:, g, :], in_=C.dram[pre + "b_s"][g].partition_broadcast(128))), writes=[("bsbc", g)], dma=True)
        P.op("pe", (lambda e, s=s: e.transpose(C.psum[s][:, 0:128], Wsl[s], C.ident)), reads=[("Wsl", s), "ident"], writes=[("bank", s)])
        P.op("act", (lambda e, g=g, s=s: e.copy(out=WsT[:, g, :], in_=C.psum[s][:, 0:128])), reads=[("bank", s)], writes=[("WsT", g)])
        _mm(P, C.psum[2 + s][:, 0:128], C.ones_bf2, WsT[:, g, :], True, True, reads=["ones_bf", ("WsT", g)], writes=[("bank", 2 + s)])
        for cb in (2 * g, 2 * g + 1):
            P.op("dve", (lambda e, g=g, s=s, cb=cb: e.scalar_tensor_tensor(
                out=Bt[:, cb, :], in0=C.psum[2 + s][:, 0:128], scalar=bv[:, cb:cb + 1], in1=bsbc[:, g, :], op0=ALU.mult, op1=ALU.add)),
                reads=[("bank", 2 + s), "bv", ("bsbc", g)], writes=[("Bt", cb)])
    A.pop()
    P.barrier()
    Wv = A.alloc((KC, 512), BF16)
    Wu = A.alloc((KC, 256), BF16)
    Wo = A.alloc((16, 512), BF16)
    z = A.alloc((4, 2048), BF16)
    uvT = A.alloc((16, 512), BF16)
    uT = A.alloc((512,))
    tmp = A.alloc((512,))
    ytmp = [A.alloc((512,)) for _ in range(2)]
    stats = A.alloc((4, 4, 6))
    mv = A.alloc((4, 2))
    rstd = A.alloc((4,))
    nmr = A.alloc((4,))
    groups = GROUPS if need_ctx else GROUPS[1:]
    ycnt = 0
    for (t0, ntile) in groups:
        ntok = ntile * 128
        for vb in range(4):
            P.op("pool", (lambda e, vb=vb: e.dma_start(out=Wv, in_=w_in[:, :, 2048 + vb * 512:2048 + (vb + 1) * 512])), writes=["Wv"], dma=True)
            for tt in range(ntile):
                t = t0 + tt
                pb = tt % 2
                _mm(P, C.psum[pb][:, :], ones_row, brow_v[:, vb * 512:(vb + 1) * 512], True, False, reads=["ones_bf", "brow_v"], writes=[("bank", pb)])
                for k in range(KC):
                    _mm(P, C.psum[pb][:, :], C.hT[:, k, t * 128:(t + 1) * 128], Wv[:, k, :], False, k == KC - 1,
                        reads=["Wv"] + hT_keys(t, 1), writes=[("bank", pb)])
                P.op("act", (lambda e, tt=tt, vb=vb, pb=pb: e.activation(out=z[:, tt, vb * 512:(vb + 1) * 512], in_=C.psum[pb][:, :], func=AF.Gelu_apprx_tanh)),
                     reads=[("bank", pb)], writes=[("z", tt, vb)])
        for tt in range(ntile):
            for vb in range(4):
                P.op("dve", (lambda e, tt=tt, vb=vb: e.bn_stats(out=stats[:, tt, vb, :], in_=z[:, tt, vb * 512:(vb + 1) * 512])),
                     reads=[("z", tt, vb)], writes=[("zst", tt, vb)])
            P.op("dve", (lambda e, tt=tt: e.bn_aggr(out=mv[:, tt, :], in_=stats[:, tt, :, :])), reads=[("zst", tt, vb) for vb in range(4)], writes=[("zmv", tt)])
        zk = [("zmv", tt) for tt in range(ntile)]
        P.op("dve", (lambda e, ntile=ntile: e.tensor_scalar(out=rstd[:, 0:ntile], in0=mv[:, 0:ntile, 1], scalar1=float(LN_EPS), scalar2=None, op0=ALU.add)),
             reads=zk, writes=["zrstd"])
        P.op("act", (lambda e, ntile=ntile: e.activation(out=rstd[:, 0:ntile], in_=rstd[:, 0:ntile], func=AF.Sqrt)), reads=["zrstd"], writes=["zrstd"])
        P.op("dve", (lambda e, ntile=ntile: e.reciprocal(out=rstd[:, 0:ntile], in_=rstd[:, 0:ntile])), reads=["zrstd"], writes=["zrstd"])
        P.op("dve", (lambda e, ntile=ntile: e.scalar_tensor_tensor(out=nmr[:, 0:ntile], in0=mv[:, 0:ntile, 0], scalar=-1.0, in1=rstd[:, 0:ntile],
                                                                  op0=ALU.mult, op1=ALU.mult)), reads=zk + ["zrstd"], writes=["znmr"])
        for tt in range(ntile):
            P.op("act", (lambda e, tt=tt: e.activation(out=z[:, tt, :], in_=z[:, tt, :], func=AF.Identity, bias=nmr[:, tt:tt + 1], scale=rstd[:, tt:tt + 1])),
                 reads=[("z", tt, vb) for vb in range(4)] + ["zrstd", "znmr"], writes=[("zn", tt)])
        for cb in range(16):
            g = cb // 2
            if cb % 2 == 0:
                P.op("pool", (lambda e, cb=cb: e.dma_start(out=Wu, in_=w_in[:, :, cb * 128:(cb + 2) * 128])), writes=["Wu"], dma=True)
            pu = C.psum[2 + cb % 2]
            ps = C.psum[4 + cb % 2]
            for k in range(KC):
                _mm(P, pu[:, 0:ntok], Wu[:, k, (cb % 2) * 128:(cb % 2 + 1) * 128], C.hT[:, k, t0 * 128:t0 * 128 + ntok],
                    k == 0, k == KC - 1, reads=["Wu"] + hT_keys(t0, ntile), writes=[("bank", 2 + cb % 2)])
            P.op("dve", (lambda e, pu=pu, cb=cb, ntok=ntok: e.tensor_scalar(out=uT[:, 0:ntok], in0=pu[:, 0:ntok], scalar1=bu[:, cb:cb + 1], scalar2=None, op0=ALU.add)),
                 reads=[("bank", 2 + cb % 2), "bu"], writes=["uT"])
            P.op("act", (lambda e, ntok=ntok: e.activation(out=uT[:, 0:ntok], in_=uT[:, 0:ntok], func=AF.Gelu_apprx_tanh)),
                 reads=["uT"], writes=["uT"])
            for tt in range(ntile):
                _mm(P, ps[:, tt * 128:(tt + 1) * 128], z[:, tt, cb * 128:(cb + 1) * 128], WsT[:, g, :], True, True,
                    reads=[("zn", tt), ("WsT", g)], writes=[("bank", 4 + cb % 2)])
            P.op("dve", (lambda e, ps=ps, cb=cb, ntile=ntile, ntok=ntok: e.scalar_tensor_tensor(
                out=tmp[:, 0:ntok].rearrange("p (a b) -> p a b", b=128), in0=ps[:, 0:ntok].rearrange("p (a b) -> p a b", b=128),
                scalar=gv[:, cb:cb + 1], in1=Bt[:, cb:cb + 1, :].broadcast_to([128, ntile, 128]), op0=ALU.mult, op1=ALU.add)),
                reads=[("bank", 4 + cb % 2), "gv", ("Bt", cb)], writes=["cm_tmp"])
            P.op("dve", (lambda e, cb=cb, ntok=ntok: e.tensor_tensor(out=uvT[:, cb, 0:ntok], in0=tmp[:, 0:ntok], in1=uT[:, 0:ntok], op=ALU.mult)),
                 reads=["cm_tmp", "uT"], writes=[("uvT", cb)])
        uk = [("uvT", cb) for cb in range(16)]
        for nh in range(2):
            P.op("pool", (lambda e, nh=nh: e.dma_start(out=Wo, in_=w_out[:, :, nh * 512:(nh + 1) * 512])), writes=["Wo"], dma=True)
            for tt in range(ntile):
                t = t0 + tt
                o = ycnt % 2
                ycnt += 1
                py = C.psum[6 + o]
                gbc = gc if t < 2 else gx
                _mm(P, py[:, :], ones_row, brow_o[:, nh * 512:(nh + 1) * 512], True, False, reads=["ones_bf", "brow_o"], writes=[("bank", 6 + o)])
                for cb in range(16):
                    _mm(P, py[:, :], uvT[:, cb, tt * 128:(tt + 1) * 128], Wo[:, cb, :], False, cb == 15,
                        reads=["Wo"] + (uk if cb in (0, 15) else []), writes=[("bank", 6 + o)])
                P.op("dve", (lambda e, py=py, o=o, gbc=gbc, nh=nh: e.tensor_tensor(out=ytmp[o], in0=py[:, :], in1=gbc[:, nh * 512:(nh + 1) * 512], op=ALU.mult)),
                     reads=[("bank", 6 + o), ("bc", "gx"), ("bc", "gc")], writes=[("ytmp", o)])
                P.op("pool", (lambda e, t=t, o=o, nh=nh: e.tensor_tensor(out=C.X[:, t, nh * 512:(nh + 1) * 512], in0=C.X[:, t, nh * 512:(nh + 1) * 512], in1=ytmp[o], op=ALU.add)),
                     reads=[("X", t), ("ytmp", o)], writes=[("X", t)])
    A.pop()
    emit_ln_bc(C, i, 0, need_ctx)


def emit_retention(C, i):
    P, A, nc = C.P, C.A, C.nc
    need_ctx = i < DEPTH - 1
    w = C.dram["l%d_ret_wqkvg" % i].rearrange("(k p) n -> p k n", p=128)
    wo = C.dram["l%d_ret_wo" % i]
    xacc = C.xacc
    P.barrier()
    A.push()
    gx = A.alloc((D,))
    gc = A.alloc((D,))
    gate_c0 = 2 * D
    P.op("sp", lambda e: e.dma_start(out=gx, in_=C.ada_scr[i, 0, gate_c0:gate_c0 + D].partition_broadcast(128)), writes=[("bc", "gx")], dma=True)
    P.op("sp", lambda e: e.dma_start(out=gc, in_=C.ada_scr[i, 1, gate_c0:gate_c0 + D].partition_broadcast(128)), writes=[("bc", "gc")], dma=True)
    emit_build_hT(C, i, 0, need_ctx=True)
    emit_scale_x(C, need_ctx=True)
    for t in range(NT):
        P.op("sp", (lambda e, t=t: e.dma_start(out=xacc[t * 128:(t + 1) * 128, :], in_=C.X[:, t, :])), reads=[("X", t)], writes=[("xacc", t)], dma=True)
    P.barrier()
    A2 = Arena(C.Xraw, NT * D)
    Kh = A2.alloc((2, CTX + SEQ), BF16)
    Vh = A2.alloc((34, 512), BF16)
    oT32 = A2.alloc((4, 512))
    ogT = A2.alloc((4, 512), BF16)
    Xt = A2.alloc((D,))
    hTo = A.alloc((KC, 512), BF16)
    ropeg = A.alloc((2, 2, 512))
    wreg_top = A.top
    Wreg = A.alloc((6144,))
    qT = [A.alloc((2, 512), BF16) for _ in range(2)]
    PT = [A.alloc((512,), BF16) for _ in range(3)]
    tmp = [A.alloc((512,)) for _ in range(4)]
    sq = A.alloc((4, 512), BF16)
    rstd = A.alloc((512,))
    ytmp = [A.alloc((D,)) for _ in range(2)]
    dec = A.alloc((8,))
    lg = A.alloc((8,))
    nlg = A.alloc((8,))
    lg128 = A.alloc((8,))
    nlg128 = A.alloc((8,))
    dji_i = A.alloc((128,), I32)
    dji = A.alloc((128,))
    mrow_i = A.alloc((40,), I32)
    mrow = A.alloc((40,))
    Ef, Eb, Df, Db, Dd = (A.alloc((128,)) for _ in range(5))
    Tf = A.alloc((4, 128))
    Tb = A.alloc((4, 128))
    pwf, pwb, npwb = A.alloc((40,)), A.alloc((40,)), A.alloc((40,))
    P.op("sp", lambda e: e.dma_start(out=dec, in_=C.dram["ret_dec"].rearrange("a b -> (a b)").partition_broadcast(128)), writes=["dec"], dma=True)
    P.op("act", lambda e: e.activation(out=lg, in_=dec, func=AF.Exp), reads=["dec"], writes=["nlg"])
    P.op("dve", lambda e: e.tensor_scalar(out=nlg, in0=lg, scalar1=1.0, scalar2=None, op0=ALU.mult), reads=["nlg"], writes=["nlg2"])
    P.op("dve", lambda e: e.tensor_scalar(out=lg, in0=nlg, scalar1=-1.0, scalar2=None, op0=ALU.mult), reads=["nlg2"], writes=["lg"])
    P.op("dve", lambda e: e.tensor_scalar(out=lg128, in0=lg, scalar1=128.0, scalar2=None, op0=ALU.mult), reads=["lg"], writes=["lg128"])
    P.op("dve", lambda e: e.tensor_scalar(out=nlg128, in0=nlg, scalar1=128.0, scalar2=None, op0=ALU.mult), reads=["nlg2"], writes=["nlg128"])
    P.op("pool", lambda e: e.iota(dji_i, pattern=[[1, 128]], base=0, channel_multiplier=-1), writes=["dji_i"])
    P.op("pool", lambda e: e.iota(mrow_i, pattern=[[1, 40]], base=0, channel_multiplier=0), writes=["mrow_i"])
    P.op("dve", lambda e: e.tensor_copy(out=dji, in_=dji_i), reads=["dji_i"], writes=["dji"])
    P.op("dve", lambda e: e.tensor_copy(out=mrow, in_=mrow_i), reads=["mrow_i"], writes=["mrow"])
    P.barrier()
    bank = lambda n: ("bank", n)
    hk_all = [("hT", t, k) for t in range(NT) for k in range(KC)]

    def do_head(hd):
        f, bb = hd, 4 + hd
        P.op("act", lambda e: e.activation(out=Ef, in_=dji, func=AF.Exp, scale=lg[:, f:f + 1]), reads=["dji", "lg"], writes=["Ef"])
        P.op("act", lambda e: e.activation(out=Eb, in_=dji, func=AF.Exp, scale=nlg[:, bb:bb + 1]), reads=["dji", "nlg2"], writes=["Eb"])
        P.op("act", lambda e: e.activation(out=pwf, in_=mrow, func=AF.Exp, scale=lg128[:, f:f + 1]), reads=["mrow", "lg128"], writes=["pwf"])
        P.op("act", lambda e: e.activation(out=pwb, in_=mrow, func=AF.Exp, scale=lg128[:, bb:bb + 1]), reads=["mrow", "lg128"], writes=["pwb"])
        P.op("act", lambda e: e.activation(out=npwb, in_=mrow, func=AF.Exp, scale=nlg128[:, bb:bb + 1]), reads=["mrow", "nlg128"], writes=["npwb"])
        P.op("pool", lambda e: e.affine_select(out=Df, in_=Ef, pattern=[[1, 128]], compare_op=ALU.is_ge, fill=0.0, base=0, channel_multiplier=-1),
             reads=["Ef"], writes=["Df"])
        P.op("pool", lambda e: e.affine_select(out=Db, in_=Eb, pattern=[[-1, 128]], compare_op=ALU.is_ge, fill=0.0, base=0, channel_multiplier=1),
             reads=["Eb"], writes=["Db"])
        P.op("dve", lambda e: e.tensor_tensor(out=Dd, in0=Df, in1=Db, op=ALU.add), reads=["Df", "Db"], writes=["Dd"])
        for m in range(4):
            P.op("dve", (lambda e, m=m: e.tensor_scalar(out=Tf[:, m, :], in0=Ef, scalar1=pwf[:, m:m + 1], scalar2=None, op0=ALU.mult)),
                 reads=["Ef", "pwf"], writes=[("Tf", m)])
            P.op("dve", (lambda e, m=m: e.tensor_scalar(out=Tb[:, m, :], in0=Eb, scalar1=npwb[:, m:m + 1], scalar2=None, op0=ALU.mult)),
                 reads=["Eb", "npwb"], writes=[("Tb", m)])
        ret_phase = int(os.environ.get("MK_RET_PHASE", "5"))
        if ret_phase < 2:
            return
        A.top = wreg_top
        Wk = A.alloc((KC, 256), BF16)
        Wkr = A.alloc((KC, 256), BF16)
        Wv = A.alloc((KC, 512), BF16)
        P.op("pool", lambda e: e.dma_start(out=Wk, in_=w[:, :, 1024 + hd * 256:1024 + (hd + 1) * 256]), writes=["Wk"], dma=True)
        P.op("pool", lambda e: e.dma_start(out=Wv, in_=w[:, :, 2048 + hd * 512:2048 + (hd + 1) * 512]), writes=["Wv"], dma=True)
        P.op("pool", lambda e: e.tensor_scalar(out=Wk, in0=Wk, scalar1=0.0625, scalar2=None, op0=ALU.mult), reads=["Wk"], writes=["Wk"])
        emit_rot_copy(P, Wkr, Wk, 64, "Wk", "Wkr")

        def kv_group(src, c0, ntile, hkeys, kcol0, ktile0, isctx, ropet):
            ntok = ntile * 128
            for dc in range(2):
                for k in range(KC):
                    _mm(P, C.psum[0][:, 0:ntok], Wk[:, k, dc * 128:(dc + 1) * 128], src[:, k, c0:c0 + ntok], k == 0, k == KC - 1,
                        reads=["Wk"] + hkeys, writes=[bank(0)])
                if isctx:
                    P.op("act", (lambda e, dc=dc: e.copy(out=Kh[:, dc, kcol0:kcol0 + ntok], in_=C.psum[0][:, 0:ntok])), reads=[bank(0)], writes=["Kh"])
                    continue
                for k in range(KC):
                    _mm(P, C.psum[1][:, 0:ntok], Wkr[:, k, dc * 128:(dc + 1) * 128], src[:, k, c0:c0 + ntok], k == 0, k == KC - 1,
                        reads=["Wkr"] + hkeys, writes=[bank(1)])
                P.op("dve", (lambda e, dc=dc: e.tensor_tensor(out=tmp[0][:, 0:ntok], in0=C.psum[0][:, 0:ntok], in1=ropet[:, dc, 0, 0:ntok], op=ALU.mult)),
                     reads=[bank(0), "ropeg"], writes=["t0"])
                P.op("dve", (lambda e, dc=dc: e.tensor_tensor(out=tmp[1][:, 0:ntok], in0=C.psum[1][:, 0:ntok], in1=ropet[:, dc, 1, 0:ntok], op=ALU.mult)),
                     reads=[bank(1), "ropeg"], writes=["t1"])
                P.op("pool", (lambda e, dc=dc: e.tensor_tensor(out=Kh[:, dc, kcol0:kcol0 + ntok], in0=tmp[0][:, 0:ntok], in1=tmp[1][:, 0:ntok], op=ALU.add)),
                     reads=["t0", "t1"], writes=["Kh"])
            for tt in range(ntile):
                pb = 2 + tt % 2
                for k in range(KC):
                    _mm(P, C.psum[pb][:, :], src[:, k, c0 + tt * 128:c0 + (tt + 1) * 128], Wv[:, k, :], k == 0, k == KC - 1,
                        reads=["Wv"] + hkeys, writes=[bank(pb)])
                P.op("act", (lambda e, tt=tt, pb=pb: e.copy(out=Vh[:, ktile0 + tt, :], in_=C.psum[pb][:, :])), reads=[bank(pb)], writes=["Vh"])

        for (t0, ntile) in GROUPS:
            if t0 < 2:
                kv_group(C.hT, 0, ntile, hk_all, 0, 0, True, None)
            else:
                c0 = (t0 - 2) * 128
                P.op("sp", (lambda e, c0=c0: e.dma_start(out=ropeg, in_=C.dram["rope_r"][:, :, :, c0:c0 + 512])), writes=["ropeg"], dma=True)
                kv_group(C.hT, t0 * 128, ntile, hk_all, CTX + c0, t0, False, ropeg)
        for g in range(4):
            P.op("sp", (lambda e, g=g: e.dma_start(out=ropeg, in_=C.dram["rope_ro"][:, :, :, g * 512:(g + 1) * 512])), writes=["ropeg"], dma=True)
            for tt in range(4):
                tg = g * 4 + tt
                P.op("sp", (lambda e, tg=tg: e.dma_start(out=Xt, in_=C.dram["x_oth"][tg * 128:(tg + 1) * 128, :])), writes=["Xt"], dma=True)
                for k in range(KC):
                    pst = C.psum[6 + k // 4][:, (k % 4) * 128:(k % 4 + 1) * 128]
                    P.op("pe", (lambda e, k=k, pst=pst: e.transpose(pst, Xt[:, k * 128:(k + 1) * 128], C.ident)),
                         reads=["Xt", "ident"], writes=[bank(6 + k // 4)])
                for k in range(KC):
                    pst = C.psum[6 + k // 4][:, (k % 4) * 128:(k % 4 + 1) * 128]
                    P.op("dve", (lambda e, k=k, pst=pst, tt=tt: e.tensor_scalar(
                        out=hTo[:, k, tt * 128:(tt + 1) * 128], in0=pst,
                        scalar1=C.sc1p[:, i, 0, k, 0:1], scalar2=C.adaT[:, i, k, 0:1], op0=ALU.mult, op1=ALU.add)),
                        reads=[bank(6 + k // 4)], writes=[("hTo", tt, k)])
            kv_group(hTo, 0, 4, [("hTo", tt, k) for tt in range(4) for k in range(KC)], CTX + TOK + g * 512, 18 + g * 4, False, ropeg)
        P.barrier()
        if ret_phase < 3:
            return
        A.top = wreg_top
        Wq = A.alloc((KC, 256), BF16)
        Wqr = A.alloc((KC, 256), BF16)
        Wg = A.alloc((KC, 512), BF16)
        Wo = A.alloc((4, D), BF16)
        P.op("pool", lambda e: e.dma_start(out=Wq, in_=w[:, :, hd * 256:(hd + 1) * 256]), writes=["Wq"], dma=True)
        P.op("pool", lambda e: e.dma_start(out=Wg, in_=w[:, :, 4096 + hd * 512:4096 + (hd + 1) * 512]), writes=["Wg"], dma=True)
        P.op("pool", lambda e: e.dma_start(out=Wo, in_=wo[hd * 512:(hd + 1) * 512, :].rearrange("(c p) n -> p c n", p=128)), writes=["Wo"], dma=True)
        emit_rot_copy(P, Wqr, Wq, 64, "Wq", "Wqr")
        ycnt_box = [0]

        def do_group(t0, ntile, a):
            ntok = ntile * 128
            isctx = t0 < 2
            lq0 = t0 - 2
            ycnt = ycnt_box[0]
            hkeys = hT_keys(t0, ntile)
            if not isctx:
                P.op("sp", (lambda e, lq0=lq0: e.dma_start(out=ropeg, in_=C.dram["rope_r"][:, :, :, lq0 * 128:lq0 * 128 + 512])), writes=["ropeg"], dma=True)
            for dc in range(2):
                for k in range(KC):
                    _mm(P, C.psum[0][:, 0:ntok], Wq[:, k, dc * 128:(dc + 1) * 128], C.hT[:, k, t0 * 128:t0 * 128 + ntok], k == 0, k == KC - 1,
                        reads=["Wq"] + hkeys, writes=[bank(0)])
                if isctx:
                    P.op("act", (lambda e, dc=dc, a=a: e.copy(out=qT[a][:, dc, 0:ntok], in_=C.psum[0][:, 0:ntok])), reads=[bank(0)], writes=[("qT", a, dc)])
                    continue
                for k in range(KC):
                    _mm(P, C.psum[1][:, 0:ntok], Wqr[:, k, dc * 128:(dc + 1) * 128], C.hT[:, k, t0 * 128:t0 * 128 + ntok], k == 0, k == KC - 1,
                        reads=["Wqr"] + hkeys, writes=[bank(1)])
                P.op("dve", (lambda e, dc=dc: e.tensor_tensor(out=tmp[0][:, 0:ntok], in0=C.psum[0][:, 0:ntok], in1=ropeg[:, dc, 0, 0:ntok], op=ALU.mult)),
                     reads=[bank(0), "ropeg"], writes=["t0"])
                P.op("dve", (lambda e, dc=dc: e.tensor_tensor(out=tmp[1][:, 0:ntok], in0=C.psum[1][:, 0:ntok], in1=ropeg[:, dc, 1, 0:ntok], op=ALU.mult)),
                     reads=[bank(1), "ropeg"], writes=["t1"])
                P.op("pool", (lambda e, dc=dc, a=a: e.tensor_tensor(out=qT[a][:, dc, 0:ntok], in0=tmp[0][:, 0:ntok], in1=tmp[1][:, 0:ntok], op=ALU.add)),
                     reads=["t0", "t1"], writes=[("qT", a, dc)])
            ktiles = [0, 1] if isctx else list(range(34))
            SBR = [2, 3, 1]

            def qk_r(ki):
                kt = ktiles[ki]
                sb = SBR[ki % 3]
                pp = ki % 3
                ps = C.psum[sb]
                for dc in range(2):
                    _mm(P, ps[:, 0:ntok], Kh[:, dc, kt * 128:(kt + 1) * 128], qT[a][:, dc, 0:ntok], dc == 0, dc == 1,
                        reads=["Kh", ("qT", a, dc)], writes=[bank(sb)])
                pt = PT[pp]
                rk = [bank(sb), "tables"]
                wk_ = [("PT", pp)]

                def one(outv, inv, scal, tab, rk=rk, wk_=wk_, wkeys=None):
                    P.op("dve", (lambda e: e.scalar_tensor_tensor(out=outv, in0=inv, scalar=scal, in1=tab, op0=ALU.mult, op1=ALU.mult)),
                         reads=rk, writes=(wk_ if wkeys is None else wkeys))

                def sub(m, mode, idx, ps=ps, pt=pt):
                    sl = slice(m * 128, (m + 1) * 128)
                    if mode == "f":
                        one(pt[:, sl], ps[:, sl], pwf[:, idx:idx + 1], Ef)
                    elif mode == "b":
                        one(pt[:, sl], ps[:, sl], pwb[:, idx:idx + 1], Eb)
                    else:
                        P.op("dve", (lambda e: e.tensor_tensor(out=pt[:, sl], in0=ps[:, sl], in1=Dd, op=ALU.mult)), reads=rk, writes=wk_)

                Tfv = Tf.rearrange("p a b -> p (a b)")
                Tbv = Tb.rearrange("p a b -> p (a b)")
                if isctx:
                    for m in range(2):
                        if kt < m:
                            sub(m, "f", 1)
                        elif kt == m:
                            sub(m, "d", 0)
                        else:
                            sub(m, "b", 1)
                elif kt < 2:
                    one(tmp[2][:, 0:ntok], ps[:, 0:ntok], pwf[:, lq0 + 2 - kt:lq0 + 3 - kt], Tfv, wkeys=["t2"])
                    P.op("dve", (lambda e, ps=ps, kt=kt: e.scalar_tensor_tensor(out=tmp[3][:, 0:ntok], in0=ps[:, 0:ntok],
                                                                               scalar=pwb[:, 32 + kt - lq0:33 + kt - lq0], in1=Tbv, op0=ALU.mult, op1=ALU.mult)),
                         reads=rk, writes=["t3"])
                    P.op("pool", (lambda e, pt=pt: e.tensor_tensor(out=pt[:, 0:ntok], in0=tmp[2][:, 0:ntok], in1=tmp[3][:, 0:ntok], op=ALU.add)),
                         reads=["t2", "t3"], writes=wk_)
                elif kt < 18:
                    lk = kt - 2
                    if lk < lq0:
                        one(pt[:, 0:ntok], ps[:, 0:ntok], pwf[:, lq0 - lk:lq0 - lk + 1], Tfv)
                    elif lk > lq0 + 3:
                        one(pt[:, 0:ntok], ps[:, 0:ntok], pwb[:, lk - lq0:lk - lq0 + 1], Tbv)
                    else:
                        for m in range(4):
                            dlt = lk - lq0 - m
                            if dlt > 0:
                                sub(m, "b", dlt)
                            elif dlt == 0:
                                sub(m, "d", 0)
                            else:
                                sub(m, "f", -dlt)
                else:
                    lk = 16 + (kt - 18)
                    one(pt[:, 0:ntok], ps[:, 0:ntok], pwb[:, lk - lq0:lk - lq0 + 1], Tbv)

            def pv_r(ki):
                kt = ktiles[ki]
                pp = ki % 3
                pt = PT[pp]
                for eb in range(4):
                    _mm(P, C.psum[4 + eb][:, 0:ntok], Vh[:, kt, eb * 128:(eb + 1) * 128], pt[:, 0:ntok], ki == 0, ki == len(ktiles) - 1,
                        reads=["Vh", ("PT", pp)], writes=[bank(4 + eb)])

            for ki in range(min(2, len(ktiles))):
                qk_r(ki)
            for ki in range(len(ktiles)):
                if ki + 2 < len(ktiles):
                    qk_r(ki + 2)
                pv_r(ki)
            if ret_phase < 4:
                return
            for eb in range(4):
                P.op("act", (lambda e, eb=eb: e.copy(out=oT32[:, eb, 0:ntok], in_=C.psum[4 + eb][:, 0:ntok])), reads=[bank(4 + eb)], writes=[("oT32", eb)])
                P.op("act", (lambda e, eb=eb: e.activation(out=sq[:, eb, 0:ntok], in_=C.psum[4 + eb][:, 0:ntok], func=AF.Square)),
                     reads=[bank(4 + eb)], writes=[("sq", eb)])
            for eb in range(4):
                _mm(P, C.psum[0][:, 0:ntok], C.ones_bf2, sq[:, eb, 0:ntok], eb == 0, eb == 3, reads=[("sq", eb), "ones_bf"], writes=[bank(0)])
            P.op("dve", lambda e: e.tensor_scalar(out=rstd[:, 0:ntok], in0=C.psum[0][:, 0:ntok], scalar1=1.0 / 512, scalar2=float(RMS_EPS),
                                                  op0=ALU.mult, op1=ALU.add), reads=[bank(0)], writes=["rstd"])
            P.op("act", lambda e: e.activation(out=rstd[:, 0:ntok], in_=rstd[:, 0:ntok], func=AF.Ln), reads=["rstd"], writes=["rstd"])
            P.op("act", lambda e: e.activation(out=rstd[:, 0:ntok], in_=rstd[:, 0:ntok], func=AF.Exp, scale=-0.5), reads=["rstd"], writes=["rstd"])
            for eb in range(4):
                pg = C.psum[1 + eb % 2] if False else C.psum[2 + eb % 2]
                pgk = bank(2 + eb % 2)
                for k in range(KC):
                    _mm(P, pg[:, 0:ntok], Wg[:, k, eb * 128:(eb + 1) * 128], C.hT[:, k, t0 * 128:t0 * 128 + ntok], k == 0, k == KC - 1,
                        reads=["Wg"] + hkeys, writes=[pgk])
                P.op("act", (lambda e, pg=pg: e.copy(out=tmp[3][:, 0:ntok], in_=pg[:, 0:ntok])), reads=[pgk], writes=["t3"])
                P.op("act", (lambda e: e.activation(out=tmp[0][:, 0:ntok], in_=tmp[3][:, 0:ntok], func=AF.Exp, scale=-1.0)), reads=["t3"], writes=["t0"])
                P.op("dve", lambda e: e.tensor_scalar(out=tmp[0][:, 0:ntok], in0=tmp[0][:, 0:ntok], scalar1=1.0, scalar2=None, op0=ALU.add),
                     reads=["t0"], writes=["t0"])
                P.op("dve", lambda e: e.reciprocal(out=tmp[0][:, 0:ntok], in_=tmp[0][:, 0:ntok]), reads=["t0"], writes=["t0"])
                P.op("dve", (lambda e: e.tensor_tensor(out=tmp[1][:, 0:ntok], in0=tmp[3][:, 0:ntok], in1=tmp[0][:, 0:ntok], op=ALU.mult)),
                     reads=["t3", "t0"], writes=["t1"])
                P.op("pool", (lambda e, eb=eb: e.tensor_tensor(out=tmp[2][:, 0:ntok], in0=oT32[:, eb, 0:ntok], in1=rstd[:, 0:ntok], op=ALU.mult)),
                     reads=[("oT32", eb), "rstd"], writes=["t2"])
                P.op("dve", (lambda e, eb=eb: e.tensor_tensor(out=ogT[:, eb, 0:ntok], in0=tmp[2][:, 0:ntok], in1=tmp[1][:, 0:ntok], op=ALU.mult)),
                     reads=["t1", "t2"], writes=[("ogT", eb)])
            if ret_phase < 5:
                return
            ok_ = [("ogT", eb) for eb in range(4)]
            for tt in range(ntile):
                t = t0 + tt
                o = ycnt % 2
                ycnt += 1
                gbc = gc if t < 2 else gx
                for nh in range(2):
                    py = C.psum[nh]
                    for eb in range(4):
                        _mm(P, py[:, :], ogT[:, eb, tt * 128:(tt + 1) * 128], Wo[:, eb, nh * 512:(nh + 1) * 512], eb == 0, eb == 3,
                            reads=ok_ + ["Wo"], writes=[bank(nh)])
                    P.op("dve", (lambda e, py=py, nh=nh, o=o, gbc=gbc: e.tensor_tensor(out=ytmp[o][:, nh * 512:(nh + 1) * 512], in0=py[:, :],
                                                                                        in1=gbc[:, nh * 512:(nh + 1) * 512], op=ALU.mult)),
                         reads=[bank(nh), ("bc", "gx"), ("bc", "gc")], writes=[("ytmp", o, nh)])
                P.op("pool", (lambda e, t=t, o=o: e.dma_start(out=xacc[t * 128:(t + 1) * 128, :], in_=ytmp[o], accum_op=ALU.add)),
                     reads=[("ytmp", o, 0), ("ytmp", o, 1)], writes=[("xacc", t)], dma=True)
            ycnt_box[0] = ycnt

        for gi, (t0_, ntile_) in enumerate(GROUPS):
            do_group(t0_, ntile_, gi % 2)
        P.barrier()

    for hd_ in range(4):
        do_head(hd_)
    for t in range(NT):
        P.op("sp", (lambda e, t=t: e.dma_start(out=C.X[:, t, :], in_=xacc[t * 128:(t + 1) * 128, :])), writes=[("X", t)], dma=True)
    A.pop()
    emit_ln_bc(C, i, 0, need_ctx)


_PROG_CACHE = {}


def _rope_tables(hd, nf):
    theta = np.float32(10000.0)
    inv = (theta ** (-(np.arange(nf, dtype=np.float32) / np.float32(nf)))).astype(np.float32)
    n = np.arange(SEQ)
    row = (n // 64).astype(np.float32)
    col = (n % 64).astype(np.float32)
    d = np.arange(hd)
    pos = np.where((d < hd // 2)[:, None], row[None, :], col[None, :]).astype(np.float32)
    ang = (pos * inv[d % nf][:, None]).astype(np.float32)
    sign = np.where((d % (hd // 2)) < nf, -1.0, 1.0).astype(np.float32)[:, None]
    return np.stack([np.cos(ang), np.sin(ang) * sign], axis=1).astype(np.float32)


def _core_inputs(inputs, layers_needed, x0=None, xc0=None, flip_odd=False):
    x = np.asarray(inputs["x"] if x0 is None else x0, dtype=np.float32)
    ctx = np.asarray(inputs["ctx"] if xc0 is None else xc0, dtype=np.float32)
    c = np.asarray(inputs["c"], dtype=np.float32)
    c_ctx = np.asarray(inputs["c_ctx"], dtype=np.float32)
    ident = np.eye(128, dtype=np.float32)
    shared = {"ident": ident}
    rope_a = _rope_tables(128, 32)
    rope_r = _rope_tables(256, 64).reshape(2, 128, 2, SEQ).transpose(1, 0, 2, 3)
    for i in layers_needed:
        for n in layer_param_names(i):
            shared[n] = np.ascontiguousarray(np.asarray(inputs[n], dtype=np.float32))
            if NEXP_RUN < NEXP and ("moe_w_gu" in n or "moe_w_down" in n):
                shared[n] = np.ascontiguousarray(shared[n][:NEXP_RUN])
    maps = []
    for r in range(8):
        b, h = r // 2, r % 2
        cc = np.stack([c[b].reshape(KC, 128).T, c_ctx.reshape(KC, 128).T], axis=-1)
        m = dict(shared)
        own = slice(h * TOK, (h + 1) * TOK)
        oth = slice((1 - h) * TOK, (2 - h) * TOK)
        rev = flip_odd and h == 1
        st = -1 if rev else 1
        m["x_in"] = np.ascontiguousarray(x[b, own][::st])
        m["ctx_in"] = np.ascontiguousarray(ctx[b][::st])
        m["x_oth"] = np.ascontiguousarray(x[b, oth][::st])
        m["cc"] = np.ascontiguousarray(cc.astype(np.float32))
        m["rope_a"] = np.ascontiguousarray(rope_a[:, :, own][:, :, ::st])
        m["rope_o"] = np.ascontiguousarray(rope_a[:, :, oth][:, :, ::st])
        m["rope_r"] = np.ascontiguousarray(rope_r[:, :, :, own][:, :, :, ::st])
        m["rope_ro"] = np.ascontiguousarray(rope_r[:, :, :, oth][:, :, :, ::st])
        for i in layers_needed:
            if i % 3 == 2:
                dec = np.asarray(inputs["l%d_ret_decay" % i], dtype=np.float32)
                m["ret_dec"] = np.ascontiguousarray(dec[::st])
            if i % 3 == 1 and rev:
                m["l%d_cmlp_w_s" % i] = np.ascontiguousarray(shared["l%d_cmlp_w_s" % i][:, ::-1, ::-1])
                m["l%d_cmlp_b_s" % i] = np.ascontiguousarray(shared["l%d_cmlp_b_s" % i][:, ::-1])
        maps.append(m)
    return maps


def run_stages(inputs, stages, x0=None, xc0=None):
    layers_needed = sorted(set(i for _, i in stages))
    key = tuple(stages)
    if key not in _PROG_CACHE:
        _PROG_CACHE[key] = build_program(stages, layers_needed)
    nc = _PROG_CACHE[key]
    flip_odd = any(k == "mix" and i % 3 == 2 for k, i in stages)
    maps = _core_inputs(inputs, layers_needed, x0, xc0, flip_odd)
    maps = [{k: v for k, v in m.items() if k in nc.mk_inputs} for m in maps]
    if os.environ.get("MK_TRACE"):
        res = run_bass_kernel_spmd(nc, maps, core_ids=list(range(8)), trace=True)
        print("MK_TRACE exec_time_ns", stages, res.exec_time_ns)
        try:
            import json, collections
            pj = res.profile_json
            if isinstance(pj, (list, tuple)):
                pj = pj[0]
            if isinstance(pj, str):
                pj = json.loads(pj)
            print("MK_TRACE profile keys", list(pj.keys())[:40] if isinstance(pj, dict) else type(pj))
            if isinstance(pj, dict):
                for k, v in pj.items():
                    if isinstance(v, (int, float, str)):
                        print("  ", k, v)
                    elif isinstance(v, dict):
                        print("  ", k, {kk: vv for kk, vv in list(v.items())[:30] if isinstance(vv, (int, float, str))})
        except Exception as ex:
            print("MK_TRACE profile dump failed", ex)
    else:
        res = run_bass_kernel_spmd(nc, maps, core_ids=list(range(8)))
    xo = np.zeros((BATCH, SEQ, D), np.float32)
    xco = np.zeros((BATCH, CTX, D), np.float32)
    for r in range(8):
        b, h = r // 2, r % 2
        st = -1 if (flip_odd and h == 1) else 1
        xo[b, h * TOK:(h + 1) * TOK] = res.results[r]["x_out"][::st]
        if h == 0:
            xco[b] = res.results[r]["xc_out"]
    return xo, xco


INPUT_NAMES = (
    "x",
    "c",
    "ctx",
    "c_ctx",
    "l0_ada_w",
    "l0_ada_b",
    "l0_ln1_g",
    "l0_ln1_b",
    "l0_ln2_g",
    "l0_ln2_b",
    "l0_attn_wqkv",
    "l0_attn_q_norm",
    "l0_attn_k_norm",
    "l0_attn_wo",
    "l0_ffn_w_gu",
    "l0_ffn_w_down",
    "l1_ada_w",
    "l1_ada_b",
    "l1_ln1_g",
    "l1_ln1_b",
    "l1_ln2_g",
    "l1_ln2_b",
    "l1_cmlp_w_in",
    "l1_cmlp_b_in",
    "l1_cmlp_v_norm_g",
    "l1_cmlp_v_norm_b",
    "l1_cmlp_w_s",
    "l1_cmlp_b_s",
    "l1_cmlp_w_out",
    "l1_cmlp_b_out",
    "l1_moe_router",
    "l1_moe_w_gu",
    "l1_moe_w_down",
    "l2_ada_w",
    "l2_ada_b",
    "l2_ln1_g",
    "l2_ln1_b",
    "l2_ln2_g",
    "l2_ln2_b",
    "l2_ret_wqkvg",
    "l2_ret_decay",
    "l2_ret_wo",
    "l2_ffn_w_gu",
    "l2_ffn_w_down",
    "l3_ada_w",
    "l3_ada_b",
    "l3_ln1_g",
    "l3_ln1_b",
    "l3_ln2_g",
    "l3_ln2_b",
    "l3_attn_wqkv",
    "l3_attn_q_norm",
    "l3_attn_k_norm",
    "l3_attn_wo",
    "l3_moe_router",
    "l3_moe_w_gu",
    "l3_moe_w_down",
)


def kernel(**inputs):
    missing = [n for n in INPUT_NAMES if n not in inputs]
    assert not missing, missing
    x, xc = inputs["x"], inputs["ctx"]
    for i in range(DEPTH):
        x, xc = run_stages(inputs, [("mix", i), ("ffn", i)], x0=x, xc0=xc)
    return x
```

```python
import os
import numpy as np
import concourse.bass as bass
import concourse.mybir as mybir
from concourse.bass_utils import run_bass_kernel_spmd

F32 = mybir.dt.float32
BF16 = mybir.dt.bfloat16
I32 = mybir.dt.int32
AF = mybir.ActivationFunctionType
ALU = mybir.AluOpType
AX = mybir.AxisListType

ENGS = ("pe", "act", "dve", "pool", "sp")
N_DMA_SEMS = 12


class _Ins:
    __slots__ = ("eng", "fn", "deps", "dma", "idx", "signal", "sig_count", "dma_slot", "dma_round", "waits")

    def __init__(self, eng, fn, dma):
        self.eng = eng
        self.fn = fn
        self.dma = dma
        self.deps = set()
        self.signal = False
        self.sig_count = 0
        self.waits = None


class Prog:
    def __init__(self, nc, sync_same_engine=True):
        self.nc = nc
        self.lists = {e: [] for e in ENGS}
        self.state = {}
        self.sync_same_engine = sync_same_engine
        self.dma_count = {"sp": 0, "pool": 0, "act": 0}

    def op(self, eng, fn, reads=(), writes=(), dma=False):
        ins = _Ins(eng, fn, dma)
        ins.idx = len(self.lists[eng])
        for k in reads:
            st = self.state.get(k)
            if st is not None and st[0] is not None:
                ins.deps.add(st[0])
        for k in writes:
            st = self.state.get(k)
            if st is not None:
                if st[0] is not None:
                    ins.deps.add(st[0])
                for r in st[1]:
                    ins.deps.add(r)
        ins.deps.discard(ins)
        for k in reads:
            st = self.state.setdefault(k, [None, []])
            st[1].append(ins)
        for k in writes:
            self.state[k] = [ins, []]
        if dma:
            n = self.dma_count[eng]
            self.dma_count[eng] = n + 1
            ins.dma_slot = n % N_DMA_SEMS
            ins.dma_round = n // N_DMA_SEMS + 1
        self.lists[eng].append(ins)
        return ins

    def finalize(self, final_waits=()):
        nc = self.nc
        for e in ENGS:
            for ins in self.lists[e]:
                for d in ins.deps:
                    if d.dma:
                        continue
                    if d.eng == ins.eng and not ins.dma:
                        if d.eng == "pe" or not self.sync_same_engine:
                            continue
                    d.signal = True
        for ins in final_waits:
            if not ins.dma:
                ins.signal = True
        for e in ENGS:
            c = 0
            for ins in self.lists[e]:
                if ins.signal and not ins.dma:
                    c += 1
                    ins.sig_count = c
        self.sig_totals = {e: sum(1 for i in self.lists[e] if i.signal and not i.dma) for e in ENGS}
        import contextlib
        with contextlib.ExitStack() as es:
            sems = {e: es.enter_context(nc.semaphore("s_" + e)) for e in ENGS}
            dsems = {q: [es.enter_context(nc.semaphore("d_%s_%d" % (q, i))) for i in range(N_DMA_SEMS)]
                     for q in ("sp", "pool", "act")}
            block = es.enter_context(nc.Block())
            engobj = {"pe": "tensor", "act": "scalar", "dve": "vector", "pool": "gpsimd", "sp": "sync"}

            def make_body(e):
                lst = self.lists[e]

                def body(engine):
                    waited = {}

                    def wait(sem, val, key):
                        if waited.get(key, 0) >= val:
                            return
                        waited[key] = val
                        engine.wait_ge(sem, val)

                    for ins in lst:
                        for d in ins.deps:
                            if d.dma:
                                wait(dsems[d.eng][d.dma_slot], 16 * d.dma_round, ("d", d.eng, d.dma_slot))
                            else:
                                if d.eng == e and not ins.dma and (e == "pe" or not self.sync_same_engine):
                                    continue
                                wait(sems[d.eng], d.sig_count, ("c", d.eng))
                        if ins.dma:
                            if ins.dma_round > 1:
                                wait(dsems[e][ins.dma_slot], 16 * (ins.dma_round - 1), ("d", e, ins.dma_slot))
                        r = ins.fn(engine)
                        if ins.dma:
                            r.then_inc(dsems[e][ins.dma_slot], 16)
                        elif ins.signal:
                            r.then_inc(sems[e], 1)
                    if e == "sp":
                        for ins in final_waits:
                            if ins.dma:
                                wait(dsems[ins.eng][ins.dma_slot], 16 * ins.dma_round, ("d", ins.eng, ins.dma_slot))
                            else:
                                wait(sems[ins.eng], ins.sig_count, ("c", ins.eng))
                return body

            for e in ENGS:
                if not self.lists[e] and e != "sp":
                    continue
                getattr(block, engobj[e])(make_body(e))
        return self


D = 1024
DEPTH = 4
SEQ = 4096
BATCH = 4
CTX = 256
TOK = 2048
NT = 18
NTOK = NT * 128
FFN = 3584
NEXP = 8
NEXP_RUN = int(os.environ.get("MK_NEXP_RUN", "8"))
ALPHA = (2 * DEPTH) ** 0.25
LN_EPS = 1e-5
RMS_EPS = 1e-6
GROUPS = [(0, 2), (2, 4), (6, 4), (10, 4), (14, 4)]
KC = D // 128


class Arena:
    def __init__(self, ap, ncols):
        self.ap = ap
        self.ncols = ncols
        self.top = 0
        self.marks = []

    def alloc(self, free_shape, dtype=F32, parts=128):
        n = 1
        for s in free_shape:
            n *= s
        words = n if dtype in (F32, I32) else (n + 1) // 2
        words = (words + 7) // 8 * 8
        assert self.top + words <= self.ncols, ("arena overflow", self.top, words, self.ncols)
        v = self.ap[0:parts, self.top:self.top + words]
        self.top += words
        if dtype not in (F32,):
            v = v.bitcast(dtype)
        v = v[:, 0:n]
        if len(free_shape) > 1:
            names = "abcdefg"[:len(free_shape)]
            pat = "p (%s) -> p %s" % (" ".join(names), " ".join(names))
            v = v.rearrange(pat, **{names[q]: free_shape[q] for q in range(1, len(free_shape))})
        return v

    def push(self):
        self.marks.append(self.top)

    def pop(self):
        self.top = self.marks.pop()


class Ctx:
    pass


def _barrier(P):
    last = []
    for e in ENGS:
        lst = P.lists[e]
        if not lst:
            continue
        for ins in reversed(lst):
            if not ins.dma:
                last.append(ins)
                break
        seen = set()
        for ins in reversed(lst):
            if ins.dma and ins.dma_slot not in seen:
                seen.add(ins.dma_slot)
                last.append(ins)
            if len(seen) == N_DMA_SEMS:
                break
    P.barrier_set = last
    P.state = {}
    P.after_barrier = {e: True for e in ENGS}


_orig_op = Prog.op


def _op_with_barrier(self, eng, fn, reads=(), writes=(), dma=False):
    ins = _orig_op(self, eng, fn, reads, writes, dma)
    if getattr(self, "after_barrier", None) and self.after_barrier.get(eng):
        for b in self.barrier_set:
            if b is not ins:
                ins.deps.add(b)
        self.after_barrier[eng] = False
    return ins


Prog.op = _op_with_barrier
Prog.barrier = _barrier


ARENA_COLS = 53200


def _mm(P, out, lhsT, rhs, start, stop, reads, writes):
    return P.op("pe", lambda e: e.matmul(out, lhsT=lhsT, rhs=rhs, start=start, stop=stop), reads, writes)


def layer_param_names(i):
    pre = "l%d_" % i
    names = ["ada_w", "ada_b", "ln1_g", "ln1_b", "ln2_g", "ln2_b"]
    kind = i % 3
    if kind == 0:
        names += ["attn_wqkv", "attn_q_norm", "attn_k_norm", "attn_wo"]
    elif kind == 1:
        names += ["cmlp_w_in", "cmlp_b_in", "cmlp_v_norm_g", "cmlp_v_norm_b", "cmlp_w_s", "cmlp_b_s",
                  "cmlp_w_out", "cmlp_b_out"]
    else:
        names += ["ret_wqkvg", "ret_decay", "ret_wo"]
    if i % 2 == 0:
        names += ["ffn_w_gu", "ffn_w_down"]
    else:
        names += ["moe_router", "moe_w_gu", "moe_w_down"]
    return [pre + n for n in names]


PARAM_SHAPES = {}


def _param_shape(name):
    n = name[3:]
    shp = {
        "ada_w": [D, 6 * D], "ada_b": [6 * D], "ln1_g": [D], "ln1_b": [D], "ln2_g": [D], "ln2_b": [D],
        "attn_wqkv": [D, 1536], "attn_q_norm": [128], "attn_k_norm": [128], "attn_wo": [D, D],
        "cmlp_w_in": [D, 4096], "cmlp_b_in": [4096], "cmlp_v_norm_g": [2048], "cmlp_v_norm_b": [2048],
        "cmlp_w_s": [8, 128, 128], "cmlp_b_s": [8, 128], "cmlp_w_out": [2048, D], "cmlp_b_out": [D],
        "ret_wqkvg": [D, 6144], "ret_decay": [2, 4], "ret_wo": [2048, D],
        "ffn_w_gu": [D, 2 * FFN], "ffn_w_down": [FFN, D],
        "moe_router": [D, NEXP], "moe_w_gu": [NEXP_RUN, D, 2 * FFN], "moe_w_down": [NEXP_RUN, FFN, D],
    }[n]
    return shp


def build_program(stages, layers_needed):
    nc = bass.Bass("TRN2", target_bir_lowering=False)
    C = Ctx()
    C.nc = nc
    dram = {}

    def din(name, shape, dtype=F32):
        dram[name] = nc.dram_tensor(name, list(shape), dtype, kind="ExternalInput").ap()
        return dram[name]

    din("x_in", [TOK, D])
    din("ctx_in", [CTX, D])
    din("cc", [128, KC, 2])
    din("ident", [128, 128])
    if any(k == "mix" and i % 3 == 0 for k, i in stages):
        din("rope_a", [128, 2, TOK])
        din("rope_o", [128, 2, TOK])
    if any(k == "mix" and i % 3 != 1 for k, i in stages):
        din("x_oth", [TOK, D])
    if any(k == "mix" and i % 3 == 2 for k, i in stages):
        din("rope_r", [128, 2, 2, TOK])
        din("rope_ro", [128, 2, 2, TOK])
        din("ret_dec", [2, 4])
        C.xacc = nc.dram_tensor("xacc", [NTOK, D], F32, kind="Internal").ap()
    for i in layers_needed:
        for n in layer_param_names(i):
            din(n, _param_shape(n))
    x_out = nc.dram_tensor("x_out", [TOK, D], F32, kind="ExternalOutput").ap()
    xc_out = nc.dram_tensor("xc_out", [CTX, D], F32, kind="ExternalOutput").ap()
    C.ada_scr = nc.dram_tensor("ada_scr", [DEPTH, 2, 6 * D], F32, kind="Internal").ap()
    C.dram = dram

    import contextlib
    with contextlib.ExitStack() as es:
        arena_t = es.enter_context(nc.sbuf_tensor("arena", [128, ARENA_COLS], F32))
        A = Arena(arena_t[:], ARENA_COLS)
        C.A = A
        C.psum = [es.enter_context(nc.psum_tensor("ps%d" % i, [128, 512], F32)) for i in range(8)]
        P = Prog(nc)
        C.P = P
        C.Xraw = A.alloc((NT * D,))
        C.X = C.Xraw.rearrange("p (t d) -> p t d", d=D)
        C.hT = A.alloc((KC, NTOK), BF16)
        C.ident = A.alloc((128,))
        C.adaT = A.alloc((DEPTH, 48, 2))
        C.sc1p = A.alloc((DEPTH, 2, KC, 2))
        C.ones_bf2 = A.alloc((128,), BF16)

        P.op("sp", lambda e: e.dma_start(out=C.ident, in_=dram["ident"]), writes=["ident"], dma=True)
        P.op("pool", lambda e: e.memset(C.ones_bf2, 1.0), writes=["ones_bf"])
        for t in range(NT):
            src = dram["ctx_in"][t * 128:(t + 1) * 128, :] if t < 2 else dram["x_in"][(t - 2) * 128:(t - 1) * 128, :]
            P.op("sp", (lambda e, t=t, src=src: e.dma_start(out=C.X[:, t, :], in_=src)), writes=[("X", t)], dma=True)

        emit_adaln(C, layers_needed)
        for kind, i in stages:
            if kind == "ffn":
                emit_ffn(C, i)
            else:
                emit_mixer(C, i)
        P.barrier()
        outs = []
        for t in range(NT):
            dst = xc_out[t * 128:(t + 1) * 128, :] if t < 2 else x_out[(t - 2) * 128:(t - 1) * 128, :]
            outs.append(P.op("sp", (lambda e, t=t, dst=dst: e.dma_start(out=dst, in_=C.X[:, t, :])),
                             reads=[("X", t)], dma=True))
        P.finalize(final_waits=outs)
    nc.mk_inputs = set(dram.keys())
    return nc


def emit_adaln(C, layers):
    P, A, nc = C.P, C.A, C.nc
    P.barrier()
    A.push()
    cc = A.alloc((KC, 2))
    scc = A.alloc((KC, 2))
    wch = [A.alloc((KC, 512)) for _ in range(2)]
    bch = [A.alloc((512,), parts=2) for _ in range(2)]
    rowc = [A.alloc((512,), parts=2) for _ in range(2)]
    psT = C.psum[2]
    P.op("sp", lambda e: e.dma_start(out=cc, in_=C.dram["cc"]), writes=["cc"], dma=True)
    P.op("act", lambda e: e.activation(out=scc, in_=cc, func=AF.Silu), reads=["cc"], writes=["scc"])
    n = 0
    for i in layers:
        w = C.dram["l%d_ada_w" % i].rearrange("(k p) n -> p k n", p=128)
        b = C.dram["l%d_ada_b" % i]
        for cch in range(12):
            s = n % 2
            n += 1
            cs = slice(cch * 512, (cch + 1) * 512)
            P.op("sp", (lambda e, s=s, cs=cs, w=w: e.dma_start(out=wch[s], in_=w[:, :, cs])), writes=[("wch", s)], dma=True)
            P.op("sp", (lambda e, s=s, cs=cs, b=b: e.dma_start(out=bch[s], in_=b[cs].partition_broadcast(2))),
                 writes=[("bch", s)], dma=True)
            ps = C.psum[s]
            for k in range(KC):
                _mm(P, ps[0:2, :], scc[:, k, :], wch[s][:, k, :], k == 0, k == KC - 1,
                    reads=["scc", ("wch", s)], writes=[("bank", s)])
            P.op("dve", (lambda e, s=s, ps=ps: e.tensor_tensor(out=rowc[s], in0=ps[0:2, :], in1=bch[s], op=ALU.add)),
                 reads=[("bank", s), ("bch", s)], writes=[("rowc", s)])
            P.op("sp", (lambda e, s=s, cs=cs, i=i: e.dma_start(out=C.ada_scr[i, :, cs], in_=rowc[s])),
                 reads=[("rowc", s)], writes=[("ada_scr", i)], dma=True)
            for q in range(4):
                ch = cch * 4 + q
                _mm(P, psT[:, ch * 2:(ch + 1) * 2], rowc[s][:, q * 128:(q + 1) * 128], C.ident[0:2, 0:2], True, True,
                    reads=[("rowc", s), "ident"], writes=[("bank", 2)])
        P.op("dve", (lambda e, i=i: e.tensor_copy(out=C.adaT[:, i, :, :], in_=psT[:, 0:96].rearrange("p (c j) -> p c j", j=2))),
             reads=[("bank", 2)], writes=[("adaT", i)])
        for sub, c0 in ((0, 8), (1, 32)):
            P.op("dve", (lambda e, i=i, sub=sub, c0=c0: e.tensor_scalar(
                out=C.sc1p[:, i, sub, :, :], in0=C.adaT[:, i, c0:c0 + 8, :], scalar1=1.0, scalar2=None, op0=ALU.add)),
                reads=[("adaT", i)], writes=[("sc1p", i, sub)])
    A.pop()
    P.barrier()


def emit_build_hT(C, i, sub, need_ctx=True, router=None):
    P, A = C.P, C.A
    shift_c0 = 0 if sub == 0 else 24
    tiles = range(NT) if need_ctx else range(2, NT)
    for t in tiles:
        j = 1 if t < 2 else 0
        s = t % 2
        ps = (C.psum[4 + 2 * s], C.psum[5 + 2 * s])
        for k in range(KC):
            pst = ps[k // 4][:, (k % 4) * 128:(k % 4 + 1) * 128]
            P.op("pe", (lambda e, t=t, k=k, pst=pst: e.transpose(pst, C.X[:, t, k * 128:(k + 1) * 128], C.ident)),
                 reads=[("X", t), "ident"], writes=[("bank", 4 + 2 * s + k // 4)])
        for k in range(KC):
            pst = ps[k // 4][:, (k % 4) * 128:(k % 4 + 1) * 128]
            P.op("dve", (lambda e, t=t, k=k, pst=pst, j=j: e.tensor_scalar(
                out=C.hT[:, k, t * 128:(t + 1) * 128], in0=pst,
                scalar1=C.sc1p[:, i, sub, k, j:j + 1], scalar2=C.adaT[:, i, shift_c0 + k, j:j + 1],
                op0=ALU.mult, op1=ALU.add)),
                reads=[("bank", 4 + 2 * s + k // 4)], writes=[("hT", t, k)])
            if router is not None:
                h32 = router["h32"][s]
                P.op("dve", (lambda e, t=t, k=k, pst=pst, j=j, h32=h32: e.tensor_scalar(
                    out=h32[:, k, :], in0=pst,
                    scalar1=C.sc1p[:, i, sub, k, j:j + 1], scalar2=C.adaT[:, i, shift_c0 + k, j:j + 1],
                    op0=ALU.mult, op1=ALU.add)),
                    reads=[("bank", 4 + 2 * s + k // 4)], writes=[("h32", s, k)])
        if router is not None and not os.environ.get("MK_DBG_NORMM"):
            psl = C.psum[s]
            for k in range(KC):
                _mm(P, psl[:, 0:NEXP], router["h32"][s][:, k, :], router["w"][:, k, :], k == 0, k == KC - 1,
                    reads=[("h32", s, k), "router_w"], writes=[("bank", s)])
            P.op("act", (lambda e, t=t, psl=psl: e.copy(out=router["logits"][:, t, :], in_=psl[:, 0:NEXP])),
                 reads=[("bank", s)], writes=[("logits", t)])


def emit_scale_x(C, need_ctx=True):
    P = C.P
    for t in (range(NT) if need_ctx else range(2, NT)):
        eng = "act" if t % 2 == 0 else "dve"
        if eng == "act":
            P.op("act", (lambda e, t=t: e.activation(out=C.X[:, t, :], in_=C.X[:, t, :], func=AF.Copy, scale=float(ALPHA))),
                 reads=[("X", t)], writes=[("X", t)])
        else:
            P.op("dve", (lambda e, t=t: e.tensor_scalar(out=C.X[:, t, :], in0=C.X[:, t, :], scalar1=float(ALPHA),
                                                        scalar2=None, op0=ALU.mult)),
                 reads=[("X", t)], writes=[("X", t)])


def emit_load_bc(C, i, sub, tiles):
    P = C.P
    gate_c0 = 2 * D if sub == 0 else 5 * D
    srcs = {
        "gx": C.ada_scr[i, 0, gate_c0:gate_c0 + D], "gc": C.ada_scr[i, 1, gate_c0:gate_c0 + D],
        "lg": C.dram["l%d_ln%d_g" % (i, sub + 1)], "lb": C.dram["l%d_ln%d_b" % (i, sub + 1)],
    }
    for n, src in srcs.items():
        P.op("sp", (lambda e, n=n, src=src: e.dma_start(out=tiles[n], in_=src.partition_broadcast(128))),
             reads=[("ada_scr", i)], writes=[("bc", n)], dma=True)


def emit_ln(C, tiles, need_ctx=True):
    P, A = C.P, C.A
    A.push()
    stats = A.alloc((NT, 2, 6))
    mv = A.alloc((NT, 2))
    rstd = A.alloc((NT,))
    nmr = A.alloc((NT,))
    t0 = 0 if need_ctx else 2
    for t in range(t0, NT):
        for hh in range(2):
            P.op("dve", (lambda e, t=t, hh=hh: e.bn_stats(out=stats[:, t, hh, :], in_=C.X[:, t, hh * 512:(hh + 1) * 512])),
                 reads=[("X", t)], writes=[("stats", t, hh)])
        P.op("dve", (lambda e, t=t: e.bn_aggr(out=mv[:, t, :], in_=stats[:, t, :, :])),
             reads=[("stats", t, 0), ("stats", t, 1)], writes=[("mv", t)])
    mvk = [("mv", t) for t in range(t0, NT)]
    P.op("dve", lambda e: e.tensor_scalar(out=rstd[:, t0:NT], in0=mv[:, t0:NT, 1], scalar1=float(LN_EPS), scalar2=None, op0=ALU.add),
         reads=mvk, writes=["rstd"])
    P.op("act", lambda e: e.activation(out=rstd[:, t0:NT], in_=rstd[:, t0:NT], func=AF.Sqrt), reads=["rstd"], writes=["rstd"])
    P.op("dve", lambda e: e.reciprocal(out=rstd[:, t0:NT], in_=rstd[:, t0:NT]), reads=["rstd"], writes=["rstd"])
    P.op("dve", lambda e: e.scalar_tensor_tensor(out=nmr[:, t0:NT], in0=mv[:, t0:NT, 0], scalar=-1.0, in1=rstd[:, t0:NT],
                                                 op0=ALU.mult, op1=ALU.mult), reads=mvk + ["rstd"], writes=["nmr"])
    for t in range(t0, NT):
        P.op("act", (lambda e, t=t: e.activation(out=C.X[:, t, :], in_=C.X[:, t, :], func=AF.Identity,
                                                 bias=nmr[:, t:t + 1], scale=rstd[:, t:t + 1])),
             reads=[("X", t), "rstd", "nmr"], writes=[("X", t)])
        P.op("dve", (lambda e, t=t: e.tensor_tensor(out=C.X[:, t, :], in0=C.X[:, t, :], in1=tiles["lg"], op=ALU.mult)),
             reads=[("X", t), ("bc", "lg")], writes=[("X", t)])
        P.op("pool" if t % 2 == 0 else "dve", (lambda e, t=t: e.tensor_tensor(out=C.X[:, t, :], in0=C.X[:, t, :], in1=tiles["lb"], op=ALU.add)),
             reads=[("X", t), ("bc", "lb")], writes=[("X", t)])
    A.pop()


def hT_keys(t0, n):
    return [("hT", t, k) for t in range(t0, t0 + n) for k in range(KC)]


def emit_ffn(C, i):
    P, A, nc = C.P, C.A, C.nc
    moe = (i % 2 == 1)
    need_ctx = i < DEPTH - 1
    P.barrier()
    A.push()
    bc = {n: A.alloc((D,)) for n in ("gx", "gc", "lg", "lb")}
    emit_load_bc(C, i, 1, bc)
    router = None
    if moe and not os.environ.get("MK_DBG_NOROUTER"):
        router = {
            "h32": [A.alloc((KC, 128)) for _ in range(2)],
            "w": A.alloc((KC, NEXP)),
            "logits": A.alloc((NT, NEXP)),
        }
        rw = C.dram["l%d_moe_router" % i].rearrange("(k p) e -> p k e", p=128)
        P.op("sp", lambda e: e.dma_start(out=router["w"], in_=rw), writes=["router_w"], dma=True)
    emit_build_hT(C, i, 1, need_ctx=need_ctx, router=router)
    emit_scale_x(C, need_ctx=need_ctx)
    gates = None
    if moe and not os.environ.get("MK_DBG_NOGATES"):
        gates = emit_gates(C, router, need_ctx)
    if moe:
        wgu = C.dram["l%d_moe_w_gu" % i]
        wdn = C.dram["l%d_moe_w_down" % i]
        blocks = [(e, j) for e in range(NEXP_RUN) for j in range(FFN // 512)]
    else:
        wgu = C.dram["l%d_ffn_w_gu" % i]
        wdn = C.dram["l%d_ffn_w_down" % i]
        blocks = [(None, j) for j in range(FFN // 512)]
    if os.environ.get("MK_DBG_NBLK"):
        blocks = blocks[:int(os.environ["MK_DBG_NBLK"])]
    Wg = [A.alloc((KC, 512), BF16) for _ in range(2)]
    Wu = [A.alloc((KC, 512), BF16) for _ in range(2)]
    Wd = [A.alloc((4, D), BF16) for _ in range(2)]
    act = [A.alloc((4, 512), BF16) for _ in range(2)]
    sg = [A.alloc((512,)) for _ in range(2)]
    tmp = [A.alloc((D,)) for _ in range(2)]
    groups = GROUPS if need_ctx else GROUPS[1:]

    def load_block(bi):
        e_, j = blocks[bi]
        s = bi % 2
        gu = (wgu[e_] if moe else wgu).rearrange("(k p) n -> p k n", p=128)
        dn = (wdn[e_] if moe else wdn)[j * 512:(j + 1) * 512, :].rearrange("(f p) n -> p f n", p=128)
        P.op("pool", (lambda e: e.dma_start(out=Wg[s], in_=gu[:, :, j * 512:(j + 1) * 512])), writes=[("Wg", s)], dma=True)
        if os.environ.get("MK_DBG_SKIPW") and bi > 1:
            return
        P.op("pool", (lambda e: e.dma_start(out=Wu[s], in_=gu[:, :, FFN + j * 512:FFN + (j + 1) * 512])),
             writes=[("Wu", s)], dma=True)
        P.op("pool", (lambda e: e.dma_start(out=Wd[s], in_=dn)), writes=[("Wd", s)], dma=True)

    acc_eng = os.environ.get("MK_ACC_ENG", "pool")
    items = [(bi, gi) for bi in range(len(blocks)) for gi in range(len(groups))]

    def emit_gu(n):
        bi, gi = items[n]
        s = bi % 2
        if gi == 0 and bi + 1 < len(blocks):
            load_block(bi + 1)
        (t0, ntile) = groups[gi]
        ntok = ntile * 128
        a = n % 2
        for fb in range(4):
            pg = C.psum[(fb % 2) * 2]
            pu = C.psum[(fb % 2) * 2 + 1]
            for k in range(KC):
                _mm(P, pg[:, 0:ntok], Wg[s][:, k, fb * 128:(fb + 1) * 128], C.hT[:, k, t0 * 128:t0 * 128 + ntok],
                    k == 0, k == KC - 1, reads=([("Wg", s)] + hT_keys(t0, ntile)) if k in (0, KC - 1) else [], writes=[("bank", (fb % 2) * 2)])
            for k in range(KC):
                _mm(P, pu[:, 0:ntok], Wu[s][:, k, fb * 128:(fb + 1) * 128], C.hT[:, k, t0 * 128:t0 * 128 + ntok],
                    k == 0, k == KC - 1, reads=([("Wu", s)] + hT_keys(t0, ntile)) if k in (0, KC - 1) else [], writes=[("bank", (fb % 2) * 2 + 1)])
            sgt = sg[fb % 2]
            P.op("act", (lambda e, pg=pg, sgt=sgt: e.activation(out=sgt[:, 0:ntok], in_=pg[:, 0:ntok], func=AF.Silu)),
                 reads=[("bank", (fb % 2) * 2)], writes=[("sg", fb % 2)])
            P.op("dve", (lambda e, pu=pu, sgt=sgt, fb=fb: e.tensor_tensor(
                out=act[a][:, fb, 0:ntok], in0=pu[:, 0:ntok], in1=sgt[:, 0:ntok], op=ALU.mult)),
                reads=[("bank", (fb % 2) * 2 + 1), ("sg", fb % 2)], writes=[("act", a, fb)])

    ocnt = [0]

    def emit_down(n):
        bi, gi = items[n]
        s = bi % 2
        e_, j = blocks[bi]
        (t0, ntile) = groups[gi]
        a = n % 2
        for tt in range(ntile):
            t = t0 + tt
            o = ocnt[0] % 2
            ocnt[0] += 1
            po = (C.psum[4 + 2 * o], C.psum[5 + 2 * o])
            for nh in range(2):
                for fb in range(4):
                    _mm(P, po[nh][:, :], act[a][:, fb, tt * 128:(tt + 1) * 128], Wd[s][:, fb, nh * 512:(nh + 1) * 512],
                        fb == 0, fb == 3, reads=[("act", a, fb), ("Wd", s)], writes=[("bank", 4 + 2 * o + nh)])
            gbc = bc["gc"] if t < 2 else bc["gx"]
            tm = tmp[o]
            for nh in range(2):
                if moe and gates is not None:
                    P.op("dve", (lambda e, po=po, nh=nh, tm=tm, gbc=gbc, t=t, e_=e_: e.scalar_tensor_tensor(
                        out=tm[:, nh * 512:(nh + 1) * 512], in0=po[nh][:, :], scalar=gates[:, t, e_:e_ + 1],
                        in1=gbc[:, nh * 512:(nh + 1) * 512], op0=ALU.mult, op1=ALU.mult)),
                        reads=[("bank", 4 + 2 * o + nh), ("bc", "gx"), ("bc", "gc"), "gates"], writes=[("tmp", o, nh)])
                else:
                    P.op("dve", (lambda e, po=po, nh=nh, tm=tm, gbc=gbc: e.tensor_tensor(
                        out=tm[:, nh * 512:(nh + 1) * 512], in0=po[nh][:, :], in1=gbc[:, nh * 512:(nh + 1) * 512], op=ALU.mult)),
                        reads=[("bank", 4 + 2 * o + nh), ("bc", "gx"), ("bc", "gc")], writes=[("tmp", o, nh)])
            eng = acc_eng if acc_eng != "alt" else ("dve" if ocnt[0] % 2 else "pool")
            P.op(eng, (lambda e, t=t, tm=tm: e.tensor_tensor(out=C.X[:, t, :], in0=C.X[:, t, :], in1=tm, op=ALU.add)),
                 reads=[("X", t), ("tmp", o, 0), ("tmp", o, 1)], writes=[("X", t)])

    if blocks:
        load_block(0)
    pipelined = bool(os.environ.get("MK_PIPE"))
    if pipelined and items:
        emit_gu(0)
        for n in range(len(items)):
            if n + 1 < len(items):
                emit_gu(n + 1)
            emit_down(n)
    else:
        for n in range(len(items)):
            emit_gu(n)
            emit_down(n)
    emit_ln(C, bc, need_ctx=need_ctx)
    A.pop()
    P.barrier()


def emit_gates(C, router, need_ctx):
    P, A = C.P, C.A
    L = router["logits"]
    t0 = 0 if need_ctx else 2
    n = NT - t0
    Lv = L[:, t0:NT, :]
    gates = A.alloc((NT, NEXP))
    m1 = A.alloc((NT,))
    m2 = A.alloc((NT,))
    mk1 = A.alloc((NT, NEXP))
    mk2 = A.alloc((NT, NEXP))
    l2 = A.alloc((NT, NEXP))
    w1 = A.alloc((NT,))
    w2 = A.alloc((NT,))
    lk = [("logits", t) for t in range(t0, NT)]

    def bcast(v):
        return v[:, t0:NT][:, :, None].broadcast_to([128, n, NEXP])

    P.op("dve", lambda e: e.tensor_reduce(out=m1[:, t0:NT], in_=Lv, axis=AX.X, op=ALU.max), reads=lk, writes=["m1"])
    P.op("dve", lambda e: e.tensor_tensor(out=mk1[:, t0:NT, :], in0=Lv, in1=bcast(m1), op=ALU.is_equal), reads=lk + ["m1"], writes=["mk1"])
    P.op("dve", lambda e: e.scalar_tensor_tensor(out=l2[:, t0:NT, :], in0=mk1[:, t0:NT, :], scalar=-1e30, in1=Lv,
                                                 op0=ALU.mult, op1=ALU.add), reads=lk + ["mk1"], writes=["l2"])
    P.op("dve", lambda e: e.tensor_reduce(out=m2[:, t0:NT], in_=l2[:, t0:NT, :], axis=AX.X, op=ALU.max), reads=["l2"], writes=["m2"])
    P.op("dve", lambda e: e.tensor_tensor(out=mk2[:, t0:NT, :], in0=l2[:, t0:NT, :], in1=bcast(m2), op=ALU.is_equal),
         reads=["l2", "m2"], writes=["mk2"])
    P.op("dve", lambda e: e.tensor_tensor(out=w2[:, t0:NT], in0=m2[:, t0:NT], in1=m1[:, t0:NT], op=ALU.subtract),
         reads=["m1", "m2"], writes=["w2"])
    P.op("act", lambda e: e.activation(out=w2[:, t0:NT], in_=w2[:, t0:NT], func=AF.Exp), reads=["w2"], writes=["w2"])
    P.op("dve", lambda e: e.tensor_scalar(out=w1[:, t0:NT], in0=w2[:, t0:NT], scalar1=1.0, scalar2=None, op0=ALU.add),
         reads=["w2"], writes=["w1"])
    P.op("dve", lambda e: e.reciprocal(out=w1[:, t0:NT], in_=w1[:, t0:NT]), reads=["w1"], writes=["w1"])
    P.op("dve", lambda e: e.tensor_tensor(out=w2[:, t0:NT], in0=w2[:, t0:NT], in1=w1[:, t0:NT], op=ALU.mult),
         reads=["w1", "w2"], writes=["w2"])
    P.op("dve", lambda e: e.tensor_tensor(out=mk1[:, t0:NT, :], in0=mk1[:, t0:NT, :], in1=bcast(w1), op=ALU.mult),
         reads=["mk1", "w1"], writes=["mk1"])
    P.op("dve", lambda e: e.tensor_tensor(out=mk2[:, t0:NT, :], in0=mk2[:, t0:NT, :], in1=bcast(w2), op=ALU.mult),
         reads=["mk2", "w2"], writes=["mk2"])
    P.op("dve", lambda e: e.tensor_tensor(out=gates[:, t0:NT, :], in0=mk1[:, t0:NT, :], in1=mk2[:, t0:NT, :], op=ALU.add),
         reads=["mk1", "mk2"], writes=["gates"])
    return gates


def emit_mixer(C, i):
    kind = i % 3
    if kind == 0:
        emit_attention(C, i)
    elif kind == 1:
        emit_cmlp(C, i)
    else:
        emit_retention(C, i)


PAIR_GROUPS = [[0, 1], [2, 3], [4, 5], [6, 7]]


def emit_rot_copy(P, dst, src, half, rkey, wkey):
    n = src.shape[-1]
    for b0 in range(0, n, 2 * half):
        P.op("pool", (lambda e, b0=b0: e.tensor_copy(out=dst[:, :, b0:b0 + half], in_=src[:, :, b0 + half:b0 + 2 * half])),
             reads=[rkey], writes=[wkey])
        P.op("pool", (lambda e, b0=b0: e.tensor_copy(out=dst[:, :, b0 + half:b0 + 2 * half], in_=src[:, :, b0:b0 + half])),
             reads=[rkey], writes=[wkey])


def emit_load_gain(C, dst, dst_rot, src, half, scale, key):
    P = C.P
    col = src.rearrange("(p o) -> p o", o=1)
    P.op("sp", lambda e: e.dma_start(out=dst, in_=col), writes=[key, "gains"], dma=True)
    for b0 in range(0, 128, 2 * half):
        P.op("sp", (lambda e, b0=b0: e.dma_start(out=dst_rot[b0:b0 + half, :], in_=col[b0 + half:b0 + 2 * half, :])),
             writes=[key + "_r", "gains"], dma=True)
        P.op("sp", (lambda e, b0=b0: e.dma_start(out=dst_rot[b0 + half:b0 + 2 * half, :], in_=col[b0:b0 + half, :])),
             writes=[key + "_r", "gains"], dma=True)
    if scale != 1.0:
        P.op("dve", lambda e: e.tensor_scalar(out=dst, in0=dst, scalar1=float(scale), scalar2=None, op0=ALU.mult),
             reads=[key], writes=[key, "gains"])
        P.op("dve", lambda e: e.tensor_scalar(out=dst_rot, in0=dst_rot, scalar1=float(scale), scalar2=None, op0=ALU.mult),
             reads=[key + "_r"], writes=[key + "_r", "gains"])


def emit_qk_norm_rope(C, ps_q, ps_qr, ps_ss, ntok, out_bf, gain, gain_r, cos, sin, tmp, eps_t, inv_d, tag, rope, out_key):
    P = C.P
    sq, rstd, ta, tb = tmp
    kq, kqr, kss = ("bank", ps_q[1]), ("bank", ps_qr[1]), ("bank", ps_ss[1])
    pq, pqr, pss = ps_q[0], ps_qr[0], ps_ss[0]
    P.op("act", lambda e: e.activation(out=sq[:, 0:ntok], in_=pq[:, 0:ntok], func=AF.Square), reads=[kq], writes=[tag + "sq"])
    _mm(P, pss[:, 0:ntok], C.ones_bf2, sq[:, 0:ntok], True, True, reads=[tag + "sq", "ones_bf"], writes=[kss])
    P.op("dve", lambda e: e.tensor_scalar(out=rstd[:, 0:ntok], in0=pss[:, 0:ntok], scalar1=float(inv_d), scalar2=float(RMS_EPS),
                                          op0=ALU.mult, op1=ALU.add), reads=[kss], writes=[tag + "rstd"])
    P.op("act", lambda e: e.activation(out=rstd[:, 0:ntok], in_=rstd[:, 0:ntok], func=AF.Ln),
         reads=[tag + "rstd"], writes=[tag + "rstd"])
    P.op("act", lambda e: e.activation(out=rstd[:, 0:ntok], in_=rstd[:, 0:ntok], func=AF.Exp, scale=-0.5),
         reads=[tag + "rstd"], writes=[tag + "rstd"])
    if not rope:
        P.op("dve", lambda e: e.scalar_tensor_tensor(out=out_bf, in0=pq[:, 0:ntok], scalar=gain, in1=rstd[:, 0:ntok],
                                                     op0=ALU.mult, op1=ALU.mult),
             reads=[kq, tag + "rstd", "gains"], writes=[out_key])
        return
    P.op("dve", lambda e: e.scalar_tensor_tensor(out=ta[:, 0:ntok], in0=pq[:, 0:ntok], scalar=gain, in1=rstd[:, 0:ntok],
                                                 op0=ALU.mult, op1=ALU.mult), reads=[kq, tag + "rstd", "gains"], writes=[tag + "ta"])
    P.op("dve", lambda e: e.scalar_tensor_tensor(out=tb[:, 0:ntok], in0=pqr[:, 0:ntok], scalar=gain_r, in1=rstd[:, 0:ntok],
                                                 op0=ALU.mult, op1=ALU.mult), reads=[kqr, tag + "rstd", "gains"], writes=[tag + "tb"])
    P.op("pool", lambda e: e.tensor_tensor(out=ta[:, 0:ntok], in0=ta[:, 0:ntok], in1=cos, op=ALU.mult),
         reads=[tag + "ta", "rope"], writes=[tag + "ta"])
    P.op("pool", lambda e: e.tensor_tensor(out=tb[:, 0:ntok], in0=tb[:, 0:ntok], in1=sin, op=ALU.mult),
         reads=[tag + "tb", "rope"], writes=[tag + "tb"])
    P.op("dve", lambda e: e.tensor_tensor(out=out_bf, in0=ta[:, 0:ntok], in1=tb[:, 0:ntok], op=ALU.add),
         reads=[tag + "ta", tag + "tb"], writes=[out_key])


def emit_attention(C, i):
    P, A, nc = C.P, C.A, C.nc
    need_ctx = i < DEPTH - 1
    wqkv = C.dram["l%d_attn_wqkv" % i].rearrange("(k p) n -> p k n", p=128)
    wo = C.dram["l%d_attn_wo" % i]
    P.barrier()
    A.push()
    gx = A.alloc((D,))
    gc = A.alloc((D,))
    bc = {"gx": gx, "gc": gc}
    gate_c0 = 2 * D
    P.op("sp", lambda e: e.dma_start(out=gx, in_=C.ada_scr[i, 0, gate_c0:gate_c0 + D].partition_broadcast(128)), writes=[("bc", "gx")], dma=True)
    P.op("sp", lambda e: e.dma_start(out=gc, in_=C.ada_scr[i, 1, gate_c0:gate_c0 + D].partition_broadcast(128)), writes=[("bc", "gc")], dma=True)
    emit_build_hT(C, i, 0, need_ctx=True)
    emit_scale_x(C, need_ctx=need_ctx)

    rope = A.alloc((2, TOK))
    P.op("sp", lambda e: e.dma_start(out=rope, in_=C.dram["rope_a"]), writes=["rope"], dma=True)
    Kall = A.alloc((2, CTX + SEQ), BF16)
    Vall = A.alloc((34, 256), BF16)
    gk, gkr, gq, gqr, eps_t = (A.alloc((1,)) for _ in range(5))
    emit_load_gain(C, gk, gkr, C.dram["l%d_attn_k_norm" % i], 32, 1.0, "gk")
    emit_load_gain(C, gq, gqr, C.dram["l%d_attn_q_norm" % i], 32, 128 ** -0.5, "gq")
    P.op("pool", lambda e: e.memset(eps_t, float(RMS_EPS)), writes=["eps"])
    tmp = (A.alloc((512,), BF16), A.alloc((512,)), A.alloc((512,)), A.alloc((512,)))
    bank = lambda n: (C.psum[n], n)

    A.push()
    Wk = A.alloc((KC, 256), BF16)
    Wkr = A.alloc((KC, 256), BF16)
    Wv = A.alloc((KC, 256), BF16)
    Xo = A.alloc((D,))
    hTo = A.alloc((KC, 512), BF16)
    rope_o = A.alloc((2, 512))
    P.op("pool", lambda e: e.dma_start(out=Wk, in_=wqkv[:, :, 1024:1280]), writes=["Wk"], dma=True)
    P.op("pool", lambda e: e.dma_start(out=Wv, in_=wqkv[:, :, 1280:1536]), writes=["Wv"], dma=True)
    emit_rot_copy(P, Wkr, Wk, 32, "Wk", "Wkr")

    def kproj(src, c0, ntok, hkeys, out, cos, sin, isctx, hk, okey):
        for k in range(KC):
            _mm(P, C.psum[0][:, 0:ntok], Wk[:, k, hk * 128:(hk + 1) * 128], src[:, k, c0:c0 + ntok],
                k == 0, k == KC - 1, reads=["Wk"] + hkeys, writes=[("bank", 0)])
        if not isctx:
            for k in range(KC):
                _mm(P, C.psum[1][:, 0:ntok], Wkr[:, k, hk * 128:(hk + 1) * 128], src[:, k, c0:c0 + ntok],
                    k == 0, k == KC - 1, reads=["Wkr"] + hkeys, writes=[("bank", 1)])
        emit_qk_norm_rope(C, bank(0), bank(1), bank(2), ntok, out, gk, gkr, cos, sin, tmp, eps_t, 1.0 / 128, "k",
                          not isctx, okey)

    def vproj(src, c0, hkeys, dst, okey, pb):
        pv = C.psum[pb]
        for k in range(KC):
            _mm(P, pv[:, 0:256], src[:, k, c0:c0 + 128], Wv[:, k, :], k == 0, k == KC - 1,
                reads=["Wv"] + hkeys, writes=[("bank", pb)])
        P.op("act", (lambda e: e.copy(out=dst, in_=pv[:, 0:256])), reads=[("bank", pb)], writes=[okey])

    for (t0, ntile) in GROUPS:
        ntok = ntile * 128
        isctx = t0 < 2
        for hk in range(2):
            if isctx:
                kproj(C.hT, 0, ntok, hT_keys(t0, ntile), Kall[:, hk, 0:CTX], None, None, True, hk, ("Kall", hk, "c"))
            else:
                c0 = (t0 - 2) * 128
                kproj(C.hT, t0 * 128, ntok, hT_keys(t0, ntile), Kall[:, hk, CTX + c0:CTX + c0 + ntok],
                      rope[:, 0, c0:c0 + ntok], rope[:, 1, c0:c0 + ntok], False, hk, ("Kall", hk, t0))
        for tt in range(ntile):
            t = t0 + tt
            vproj(C.hT, t * 128, hT_keys(t, 1), Vall[:, t, :], ("Vall", t), 4 + t % 2)
    xoth = C.dram["x_oth"]
    for g in range(4):
        P.op("sp", (lambda e, g=g: e.dma_start(out=rope_o, in_=C.dram["rope_o"][:, :, g * 512:(g + 1) * 512])),
             writes=["rope_o"], dma=True)
        for tt in range(4):
            tg = g * 4 + tt
            P.op("sp", (lambda e, tg=tg: e.dma_start(out=Xo, in_=xoth[tg * 128:(tg + 1) * 128, :])), writes=["Xo"], dma=True)
            for k in range(KC):
                pst = C.psum[6 + k // 4][:, (k % 4) * 128:(k % 4 + 1) * 128]
                P.op("pe", (lambda e, k=k, pst=pst: e.transpose(pst, Xo[:, k * 128:(k + 1) * 128], C.ident)),
                     reads=["Xo", "ident"], writes=[("bank", 6 + k // 4)])
            for k in range(KC):
                pst = C.psum[6 + k // 4][:, (k % 4) * 128:(k % 4 + 1) * 128]
                P.op("dve", (lambda e, k=k, pst=pst, tt=tt: e.tensor_scalar(
                    out=hTo[:, k, tt * 128:(tt + 1) * 128], in0=pst,
                    scalar1=C.sc1p[:, i, 0, k, 0:1], scalar2=C.adaT[:, i, k, 0:1], op0=ALU.mult, op1=ALU.add)),
                    reads=[("bank", 6 + k // 4)], writes=[("hTo", tt, k)])
        hk_o = [("hTo", tt, k) for tt in range(4) for k in range(KC)]
        c0 = CTX + TOK + g * 512
        for hk in range(2):
            kproj(hTo, 0, 512, hk_o, Kall[:, hk, c0:c0 + 512], rope_o[:, 0, :], rope_o[:, 1, :], False, hk, ("Kall", hk, "o", g))
        for tt in range(4):
            vproj(hTo, tt * 128, [("hTo", tt, k) for k in range(KC)], Vall[:, 18 + g * 4 + tt, :], ("Vall", 18 + g * 4 + tt), 4 + tt % 2)
    A.pop()
    P.barrier()

    Wq = [A.alloc((KC, 128), BF16) for _ in range(2)]
    Wqr = [A.alloc((KC, 128), BF16) for _ in range(2)]
    Wo = [A.alloc((D,), BF16) for _ in range(2)]
    qT = [A.alloc((512,), BF16) for _ in range(2)]
    PT = [A.alloc((512,), BF16) for _ in range(3)]
    oT = [A.alloc((512,), BF16) for _ in range(2)]
    rden = A.alloc((512,))
    ytmp = [A.alloc((D,)) for _ in range(2)]
    groups = GROUPS if need_ctx else GROUPS[1:]

    def load_head(h):
        s = h % 2
        P.op("pool", lambda e: e.dma_start(out=Wq[s], in_=wqkv[:, :, h * 128:(h + 1) * 128]), writes=[("Wq", s)], dma=True)
        P.op("pool", lambda e: e.dma_start(out=Wo[s], in_=wo[h * 128:(h + 1) * 128, :]), writes=[("Wo", s)], dma=True)
        emit_rot_copy(P, Wqr[s], Wq[s], 32, ("Wq", s), ("Wqr", s))

    items = [(h, gi) for h in range(8) for gi in range(len(groups))]
    SB = [4, 5, 6]
    LOOK = 2
    ycnt = [0]

    def qproj(n):
        h, gi = items[n]
        s = h % 2
        (t0, ntile) = groups[gi]
        ntok = ntile * 128
        isctx = t0 < 2
        a = n % 2
        for k in range(KC):
            _mm(P, C.psum[0][:, 0:ntok], Wq[s][:, k, :], C.hT[:, k, t0 * 128:t0 * 128 + ntok],
                k == 0, k == KC - 1, reads=[("Wq", s)] + hT_keys(t0, ntile), writes=[("bank", 0)])
        if not isctx:
            for k in range(KC):
                _mm(P, C.psum[1][:, 0:ntok], Wqr[s][:, k, :], C.hT[:, k, t0 * 128:t0 * 128 + ntok],
                    k == 0, k == KC - 1, reads=[("Wqr", s)] + hT_keys(t0, ntile), writes=[("bank", 1)])
            c0 = (t0 - 2) * 128
            emit_qk_norm_rope(C, bank(0), bank(1), bank(2), ntok, qT[a][:, 0:ntok], gq, gqr, rope[:, 0, c0:c0 + ntok],
                              rope[:, 1, c0:c0 + ntok], tmp, eps_t, 1.0 / 128, "q", True, ("qT", a))
        else:
            emit_qk_norm_rope(C, bank(0), bank(1), bank(2), ntok, qT[a][:, 0:ntok], gq, gqr, None, None, tmp, eps_t,
                              1.0 / 128, "q", False, ("qT", a))

    def keyloop(n):
        h, gi = items[n]
        hk = h // 4
        (t0, ntile) = groups[gi]
        ntok = ntile * 128
        isctx = t0 < 2
        a = n % 2
        nkt = 2 if isctx else 34

        def qk(kt):
            sb = SB[kt % 3]
            pp = kt % 3
            _mm(P, C.psum[sb][:, 0:ntok], Kall[:, hk, kt * 128:(kt + 1) * 128], qT[a][:, 0:ntok], True, True,
                reads=["Kall", ("qT", a)], writes=[("bank", sb)])
            P.op("act", (lambda e: e.activation(out=PT[pp][:, 0:ntok], in_=C.psum[sb][:, 0:ntok], func=AF.Exp)),
                 reads=[("bank", sb)], writes=[("PT", pp)])

        def pv(kt):
            pp = kt % 3
            _mm(P, C.psum[3][:, 0:ntok], Vall[:, kt, hk * 128:(hk + 1) * 128], PT[pp][:, 0:ntok], kt == 0, kt == nkt - 1,
                reads=["Vall", ("PT", pp)], writes=[("bank", 3)])
            _mm(P, C.psum[2][:, 0:ntok], C.ones_bf2, PT[pp][:, 0:ntok], kt == 0, kt == nkt - 1,
                reads=["ones_bf", ("PT", pp)], writes=[("bank", 2)])

        for kt in range(min(LOOK, nkt)):
            qk(kt)
        for kt in range(nkt):
            if kt + LOOK < nkt:
                qk(kt + LOOK)
            pv(kt)
        P.op("dve", (lambda e: e.reciprocal(out=rden[:, 0:ntok], in_=C.psum[2][:, 0:ntok])),
             reads=[("bank", 2)], writes=["rden"])
        P.op("dve", (lambda e: e.tensor_tensor(out=oT[a][:, 0:ntok], in0=C.psum[3][:, 0:ntok], in1=rden[:, 0:ntok], op=ALU.mult)),
             reads=[("bank", 3), "rden"], writes=[("oT", a)])

    def yproj(n):
        h, gi = items[n]
        s = h % 2
        (t0, ntile) = groups[gi]
        a = n % 2
        for tt in range(ntile):
            t = t0 + tt
            o = ycnt[0] % 2
            ycnt[0] += 1
            gbc = gc if t < 2 else gx
            yb = (7, 0) if o == 0 else (1, 2)
            for nh in range(2):
                _mm(P, C.psum[yb[nh]][:, :], oT[a][:, tt * 128:(tt + 1) * 128], Wo[s][:, nh * 512:(nh + 1) * 512], True, True,
                    reads=[("oT", a), ("Wo", s)], writes=[("bank", yb[nh])])
                P.op("dve", (lambda e, nh=nh, o=o, gbc=gbc, yb=yb: e.tensor_tensor(
                    out=ytmp[o][:, nh * 512:(nh + 1) * 512], in0=C.psum[yb[nh]][:, :], in1=gbc[:, nh * 512:(nh + 1) * 512], op=ALU.mult)),
                    reads=[("bank", yb[nh]), ("bc", "gx"), ("bc", "gc")], writes=[("ytmp", o, nh)])
            P.op("pool" if o == 0 else "dve", (lambda e, t=t, o=o: e.tensor_tensor(out=C.X[:, t, :], in0=C.X[:, t, :], in1=ytmp[o], op=ALU.add)),
                 reads=[("X", t), ("ytmp", o, 0), ("ytmp", o, 1)], writes=[("X", t)])

    load_head(0)
    load_head(1)
    qproj(0)
    for n in range(len(items)):
        keyloop(n)
        if n + 1 < len(items):
            qproj(n + 1)
        yproj(n)
        h_, gi_ = items[n]
        if gi_ == len(groups) - 1 and h_ + 2 < 8:
            load_head(h_ + 2)
    A.pop()
    P.barrier()
    A.push()
    bc2 = {"lg": A.alloc((D,)), "lb": A.alloc((D,))}
    for n, src in (("lg", C.dram["l%d_ln1_g" % i]), ("lb", C.dram["l%d_ln1_b" % i])):
        P.op("sp", (lambda e, n=n, src=src: e.dma_start(out=bc2[n], in_=src.partition_broadcast(128))), writes=[("bc", n)], dma=True)
    emit_ln(C, bc2, need_ctx=need_ctx)
    A.pop()
    P.barrier()


def emit_ln_bc(C, i, sub, need_ctx):
    P, A = C.P, C.A
    P.barrier()
    A.push()
    bc2 = {"lg": A.alloc((D,)), "lb": A.alloc((D,))}
    for n, src in (("lg", C.dram["l%d_ln%d_g" % (i, sub + 1)]), ("lb", C.dram["l%d_ln%d_b" % (i, sub + 1)])):
        P.op("sp", (lambda e, n=n, src=src: e.dma_start(out=bc2[n], in_=src.partition_broadcast(128))), writes=[("bc", n)], dma=True)
    emit_ln(C, bc2, need_ctx=need_ctx)
    A.pop()
    P.barrier()


def emit_cmlp(C, i):
    P, A, nc = C.P, C.A, C.nc
    need_ctx = i < DEPTH - 1
    pre = "l%d_cmlp_" % i
    w_in = C.dram[pre + "w_in"].rearrange("(k p) n -> p k n", p=128)
    w_out = C.dram[pre + "w_out"].rearrange("(c p) n -> p c n", p=128)
    P.barrier()
    A.push()
    gx = A.alloc((D,))
    gc = A.alloc((D,))
    gate_c0 = 2 * D
    P.op("sp", lambda e: e.dma_start(out=gx, in_=C.ada_scr[i, 0, gate_c0:gate_c0 + D].partition_broadcast(128)), writes=[("bc", "gx")], dma=True)
    P.op("sp", lambda e: e.dma_start(out=gc, in_=C.ada_scr[i, 1, gate_c0:gate_c0 + D].partition_broadcast(128)), writes=[("bc", "gc")], dma=True)
    emit_build_hT(C, i, 0, need_ctx=need_ctx)
    emit_scale_x(C, need_ctx=need_ctx)
    gv, bv, bu = A.alloc((16,)), A.alloc((16,)), A.alloc((16,))
    P.op("sp", lambda e: e.dma_start(out=gv, in_=C.dram[pre + "v_norm_g"].rearrange("(c p) -> p c", p=128), allow_slow_non_contiguous=True), writes=["gv"], dma=True)
    P.op("sp", lambda e: e.dma_start(out=bv, in_=C.dram[pre + "v_norm_b"].rearrange("(c p) -> p c", p=128), allow_slow_non_contiguous=True), writes=["bv"], dma=True)
    P.op("sp", lambda e: e.dma_start(out=bu, in_=C.dram[pre + "b_in"][0:2048].rearrange("(c p) -> p c", p=128), allow_slow_non_contiguous=True), writes=["bu"], dma=True)
    brow_v = A.alloc((2048,), BF16, parts=1)
    brow_o = A.alloc((D,), BF16, parts=1)
    P.op("pool", lambda e: e.dma_start(out=brow_v, in_=C.dram[pre + "b_in"][2048:4096].rearrange("(o n) -> o n", o=1)), writes=["brow_v"], dma=True)
    P.op("pool", lambda e: e.dma_start(out=brow_o, in_=C.dram[pre + "b_out"].rearrange("(o n) -> o n", o=1)), writes=["brow_o"], dma=True)
    WsT = A.alloc((8, 128), BF16)
    Bt = A.alloc((16, 128))
    ones_row = C.ones_bf2[0:1, :]
    A.push()
    Wsl = [A.alloc((128,)) for _ in range(2)]
    bsbc = A.alloc((8, 128))
    for g in range(8):
        s = g % 2
        P.op("sp", (lambda e, g=g, s=s: e.dma_start(out=Wsl[s], in_=C.dram[pre + "w_s"][g])), writes=[("Wsl", s)], dma=True)
        P.op("sp", (lambda e, g=g: e.dma_start(out=bsbc[:, g, :], in_=C.dram[pre + "b_s"][g].partition_broadcast(128))), writes=[("bsbc", g)], dma=True)
        P.op("pe", (lambda e, s=s: e.transpose(C.psum[s][:, 0:128], Wsl[s], C.ident)), reads=[("Wsl", s), "ident"], writes=[("bank", s)])
        P.op("act", (lambda e, g=g, s=s: e.copy(out=WsT[:, g, :], in_=C.psum[s][:, 0:128])), reads=[("bank", s)], writes=[("WsT", g)])
        _mm(P, C.psum[2 + s][:, 0:128], C.ones_bf2, WsT[:, g, :], True, True, reads=["ones_bf", ("WsT", g)], writes=[("bank", 2 + s)])
        for cb in (2 * g, 2 * g + 1):
            P.op("dve", (lambda e, g=g, s=s, cb=cb: e.scalar_tensor_tensor(
                out=Bt[:, cb, :], in0=C.psum[2 + s][:, 0:128], scalar=bv[:, cb:cb + 1], in1=bsbc[:, g, :], op0=ALU.mult, op1=ALU.add)),
                reads=[("bank", 2 + s), "bv", ("bsbc", g)], writes=[("Bt", cb)])
    A.pop()
    P.barrier()
    Wv = A.alloc((KC, 512), BF16)
    Wu = A.alloc((KC, 256), BF16)
    Wo = A.alloc((16, 512), BF16)
    z = A.alloc((4, 2048), BF16)
    uvT = A.alloc((16, 512), BF16)
    uT = A.alloc((512,))
    tmp = A.alloc((512,))
    ytmp = [A.alloc((512,)) for _ in range(2)]
    stats = A.alloc((4, 4, 6))
    mv = A.alloc((4, 2))
    rstd = A.alloc((4,))
    nmr = A.alloc((4,))
    groups = GROUPS if need_ctx else GROUPS[1:]
    ycnt = 0
    for (t0, ntile) in groups:
        ntok = ntile * 128
        for vb in range(4):
            P.op("pool", (lambda e, vb=vb: e.dma_start(out=Wv, in_=w_in[:, :, 2048 + vb * 512:2048 + (vb + 1) * 512])), writes=["Wv"], dma=True)
            for tt in range(ntile):
                t = t0 + tt
                pb = tt % 2
                _mm(P, C.psum[pb][:, :], ones_row, brow_v[:, vb * 512:(vb + 1) * 512], True, False, reads=["ones_bf", "brow_v"], writes=[("bank", pb)])
                for k in range(KC):
                    _mm(P, C.psum[pb][:, :], C.hT[:, k, t * 128:(t + 1) * 128], Wv[:, k, :], False, k == KC - 1,
                        reads=["Wv"] + hT_keys(t, 1), writes=[("bank", pb)])
                P.op("act", (lambda e, tt=tt, vb=vb, pb=pb: e.activation(out=z[:, tt, vb * 512:(vb + 1) * 512], in_=C.psum[pb][:, :], func=AF.Gelu_apprx_tanh)),
                     reads=[("bank", pb)], writes=[("z", tt, vb)])
        for tt in range(ntile):
            for vb in range(4):
                P.op("dve", (lambda e, tt=tt, vb=vb: e.bn_stats(out=stats[:, tt, vb, :], in_=z[:, tt, vb * 512:(vb + 1) * 512])),
                     reads=[("z", tt, vb)], writes=[("zst", tt, vb)])
            P.op("dve", (lambda e, tt=tt: e.bn_aggr(out=mv[:, tt, :], in_=stats[:, tt, :, :])), reads=[("zst", tt, vb) for vb in range(4)], writes=[("zmv", tt)])
        zk = [("zmv", tt) for tt in range(ntile)]
        P.op("dve", (lambda e, ntile=ntile: e.tensor_scalar(out=rstd[:, 0:ntile], in0=mv[:, 0:ntile, 1], scalar1=float(LN_EPS), scalar2=None, op0=ALU.add)),
             reads=zk, writes=["zrstd"])
        P.op("act", (lambda e, ntile=ntile: e.activation(out=rstd[:, 0:ntile], in_=rstd[:, 0:ntile], func=AF.Sqrt)), reads=["zrstd"], writes=["zrstd"])
        P.op("dve", (lambda e, ntile=ntile: e.reciprocal(out=rstd[:, 0:ntile], in_=rstd[:, 0:ntile])), reads=["zrstd"], writes=["zrstd"])
        P.op("dve", (lambda e, ntile=ntile: e.scalar_tensor_tensor(out=nmr[:, 0:ntile], in0=mv[:, 0:ntile, 0], scalar=-1.0, in1=rstd[:, 0:ntile],
                                                                  op0=ALU.mult, op1=ALU.mult)), reads=zk + ["zrstd"], writes=["znmr"])
        for tt in range(ntile):
            P.op("act", (lambda e, tt=tt: e.activation(out=z[:, tt, :], in_=z[:, tt, :], func=AF.Identity, bias=nmr[:, tt:tt + 1], scale=rstd[:, tt:tt + 1])),
                 reads=[("z", tt, vb) for vb in range(4)] + ["zrstd", "znmr"], writes=[("zn", tt)])
        for cb in range(16):
            g = cb // 2
            if cb % 2 == 0:
                P.op("pool", (lambda e, cb=cb: e.dma_start(out=Wu, in_=w_in[:, :, cb * 128:(cb + 2) * 128])), writes=["Wu"], dma=True)
            pu = C.psum[2 + cb % 2]
            ps = C.psum[4 + cb % 2]
            for k in range(KC):
                _mm(P, pu[:, 0:ntok], Wu[:, k, (cb % 2) * 128:(cb % 2 + 1) * 128], C.hT[:, k, t0 * 128:t0 * 128 + ntok],
                    k == 0, k == KC - 1, reads=["Wu"] + hT_keys(t0, ntile), writes=[("bank", 2 + cb % 2)])
            P.op("dve", (lambda e, pu=pu, cb=cb, ntok=ntok: e.tensor_scalar(out=uT[:, 0:ntok], in0=pu[:, 0:ntok], scalar1=bu[:, cb:cb + 1], scalar2=None, op0=ALU.add)),
                 reads=[("bank", 2 + cb % 2), "bu"], writes=["uT"])
            P.op("act", (lambda e, ntok=ntok: e.activation(out=uT[:, 0:ntok], in_=uT[:, 0:ntok], func=AF.Gelu_apprx_tanh)),
                 reads=["uT"], writes=["uT"])
            for tt in range(ntile):
                _mm(P, ps[:, tt * 128:(tt + 1) * 128], z[:, tt, cb * 128:(cb + 1) * 128], WsT[:, g, :], True, True,
                    reads=[("zn", tt), ("WsT", g)], writes=[("bank", 4 + cb % 2)])
            P.op("dve", (lambda e, ps=ps, cb=cb, ntile=ntile, ntok=ntok: e.scalar_tensor_tensor(
                out=tmp[:, 0:ntok].rearrange("p (a b) -> p a b", b=128), in0=ps[:, 0:ntok].rearrange("p (a b) -> p a b", b=128),
                scalar=gv[:, cb:cb + 1], in1=Bt[:, cb:cb + 1, :].broadcast_to([128, ntile, 128]), op0=ALU.mult, op1=ALU.add)),
                reads=[("bank", 4 + cb % 2), "gv", ("Bt", cb)], writes=["cm_tmp"])
            P.op("dve", (lambda e, cb=cb, ntok=ntok: e.tensor_tensor(out=uvT[:, cb, 0:ntok], in0=tmp[:, 0:ntok], in1=uT[:, 0:ntok], op=ALU.mult)),
                 reads=["cm_tmp", "uT"], writes=[("uvT", cb)])
        uk = [("uvT", cb) for cb in range(16)]
        for nh in range(2):
            P.op("pool", (lambda e, nh=nh: e.dma_start(out=Wo, in_=w_out[:, :, nh * 512:(nh + 1) * 512])), writes=["Wo"], dma=True)
            for tt in range(ntile):
                t = t0 + tt
                o = ycnt % 2
                ycnt += 1
                py = C.psum[6 + o]
                gbc = gc if t < 2 else gx
                _mm(P, py[:, :], ones_row, brow_o[:, nh * 512:(nh + 1) * 512], True, False, reads=["ones_bf", "brow_o"], writes=[("bank", 6 + o)])
                for cb in range(16):
                    _mm(P, py[:, :], uvT[:, cb, tt * 128:(tt + 1) * 128], Wo[:, cb, :], False, cb == 15,
                        reads=["Wo"] + (uk if cb in (0, 15) else []), writes=[("bank", 6 + o)])
                P.op("dve", (lambda e, py=py, o=o, gbc=gbc, nh=nh: e.tensor_tensor(out=ytmp[o], in0=py[:, :], in1=gbc[:, nh * 512:(nh + 1) * 512], op=ALU.mult)),
                     reads=[("bank", 6 + o), ("bc", "gx"), ("bc", "gc")], writes=[("ytmp", o)])
                P.op("pool", (lambda e, t=t, o=o, nh=nh: e.tensor_tensor(out=C.X[:, t, nh * 512:(nh + 1) * 512], in0=C.X[:, t, nh * 512:(nh + 1) * 512], in1=ytmp[o], op=ALU.add)),
                     reads=[("X", t), ("ytmp", o)], writes=[("X", t)])
    A.pop()
    emit_ln_bc(C, i, 0, need_ctx)


def emit_retention(C, i):
    P, A, nc = C.P, C.A, C.nc
    need_ctx = i < DEPTH - 1
    w = C.dram["l%d_ret_wqkvg" % i].rearrange("(k p) n -> p k n", p=128)
    wo = C.dram["l%d_ret_wo" % i]
    xacc = C.xacc
    P.barrier()
    A.push()
    gx = A.alloc((D,))
    gc = A.alloc((D,))
    gate_c0 = 2 * D
    P.op("sp", lambda e: e.dma_start(out=gx, in_=C.ada_scr[i, 0, gate_c0:gate_c0 + D].partition_broadcast(128)), writes=[("bc", "gx")], dma=True)
    P.op("sp", lambda e: e.dma_start(out=gc, in_=C.ada_scr[i, 1, gate_c0:gate_c0 + D].partition_broadcast(128)), writes=[("bc", "gc")], dma=True)
    emit_build_hT(C, i, 0, need_ctx=True)
    emit_scale_x(C, need_ctx=True)
    for t in range(NT):
        P.op("sp", (lambda e, t=t: e.dma_start(out=xacc[t * 128:(t + 1) * 128, :], in_=C.X[:, t, :])), reads=[("X", t)], writes=[("xacc", t)], dma=True)
    P.barrier()
    A2 = Arena(C.Xraw, NT * D)
    Kh = A2.alloc((2, CTX + SEQ), BF16)
    Vh = A2.alloc((34, 512), BF16)
    oT32 = A2.alloc((4, 512))
    ogT = A2.alloc((4, 512), BF16)
    Xt = A2.alloc((D,))
    hTo = A.alloc((KC, 512), BF16)
    ropeg = A.alloc((2, 2, 512))
    wreg_top = A.top
    Wreg = A.alloc((6144,))
    qT = [A.alloc((2, 512), BF16) for _ in range(2)]
    PT = [A.alloc((512,), BF16) for _ in range(3)]
    tmp = [A.alloc((512,)) for _ in range(4)]
    sq = A.alloc((4, 512), BF16)
    rstd = A.alloc((512,))
    ytmp = [A.alloc((D,)) for _ in range(2)]
    dec = A.alloc((8,))
    lg = A.alloc((8,))
    nlg = A.alloc((8,))
    lg128 = A.alloc((8,))
    nlg128 = A.alloc((8,))
    dji_i = A.alloc((128,), I32)
    dji = A.alloc((128,))
    mrow_i = A.alloc((40,), I32)
    mrow = A.alloc((40,))
    Ef, Eb, Df, Db, Dd = (A.alloc((128,)) for _ in range(5))
    Tf = A.alloc((4, 128))
    Tb = A.alloc((4, 128))
    pwf, pwb, npwb = A.alloc((40,)), A.alloc((40,)), A.alloc((40,))
    P.op("sp", lambda e: e.dma_start(out=dec, in_=C.dram["ret_dec"].rearrange("a b -> (a b)").partition_broadcast(128)), writes=["dec"], dma=True)
    P.op("act", lambda e: e.activation(out=lg, in_=dec, func=AF.Exp), reads=["dec"], writes=["nlg"])
    P.op("dve", lambda e: e.tensor_scalar(out=nlg, in0=lg, scalar1=1.0, scalar2=None, op0=ALU.mult), reads=["nlg"], writes=["nlg2"])
    P.op("dve", lambda e: e.tensor_scalar(out=lg, in0=nlg, scalar1=-1.0, scalar2=None, op0=ALU.mult), reads=["nlg2"], writes=["lg"])
    P.op("dve", lambda e: e.tensor_scalar(out=lg128, in0=lg, scalar1=128.0, scalar2=None, op0=ALU.mult), reads=["lg"], writes=["lg128"])
    P.op("dve", lambda e: e.tensor_scalar(out=nlg128, in0=nlg, scalar1=128.0, scalar2=None, op0=ALU.mult), reads=["nlg2"], writes=["nlg128"])
    P.op("pool", lambda e: e.iota(dji_i, pattern=[[1, 128]], base=0, channel_multiplier=-1), writes=["dji_i"])
    P.op("pool", lambda e: e.iota(mrow_i, pattern=[[1, 40]], base=0, channel_multiplier=0), writes=["mrow_i"])
    P.op("dve", lambda e: e.tensor_copy(out=dji, in_=dji_i), reads=["dji_i"], writes=["dji"])
    P.op("dve", lambda e: e.tensor_copy(out=mrow, in_=mrow_i), reads=["mrow_i"], writes=["mrow"])
    P.barrier()
    bank = lambda n: ("bank", n)
    hk_all = [("hT", t, k) for t in range(NT) for k in range(KC)]

    def do_head(hd):
        f, bb = hd, 4 + hd
        P.op("act", lambda e: e.activation(out=Ef, in_=dji, func=AF.Exp, scale=lg[:, f:f + 1]), reads=["dji", "lg"], writes=["Ef"])
        P.op("act", lambda e: e.activation(out=Eb, in_=dji, func=AF.Exp, scale=nlg[:, bb:bb + 1]), reads=["dji", "nlg2"], writes=["Eb"])
        P.op("act", lambda e: e.activation(out=pwf, in_=mrow, func=AF.Exp, scale=lg128[:, f:f + 1]), reads=["mrow", "lg128"], writes=["pwf"])
        P.op("act", lambda e: e.activation(out=pwb, in_=mrow, func=AF.Exp, scale=lg128[:, bb:bb + 1]), reads=["mrow", "lg128"], writes=["pwb"])
        P.op("act", lambda e: e.activation(out=npwb, in_=mrow, func=AF.Exp, scale=nlg128[:, bb:bb + 1]), reads=["mrow", "nlg128"], writes=["npwb"])
        P.op("pool", lambda e: e.affine_select(out=Df, in_=Ef, pattern=[[1, 128]], compare_op=ALU.is_ge, fill=0.0, base=0, channel_multiplier=-1),
             reads=["Ef"], writes=["Df"])
        P.op("pool", lambda e: e.affine_select(out=Db, in_=Eb, pattern=[[-1, 128]], compare_op=ALU.is_ge, fill=0.0, base=0, channel_multiplier=1),
             reads=["Eb"], writes=["Db"])
        P.op("dve", lambda e: e.tensor_tensor(out=Dd, in0=Df, in1=Db, op=ALU.add), reads=["Df", "Db"], writes=["Dd"])
        for m in range(4):
            P.op("dve", (lambda e, m=m: e.tensor_scalar(out=Tf[:, m, :], in0=Ef, scalar1=pwf[:, m:m + 1], scalar2=None, op0=ALU.mult)),
                 reads=["Ef", "pwf"], writes=[("Tf", m)])
            P.op("dve", (lambda e, m=m: e.tensor_scalar(out=Tb[:, m, :], in0=Eb, scalar1=npwb[:, m:m + 1], scalar2=None, op0=ALU.mult)),
                 reads=["Eb", "npwb"], writes=[("Tb", m)])
        ret_phase = int(os.environ.get("MK_RET_PHASE", "5"))
        if ret_phase < 2:
            return
        A.top = wreg_top
        Wk = A.alloc((KC, 256), BF16)
        Wkr = A.alloc((KC, 256), BF16)
        Wv = A.alloc((KC, 512), BF16)
        P.op("pool", lambda e: e.dma_start(out=Wk, in_=w[:, :, 1024 + hd * 256:1024 + (hd + 1) * 256]), writes=["Wk"], dma=True)
        P.op("pool", lambda e: e.dma_start(out=Wv, in_=w[:, :, 2048 + hd * 512:2048 + (hd + 1) * 512]), writes=["Wv"], dma=True)
        P.op("pool", lambda e: e.tensor_scalar(out=Wk, in0=Wk, scalar1=0.0625, scalar2=None, op0=ALU.mult), reads=["Wk"], writes=["Wk"])
        emit_rot_copy(P, Wkr, Wk, 64, "Wk", "Wkr")

        def kv_group(src, c0, ntile, hkeys, kcol0, ktile0, isctx, ropet):
            ntok = ntile * 128
            for dc in range(2):
                for k in range(KC):
                    _mm(P, C.psum[0][:, 0:ntok], Wk[:, k, dc * 128:(dc + 1) * 128], src[:, k, c0:c0 + ntok], k == 0, k == KC - 1,
                        reads=["Wk"] + hkeys, writes=[bank(0)])
                if isctx:
                    P.op("act", (lambda e, dc=dc: e.copy(out=Kh[:, dc, kcol0:kcol0 + ntok], in_=C.psum[0][:, 0:ntok])), reads=[bank(0)], writes=["Kh"])
                    continue
                for k in range(KC):
                    _mm(P, C.psum[1][:, 0:ntok], Wkr[:, k, dc * 128:(dc + 1) * 128], src[:, k, c0:c0 + ntok], k == 0, k == KC - 1,
                        reads=["Wkr"] + hkeys, writes=[bank(1)])
                P.op("dve", (lambda e, dc=dc: e.tensor_tensor(out=tmp[0][:, 0:ntok], in0=C.psum[0][:, 0:ntok], in1=ropet[:, dc, 0, 0:ntok], op=ALU.mult)),
                     reads=[bank(0), "ropeg"], writes=["t0"])
                P.op("dve", (lambda e, dc=dc: e.tensor_tensor(out=tmp[1][:, 0:ntok], in0=C.psum[1][:, 0:ntok], in1=ropet[:, dc, 1, 0:ntok], op=ALU.mult)),
                     reads=[bank(1), "ropeg"], writes=["t1"])
                P.op("pool", (lambda e, dc=dc: e.tensor_tensor(out=Kh[:, dc, kcol0:kcol0 + ntok], in0=tmp[0][:, 0:ntok], in1=tmp[1][:, 0:ntok], op=ALU.add)),
                     reads=["t0", "t1"], writes=["Kh"])
            for tt in range(ntile):
                pb = 2 + tt % 2
                for k in range(KC):
                    _mm(P, C.psum[pb][:, :], src[:, k, c0 + tt * 128:c0 + (tt + 1) * 128], Wv[:, k, :], k == 0, k == KC - 1,
                        reads=["Wv"] + hkeys, writes=[bank(pb)])
                P.op("act", (lambda e, tt=tt, pb=pb: e.copy(out=Vh[:, ktile0 + tt, :], in_=C.psum[pb][:, :])), reads=[bank(pb)], writes=["Vh"])

        for (t0, ntile) in GROUPS:
            if t0 < 2:
                kv_group(C.hT, 0, ntile, hk_all, 0, 0, True, None)
            else:
                c0 = (t0 - 2) * 128
                P.op("sp", (lambda e, c0=c0: e.dma_start(out=ropeg, in_=C.dram["rope_r"][:, :, :, c0:c0 + 512])), writes=["ropeg"], dma=True)
                kv_group(C.hT, t0 * 128, ntile, hk_all, CTX + c0, t0, False, ropeg)
        for g in range(4):
            P.op("sp", (lambda e, g=g: e.dma_start(out=ropeg, in_=C.dram["rope_ro"][:, :, :, g * 512:(g + 1) * 512])), writes=["ropeg"], dma=True)
            for tt in range(4):
                tg = g * 4 + tt
                P.op("sp", (lambda e, tg=tg: e.dma_start(out=Xt, in_=C.dram["x_oth"][tg * 128:(tg + 1) * 128, :])), writes=["Xt"], dma=True)
                for k in range(KC):
                    pst = C.psum[6 + k // 4][:, (k % 4) * 128:(k % 4 + 1) * 128]
                    P.op("pe", (lambda e, k=k, pst=pst: e.transpose(pst, Xt[:, k * 128:(k + 1) * 128], C.ident)),
                         reads=["Xt", "ident"], writes=[bank(6 + k // 4)])
                for k in range(KC):
                    pst = C.psum[6 + k // 4][:, (k % 4) * 128:(k % 4 + 1) * 128]
                    P.op("dve", (lambda e, k=k, pst=pst, tt=tt: e.tensor_scalar(
                        out=hTo[:, k, tt * 128:(tt + 1) * 128], in0=pst,
                        scalar1=C.sc1p[:, i, 0, k, 0:1], scalar2=C.adaT[:, i, k, 0:1], op0=ALU.mult, op1=ALU.add)),
                        reads=[bank(6 + k // 4)], writes=[("hTo", tt, k)])
            kv_group(hTo, 0, 4, [("hTo", tt, k) for tt in range(4) for k in range(KC)], CTX + TOK + g * 512, 18 + g * 4, False, ropeg)
        P.barrier()
        if ret_phase < 3:
            return
        A.top = wreg_top
        Wq = A.alloc((KC, 256), BF16)
        Wqr = A.alloc((KC, 256), BF16)
        Wg = A.alloc((KC, 512), BF16)
        Wo = A.alloc((4, D), BF16)
        P.op("pool", lambda e: e.dma_start(out=Wq, in_=w[:, :, hd * 256:(hd + 1) * 256]), writes=["Wq"], dma=True)
        P.op("pool", lambda e: e.dma_start(out=Wg, in_=w[:, :, 4096 + hd * 512:4096 + (hd + 1) * 512]), writes=["Wg"], dma=True)
        P.op("pool", lambda e: e.dma_start(out=Wo, in_=wo[hd * 512:(hd + 1) * 512, :].rearrange("(c p) n -> p c n", p=128)), writes=["Wo"], dma=True)
        emit_rot_copy(P, Wqr, Wq, 64, "Wq", "Wqr")
        ycnt_box = [0]

        def do_group(t0, ntile, a):
            ntok = ntile * 128
            isctx = t0 < 2
            lq0 = t0 - 2
            ycnt = ycnt_box[0]
            hkeys = hT_keys(t0, ntile)
            if not isctx:
                P.op("sp", (lambda e, lq0=lq0: e.dma_start(out=ropeg, in_=C.dram["rope_r"][:, :, :, lq0 * 128:lq0 * 128 + 512])), writes=["ropeg"], dma=True)
            for dc in range(2):
                for k in range(KC):
                    _mm(P, C.psum[0][:, 0:ntok], Wq[:, k, dc * 128:(dc + 1) * 128], C.hT[:, k, t0 * 128:t0 * 128 + ntok], k == 0, k == KC - 1,
                        reads=["Wq"] + hkeys, writes=[bank(0)])
                if isctx:
                    P.op("act", (lambda e, dc=dc, a=a: e.copy(out=qT[a][:, dc, 0:ntok], in_=C.psum[0][:, 0:ntok])), reads=[bank(0)], writes=[("qT", a, dc)])
                    continue
                for k in range(KC):
                    _mm(P, C.psum[1][:, 0:ntok], Wqr[:, k, dc * 128:(dc + 1) * 128], C.hT[:, k, t0 * 128:t0 * 128 + ntok], k == 0, k == KC - 1,
                        reads=["Wqr"] + hkeys, writes=[bank(1)])
                P.op("dve", (lambda e, dc=dc: e.tensor_tensor(out=tmp[0][:, 0:ntok], in0=C.psum[0][:, 0:ntok], in1=ropeg[:, dc, 0, 0:ntok], op=ALU.mult)),
                     reads=[bank(0), "ropeg"], writes=["t0"])
                P.op("dve", (lambda e, dc=dc: e.tensor_tensor(out=tmp[1][:, 0:ntok], in0=C.psum[1][:, 0:ntok], in1=ropeg[:, dc, 1, 0:ntok], op=ALU.mult)),
                     reads=[bank(1), "ropeg"], writes=["t1"])
                P.op("pool", (lambda e, dc=dc, a=a: e.tensor_tensor(out=qT[a][:, dc, 0:ntok], in0=tmp[0][:, 0:ntok], in1=tmp[1][:, 0:ntok], op=ALU.add)),
                     reads=["t0", "t1"], writes=[("qT", a, dc)])
            ktiles = [0, 1] if isctx else list(range(34))
            SBR = [2, 3, 1]

            def qk_r(ki):
                kt = ktiles[ki]
                sb = SBR[ki % 3]
                pp = ki % 3
                ps = C.psum[sb]
                for dc in range(2):
                    _mm(P, ps[:, 0:ntok], Kh[:, dc, kt * 128:(kt + 1) * 128], qT[a][:, dc, 0:ntok], dc == 0, dc == 1,
                        reads=["Kh", ("qT", a, dc)], writes=[bank(sb)])
                pt = PT[pp]
                rk = [bank(sb), "tables"]
                wk_ = [("PT", pp)]

                def one(outv, inv, scal, tab, rk=rk, wk_=wk_, wkeys=None):
                    P.op("dve", (lambda e: e.scalar_tensor_tensor(out=outv, in0=inv, scalar=scal, in1=tab, op0=ALU.mult, op1=ALU.mult)),
                         reads=rk, writes=(wk_ if wkeys is None else wkeys))

                def sub(m, mode, idx, ps=ps, pt=pt):
                    sl = slice(m * 128, (m + 1) * 128)
                    if mode == "f":
                        one(pt[:, sl], ps[:, sl], pwf[:, idx:idx + 1], Ef)
                    elif mode == "b":
                        one(pt[:, sl], ps[:, sl], pwb[:, idx:idx + 1], Eb)
                    else:
                        P.op("dve", (lambda e: e.tensor_tensor(out=pt[:, sl], in0=ps[:, sl], in1=Dd, op=ALU.mult)), reads=rk, writes=wk_)

                Tfv = Tf.rearrange("p a b -> p (a b)")
                Tbv = Tb.rearrange("p a b -> p (a b)")
                if isctx:
                    for m in range(2):
                        if kt < m:
                            sub(m, "f", 1)
                        elif kt == m:
                            sub(m, "d", 0)
                        else:
                            sub(m, "b", 1)
                elif kt < 2:
                    one(tmp[2][:, 0:ntok], ps[:, 0:ntok], pwf[:, lq0 + 2 - kt:lq0 + 3 - kt], Tfv, wkeys=["t2"])
                    P.op("dve", (lambda e, ps=ps, kt=kt: e.scalar_tensor_tensor(out=tmp[3][:, 0:ntok], in0=ps[:, 0:ntok],
                                                                               scalar=pwb[:, 32 + kt - lq0:33 + kt - lq0], in1=Tbv, op0=ALU.mult, op1=ALU.mult)),
                         reads=rk, writes=["t3"])
                    P.op("pool", (lambda e, pt=pt: e.tensor_tensor(out=pt[:, 0:ntok], in0=tmp[2][:, 0:ntok], in1=tmp[3][:, 0:ntok], op=ALU.add)),
                         reads=["t2", "t3"], writes=wk_)
                elif kt < 18:
                    lk = kt - 2
                    if lk < lq0:
                        one(pt[:, 0:ntok], ps[:, 0:ntok], pwf[:, lq0 - lk:lq0 - lk + 1], Tfv)
                    elif lk > lq0 + 3:
                        one(pt[:, 0:ntok], ps[:, 0:ntok], pwb[:, lk - lq0:lk - lq0 + 1], Tbv)
                    else:
                        for m in range(4):
                            dlt = lk - lq0 - m
                            if dlt > 0:
                                sub(m, "b", dlt)
                            elif dlt == 0:
                                sub(m, "d", 0)
                            else:
                                sub(m, "f", -dlt)
                else:
                    lk = 16 + (kt - 18)
                    one(pt[:, 0:ntok], ps[:, 0:ntok], pwb[:, lk - lq0:lk - lq0 + 1], Tbv)

            def pv_r(ki):
                kt = ktiles[ki]
                pp = ki % 3
                pt = PT[pp]
                for eb in range(4):
                    _mm(P, C.psum[4 + eb][:, 0:ntok], Vh[:, kt, eb * 128:(eb + 1) * 128], pt[:, 0:ntok], ki == 0, ki == len(ktiles) - 1,
                        reads=["Vh", ("PT", pp)], writes=[bank(4 + eb)])

            for ki in range(min(2, len(ktiles))):
                qk_r(ki)
            for ki in range(len(ktiles)):
                if ki + 2 < len(ktiles):
                    qk_r(ki + 2)
                pv_r(ki)
            if ret_phase < 4:
                return
            for eb in range(4):
                P.op("act", (lambda e, eb=eb: e.copy(out=oT32[:, eb, 0:ntok], in_=C.psum[4 + eb][:, 0:ntok])), reads=[bank(4 + eb)], writes=[("oT32", eb)])
                P.op("act", (lambda e, eb=eb: e.activation(out=sq[:, eb, 0:ntok], in_=C.psum[4 + eb][:, 0:ntok], func=AF.Square)),
                     reads=[bank(4 + eb)], writes=[("sq", eb)])
            for eb in range(4):
                _mm(P, C.psum[0][:, 0:ntok], C.ones_bf2, sq[:, eb, 0:ntok], eb == 0, eb == 3, reads=[("sq", eb), "ones_bf"], writes=[bank(0)])
            P.op("dve", lambda e: e.tensor_scalar(out=rstd[:, 0:ntok], in0=C.psum[0][:, 0:ntok], scalar1=1.0 / 512, scalar2=float(RMS_EPS),
                                                  op0=ALU.mult, op1=ALU.add), reads=[bank(0)], writes=["rstd"])
            P.op("act", lambda e: e.activation(out=rstd[:, 0:ntok], in_=rstd[:, 0:ntok], func=AF.Ln), reads=["rstd"], writes=["rstd"])
            P.op("act", lambda e: e.activation(out=rstd[:, 0:ntok], in_=rstd[:, 0:ntok], func=AF.Exp, scale=-0.5), reads=["rstd"], writes=["rstd"])
            for eb in range(4):
                pg = C.psum[1 + eb % 2] if False else C.psum[2 + eb % 2]
                pgk = bank(2 + eb % 2)
                for k in range(KC):
                    _mm(P, pg[:, 0:ntok], Wg[:, k, eb * 128:(eb + 1) * 128], C.hT[:, k, t0 * 128:t0 * 128 + ntok], k == 0, k == KC - 1,
                        reads=["Wg"] + hkeys, writes=[pgk])
                P.op("act", (lambda e, pg=pg: e.copy(out=tmp[3][:, 0:ntok], in_=pg[:, 0:ntok])), reads=[pgk], writes=["t3"])
                P.op("act", (lambda e: e.activation(out=tmp[0][:, 0:ntok], in_=tmp[3][:, 0:ntok], func=AF.Exp, scale=-1.0)), reads=["t3"], writes=["t0"])
                P.op("dve", lambda e: e.tensor_scalar(out=tmp[0][:, 0:ntok], in0=tmp[0][:, 0:ntok], scalar1=1.0, scalar2=None, op0=ALU.add),
                     reads=["t0"], writes=["t0"])
                P.op("dve", lambda e: e.reciprocal(out=tmp[0][:, 0:ntok], in_=tmp[0][:, 0:ntok]), reads=["t0"], writes=["t0"])
                P.op("dve", (lambda e: e.tensor_tensor(out=tmp[1][:, 0:ntok], in0=tmp[3][:, 0:ntok], in1=tmp[0][:, 0:ntok], op=ALU.mult)),
                     reads=["t3", "t0"], writes=["t1"])
                P.op("pool", (lambda e, eb=eb: e.tensor_tensor(out=tmp[2][:, 0:ntok], in0=oT32[:, eb, 0:ntok], in1=rstd[:, 0:ntok], op=ALU.mult)),
                     reads=[("oT32", eb), "rstd"], writes=["t2"])
                P.op("dve", (lambda e, eb=eb: e.tensor_tensor(out=ogT[:, eb, 0:ntok], in0=tmp[2][:, 0:ntok], in1=tmp[1][:, 0:ntok], op=ALU.mult)),
                     reads=["t1", "t2"], writes=[("ogT", eb)])
            if ret_phase < 5:
                return
            ok_ = [("ogT", eb) for eb in range(4)]
            for tt in range(ntile):
                t = t0 + tt
                o = ycnt % 2
                ycnt += 1
                gbc = gc if t < 2 else gx
                for nh in range(2):
                    py = C.psum[nh]
                    for eb in range(4):
                        _mm(P, py[:, :], ogT[:, eb, tt * 128:(tt + 1) * 128], Wo[:, eb, nh * 512:(nh + 1) * 512], eb == 0, eb == 3,
                            reads=ok_ + ["Wo"], writes=[bank(nh)])
                    P.op("dve", (lambda e, py=py, nh=nh, o=o, gbc=gbc: e.tensor_tensor(out=ytmp[o][:, nh * 512:(nh + 1) * 512], in0=py[:, :],
                                                                                        in1=gbc[:, nh * 512:(nh + 1) * 512], op=ALU.mult)),
                         reads=[bank(nh), ("bc", "gx"), ("bc", "gc")], writes=[("ytmp", o, nh)])
                P.op("pool", (lambda e, t=t, o=o: e.dma_start(out=xacc[t * 128:(t + 1) * 128, :], in_=ytmp[o], accum_op=ALU.add)),
                     reads=[("ytmp", o, 0), ("ytmp", o, 1)], writes=[("xacc", t)], dma=True)
            ycnt_box[0] = ycnt

        for gi, (t0_, ntile_) in enumerate(GROUPS):
            do_group(t0_, ntile_, gi % 2)
        P.barrier()

    for hd_ in range(4):
        do_head(hd_)
    for t in range(NT):
        P.op("sp", (lambda e, t=t: e.dma_start(out=C.X[:, t, :], in_=xacc[t * 128:(t + 1) * 128, :])), writes=[("X", t)], dma=True)
    A.pop()
    emit_ln_bc(C, i, 0, need_ctx)


_PROG_CACHE = {}


def _rope_tables(hd, nf):
    theta = np.float32(10000.0)
    inv = (theta ** (-(np.arange(nf, dtype=np.float32) / np.float32(nf)))).astype(np.float32)
    n = np.arange(SEQ)
    row = (n // 64).astype(np.float32)
    col = (n % 64).astype(np.float32)
    d = np.arange(hd)
    pos = np.where((d < hd // 2)[:, None], row[None, :], col[None, :]).astype(np.float32)
    ang = (pos * inv[d % nf][:, None]).astype(np.float32)
    sign = np.where((d % (hd // 2)) < nf, -1.0, 1.0).astype(np.float32)[:, None]
    return np.stack([np.cos(ang), np.sin(ang) * sign], axis=1).astype(np.float32)


def _core_inputs(inputs, layers_needed, x0=None, xc0=None, flip_odd=False):
    x = np.asarray(inputs["x"] if x0 is None else x0, dtype=np.float32)
    ctx = np.asarray(inputs["ctx"] if xc0 is None else xc0, dtype=np.float32)
    c = np.asarray(inputs["c"], dtype=np.float32)
    c_ctx = np.asarray(inputs["c_ctx"], dtype=np.float32)
    ident = np.eye(128, dtype=np.float32)
    shared = {"ident": ident}
    rope_a = _rope_tables(128, 32)
    rope_r = _rope_tables(256, 64).reshape(2, 128, 2, SEQ).transpose(1, 0, 2, 3)
    for i in layers_needed:
        for n in layer_param_names(i):
            shared[n] = np.ascontiguousarray(np.asarray(inputs[n], dtype=np.float32))
            if NEXP_RUN < NEXP and ("moe_w_gu" in n or "moe_w_down" in n):
                shared[n] = np.ascontiguousarray(shared[n][:NEXP_RUN])
    maps = []
    for r in range(8):
        b, h = r // 2, r % 2
        cc = np.stack([c[b].reshape(KC, 128).T, c_ctx.reshape(KC, 128).T], axis=-1)
        m = dict(shared)
        own = slice(h * TOK, (h + 1) * TOK)
        oth = slice((1 - h) * TOK, (2 - h) * TOK)
        rev = flip_odd and h == 1
        st = -1 if rev else 1
        m["x_in"] = np.ascontiguousarray(x[b, own][::st])
        m["ctx_in"] = np.ascontiguousarray(ctx[b][::st])
        m["x_oth"] = np.ascontiguousarray(x[b, oth][::st])
        m["cc"] = np.ascontiguousarray(cc.astype(np.float32))
        m["rope_a"] = np.ascontiguousarray(rope_a[:, :, own][:, :, ::st])
        m["rope_o"] = np.ascontiguousarray(rope_a[:, :, oth][:, :, ::st])
        m["rope_r"] = np.ascontiguousarray(rope_r[:, :, :, own][:, :, :, ::st])
        m["rope_ro"] = np.ascontiguousarray(rope_r[:, :, :, oth][:, :, :, ::st])
        for i in layers_needed:
            if i % 3 == 2:
                dec = np.asarray(inputs["l%d_ret_decay" % i], dtype=np.float32)
                m["ret_dec"] = np.ascontiguousarray(dec[::st])
            if i % 3 == 1 and rev:
                m["l%d_cmlp_w_s" % i] = np.ascontiguousarray(shared["l%d_cmlp_w_s" % i][:, ::-1, ::-1])
                m["l%d_cmlp_b_s" % i] = np.ascontiguousarray(shared["l%d_cmlp_b_s" % i][:, ::-1])
        maps.append(m)
    return maps


def run_stages(inputs, stages, x0=None, xc0=None):
    layers_needed = sorted(set(i for _, i in stages))
    key = tuple(stages)
    if key not in _PROG_CACHE:
        _PROG_CACHE[key] = build_program(stages, layers_needed)
    nc = _PROG_CACHE[key]
    flip_odd = any(k == "mix" and i % 3 == 2 for k, i in stages)
    maps = _core_inputs(inputs, layers_needed, x0, xc0, flip_odd)
    maps = [{k: v for k, v in m.items() if k in nc.mk_inputs} for m in maps]
    if os.environ.get("MK_TRACE"):
        res = run_bass_kernel_spmd(nc, maps, core_ids=list(range(8)), trace=True)
        print("MK_TRACE exec_time_ns", stages, res.exec_time_ns)
        try:
            import json, collections
            pj = res.profile_json
            if isinstance(pj, (list, tuple)):
                pj = pj[0]
            if isinstance(pj, str):
                pj = json.loads(pj)
            print("MK_TRACE profile keys", list(pj.keys())[:40] if isinstance(pj, dict) else type(pj))
            if isinstance(pj, dict):
                for k, v in pj.items():
                    if isinstance(v, (int, float, str)):
                        print("  ", k, v)
                    elif isinstance(v, dict):
                        print("  ", k, {kk: vv for kk, vv in list(v.items())[:30] if isinstance(vv, (int, float, str))})
        except Exception as ex:
            print("MK_TRACE profile dump failed", ex)
    else:
        res = run_bass_kernel_spmd(nc, maps, core_ids=list(range(8)))
    xo = np.zeros((BATCH, SEQ, D), np.float32)
    xco = np.zeros((BATCH, CTX, D), np.float32)
    for r in range(8):
        b, h = r // 2, r % 2
        st = -1 if (flip_odd and h == 1) else 1
        xo[b, h * TOK:(h + 1) * TOK] = res.results[r]["x_out"][::st]
        if h == 0:
            xco[b] = res.results[r]["xc_out"]
    return xo, xco


INPUT_NAMES = (
    "x",
    "c",
    "ctx",
    "c_ctx",
    "l0_ada_w",
    "l0_ada_b",
    "l0_ln1_g",
    "l0_ln1_b",
    "l0_ln2_g",
    "l0_ln2_b",
    "l0_attn_wqkv",
    "l0_attn_q_norm",
    "l0_attn_k_norm",
    "l0_attn_wo",
    "l0_ffn_w_gu",
    "l0_ffn_w_down",
    "l1_ada_w",
    "l1_ada_b",
    "l1_ln1_g",
    "l1_ln1_b",
    "l1_ln2_g",
    "l1_ln2_b",
    "l1_cmlp_w_in",
    "l1_cmlp_b_in",
    "l1_cmlp_v_norm_g",
    "l1_cmlp_v_norm_b",
    "l1_cmlp_w_s",
    "l1_cmlp_b_s",
    "l1_cmlp_w_out",
    "l1_cmlp_b_out",
    "l1_moe_router",
    "l1_moe_w_gu",
    "l1_moe_w_down",
    "l2_ada_w",
    "l2_ada_b",
    "l2_ln1_g",
    "l2_ln1_b",
    "l2_ln2_g",
    "l2_ln2_b",
    "l2_ret_wqkvg",
    "l2_ret_decay",
    "l2_ret_wo",
    "l2_ffn_w_gu",
    "l2_ffn_w_down",
    "l3_ada_w",
    "l3_ada_b",
    "l3_ln1_g",
    "l3_ln1_b",
    "l3_ln2_g",
    "l3_ln2_b",
    "l3_attn_wqkv",
    "l3_attn_q_norm",
    "l3_attn_k_norm",
    "l3_attn_wo",
    "l3_moe_router",
    "l3_moe_w_gu",
    "l3_moe_w_down",
)


def kernel(**inputs):
    missing = [n for n in INPUT_NAMES if n not in inputs]
    assert not missing, missing
    x, xc = inputs["x"], inputs["ctx"]
    for i in range(DEPTH):
        x, xc = run_stages(inputs, [("mix", i), ("ffn", i)], x0=x, xc0=xc)
    return x
```
